# Optimizing a Trainium2 kernel written in Bass

```python
import math
import jax, jax.numpy as jnp
from jax import lax
import numpy as np

D_MODEL = 1024
BATCH = 8
SEQ = 4096
DEPTH = 2

GRID_W = 64
CTX_LEN = 256
ROPE_BASE = 10000.0
EPS = 1e-6
NEG_INF = -1e30

A_HEADS = 8
A_KV_HEADS = 2
A_GROUP = A_HEADS // A_KV_HEADS
A_HEAD_DIM = 64
A_WIDTH = A_HEADS * A_HEAD_DIM
WINDOW = 128

B_HEADS = 8
B_NOPE = 64
B_ROPE = 32
B_VDIM = 64
B_WIDTH = B_HEADS * B_VDIM
Q_LORA = 256
KV_LORA = 256
Q_BLOCK = 128

POOL_WINDOWS = (2, 4, 8, 16)
C_GROUPS = 4
C_GROUP_DIM = 128
C_WIDTH = C_GROUPS * C_GROUP_DIM

N_BRANCH = 3
IN_SPLITS = (A_WIDTH, A_KV_HEADS * A_HEAD_DIM, A_KV_HEADS * A_HEAD_DIM,
             Q_LORA, KV_LORA, B_ROPE, C_WIDTH, N_BRANCH * D_MODEL)
IN_WIDTH = sum(IN_SPLITS)

N_GROUPS = 4
EXPERTS_PER_GROUP = 8
N_EXPERTS = N_GROUPS * EXPERTS_PER_GROUP
TOP_K = 2
EXPERT_FF = 256
EXPERT_BLOCK = 128

kernel_name = 'hybrid_gated_swa_mla_pool_hmoe_dit'


def rmsnorm(x, g):
    xf = x.astype(jnp.float32)
    y = xf * lax.rsqrt(jnp.mean(xf * xf, axis=-1, keepdims=True) + EPS)
    return (y * g.astype(jnp.float32)).astype(x.dtype)


def modulate(h, g, shift, scale):
    return rmsnorm(h, g) * (1 + scale) + shift


def axial_rope_tables(rows, rot_dim):
    row = jnp.repeat(jnp.arange(rows), GRID_W).astype(jnp.float32)
    col = jnp.tile(jnp.arange(GRID_W), rows).astype(jnp.float32)
    axis_dim = rot_dim // 2
    inv = ROPE_BASE ** (-jnp.arange(0, axis_dim, 2, dtype=jnp.float32) / axis_dim)
    ang = jnp.concatenate([row[:, None] * inv, col[:, None] * inv], axis=-1)
    return jnp.cos(ang), jnp.sin(ang)


def apply_rope(x, cos, sin):
    xf = x.astype(jnp.float32)
    x1, x2 = xf[..., 0::2], xf[..., 1::2]
    c, s = cos[:, None, :], sin[:, None, :]
    out = jnp.stack([x1 * c - x2 * s, x1 * s + x2 * c], axis=-1).reshape(x.shape)
    return out.astype(x.dtype)


def split_in(p):
    idx, acc = [], 0
    for w in IN_SPLITS[:-1]:
        acc += w
        idx.append(acc)
    return jnp.split(p, idx, axis=-1)


def window_attn(q, k, v, kc, vc, sink):
    B, N = q.shape[:2]
    nb = N // WINDOW
    scale = A_HEAD_DIM ** -0.5
    qb = q.reshape(B, nb, WINDOW, A_KV_HEADS, A_GROUP, A_HEAD_DIM)
    pad = ((0, 0), (WINDOW, WINDOW), (0, 0), (0, 0))
    kp = jnp.pad(k, pad).reshape(B, nb + 2, WINDOW, A_KV_HEADS, A_HEAD_DIM)
    vp = jnp.pad(v, pad).reshape(B, nb + 2, WINDOW, A_KV_HEADS, A_HEAD_DIM)
    kband = jnp.concatenate([kp[:, :-2], kp[:, 1:-1], kp[:, 2:]], axis=2)
    vband = jnp.concatenate([vp[:, :-2], vp[:, 1:-1], vp[:, 2:]], axis=2)
    s_loc = jnp.einsum('bnqhgd,bnkhd->bnhgqk', qb, kband).astype(jnp.float32) * scale
    s_ctx = jnp.einsum('bnqhgd,bchd->bnhgqc', qb, kc).astype(jnp.float32) * scale
    rel = jnp.arange(3 * WINDOW)[None, :] - WINDOW - jnp.arange(WINDOW)[:, None]
    band = jnp.abs(rel) <= WINDOW
    kpos = jnp.arange(nb)[:, None] * WINDOW - WINDOW + jnp.arange(3 * WINDOW)[None, :]
    valid = (kpos >= 0) & (kpos < N)
    mask = band[None, :, :] & valid[:, None, :]
    s_loc = jnp.where(mask[None, :, None, None, :, :], s_loc, NEG_INF)
    sk = jnp.broadcast_to(sink.astype(jnp.float32).reshape(A_KV_HEADS, A_GROUP)[None, None, :, :, None, None],
                          s_loc.shape[:-1] + (1,))
    p = jax.nn.softmax(jnp.concatenate([s_loc, s_ctx, sk], axis=-1), axis=-1)
    p_loc = p[..., :3 * WINDOW].astype(v.dtype)
    p_ctx = p[..., 3 * WINDOW:3 * WINDOW + kc.shape[1]].astype(v.dtype)
    o = (jnp.einsum('bnhgqk,bnkhd->bnqhgd', p_loc, vband)
         + jnp.einsum('bnhgqc,bchd->bnqhgd', p_ctx, vc))
    return o.reshape(B, N, A_WIDTH)


def ctx_gqa_attn(q, k, v, sink):
    B, L = q.shape[:2]
    qg = q.reshape(B, L, A_KV_HEADS, A_GROUP, A_HEAD_DIM)
    s = jnp.einsum('bqhgd,bkhd->bhgqk', qg, k).astype(jnp.float32) * (A_HEAD_DIM ** -0.5)
    sk = jnp.broadcast_to(sink.astype(jnp.float32).reshape(A_KV_HEADS, A_GROUP)[None, :, :, None, None],
                          s.shape[:-1] + (1,))
    p = jax.nn.softmax(jnp.concatenate([s, sk], axis=-1), axis=-1)[..., :-1].astype(v.dtype)
    o = jnp.einsum('bhgqk,bkhd->bqhgd', p, v)
    return o.reshape(B, L, A_WIDTH)


def mla_qkv(cq, ckv, kr, q_norm_g, w_uq, kv_norm_g, w_ukv, rope):
    B, L, _ = cq.shape
    q = (rmsnorm(cq, q_norm_g) @ w_uq).reshape(B, L, B_HEADS, B_NOPE + B_ROPE)
    kv = (rmsnorm(ckv, kv_norm_g) @ w_ukv).reshape(B, L, B_HEADS, B_NOPE + B_VDIM)
    q_nope, q_rope = q[..., :B_NOPE], q[..., B_NOPE:]
    k_nope, v = kv[..., :B_NOPE], kv[..., B_NOPE:]
    k_rope = kr[:, :, None, :]
    if rope is not None:
        q_rope = apply_rope(q_rope, *rope)
        k_rope = apply_rope(k_rope, *rope)
    q = jnp.concatenate([q_nope, q_rope], axis=-1)
    k = jnp.concatenate([k_nope, jnp.broadcast_to(k_rope, (B, L, B_HEADS, B_ROPE))], axis=-1)
    return q, k, v


def dense_attn(q, k, v):
    s = jnp.einsum('bqhd,bkhd->bhqk', q, k).astype(jnp.float32) * (q.shape[-1] ** -0.5)
    p = jax.nn.softmax(s, axis=-1).astype(v.dtype)
    o = jnp.einsum('bhqk,bkhd->bqhd', p, v)
    return o.reshape(q.shape[0], q.shape[1], -1)


def blocked_dense_attn(q, k, v):
    B, N, H, dq = q.shape
    nb = N // Q_BLOCK
    qb = q.reshape(B, nb, Q_BLOCK, H, dq).transpose(1, 0, 2, 3, 4)
    o = lax.map(lambda qblk: dense_attn(qblk, k, v), qb)
    return o.transpose(1, 0, 2, 3).reshape(B, N, -1)


def pool_mixer(u, w_pool, pool_scale):
    B, L, _ = u.shape
    uf = u.astype(jnp.float32)
    cs = jnp.concatenate([jnp.zeros((B, 1, C_WIDTH), jnp.float32), jnp.cumsum(uf, axis=1)], axis=1)
    t = jnp.arange(L)
    parts = []
    for g, w in enumerate(POOL_WINDOWS):
        r = w // 2
        lo = jnp.maximum(t - r, 0)
        hi = jnp.minimum(t + r + 1, L)
        sl = slice(g * C_GROUP_DIM, (g + 1) * C_GROUP_DIM)
        win_sum = cs[:, hi, sl] - cs[:, lo, sl]
        parts.append(win_sum / (hi - lo).astype(jnp.float32)[:, None] - uf[:, :, sl])
    pooled = jnp.concatenate(parts, axis=-1).reshape(B, L, C_GROUPS, C_GROUP_DIM).astype(u.dtype)
    mixed = jnp.einsum('blgc,gcd->blgd', pooled, w_pool).reshape(B, L, C_WIDTH)
    return mixed * pool_scale


def merge_branches(gates, oa, ob, oc, lp):
    ga, gb, gc = jnp.split(gates, N_BRANCH, axis=-1)
    y = (jax.nn.sigmoid(ga) * (oa @ lp['w_br_a'])
         + jax.nn.sigmoid(gb) * (ob @ lp['w_br_b'])
         + jax.nn.sigmoid(gc) * (oc @ lp['w_br_c']))
    return y @ lp['w_out']


def mixer_block(hx, hc, lp, rope_a, rope_b, last):
    B, N, _ = hx.shape
    Lc = hc.shape[1]
    qa, ka, va, cq, ckv, kr, u, gates = split_in(hx @ lp['w_in'])
    qa_c, ka_c, va_c, cq_c, ckv_c, kr_c, u_c, gates_c = split_in(hc @ lp['w_in'])
    qa = apply_rope(qa.reshape(B, N, A_HEADS, A_HEAD_DIM), *rope_a)
    ka = apply_rope(ka.reshape(B, N, A_KV_HEADS, A_HEAD_DIM), *rope_a)
    va = va.reshape(B, N, A_KV_HEADS, A_HEAD_DIM)
    ka_c = ka_c.reshape(B, Lc, A_KV_HEADS, A_HEAD_DIM)
    va_c = va_c.reshape(B, Lc, A_KV_HEADS, A_HEAD_DIM)
    oa = window_attn(qa, ka, va, ka_c, va_c, lp['sink'])
    qb, kb, vb = mla_qkv(cq, ckv, kr, lp['q_norm_g'], lp['w_uq'], lp['kv_norm_g'], lp['w_ukv'], rope_b)
    qb_c, kb_c, vb_c = mla_qkv(cq_c, ckv_c, kr_c, lp['q_norm_g'], lp['w_uq'], lp['kv_norm_g'], lp['w_ukv'], None)
    ob = blocked_dense_attn(qb, jnp.concatenate([kb_c, kb], axis=1), jnp.concatenate([vb_c, vb], axis=1))
    oc = pool_mixer(u, lp['w_pool'], lp['pool_scale'])
    out_x = merge_branches(gates, oa, ob, oc, lp)
    if last:
        return out_x, None
    oa_c = ctx_gqa_attn(qa_c.reshape(B, Lc, A_HEADS, A_HEAD_DIM), ka_c, va_c, lp['sink'])
    ob_c = dense_attn(qb_c, kb_c, vb_c)
    oc_c = pool_mixer(u_c, lp['w_pool'], lp['pool_scale'])
    out_c = merge_branches(gates_c, oa_c, ob_c, oc_c, lp)
    return out_x, out_c


def hier_moe(h, w_rg, b_rg, w_re, b_re, w_gu, w_dn):
    T, D = h.shape
    g_logits = (h @ w_rg + b_rg).astype(jnp.float32)
    e_logits = (h @ w_re + b_re).astype(jnp.float32).reshape(T, N_GROUPS, EXPERTS_PER_GROUP)
    g_sel = jnp.argmax(g_logits, axis=-1)
    g_p = jnp.take_along_axis(jax.nn.softmax(g_logits, axis=-1), g_sel[:, None], axis=1)
    e_grp = jnp.take_along_axis(e_logits, g_sel[:, None, None], axis=1)[:, 0]
    top_v, top_i = lax.top_k(e_grp, TOP_K)
    gate = jax.nn.softmax(top_v, axis=-1) * g_p
    expert = g_sel[:, None] * EXPERTS_PER_GROUP + top_i
    A = T * TOP_K
    flat_e = expert.reshape(A)
    order = jnp.argsort(flat_e)
    e_sorted = flat_e[order]
    tok_sorted = order // TOP_K
    gate_sorted = gate.reshape(A)[order]
    counts = jax.ops.segment_sum(jnp.ones((A,), jnp.int32), flat_e, num_segments=N_EXPERTS)
    padded = (counts + EXPERT_BLOCK - 1) // EXPERT_BLOCK * EXPERT_BLOCK
    pad_end = jnp.cumsum(padded)
    pad_start = pad_end - padded
    start = jnp.cumsum(counts) - counts
    slot = pad_start[e_sorted] + (jnp.arange(A) - start[e_sorted])
    n_blocks = -(-(A + N_EXPERTS * (EXPERT_BLOCK - 1)) // EXPERT_BLOCK)
    P = n_blocks * EXPERT_BLOCK
    buf = jnp.zeros((P, D), h.dtype).at[slot].set(h[tok_sorted])
    blk_e = jnp.minimum(jnp.searchsorted(pad_end, jnp.arange(n_blocks) * EXPERT_BLOCK, side='right'),
                        N_EXPERTS - 1)

    def run_block(args):
        xb, e = args
        gt, up = jnp.split(xb @ w_gu[e], 2, axis=-1)
        return (jax.nn.silu(gt) * up) @ w_dn[e]

    yb = lax.map(run_block, (buf.reshape(n_blocks, EXPERT_BLOCK, D), blk_e))
    y = yb.reshape(P, D)[slot] * gate_sorted[:, None].astype(h.dtype)
    return jax.ops.segment_sum(y, tok_sorted, num_segments=T)


def setup_inputs(seed: int = 0) -> dict:
    key = jax.random.key(seed)
    ks = jax.random.split(key, 32)
    nrm = jax.random.normal
    f32 = jnp.float32
    D = D_MODEL
    return {
        'x': nrm(ks[0], (BATCH, SEQ, D), f32),
        'c': nrm(ks[1], (BATCH, D), f32),
        'ctx': nrm(ks[2], (BATCH, CTX_LEN, D), f32),
        'c_ctx': nrm(ks[3], (D,), f32),
        'w_mod': nrm(ks[4], (DEPTH, D, 6 * D), f32) * (0.5 * D ** -0.5),
        'b_mod': nrm(ks[5], (DEPTH, 6 * D), f32) * 0.01,
        'norm_mix_g': 1.0 + 0.02 * nrm(ks[6], (DEPTH, D), f32),
        'norm_ffn_g': 1.0 + 0.02 * nrm(ks[7], (DEPTH, D), f32),
        'w_in': nrm(ks[8], (DEPTH, D, IN_WIDTH), f32) * D ** -0.5,
        'sink': 0.5 * nrm(ks[9], (DEPTH, A_HEADS), f32),
        'q_norm_g': 1.0 + 0.02 * nrm(ks[10], (DEPTH, Q_LORA), f32),
        'w_uq': nrm(ks[11], (DEPTH, Q_LORA, B_HEADS * (B_NOPE + B_ROPE)), f32) * Q_LORA ** -0.5,
        'kv_norm_g': 1.0 + 0.02 * nrm(ks[12], (DEPTH, KV_LORA), f32),
        'w_ukv': nrm(ks[13], (DEPTH, KV_LORA, B_HEADS * (B_NOPE + B_VDIM)), f32) * KV_LORA ** -0.5,
        'w_pool': nrm(ks[14], (DEPTH, C_GROUPS, C_GROUP_DIM, C_GROUP_DIM), f32) * C_GROUP_DIM ** -0.5,
        'pool_scale': 1.0 + 0.02 * nrm(ks[15], (DEPTH, C_WIDTH), f32),
        'w_br_a': nrm(ks[16], (DEPTH, A_WIDTH, D), f32) * A_WIDTH ** -0.5,
        'w_br_b': nrm(ks[17], (DEPTH, B_WIDTH, D), f32) * B_WIDTH ** -0.5,
        'w_br_c': nrm(ks[18], (DEPTH, C_WIDTH, D), f32) * C_WIDTH ** -0.5,
        'w_out': nrm(ks[19], (DEPTH, D, D), f32) * D ** -0.5,
        'w_rg': nrm(ks[20], (DEPTH, D, N_GROUPS), f32) * D ** -0.5,
        'b_rg': 0.01 * nrm(ks[21], (DEPTH, N_GROUPS), f32),
        'w_re': nrm(ks[22], (DEPTH, D, N_EXPERTS), f32) * D ** -0.5,
        'b_re': 0.01 * nrm(ks[23], (DEPTH, N_EXPERTS), f32),
        'w_gu': nrm(ks[24], (DEPTH, N_EXPERTS, D, 2 * EXPERT_FF), f32) * D ** -0.5,
        'w_dn': nrm(ks[25], (DEPTH, N_EXPERTS, EXPERT_FF, D), f32) * EXPERT_FF ** -0.5,
        'final_g': 1.0 + 0.02 * nrm(ks[26], (D,), f32),
    }


def reference(x, c, ctx, c_ctx, w_mod, b_mod, norm_mix_g, norm_ffn_g, w_in, sink, q_norm_g, w_uq,
              kv_norm_g, w_ukv, w_pool, pool_scale, w_br_a, w_br_b, w_br_c, w_out, w_rg, b_rg, w_re,
              b_re, w_gu, w_dn, final_g):
    B, N, D = x.shape
    Lc = ctx.shape[1]
    ROWS = N // GRID_W
    rope_a = axial_rope_tables(ROWS, A_HEAD_DIM)
    rope_b = axial_rope_tables(ROWS, B_ROPE)
    h, hcs = x, ctx
    for l in range(DEPTH):
        last = l == DEPTH - 1
        lp = {'w_in': w_in[l], 'sink': sink[l], 'q_norm_g': q_norm_g[l], 'w_uq': w_uq[l],
              'kv_norm_g': kv_norm_g[l], 'w_ukv': w_ukv[l], 'w_pool': w_pool[l],
              'pool_scale': pool_scale[l], 'w_br_a': w_br_a[l], 'w_br_b': w_br_b[l],
              'w_br_c': w_br_c[l], 'w_out': w_out[l]}
        mod_x = jax.nn.silu(c) @ w_mod[l] + b_mod[l]
        mod_c = (jax.nn.silu(c_ctx) @ w_mod[l] + b_mod[l])[None]
        sh1, sc1, g1, sh2, sc2, g2 = jnp.split(mod_x[:, None, :], 6, axis=-1)
        sh1c, sc1c, g1c, sh2c, sc2c, g2c = jnp.split(mod_c[:, None, :], 6, axis=-1)
        hx = modulate(h, norm_mix_g[l], sh1, sc1)
        hc = modulate(hcs, norm_mix_g[l], sh1c, sc1c)
        mix_x, mix_c = mixer_block(hx, hc, lp, rope_a, rope_b, last)
        h = h + g1 * mix_x
        fx = modulate(h, norm_ffn_g[l], sh2, sc2).reshape(B * N, D)
        if last:
            ffn = hier_moe(fx, w_rg[l], b_rg[l], w_re[l], b_re[l], w_gu[l], w_dn[l])
            h = h + g2 * ffn.reshape(B, N, D)
        else:
            hcs = hcs + g1c * mix_c
            fc = modulate(hcs, norm_ffn_g[l], sh2c, sc2c).reshape(B * Lc, D)
            ffn = hier_moe(jnp.concatenate([fx, fc], axis=0), w_rg[l], b_rg[l], w_re[l], b_re[l],
                           w_gu[l], w_dn[l])
            h = h + g2 * ffn[:B * N].reshape(B, N, D)
            hcs = hcs + g2c * ffn[B * N:].reshape(B, Lc, D)
    return rmsnorm(h, final_g)
```

```python
import numpy as np
import concourse.bass as bass
import concourse.mybir as mybir
from concourse.bass_utils import run_bass_kernel_spmd

F32 = mybir.dt.float32
BF16 = mybir.dt.bfloat16
I32 = mybir.dt.int32
U32 = mybir.dt.uint32
AF = mybir.ActivationFunctionType
ALU = mybir.AluOpType
AX = mybir.AxisListType

ENGS = ("pe", "act", "dve", "pool", "sp")
EPOCH = 12000
NSLOT = 24


class Tok:
    __slots__ = ("name", "writers", "readers")

    def __init__(self, name=""):
        self.name = name
        self.writers = []
        self.readers = []


class Op:
    __slots__ = ("eng", "fn", "deps", "idx", "needed", "cnt", "dma", "slot", "val", "waits")

    def __init__(self, eng, fn, dma=False):
        self.eng = eng
        self.fn = fn
        self.deps = []
        self.idx = -1
        self.needed = False
        self.cnt = -1
        self.dma = dma
        self.slot = -1
        self.val = -1
        self.waits = []


class Sched:
    def __init__(self, nc):
        self.nc = nc
        self.ops = {e: [] for e in ENGS}
        self.ndma = {e: 0 for e in ENGS}
        self.seen = {e: {} for e in ENGS}
        self.pending = {e: [] for e in ENGS}

    def barrier(self):
        waits = []
        for e in ENGS:
            last = None
            for o in reversed(self.ops[e]):
                if not o.dma:
                    last = o
                    break
            if last is not None:
                waits.append(("c", last))
            slots = {}
            for o in self.ops[e]:
                if o.dma:
                    slots[o.slot] = o
            for o in slots.values():
                waits.append(("d", o))
        for e in ENGS:
            self.pending[e] = list(waits)

    def _add(self, op, reads, writes, acc):
        deps = []
        for t in reads:
            deps.extend(t.writers)
        for t in writes:
            deps.extend(t.readers)
            if not acc:
                deps.extend(t.writers)
            else:
                deps.extend(w for w in t.writers if w.eng != op.eng or w.dma != op.dma)
        e = op.eng
        op.idx = len(self.ops[e])
        seen = self.seen[e]
        if self.pending[e]:
            for w in self.pending[e]:
                if w[0] == "c":
                    deps.append(w[1])
                else:
                    deps.append(w[1])
            self.pending[e] = []
            barrier_dep = True
        else:
            barrier_dep = False
        if op.dma:
            n = self.ndma[e]
            self.ndma[e] = n + 1
            op.slot = n % NSLOT
            op.val = 16 * (n // NSLOT + 1)
            if op.val > 16:
                key = ("d", e, op.slot)
                if seen.get(key, 0) < op.val - 16:
                    seen[key] = op.val - 16
                    op.waits.append(("d", e, op.slot, op.val - 16))
        for d in deps:
            if d.dma:
                key = ("d", d.eng, d.slot)
                if seen.get(key, 0) >= d.val:
                    continue
                seen[key] = d.val
                op.waits.append(("d", d.eng, d.slot, d.val))
            else:
                if d.eng == e:
                    if e in ("pe", "sp"):
                        continue
                key = ("c", d.eng)
                if seen.get(key, -1) >= d.idx:
                    continue
                seen[key] = d.idx
                d.needed = True
                op.waits.append(("c", d))
        for t in reads:
            t.readers.append(op)
        for t in writes:
            if acc:
                t.writers.append(op)
            else:
                t.writers = [op]
                t.readers = []
        self.ops[e].append(op)
        return op

    def op(self, eng, fn, reads=(), writes=(), acc=False):
        return self._add(Op(eng, fn), list(reads), list(writes), acc)

    def dma(self, eng, out, in_, reads=(), writes=(), acc=False, **kw):
        def fn(en):
            return en.dma_start(out=out, in_=in_, **kw)
        return self._add(Op(eng, fn, dma=True), list(reads), list(writes), acc)

    def dma_fn(self, eng, fn, reads=(), writes=(), acc=False):
        return self._add(Op(eng, fn, dma=True), list(reads), list(writes), acc)

    def emit(self):
        nc = self.nc
        nep = {}
        for e in ENGS:
            c = 0
            for o in self.ops[e]:
                if o.needed and not o.dma:
                    c += 1
                    o.cnt = c
            nep[e] = max(1, (c + EPOCH - 1) // EPOCH)
        import contextlib
        with contextlib.ExitStack() as st:
            csem = {e: [st.enter_context(nc.semaphore(f"c_{e}_{i}")) for i in range(nep[e])]
                    for e in ENGS}
            dsem = {e: [st.enter_context(nc.semaphore(f"d_{e}_{i}")) for i in range(NSLOT)]
                    for e in ENGS if self.ndma[e] > 0}
            block = st.enter_context(nc.Block())
            hw = {"pe": block.tensor, "act": block.scalar, "dve": block.vector,
                  "pool": block.gpsimd, "sp": block.sync}

            def body(e):
                def run(en):
                    for o in self.ops[e]:
                        for w in o.waits:
                            if w[0] == "d":
                                en.wait_ge(dsem[w[1]][w[2]], w[3])
                            else:
                                d = w[1]
                                en.wait_ge(csem[d.eng][(d.cnt - 1) // EPOCH], (d.cnt - 1) % EPOCH + 1)
                        ins = o.fn(en)
                        if o.dma:
                            ins.then_inc(dsem[e][o.slot], 16)
                        elif o.needed:
                            ins.then_inc(csem[e][(o.cnt - 1) // EPOCH], 1)
                    if self.ndma[e] > 0:
                        last = {}
                        for o in self.ops[e]:
                            if o.dma:
                                last[o.slot] = o.val
                        for s, v in last.items():
                            en.wait_ge(dsem[e][s], v)
                return run

            for e in ENGS:
                if self.ops[e]:
                    hw[e](body(e))


DM = 1024
SEQ = 4096
LC = 256
NTOK = SEQ + LC
NT = NTOK // 128
DEPTH = 2
EPS = 1e-6
STOP = 0
QT_LIMIT = 0


class Ctx:
    def __init__(self, nc, S):
        self.nc = nc
        self.S = S
        self.d = {}
        self.tok = {}

    def T(self, name):
        t = self.tok.get(name)
        if t is None:
            t = Tok(name)
            self.tok[name] = t
        return t


def phase_mod(C, l):
    nc, S = C.nc, C.S
    wmod = C.d["w_mod"][l].rearrange("(kc p) n -> p kc n", p=128)
    with nc.sbuf_tensor(f"m_ct{l}", [128, 8, 2], F32) as ct, \
            nc.sbuf_tensor(f"m_sct{l}", [128, 8, 2], F32) as sct, \
            nc.sbuf_tensor(f"m_w0{l}", [128, 8, 512], F32) as w0, \
            nc.sbuf_tensor(f"m_w1{l}", [128, 8, 512], F32) as w1, \
            nc.sbuf_tensor(f"m_b{l}", [2, 6144], F32) as bm, \
            nc.sbuf_tensor(f"m_row{l}", [2, 6144], F32) as mrow, \
            nc.psum_tensor(f"m_p0{l}", [2, 512], F32) as p0, \
            nc.psum_tensor(f"m_p1{l}", [2, 512], F32) as p1:
        t_ct, t_sct, t_bm, t_row = Tok(), Tok(), Tok(), Tok()
        t_w = [Tok(), Tok()]
        t_p = [Tok(), Tok()]
        wb = [w0, w1]
        pb = [p0, p1]
        S.dma("sp", ct[:], C.d["cT"][:], writes=[t_ct])
        S.dma("sp", bm[:], C.d["b_mod"][l:l + 1, :].to_broadcast([2, 6144]), writes=[t_bm])
        S.op("act", lambda e: e.activation(out=sct[:], in_=ct[:], func=AF.Silu),
             reads=[t_ct], writes=[t_sct])
        for n in range(12):
            S.dma("sp", wb[n % 2][:], wmod[:, :, n * 512:(n + 1) * 512], writes=[t_w[n % 2]])
            for kc in range(8):
                S.op("pe", lambda e, n=n, kc=kc: e.matmul(pb[n % 2][:], lhsT=sct[:, kc, :],
                                                       rhs=wb[n % 2][:, kc, :],
                                                       start=(kc == 0), stop=(kc == 7)),
                     reads=[t_sct, t_w[n % 2]], writes=[t_p[n % 2]], acc=(kc > 0))
            S.op("dve", lambda e, n=n: e.tensor_tensor(out=mrow[:, n * 512:(n + 1) * 512],
                                                      in0=pb[n % 2][:],
                                                      in1=bm[:, n * 512:(n + 1) * 512], op=ALU.add),
                 reads=[t_p[n % 2], t_bm], writes=[t_row], acc=True)
        S.dma("sp", C.d["modv"][l], mrow[:], reads=[t_row], writes=[C.T(f"modv{l}")])
    S.barrier()


class PsumPool:
    def __init__(self, C, st, n, name, shape=(128, 512), dtype=F32):
        self.tiles = [st.enter_context(C.nc.psum_tensor(f"{name}{i}", list(shape), dtype)) for i in range(n)]
        self.toks = [Tok(f"{name}{i}") for i in range(n)]
        self.i = 0

    def get(self):
        i = self.i
        self.i = (i + 1) % len(self.tiles)
        return self.tiles[i], self.toks[i]


class SbPool:
    def __init__(self, C, st, n, name, shape, dtype):
        self.tiles = [st.enter_context(C.nc.sbuf_tensor(f"{name}{i}", list(shape), dtype)) for i in range(n)]
        self.toks = [Tok(f"{name}{i}") for i in range(n)]
        self.i = 0

    def get(self):
        i = self.i
        self.i = (i + 1) % len(self.tiles)
        return self.tiles[i], self.toks[i]


def mm(S, out, lhsT, rhs, first, last, reads, wtok):
    S.op("pe", lambda e: e.matmul(out, lhsT=lhsT, rhs=rhs, start=first, stop=last),
         reads=reads, writes=[wtok], acc=not first)


def act(S, out, in_, func, reads, writes, acc=False, **kw):
    S.op("act", lambda e: e.activation(out=out, in_=in_, func=func, **kw), reads=reads, writes=writes, acc=acc)


def tt(S, eng, out, in0, in1, op, reads, writes, acc=False):
    S.op(eng, lambda e: e.tensor_tensor(out=out, in0=in0, in1=in1, op=op), reads=reads, writes=writes, acc=acc)


def ts(S, eng, out, in0, s1, s2, op0, op1, reads, writes, acc=False):
    if op1 is None:
        S.op(eng, lambda e: e.tensor_scalar(out=out, in0=in0, scalar1=s1, scalar2=None, op0=op0),
             reads=reads, writes=writes, acc=acc)
    else:
        S.op(eng, lambda e: e.tensor_scalar(out=out, in0=in0, scalar1=s1, scalar2=s2, op0=op0, op1=op1),
             reads=reads, writes=writes, acc=acc)


def stt(S, out, in0, scalar, in1, op0, op1, reads, writes, acc=False):
    S.op("dve", lambda e: e.scalar_tensor_tensor(out=out, in0=in0, scalar=scalar, in1=in1, op0=op0, op1=op1),
         reads=reads, writes=writes, acc=acc)


def cp(S, eng, out, in_, reads, writes, acc=False):
    if eng == "act":
        S.op("act", lambda e: e.copy(out=out, in_=in_), reads=reads, writes=writes, acc=acc)
    else:
        S.op(eng, lambda e: e.tensor_copy(out=out, in_=in_), reads=reads, writes=writes, acc=acc)


def make_ident(C, st):
    nc, S = C.nc, C.S
    idi = st.enter_context(nc.sbuf_tensor("c_idi", [128, 128], I32))
    idb = st.enter_context(nc.sbuf_tensor("c_idb", [128, 128], BF16))
    idf = st.enter_context(nc.sbuf_tensor("c_idf", [128, 128], F32))
    onb = st.enter_context(nc.sbuf_tensor("c_onb", [128, 128], BF16))
    t_i, t_c = Tok(), Tok("consts")
    S.op("pool", lambda e: e.iota(idi[:], pattern=[[1, 128]], base=0, channel_multiplier=-1), writes=[t_i])
    ts(S, "dve", idb[:], idi[:], 0, None, ALU.is_equal, None, [t_i], [t_c], acc=True)
    ts(S, "dve", idf[:], idi[:], 0, None, ALU.is_equal, None, [t_i], [t_c], acc=True)
    S.op("dve", lambda e: e.memset(onb[:], 1.0), writes=[t_c], acc=True)
    utri = st.enter_context(nc.sbuf_tensor("c_utri", [128, 128], BF16))
    ts(S, "dve", utri[:], idi[:], 0, None, ALU.is_gt, None, [t_i], [t_c], acc=True)
    C.utri = utri
    C.idb, C.idf, C.onb, C.t_c = idb, idf, onb, t_c


O_QA, O_QAS, O_KA, O_KAS, O_KR, O_KRS, O_CQ, O_CKV, O_U, O_VA, O_G, W_EXT = (
    0, 512, 1024, 1152, 1280, 1312, 1344, 1600, 1856, 2368, 2496, 5568)
W_N = O_G
GROUPS = [(0, 256)] + [(256 + 512 * i, 512) for i in range(8)]


def bcast_row(ap_row, n):
    return ap_row.to_broadcast([128, n])


def load_cast(C, st, name, dst, src3, nk, ncol, t_dst, scale_ap=None, scale_tok=None, half=2784):
    S = C.S
    stg = SbPool(C, st, 2, name, [128, half], F32)
    for kc in range(nk):
        for c0 in range(0, ncol, half):
            w = min(half, ncol - c0)
            s_t, s_k = stg.get()
            S.dma("sp", s_t[:, 0:w], src3[:, kc, c0:c0 + w], writes=[s_k])
            if scale_ap is None:
                cp(S, "act", dst[:, kc, c0:c0 + w], s_t[:, 0:w], [s_k], [t_dst], acc=True)
            else:
                ts(S, "dve", dst[:, kc, c0:c0 + w], s_t[:, 0:w], scale_ap(kc), None, ALU.mult, None,
                   [s_k, scale_tok], [t_dst], acc=True)


def phase_inproj(C, l):
    nc, S = C.nc, C.S
    import contextlib
    d = C.d
    hsrc = d["h0"] if l == 0 else d["h_s"]
    with contextlib.ExitStack() as st:
        sb = lambda name, shape, dt: st.enter_context(nc.sbuf_tensor(f"n{l}_{name}", list(shape), dt))
        wsb = sb("w", [128, 8, W_N], BF16)
        wq = sb("wq", [128, 2, 1024], BF16)
        wkv = sb("wkv", [128, 2, 1024], BF16)
        gq = sb("gq", [128, 4], F32)
        Gm, Sm = sb("Gm", [128, 2, 1024], F32), sb("Sm", [128, 2, 1024], F32)
        t_w, t_wq, t_gq, t_mod = Tok(), Tok(), Tok(), Tok()
        wext = d["w_in_ext"][l].rearrange("(kc p) n -> p kc n", p=128)
        load_cast(C, st, f"n{l}_stg", wsb, wext, 8, W_N, t_w)
        S.dma("sp", gq[:, 0:2], d["q_norm_gT"][l], writes=[t_gq], acc=True)
        S.dma("sp", gq[:, 2:4], d["kv_norm_gT"][l], writes=[t_gq], acc=True)
        for wi, (wname, wdst) in enumerate((("w_uq_ext", wq), ("w_ukv_ext", wkv))):
            wsrc = d[wname][l].rearrange("(kc p) n -> p kc n", p=128)
            load_cast(C, st, f"n{l}_stg{wi}", wdst, wsrc, 2, 1024, t_wq,
                      scale_ap=lambda kc, wi=wi: gq[:, 2 * wi + kc:2 * wi + kc + 1], scale_tok=t_gq, half=1024)
        mv = d["modv"][l]
        tmpp = SbPool(C, st, 2, f"n{l}_tmp", [128, 1024], F32)
        gn, k_gn = tmpp.get()
        S.dma("sp", gn[:], bcast_row(d["norm_mix_g"][l:l + 1, :], 1024), writes=[k_gn])
        for r in range(2):
            S.dma("sp", Sm[:, r, :], bcast_row(mv[r:r + 1, 0:1024], 1024), reads=[C.T(f"modv{l}")], writes=[t_mod], acc=True)
            S.dma("sp", Gm[:, r, :], bcast_row(mv[r:r + 1, 1024:2048], 1024), reads=[C.T(f"modv{l}")], writes=[t_mod], acc=True)
        t_mod2 = Tok()
        for r in range(2):
            stt(S, Gm[:, r, :], Gm[:, r, :], 1.0, gn[:], ALU.add, ALU.mult, [t_mod, k_gn], [t_mod2], acc=True)
        if STOP == 1:
            return
        pp = PsumPool(C, st, 5, f"n{l}_pp")
        ptp = PsumPool(C, st, 2, f"n{l}_ptp", (128, 8, 128), BF16)
        hp = SbPool(C, st, 2, f"n{l}_h", [128, 1024], F32)
        hxp = SbPool(C, st, 2, f"n{l}_hx", [128, 1024], BF16)
        hxTp = SbPool(C, st, 2, f"n{l}_hxT", [128, 8, 512], BF16)
        ssp = SbPool(C, st, 4, f"n{l}_ss", [128, 4], F32)
        sqj = sb("sqj", [128, 1024], BF16)
        t_sqj = Tok()
        ropep = SbPool(C, st, 1, f"n{l}_rope", [128, 4, 512], F32)
        r1p = SbPool(C, st, 1, f"n{l}_r1", [128, 512], F32)
        r2p = SbPool(C, st, 1, f"n{l}_r2", [128, 512], F32)
        o4p = SbPool(C, st, 4, f"n{l}_o4", [128, 4, 512], BF16)
        krTp = SbPool(C, st, 1, f"n{l}_krT", [32, 512], BF16)
        latfp = SbPool(C, st, 1, f"n{l}_latf", [128, 2, 512], F32)
        latqp = SbPool(C, st, 1, f"n{l}_latq", [128, 2, 512], BF16)
        rbcp = SbPool(C, st, 1, f"n{l}_rbc", [128, 512], F32)
        cqnp = SbPool(C, st, 1, f"n{l}_cqn", [128, 2, 512], BF16)
        ckvnp = SbPool(C, st, 1, f"n{l}_ckvn", [128, 2, 512], BF16)
        vap = SbPool(C, st, 1, f"n{l}_va", [128, 4, 2, 65], BF16)
        vbp = SbPool(C, st, 1, f"n{l}_vb", [128, 4, 8, 65], BF16)
        for pool_ in (vap, vbp):
            for t_, k_ in zip(pool_.tiles, pool_.toks):
                S.op("pool", lambda e, t_=t_: e.memset(t_[:], 1.0), writes=[k_])
        t_all = [t_mod2, C.t_c]

        for g, (t0, n) in enumerate(GROUPS):
            ntl = n // 128
            isx = g > 0
            mr = 0 if isx else 1
            hxT, k_hxT = hxTp.get()
            if isx:
                rp, k_rp = ropep.get()
                p0 = t0 - 256
                for i, nm in enumerate(("ropeA_c", "ropeA_s", "ropeB_c", "ropeB_s")):
                    S.dma("sp", rp[:, i, :], d[nm][:, p0:p0 + 512], writes=[k_rp], acc=(i > 0))
            for ti in range(ntl):
                r0 = t0 + ti * 128
                ht, k_h = hp.get()
                S.dma("sp", ht[:], hsrc[r0:r0 + 128, :], reads=[C.T(f"h:{r0 // 128}")], writes=[k_h])
                ss, k_ss = ssp.get()
                act(S, sqj[:], ht[:], AF.Square, [k_h], [t_sqj, k_ss], accum_out=ss[:, 0:1])
                act(S, ss[:, 1:2], ss[:, 0:1], AF.Sqrt, [k_ss], [k_ss], acc=True, scale=1.0 / DM, bias=EPS)
                S.op("dve", lambda e, ss=ss: e.reciprocal(out=ss[:, 2:3], in_=ss[:, 1:2]), reads=[k_ss], writes=[k_ss], acc=True)
                tm, k_tm = tmpp.get()
                stt(S, tm[:], ht[:], ss[:, 2:3], Gm[:, mr, :], ALU.mult, ALU.mult, [k_h, k_ss] + t_all, [k_tm])
                hx, k_hx = hxp.get()
                tt(S, "dve", hx[:], tm[:], Sm[:, mr, :], ALU.add, [k_tm] + t_all, [k_hx])
                tp, k_tp = ptp.get()
                for kc in range(8):
                    S.op("pe", lambda e, tp=tp, hx=hx, kc=kc: e.transpose(tp[:, kc, :], hx[:, kc * 128:(kc + 1) * 128], C.idb[:]),
                         reads=[k_hx, C.t_c], writes=[k_tp], acc=(kc > 0))
                cp(S, "act", hxT[:, :, ti * 128:(ti + 1) * 128], tp[:], [k_tp], [k_hxT], acc=(ti > 0))
            if STOP == 2:
                continue
            S.dma("pool", d["hxT_s"].rearrange("c p t -> p c t")[:, :, t0:t0 + n], hxT[:, :, 0:n],
                  reads=[k_hxT], writes=[C.T(f"hxT:{g}")])
            if STOP == 3:
                continue

            def proj(col0, m, ps):
                pt, pk = ps
                for kc in range(8):
                    mm(S, pt[0:m, 0:n], wsb[:, kc, col0:col0 + m], hxT[:, kc, 0:n], kc == 0, kc == 7,
                       [t_w, k_hxT], pk)

            def rope_evac(prj, col0, cols0, m, out_ap, out_k, ri, acc):
                ps = pp.get()
                prj(col0, m, ps)
                if not isx:
                    cp(S, "act", out_ap, ps[0][0:m, 0:n], [ps[1]], [out_k], acc=acc)
                    return
                ps2 = pp.get()
                prj(cols0, m, ps2)
                r1, k1 = r1p.get()
                r2, k2 = r2p.get()
                tt(S, "dve", r1[0:m, 0:n], ps[0][0:m, 0:n], rp[0:m, ri, 0:n], ALU.mult, [ps[1], k_rp], [k1])
                tt(S, "dve", r2[0:m, 0:n], ps2[0][0:m, 0:n], rp[0:m, ri + 1, 0:n], ALU.mult, [ps2[1], k_rp], [k2])
                tt(S, "dve", out_ap, r1[0:m, 0:n], r2[0:m, 0:n], ALU.add, [k1, k2], [out_k], acc=acc)

            qaT, k_qaT = o4p.get()
            for c in range(4):
                rope_evac(proj, O_QA + c * 128, O_QAS + c * 128, 128, qaT[:, c, 0:n], k_qaT, 0, c > 0)
            S.dma("pool", d["qaT_s"].rearrange("c p t -> p c t")[:, :, t0:t0 + n], qaT[:, :, 0:n],
                  reads=[k_qaT], writes=[C.T(f"qaT:{g}")])
            kaT, k_kaT = o4p.get()
            rope_evac(proj, O_KA, O_KAS, 128, kaT[:, 0, 0:n], k_kaT, 0, False)
            S.dma("pool", d["kaT_s"][:, t0:t0 + n], kaT[:, 0, 0:n], reads=[k_kaT], writes=[C.T(f"kaT:{g}")])
            krT, k_krT = krTp.get()
            rope_evac(proj, O_KR, O_KRS, 32, krT[:, 0:n], k_krT, 2, False)
            for h in range(8):
                S.dma("pool", d["kbT_s"][h, 64:96, t0:t0 + n], krT[:, 0:n], reads=[k_krT],
                      writes=[C.T(f"kbT:{g}")], acc=True)
            if STOP == 4:
                continue
            uT, k_uT = o4p.get()
            for c in range(4):
                ps = pp.get()
                proj(O_U + c * 128, 128, ps)
                cp(S, "act", uT[:, c, 0:n], ps[0][:, 0:n], [ps[1]], [k_uT], acc=(c > 0))
            S.dma("pool", d["uT_s"].rearrange("c p t -> p c t")[:, :, t0:t0 + n], uT[:, :, 0:n],
                  reads=[k_uT], writes=[C.T(f"uT:{g}")])
            va, k_va = vap.get()
            for ti in range(ntl):
                pt, pk = pp.get()
                for kc in range(8):
                    mm(S, pt[:, 0:128], hxT[:, kc, ti * 128:(ti + 1) * 128], wsb[:, kc, O_VA:O_VA + 128],
                       kc == 0, kc == 7, [t_w, k_hxT], pk)
                cp(S, "act", va[:, ti, :, 0:64], pt[:, 0:128].rearrange("p (g e) -> p g e", g=2), [pk], [k_va], acc=(ti > 0))
            S.dma("pool", d["va_s"][t0:t0 + n, :].rearrange("(i p) f -> p i f", p=128),
                  va[:, 0:ntl, :, :].rearrange("p i g e -> p i (g e)"), reads=[k_va], writes=[C.T(f"va:{g}")])
            if STOP == 5:
                continue
            lat_out = []
            for (col0, npool) in ((O_CQ, cqnp), (O_CKV, ckvnp)):
                lf, k_lf = latfp.get()
                lq, k_lq = latqp.get()
                for c in range(2):
                    ps = pp.get()
                    proj(col0 + c * 128, 128, ps)
                    act(S, lq[:, c, 0:n], ps[0][:, 0:n], AF.Square, [ps[1]], [k_lq], acc=(c > 0))
                    cp(S, "act", lf[:, c, 0:n], ps[0][:, 0:n], [ps[1]], [k_lf], acc=(c > 0))
                if STOP == 61:
                    continue
                pt, pk = pp.get()
                for c in range(2):
                    mm(S, pt[:, 0:n], C.onb[:], lq[:, c, 0:n], c == 0, c == 1, [k_lq, C.t_c], pk)
                rb, k_rb = rbcp.get()
                if STOP == 62:
                    continue
                act(S, rb[:, 0:n], pt[:, 0:n], AF.Sqrt, [pk], [k_rb], scale=1.0 / 256, bias=EPS)
                if STOP == 63:
                    continue
                S.op("dve", lambda e, rb=rb, n=n: e.reciprocal(out=rb[:, 0:n], in_=rb[:, 0:n]), reads=[k_rb], writes=[k_rb], acc=True)
                if STOP == 64:
                    continue
                ln, k_ln = npool.get()
                for c in range(2):
                    tt(S, "dve", ln[:, c, 0:n], lf[:, c, 0:n], rb[:, 0:n], ALU.mult, [k_lf, k_rb], [k_ln], acc=(c > 0))
                lat_out.append((ln, k_ln))
            if STOP > 60:
                continue
            (cqn, k_cqn), (ckvn, k_ckvn) = lat_out
            if STOP == 6:
                continue

            def mkproj2(wt, lat, k_lat):
                def proj2(col0, m, ps):
                    pt, pk = ps
                    for kc in range(2):
                        mm(S, pt[0:m, 0:n], wt[:, kc, col0:col0 + m], lat[:, kc, 0:n], kc == 0, kc == 1, [t_wq, k_lat], pk)
                return proj2
            pq = mkproj2(wq, cqn, k_cqn)
            pk_ = mkproj2(wkv, ckvn, k_ckvn)
            qnT, k_qnT = o4p.get()
            for c in range(4):
                ps = pp.get()
                pq(c * 128, 128, ps)
                cp(S, "act", qnT[:, c, 0:n], ps[0][:, 0:n], [ps[1]], [k_qnT], acc=(c > 0))
            for h in range(8):
                S.dma("pool", d["qbT_s"][h, 0:64, t0:t0 + n], qnT[(h % 2) * 64:(h % 2) * 64 + 64, h // 2, 0:n],
                      reads=[k_qnT], writes=[C.T(f"qbT:{g}")], acc=True)
            if STOP == 7:
                continue
            qrT, k_qrT = o4p.get()
            for c in range(2):
                rope_evac(pq, 512 + c * 128, 768 + c * 128, 128, qrT[:, c, 0:n], k_qrT, 2, c > 0)
            for h in range(8):
                S.dma("pool", d["qbT_s"][h, 64:96, t0:t0 + n], qrT[(h % 4) * 32:(h % 4) * 32 + 32, h // 4, 0:n],
                      reads=[k_qrT], writes=[C.T(f"qbT:{g}")], acc=True)
            if STOP == 8:
                continue
            knT, k_knT = o4p.get()
            for c in range(4):
                ps = pp.get()
                pk_(c * 128, 128, ps)
                cp(S, "act", knT[:, c, 0:n], ps[0][:, 0:n], [ps[1]], [k_knT], acc=(c > 0))
            for h in range(8):
                S.dma("pool", d["kbT_s"][h, 0:64, t0:t0 + n], knT[(h % 2) * 64:(h % 2) * 64 + 64, h // 2, 0:n],
                      reads=[k_knT], writes=[C.T(f"kbT:{g}")], acc=True)
            if STOP == 9:
                continue
            vb, k_vb = vbp.get()
            for ti in range(ntl):
                pt, pk = pp.get()
                for kc in range(2):
                    mm(S, pt[:, :], ckvn[:, kc, ti * 128:(ti + 1) * 128], wkv[:, kc, 512:1024], kc == 0, kc == 1,
                       [t_wq, k_ckvn], pk)
                cp(S, "act", vb[:, ti, :, 0:64], pt[:, :].rearrange("p (g e) -> p g e", g=8), [pk], [k_vb], acc=(ti > 0))
            S.dma("pool", d["vb_s"][t0:t0 + n, :].rearrange("(i p) f -> p i f", p=128),
                  vb[:, 0:ntl, :, :].rearrange("p i g e -> p i (g e)"), reads=[k_vb], writes=[C.T(f"vb:{g}")])
    S.barrier()


def _deint(n):
    return np.concatenate([np.arange(0, n, 2), np.arange(1, n, 2)])


def _deint_sw(n):
    return np.concatenate([np.arange(1, n, 2), np.arange(0, n, 2)])


def _w_in_cols():
    cols = []
    for sw in (False, True):
        for c in range(4):
            for h in (c, 4 + c):
                cols.append(h * 64 + (_deint_sw(64) if sw else _deint(64)))
    for sw in (False, True):
        for g in range(2):
            cols.append(512 + g * 64 + (_deint_sw(64) if sw else _deint(64)))
    cols.append(1280 + _deint(32))
    cols.append(1280 + _deint_sw(32))
    cols.append(np.arange(768, 1280))
    cols.append(np.arange(1312, 1824))
    cols.append(np.arange(640, 768))
    cols.append(np.arange(1824, 4896))
    out = np.concatenate(cols)
    assert out.shape[0] == W_EXT
    return out


def _w_uq_cols():
    cols = [h * 96 + np.arange(64) for h in range(8)]
    cols += [h * 96 + 64 + _deint(32) for h in range(8)]
    cols += [h * 96 + 64 + _deint_sw(32) for h in range(8)]
    return np.concatenate(cols)


def _w_ukv_cols():
    cols = [h * 128 + np.arange(64) for h in range(8)]
    cols += [h * 128 + 64 + np.arange(64) for h in range(8)]
    return np.concatenate(cols)


def _rope_tables():
    t = np.arange(SEQ)
    row = (t // 64).astype(np.float32)
    col = (t % 64).astype(np.float32)

    def tabs(rot):
        axis_dim = rot // 2
        inv = (np.float32(10000.0) ** (-np.arange(0, axis_dim, 2, dtype=np.float32) / np.float32(axis_dim))).astype(np.float32)
        ang = np.concatenate([row[:, None] * inv, col[:, None] * inv], axis=-1).astype(np.float32)
        cos, sin = np.cos(ang).astype(np.float32), np.sin(ang).astype(np.float32)
        half = rot // 2
        ct = np.concatenate([cos, cos], axis=1).T
        st_ = np.concatenate([-sin, sin], axis=1).T
        rep = 128 // rot
        return np.ascontiguousarray(np.tile(ct, (rep, 1))), np.ascontiguousarray(np.tile(st_, (rep, 1)))
    ca, sa = tabs(64)
    cb, sb_ = tabs(32)
    return ca, sa, cb, sb_


SCRATCH = {
    "modv": ([2, 2, 6144], F32),
    "h_s": ([NTOK, DM], F32),
    "hxT_s": ([8, 128, NTOK], BF16),
    "qaT_s": ([4, 128, NTOK], BF16),
    "kaT_s": ([128, NTOK], BF16),
    "va_s": ([NTOK, 130], BF16),
    "qbT_s": ([8, 96, NTOK], BF16),
    "kbT_s": ([8, 96, NTOK], BF16),
    "vb_s": ([NTOK, 520], BF16),
    "uT_s": ([4, 128, NTOK], BF16),
    "oaT_s": ([4, 128, NTOK], BF16),
    "obT_s": ([4, 128, NTOK], BF16),
    "ocT_s": ([4, 128, NTOK], BF16),
    "gT_s": ([32, NTOK], F32),
    "wgu_b": ([4096, 4096], BF16),
    "wdn_b": ([4096, 2048], BF16),
    "xbuf": ([12800, DM], BF16),
    "ybuf": ([12800, DM], F32),
}
INPUTS = {
    "h0": ([NTOK, DM], F32),
    "cT": ([128, 8, 2], F32),
    "w_mod": ([2, DM, 6144], F32),
    "b_mod": ([2, 6144], F32),
    "norm_mix_g": ([2, DM], F32),
    "norm_ffn_g": ([2, DM], F32),
    "w_in_ext": ([2, DM, W_EXT], F32),
    "w_uq_ext": ([2, 256, 1024], F32),
    "w_ukv_ext": ([2, 256, 1024], F32),
    "q_norm_gT": ([2, 128, 2], F32),
    "kv_norm_gT": ([2, 128, 2], F32),
    "ropeA_c": ([128, SEQ], F32),
    "ropeA_s": ([128, SEQ], F32),
    "ropeB_c": ([128, SEQ], F32),
    "ropeB_s": ([128, SEQ], F32),
    "sink": ([2, 8], F32),
    "sink_row": ([2, 2, 512], F32),
    "w_pool": ([2, 4, 128, 128], F32),
    "pool_scaleT": ([2, 128, 4], F32),
    "invx": ([4, SEQ], F32),
    "invc": ([4, LC], F32),
    "w_br": ([2, 3, 512, DM], F32),
    "w_out": ([2, DM, DM], F32),
    "w_r": ([2, DM, 36], F32),
    "b_r": ([2, 36], F32),
    "w_gu_h": ([2, 32 * 128, 4096], F32),
    "w_dn_h": ([2, 32 * 128, 2048], F32),
    "final_g": ([1, DM], F32),
}


def prep_shared(inp):
    out = {}
    f = lambda a: np.ascontiguousarray(np.asarray(a, dtype=np.float32))
    out["w_mod"] = f(inp["w_mod"])
    out["b_mod"] = f(inp["b_mod"])
    out["norm_mix_g"] = f(inp["norm_mix_g"])
    out["norm_ffn_g"] = f(inp["norm_ffn_g"])
    out["sink"] = f(inp["sink"])
    out["sink_row"] = f(np.repeat(np.asarray(inp["sink"]).reshape(2, 2, 4), 128, axis=-1))
    out["w_pool"] = f(inp["w_pool"])
    out["pool_scaleT"] = f(np.asarray(inp["pool_scale"]).reshape(2, 4, 128).transpose(0, 2, 1))
    out["w_br"] = f(np.stack([np.asarray(inp[k]) for k in ("w_br_a", "w_br_b", "w_br_c")], axis=1))
    out["w_out"] = f(inp["w_out"])
    out["w_r"] = f(np.concatenate([np.asarray(inp["w_rg"]), np.asarray(inp["w_re"])], axis=-1))
    out["b_r"] = f(np.concatenate([np.asarray(inp["b_rg"]), np.asarray(inp["b_re"])], axis=-1))
    out["w_gu_h"] = f(np.asarray(inp["w_gu"]).reshape(2, 32, 8, 128, 512).transpose(0, 1, 3, 2, 4).reshape(2, 32 * 128, 4096))
    out["w_dn_h"] = f(np.asarray(inp["w_dn"]).reshape(2, 32, 2, 128, 1024).transpose(0, 1, 3, 2, 4).reshape(2, 32 * 128, 2048))
    out["final_g"] = f(np.asarray(inp["final_g"]).reshape(1, DM))
    for nm, L in (("invx", SEQ), ("invc", LC)):
        t = np.arange(L)
        rows = []
        for r in (1, 2, 4, 8):
            cnt = np.minimum(t + r + 1, L) - np.maximum(t - r, 0)
            rows.append((1.0 / cnt.astype(np.float32)).astype(np.float32))
        out[nm] = np.stack(rows)
    out["w_in_ext"] = f(np.asarray(inp["w_in"])[:, :, _w_in_cols()])
    out["w_uq_ext"] = f(np.asarray(inp["w_uq"])[:, :, _w_uq_cols()])
    out["w_ukv_ext"] = f(np.asarray(inp["w_ukv"])[:, :, _w_ukv_cols()])
    out["q_norm_gT"] = f(np.asarray(inp["q_norm_g"]).reshape(2, 2, 128).transpose(0, 2, 1))
    out["kv_norm_gT"] = f(np.asarray(inp["kv_norm_g"]).reshape(2, 2, 128).transpose(0, 2, 1))
    ca, sa, cb, sb_ = _rope_tables()
    out["ropeA_c"], out["ropeA_s"], out["ropeB_c"], out["ropeB_s"] = ca, sa, cb, sb_
    return out


def prep_core(inp, b):
    out = {}
    x, ctx, c, cc = (np.asarray(inp[k], dtype=np.float32) for k in ("x", "ctx", "c", "c_ctx"))
    out["h0"] = np.ascontiguousarray(np.concatenate([ctx[b], x[b]], axis=0))
    out["cT"] = np.ascontiguousarray(np.stack([c[b].reshape(8, 128).T, cc.reshape(8, 128).T], axis=-1))
    return out


def build_program(phases, debug_out=()):
    nc = bass.Bass("TRN2", target_bir_lowering=False)
    S = Sched(nc)
    C = Ctx(nc, S)
    for k, (shape, dt) in INPUTS.items():
        C.d[k] = nc.dram_tensor(k, list(shape), dt, kind="ExternalInput").ap()
    for k, (shape, dt) in SCRATCH.items():
        kind = "ExternalOutput" if k in debug_out else "Internal"
        C.d[k] = nc.dram_tensor(k, list(shape), dt, kind=kind).ap()
    import contextlib
    with contextlib.ExitStack() as st:
        make_ident(C, st)
        phases(C)
        S.emit()
    return nc


NEG = -30000.0


def attn_pipeline(C, st, name, items, scale, hook=None, hook_every=1):
    S = C.S
    pps = PsumPool(C, st, 2, f"{name}_ps", (128, 2, 512), F32)
    pac = PsumPool(C, st, 2, f"{name}_ac", (128, 512), F32)
    ptp = SbPool(C, st, 3, f"{name}_pt", [128, 2, 512], BF16)
    steps = []
    for it in items:
        ks = it["keys"]
        for a in range(0, len(ks), 2):
            steps.append((it, a, ks[a:a + 2]))
    state = {}

    def emit_S(si):
        it, a, chunk = steps[si]
        n = it["n"]
        if "q" not in it:
            it["load_q"](it)
        ps, k_ps = pps.get()
        for jj, (k_ap, v_ap, msk) in enumerate(chunk):
            mm(S, ps[:, jj, 0:n], k_ap, it["q"], True, msk is None, it["kv_reads"] + it["q_reads"], k_ps)
            if msk is not None:
                mm(S, ps[:, jj, 0:n], C.idb[:], msk[:, 0:n], False, True, [it["m_tok"], C.t_c], k_ps)
        pt, k_pt = ptp.get()
        act(S, pt[:, 0:len(chunk), 0:n], ps[:, 0:len(chunk), 0:n], AF.Exp, [k_ps], [k_pt], scale=scale)
        state[si] = (pt, k_pt)

    def emit_PV(si):
        it, a, chunk = steps[si]
        n = it["n"]
        pt, k_pt = state.pop(si)
        if a == 0:
            it["acc"] = pac.get()
        acc, k_acc = it["acc"]
        nk = len(it["keys"])
        for jj, (k_ap, v_ap, msk) in enumerate(chunk):
            mm(S, acc[0:65, 0:n], v_ap, pt[:, jj, 0:n], a + jj == 0, a + jj == nk - 1, [k_pt] + it["kv_reads"], k_acc)
        if a + len(chunk) == nk:
            it["finish"](acc, k_acc)

    if not steps:
        return
    emit_S(0)
    for si in range(len(steps)):
        if hook is not None and si % hook_every == 0:
            hook(si // hook_every)
        if si + 1 < len(steps):
            emit_S(si + 1)
        emit_PV(si)


def _norm_store(C, pools, acc, k_acc, n, add_ap, add_tok, dst_ap, wtoks):
    S = C.S
    rsp, bcp, bcsp, op_, onesf = pools
    rs, k_rs = rsp.get()
    if add_ap is not None:
        tt(S, "dve", rs[64:65, 0:n], acc[64:65, 0:n], add_ap, ALU.add, [k_acc, add_tok], [k_rs])
        S.op("dve", lambda e, rs=rs, n=n: e.reciprocal(out=rs[64:65, 0:n], in_=rs[64:65, 0:n]), reads=[k_rs], writes=[k_rs], acc=True)
    else:
        S.op("dve", lambda e, rs=rs, n=n, acc=acc: e.reciprocal(out=rs[64:65, 0:n], in_=acc[64:65, 0:n]), reads=[k_acc], writes=[k_rs])
    bc, k_bc = bcp.get()
    mm(S, bc[0:64, 0:n], onesf[64:65, 0:64], rs[64:65, 0:n], True, True, [k_rs, C.t_c], k_bc)
    bcs, k_bcs = bcsp.get()
    cp(S, "act", bcs[:, 0:n], bc[0:64, 0:n], [k_bc], [k_bcs])
    o, k_o = op_.get()
    tt(S, "dve", o[:, 0:n], acc[0:64, 0:n], bcs[:, 0:n], ALU.mult, [k_acc, k_bcs], [k_o])
    S.dma("pool", dst_ap(o), o[:, 0:n] if dst_ap.flat else o[:, 0:n].rearrange("p (u t) -> p u t", u=4), reads=[k_o], writes=wtoks, acc=True)


class _Dst:
    def __init__(self, ap, flat):
        self.ap, self.flat = ap, flat

    def __call__(self, o):
        return self.ap


def _norm_pools(C, st, name):
    nc = C.nc
    rsp = SbPool(C, st, 2, f"{name}_rs", [65, 512], F32)
    bcp = PsumPool(C, st, 1, f"{name}_bc", (64, 512), F32)
    bcsp = SbPool(C, st, 2, f"{name}_bcs", [64, 512], F32)
    op_ = SbPool(C, st, 3, f"{name}_o", [64, 512], BF16)
    onesf = st.enter_context(nc.sbuf_tensor(f"{name}_onesf", [128, 64], F32))
    C.S.op("dve", lambda e: e.memset(onesf[:], 1.0), writes=[C.t_c], acc=True)
    return (rsp, bcp, bcsp, op_, onesf)


def phase_attn_a(C, l):
    nc, S, d = C.nc, C.S, C.d
    import contextlib
    last = (l == DEPTH - 1)
    with contextlib.ExitStack() as st:
        sb = lambda name, shape, dt: st.enter_context(nc.sbuf_tensor(f"a{l}_{name}", list(shape), dt))
        ka = sb("ka", [128, NTOK], BF16)
        va = sb("va", [128, NT, 130], BF16)
        esk = sb("esk", [65, 2, 512], F32)
        mi = sb("mi", [128, 512], I32)
        mP, mN = sb("mP", [128, 512], BF16), sb("mN", [128, 512], BF16)
        t_ka, t_va, t_es, t_mi, t_m = Tok(), Tok(), Tok(), Tok(), Tok()
        S.dma("sp", ka[:], d["kaT_s"][:, :], reads=[C.T(f"kaT:{g}") for g in range(9)], writes=[t_ka])
        S.dma("sp", va[:], d["va_s"].rearrange("(i p) f -> p i f", p=128), reads=[C.T(f"va:{g}") for g in range(9)], writes=[t_va])
        S.dma("sp", esk[64:65, :, :], d["sink_row"][l:l + 1, :, :], writes=[t_es])
        act(S, esk[64:65, :, :], esk[64:65, :, :], AF.Exp, [t_es], [t_es])
        S.op("pool", lambda e: e.iota(mi[:], pattern=[[0, 4], [1, 128]], base=0, channel_multiplier=-1), writes=[t_mi])
        ts(S, "dve", mP[:], mi[:], 0, NEG, ALU.is_gt, ALU.mult, [t_mi], [t_m], acc=True)
        ts(S, "dve", mN[:], mi[:], 0, NEG, ALU.is_lt, ALU.mult, [t_mi], [t_m], acc=True)
        pools = _norm_pools(C, st, f"a{l}")
        qp = SbPool(C, st, 3, f"a{l}_q", [128, 4, 128], BF16)
        qtiles = list(range(2, NT)) if last else list(range(NT))
        if QT_LIMIT:
            qtiles = qtiles[:QT_LIMIT]
        items = []
        for qt in qtiles:
            if qt < 2:
                keys = [(0, None), (1, None)]
            else:
                xq = qt - 2
                keys = [(0, None), (1, None)]
                if xq > 0:
                    keys.append((qt - 1, mP))
                keys.append((qt, None))
                if xq < 31:
                    keys.append((qt + 1, mN))
            for g in range(2):
                it = {"qt": qt, "g": g, "n": 512, "m_tok": t_m, "kv_reads": [t_ka, t_va],
                      "keys": [(ka[g * 64:(g + 1) * 64, kt * 128:(kt + 1) * 128], va[:, kt, g * 65:(g + 1) * 65], msk)
                               for kt, msk in keys]}
                items.append(it)
        cur = {}

        def load_q(it):
            qt, g = it["qt"], it["g"]
            if qt not in cur:
                q, k_q = qp.get()
                S.dma("sp", q[:], d["qaT_s"].rearrange("c p t -> p c t")[:, :, qt * 128:(qt + 1) * 128],
                      reads=[C.T(f"qaT:{gg}") for gg in range(9)], writes=[k_q])
                cur.clear()
                cur[qt] = (q, k_q)
            q, k_q = cur[qt]
            it["q"] = q[g * 64:(g + 1) * 64, :, :].rearrange("p c t -> p (c t)")
            it["q_reads"] = [k_q]

        for it in items:
            qt, g = it["qt"], it["g"]
            it["load_q"] = load_q
            dst = d["oaT_s"][2 * g:2 * g + 2].rearrange("u2 (u1 e) t -> e (u2 u1) t", u1=2)[:, :, qt * 128:(qt + 1) * 128]
            it["finish"] = (lambda acc, k_acc, g=g, qt=qt, dst=dst:
                            _norm_store(C, pools, acc, k_acc, 512, esk[64:65, g, :], t_es, _Dst(dst, False),
                                        [C.T(f"oaT_s:{qt}")]))
        attn_pipeline(C, st, f"a{l}", items, 0.125)
    S.barrier()


def phase_attn_b(C, l):
    nc, S, d = C.nc, C.S, C.d
    import contextlib
    last = (l == DEPTH - 1)
    scale = 96.0 ** -0.5
    with contextlib.ExitStack() as st:
        sb = lambda name, shape, dt: st.enter_context(nc.sbuf_tensor(f"b{l}_{name}", list(shape), dt))
        kb = sb("kb", [96, 8, NTOK], BF16)
        vb = sb("vb", [128, NT, 520], BF16)
        t_kb, t_vb = Tok(), Tok()
        for h in range(8):
            S.dma("sp", kb[:, h, :], d["kbT_s"][h], reads=[C.T(f"kbT:{g}") for g in range(9)], writes=[t_kb], acc=True)
        S.dma("sp", vb[:], d["vb_s"].rearrange("(i p) f -> p i f", p=128), reads=[C.T(f"vb:{g}") for g in range(9)], writes=[t_vb])
        pools = _norm_pools(C, st, f"b{l}")
        qp = SbPool(C, st, 2, f"b{l}_q", [96, 8, 512], BF16)
        groups = list(enumerate(GROUPS))
        if last:
            groups = groups[1:]
        if QT_LIMIT:
            groups = groups[:2]
        items = []
        curq = {}

        def load_q(it):
            g, h = it["g"], it["h"]
            t0, n = GROUPS[g]
            if g not in curq:
                q, k_q = qp.get()
                S.dma("sp", q[:, :, 0:n], d["qbT_s"].rearrange("h p t -> p h t")[:, :, t0:t0 + n],
                      reads=[C.T(f"qbT:{g}")], writes=[k_q])
                curq.clear()
                curq[g] = (q, k_q)
            q, k_q = curq[g]
            it["q"] = q[:, h, 0:n]
            it["q_reads"] = [k_q]

        for g, (t0, n) in groups:
            keys = [0, 1] if g == 0 else list(range(NT))
            tiles = list(range(t0 // 128, (t0 + n) // 128))
            for h in range(8):
                dst = d["obT_s"][h // 2, (h % 2) * 64:(h % 2) * 64 + 64, t0:t0 + n]
                items.append({"n": n, "g": g, "h": h, "load_q": load_q, "kv_reads": [t_kb, t_vb], "m_tok": None,
                              "keys": [(kb[:, h, kt * 128:(kt + 1) * 128], vb[:, kt, h * 65:(h + 1) * 65], None) for kt in keys],
                              "finish": (lambda acc, k_acc, n=n, dst=dst, tiles=tiles:
                                         _norm_store(C, pools, acc, k_acc, n, None, None, _Dst(dst, True),
                                                     [C.T(f"obT_s:{t}") for t in tiles]))})
        stg_g = SbPool(C, st, 1, f"b{l}_sgg", [128, 4096], F32)
        stg_d = SbPool(C, st, 1, f"b{l}_sgd", [128, 2048], F32)
        cb_g = SbPool(C, st, 1, f"b{l}_cbg", [128, 4096], BF16)
        cb_d = SbPool(C, st, 1, f"b{l}_cbd", [128, 2048], BF16)

        def precast(k):
            if k >= 64 or QT_LIMIT:
                return
            e_, part = k // 2, k % 2
            srcw, dstw, sp_, cp_ = ((d["w_gu_h"], d["wgu_b"], stg_g, cb_g) if part == 0 else (d["w_dn_h"], d["wdn_b"], stg_d, cb_d))
            s_t, s_k = sp_.get()
            S.dma("sp", s_t[:], srcw[l, e_ * 128:(e_ + 1) * 128, :], writes=[s_k])
            c_t, c_k = cp_.get()
            cp(S, "dve", c_t[:], s_t[:], [s_k], [c_k])
            S.dma("sp", dstw[e_ * 128:(e_ + 1) * 128, :], c_t[:], reads=[c_k], writes=[C.T("wexp_b")], acc=True)

        nsteps = sum((len(it["keys"]) + 1) // 2 for it in items)
        attn_pipeline(C, st, f"b{l}", items, scale, hook=precast, hook_every=max(1, nsteps // 66))
    S.barrier()


def phase_pool(C, l):
    nc, S, d = C.nc, C.S, C.d
    import contextlib
    last = (l == DEPTH - 1)
    seqs = [(256, SEQ, "invx")] + ([] if last else [(0, LC, "invc")])
    with contextlib.ExitStack() as st:
        sb = lambda name, shape, dt: st.enter_context(nc.sbuf_tensor(f"c{l}_{name}", list(shape), dt))
        wps = sb("wps", [128, 4, 128], F32)
        wp = sb("wp", [128, 4, 128], BF16)
        psc = sb("psc", [128, 4], F32)
        Up = sb("Up", [128, SEQ + 16], F32)
        Aa, Ab = sb("Aa", [128, SEQ + 16], F32), sb("Ab", [128, SEQ + 16], F32)
        inv = sb("inv", [128, SEQ], F32)
        ub = sb("ub", [128, SEQ], BF16)
        pl = sb("pl", [128, SEQ], BF16)
        oc = sb("oc", [128, SEQ], BF16)
        t_wp, t_psc, t_Up, t_A, t_B, t_inv, t_ub, t_pl, t_oc = (Tok() for _ in range(9))
        S.dma("sp", wps[:], d["w_pool"][l].rearrange("g c e -> c g e"), writes=[t_wp])
        cp(S, "act", wp[:], wps[:], [t_wp], [t_wp])
        S.dma("sp", psc[:], d["pool_scaleT"][l], writes=[t_psc])
        pp = PsumPool(C, st, 3, f"c{l}_pp")
        eng = ["dve", "dve"]
        ei = 0
        for (t0, L, invname) in seqs:
            for g in range(4):
                r = (1, 2, 4, 8)[g]
                S.dma("sp", ub[:, 0:L], d["uT_s"][g, :, t0:t0 + L], reads=[C.T(f"uT:{q}") for q in range(9)], writes=[t_ub])
                S.dma("sp", inv[:, 0:L], bcast_row(d[invname][g:g + 1, :], L), writes=[t_inv])
                S.op("dve", lambda e, L=L: e.memset(Up[:, 0:L + 16], 0.0), writes=[t_Up])
                cp(S, "dve", Up[:, 8:8 + L], ub[:, 0:L], [t_ub], [t_Up])
                src, k_src, ln = Up, t_Up, L + 16
                bufs = [(Aa, t_A), (Ab, t_B)]
                step = 1
                bi = 0
                while step <= r:
                    dst, k_dst = bufs[bi]
                    bi ^= 1
                    nl = ln - step
                    tt(S, eng[ei % 2], dst[:, 0:nl], src[:, 0:nl], src[:, step:step + nl], ALU.add, [k_src], [k_dst])
                    ei += 1
                    src, k_src, ln = dst, k_dst, nl
                    step *= 2
                dst, k_dst = bufs[bi]
                tt(S, eng[ei % 2], dst[:, 0:L], src[:, 8 - r:8 - r + L], Up[:, 8 + r:8 + r + L], ALU.add, [k_src, t_Up], [k_dst])
                ei += 1
                tt(S, "dve", dst[:, 0:L], dst[:, 0:L], inv[:, 0:L], ALU.mult, [k_dst, t_inv], [k_dst])
                tt(S, "dve", pl[:, 0:L], dst[:, 0:L], Up[:, 8:8 + L], ALU.subtract, [k_dst, t_Up], [t_pl])
                for c0 in range(0, L, 512):
                    n = min(512, L - c0)
                    ps, k_ps = pp.get()
                    mm(S, ps[:, 0:n], wp[:, g, :], pl[:, c0:c0 + n], True, True, [t_wp, t_pl], k_ps)
                    act(S, oc[:, c0:c0 + n], ps[:, 0:n], AF.Copy, [k_ps, t_psc], [t_oc], acc=(c0 > 0), scale=psc[:, g:g + 1])
                S.dma("pool", d["ocT_s"][g, :, t0:t0 + L], oc[:, 0:L], reads=[t_oc], writes=[C.T(f"ocT:{g}:{t0}")])
    S.barrier()


def phase_merge(C, l):
    nc, S, d = C.nc, C.S, C.d
    import contextlib
    last = (l == DEPTH - 1)
    hsrc = d["h0"] if l == 0 else d["h_s"]
    with contextlib.ExitStack() as st:
        sb = lambda name, shape, dt: st.enter_context(nc.sbuf_tensor(f"g{l}_{name}", list(shape), dt))
        wg = sb("wg", [128, 8, 3072], BF16)
        wbr = sb("wbr", [128, 12, 1024], BF16)
        wo = sb("wo", [128, 2, 8, 1024], BF16)
        G1 = sb("G1", [128, 2, 1024], F32)
        t_wg, t_wbr, t_wo, t_G1 = Tok(), Tok(), Tok(), Tok()
        wext = d["w_in_ext"][l].rearrange("(kc p) n -> p kc n", p=128)[:, :, O_G:W_EXT]
        load_cast(C, st, f"g{l}_stg", wg, wext, 8, 3072, t_wg, half=768)
        wbsrc = d["w_br"][l].rearrange("b (kc p) n -> p (b kc) n", p=128)
        load_cast(C, st, f"g{l}_stg2", wbr, wbsrc, 12, 1024, t_wbr, half=512)
        for r in range(2):
            S.dma("sp", G1[:, r, :], bcast_row(d["modv"][l][r:r + 1, 2048:3072], 1024), reads=[C.T(f"modv{l}")], writes=[t_G1], acc=True)
        stgo = SbPool(C, st, 2, f"g{l}_stgo", [128, 1024], F32)
        wosrc = d["w_out"][l].rearrange("(kc p) n -> p kc n", p=128)
        for kc in range(8):
            s_t, s_k = stgo.get()
            S.dma("sp", s_t[:], wosrc[:, kc, :], writes=[s_k])
            for r in range(2):
                tt(S, "dve", wo[:, r, kc, :], s_t[:], G1[:, r, :], ALU.mult, [s_k, t_G1], [t_wo], acc=True)
        pp = PsumPool(C, st, 6, f"g{l}_pp")
        hxTp = SbPool(C, st, 1, f"g{l}_hxT", [128, 8, 512], BF16)
        oTp = SbPool(C, st, 1, f"g{l}_oT", [128, 12, 512], BF16)
        YTp = SbPool(C, st, 1, f"g{l}_YT", [128, 8, 512], BF16)
        sgp = SbPool(C, st, 3, f"g{l}_sg", [128, 512], BF16)
        tbp = SbPool(C, st, 4, f"g{l}_tb", [128, 512], F32)
        y1p = SbPool(C, st, 2, f"g{l}_y1", [128, 512], F32)
        hp = SbPool(C, st, 2, f"g{l}_h", [128, 1024], F32)
        hnp = SbPool(C, st, 2, f"g{l}_hn", [128, 1024], F32)
        groups = list(enumerate(GROUPS))
        if last:
            groups = groups[1:]
        if QT_LIMIT:
            groups = groups[:2]
        oc_toks = [C.T(f"ocT:{g}:{t0}") for g in range(4) for t0 in (0, 256)]
        for g, (t0, n) in groups:
            r = 0 if g > 0 else 1
            hxT, k_hxT = hxTp.get()
            S.dma("sp", hxT[:, :, 0:n], d["hxT_s"].rearrange("c p t -> p c t")[:, :, t0:t0 + n], reads=[C.T(f"hxT:{g}")], writes=[k_hxT])
            oT, k_oT = oTp.get()
            tiles = list(range(t0 // 128, (t0 + n) // 128))
            for bi, (nm, rd) in enumerate((("oaT_s", [C.T(f"oaT_s:{t}") for t in tiles]),
                                           ("obT_s", [C.T(f"obT_s:{t}") for t in tiles]),
                                           ("ocT_s", oc_toks))):
                S.dma("sp", oT[:, bi * 4:(bi + 1) * 4, 0:n], d[nm].rearrange("c p t -> p c t")[:, :, t0:t0 + n],
                      reads=rd, writes=[k_oT], acc=(bi > 0))
            YT, k_YT = YTp.get()
            for m in range(8):
                tbs = []
                for br in range(3):
                    psg, k_psg = pp.get()
                    for kc in range(8):
                        mm(S, psg[:, 0:n], wg[:, kc, br * 1024 + m * 128:br * 1024 + (m + 1) * 128], hxT[:, kc, 0:n],
                           kc == 0, kc == 7, [t_wg, k_hxT], k_psg)
                    sg, k_sg = sgp.get()
                    act(S, sg[:, 0:n], psg[:, 0:n], AF.Sigmoid, [k_psg], [k_sg])
                    psv, k_psv = pp.get()
                    for kc in range(4):
                        mm(S, psv[:, 0:n], wbr[:, br * 4 + kc, m * 128:(m + 1) * 128], oT[:, br * 4 + kc, 0:n],
                           kc == 0, kc == 3, [t_wbr, k_oT], k_psv)
                    tb, k_tb = tbp.get()
                    tt(S, "dve", tb[:, 0:n], psv[:, 0:n], sg[:, 0:n], ALU.mult, [k_psv, k_sg], [k_tb])
                    tbs.append((tb, k_tb))
                y1, k_y1 = y1p.get()
                tt(S, "dve", y1[:, 0:n], tbs[0][0][:, 0:n], tbs[1][0][:, 0:n], ALU.add, [tbs[0][1], tbs[1][1]], [k_y1])
                tt(S, "dve", YT[:, m, 0:n], y1[:, 0:n], tbs[2][0][:, 0:n], ALU.add, [k_y1, tbs[2][1]], [k_YT], acc=(m > 0))
            for ti, tile in enumerate(tiles):
                ht, k_h = hp.get()
                S.dma("sp", ht[:], hsrc[tile * 128:(tile + 1) * 128, :], reads=[C.T(f"h:{tile}")], writes=[k_h])
                hn, k_hn = hnp.get()
                for half in range(2):
                    ps, k_ps = pp.get()
                    for m in range(8):
                        mm(S, ps[:, :], YT[:, m, ti * 128:(ti + 1) * 128], wo[:, r, m, half * 512:(half + 1) * 512],
                           m == 0, m == 7, [k_YT, t_wo], k_ps)
                    tt(S, "dve", hn[:, half * 512:(half + 1) * 512], ps[:, :], ht[:, half * 512:(half + 1) * 512], ALU.add,
                       [k_ps, k_h], [k_hn], acc=(half > 0))
                S.dma("pool", d["h_s"][tile * 128:(tile + 1) * 128, :], hn[:], reads=[k_hn], writes=[C.T(f"h:{tile}")])
    S.barrier()


def phase_route(C, l):
    nc, S, d = C.nc, C.S, C.d
    import contextlib
    last = (l == DEPTH - 1)
    with contextlib.ExitStack() as st:
        sb = lambda name, shape, dt: st.enter_context(nc.sbuf_tensor(f"r{l}_{name}", list(shape), dt))
        Gm, Sm = sb("Gm", [128, 2, 1024], F32), sb("Sm", [128, 2, 1024], F32)
        wr = sb("wr", [128, 8, 36], F32)
        br = sb("br", [128, 36], F32)
        t_mod, t_mod2, t_wr = Tok(), Tok(), Tok()
        mv = d["modv"][l]
        tmpp = SbPool(C, st, 2, f"r{l}_tmp", [128, 1024], F32)
        gn, k_gn = tmpp.get()
        S.dma("sp", gn[:], bcast_row(d["norm_ffn_g"][l:l + 1, :], 1024), writes=[k_gn])
        for r in range(2):
            S.dma("sp", Sm[:, r, :], bcast_row(mv[r:r + 1, 3072:4096], 1024), reads=[C.T(f"modv{l}")], writes=[t_mod], acc=True)
            S.dma("sp", Gm[:, r, :], bcast_row(mv[r:r + 1, 4096:5120], 1024), reads=[C.T(f"modv{l}")], writes=[t_mod], acc=True)
        for r in range(2):
            stt(S, Gm[:, r, :], Gm[:, r, :], 1.0, gn[:], ALU.add, ALU.mult, [t_mod, k_gn], [t_mod2], acc=True)
        S.dma("sp", wr[:], d["w_r"][l].rearrange("(kc p) n -> p kc n", p=128), writes=[t_wr], acc=True)
        S.dma("sp", br[:], bcast_row(d["b_r"][l:l + 1, :], 36), writes=[t_wr], acc=True)
        pt32 = PsumPool(C, st, 2, f"r{l}_pt", (128, 4, 128), F32)
        pp = PsumPool(C, st, 2, f"r{l}_pp", (128, 128), F32)
        ptb = PsumPool(C, st, 2, f"r{l}_ptb", (128, 8, 128), BF16)
        hp = SbPool(C, st, 2, f"r{l}_h", [128, 1024], F32)
        fxp = SbPool(C, st, 2, f"r{l}_fx", [128, 1024], F32)
        fxbp = SbPool(C, st, 2, f"r{l}_fxb", [128, 1024], BF16)
        fxTp = SbPool(C, st, 2, f"r{l}_fxT", [128, 8, 128], F32)
        fxTbp = SbPool(C, st, 2, f"r{l}_fxTb", [128, 8, 128], BF16)
        ssp = SbPool(C, st, 2, f"r{l}_ss", [128, 4], F32)
        sqj = sb("sqj", [128, 1024], BF16)
        t_sqj = Tok()
        rtp = SbPool(C, st, 2, f"r{l}_rt", [128, 160], F32)
        gTp = SbPool(C, st, 2, f"r{l}_gT", [32, 128], F32)
        tiles = list(range(2, NT)) if last else list(range(NT))
        if QT_LIMIT:
            tiles = tiles[:QT_LIMIT]
        for tile in tiles:
            mr = 0 if tile >= 2 else 1
            ht, k_h = hp.get()
            S.dma("sp", ht[:], d["h_s"][tile * 128:(tile + 1) * 128, :], reads=[C.T(f"h:{tile}")], writes=[k_h])
            ss, k_ss = ssp.get()
            act(S, sqj[:], ht[:], AF.Square, [k_h], [t_sqj, k_ss], accum_out=ss[:, 0:1])
            act(S, ss[:, 1:2], ss[:, 0:1], AF.Sqrt, [k_ss], [k_ss], acc=True, scale=1.0 / DM, bias=EPS)
            S.op("dve", lambda e, ss=ss: e.reciprocal(out=ss[:, 2:3], in_=ss[:, 1:2]), reads=[k_ss], writes=[k_ss], acc=True)
            tm, k_tm = tmpp.get()
            stt(S, tm[:], ht[:], ss[:, 2:3], Gm[:, mr, :], ALU.mult, ALU.mult, [k_h, k_ss, t_mod2], [k_tm])
            fx, k_fx = fxp.get()
            tt(S, "dve", fx[:], tm[:], Sm[:, mr, :], ALU.add, [k_tm, t_mod2], [k_fx])
            fxb, k_fxb = fxbp.get()
            cp(S, "act", fxb[:], fx[:], [k_fx], [k_fxb])
            tpb, k_tpb = ptb.get()
            for kc in range(8):
                S.op("pe", lambda e, tpb=tpb, fxb=fxb, kc=kc: e.transpose(tpb[:, kc, :], fxb[:, kc * 128:(kc + 1) * 128], C.idb[:]),
                     reads=[k_fxb, C.t_c], writes=[k_tpb], acc=(kc > 0))
            fxTb, k_fxTb = fxTbp.get()
            cp(S, "act", fxTb[:], tpb[:], [k_tpb], [k_fxTb])
            S.dma("pool", d["hxT_s"].rearrange("c p t -> p c t")[:, :, tile * 128:(tile + 1) * 128], fxTb[:],
                  reads=[k_fxTb], writes=[C.T(f"fxT:{tile}")])
            fxT, k_fxT = fxTp.get()
            for hf in range(2):
                tp, k_tp = pt32.get()
                for kc in range(4):
                    S.op("pe", lambda e, tp=tp, fx=fx, kc=kc, hf=hf: e.transpose(tp[:, kc, :], fx[:, (hf * 4 + kc) * 128:(hf * 4 + kc + 1) * 128], C.idf[:]),
                         reads=[k_fx, C.t_c], writes=[k_tp], acc=(kc > 0))
                cp(S, "act", fxT[:, hf * 4:(hf + 1) * 4, :], tp[:], [k_tp], [k_fxT], acc=(hf > 0))
            pl, k_pl = pp.get()
            for kc in range(8):
                mm(S, pl[:, 0:36], fxT[:, kc, :], wr[:, kc, :], kc == 0, kc == 7, [k_fxT, t_wr], k_pl)
            rt, k = rtp.get()
            tt(S, "dve", rt[:, 0:36], pl[:, 0:36], br[:], ALU.add, [k_pl, t_wr], [k])
            S.op("dve", lambda e, rt=rt: e.reduce_max(out=rt[:, 36:37], in_=rt[:, 0:4], axis=AX.X), reads=[k], writes=[k], acc=True)
            ts(S, "dve", rt[:, 37:38], rt[:, 36:37], -1.0, None, ALU.mult, None, [k], [k], acc=True)
            act(S, rt[:, 44:48], rt[:, 0:4], AF.Exp, [k], [k], acc=True, bias=rt[:, 37:38], accum_out=rt[:, 38:39])
            S.op("dve", lambda e, rt=rt: e.reciprocal(out=rt[:, 39:40], in_=rt[:, 38:39]), reads=[k], writes=[k], acc=True)
            ts(S, "dve", rt[:, 40:44], rt[:, 0:4], rt[:, 36:37], None, ALU.is_equal, None, [k], [k], acc=True)
            ts(S, "dve", rt[:, 40:44], rt[:, 40:44], -1.0, 30000.0, ALU.add, ALU.mult, [k], [k], acc=True)
            for g in range(4):
                ts(S, "dve", rt[:, 48 + g * 8:56 + g * 8], rt[:, 4 + g * 8:12 + g * 8], rt[:, 40 + g:41 + g], None,
                   ALU.add, None, [k], [k], acc=True)
            S.op("dve", lambda e, rt=rt: e.max(out=rt[:, 80:88], in_=rt[:, 48:80]), reads=[k], writes=[k], acc=True)
            tt(S, "dve", rt[:, 88:89], rt[:, 81:82], rt[:, 80:81], ALU.subtract, [k], [k], acc=True)
            act(S, rt[:, 89:90], rt[:, 88:89], AF.Exp, [k], [k], acc=True)
            ts(S, "dve", rt[:, 90:91], rt[:, 89:90], 1.0, None, ALU.add, None, [k], [k], acc=True)
            S.op("dve", lambda e, rt=rt: e.reciprocal(out=rt[:, 90:91], in_=rt[:, 90:91]), reads=[k], writes=[k], acc=True)
            tt(S, "dve", rt[:, 91:92], rt[:, 90:91], rt[:, 39:40], ALU.mult, [k], [k], acc=True)
            tt(S, "dve", rt[:, 92:93], rt[:, 91:92], rt[:, 89:90], ALU.mult, [k], [k], acc=True)
            ts(S, "dve", rt[:, 96:128], rt[:, 48:80], rt[:, 80:81], rt[:, 91:92], ALU.is_equal, ALU.mult, [k], [k], acc=True)
            ts(S, "dve", rt[:, 128:160], rt[:, 48:80], rt[:, 81:82], rt[:, 92:93], ALU.is_equal, ALU.mult, [k], [k], acc=True)
            tt(S, "dve", rt[:, 96:128], rt[:, 96:128], rt[:, 128:160], ALU.add, [k], [k], acc=True)
            pg, k_pg = pp.get()
            S.op("pe", lambda e, pg=pg, rt=rt: e.transpose(pg[0:32, 0:128], rt[:, 96:128], C.idf[:]),
                 reads=[k, C.t_c], writes=[k_pg])
            gT, k_gT = gTp.get()
            cp(S, "act", gT[:], pg[0:32, 0:128], [k_pg], [k_gT])
            S.dma("pool", d["gT_s"][:, tile * 128:(tile + 1) * 128], gT[:], reads=[k_gT], writes=[C.T(f"gT:{tile}")])
    S.barrier()


def phase_experts(C, l):
    nc, S, d = C.nc, C.S, C.d
    import contextlib
    last = (l == DEPTH - 1)
    with contextlib.ExitStack() as st:
        sb = lambda name, shape, dt: st.enter_context(nc.sbuf_tensor(f"e{l}_{name}", list(shape), dt))
        g2T = sb("g2T", [128, 2, 8], F32)
        fg = sb("fg", [128, 1024], F32)
        t_g2 = Tok()
        for r in range(2):
            S.dma("sp", g2T[:, r, :], d["modv"][l][r, 5120:6144].rearrange("(m p) -> p m", p=128), reads=[C.T(f"modv{l}")],
                  writes=[t_g2], acc=True, allow_slow_non_contiguous=True)
        if last:
            S.dma("sp", fg[:], bcast_row(d["final_g"][0:1, :], 1024), writes=[t_g2], acc=True)
        ppg = PsumPool(C, st, 4, f"e{l}_ppg")
        pp = PsumPool(C, st, 3, f"e{l}_pp")
        wgs = SbPool(C, st, 1, f"e{l}_wgs", [128, 8, 512], F32)
        wds = SbPool(C, st, 1, f"e{l}_wds", [128, 2, 1024], F32)
        wgp = SbPool(C, st, 2, f"e{l}_wg", [128, 8, 512], BF16)
        wdp = SbPool(C, st, 2, f"e{l}_wd", [128, 2, 1024], BF16)
        fxTp = SbPool(C, st, 1, f"e{l}_fxT", [128, 8, 2176], BF16)
        yacc = sb("yacc", [128, 8, 2176], F32)
        t_y = Tok()
        gwp = SbPool(C, st, 3, f"e{l}_gw", [128, 512], F32)
        sip = SbPool(C, st, 2, f"e{l}_si", [128, 512], F32)
        a1p = SbPool(C, st, 2, f"e{l}_a1", [128, 512], F32)
        actp = SbPool(C, st, 3, f"e{l}_act", [128, 2, 512], BF16)
        hp = SbPool(C, st, 2, f"e{l}_h", [128, 1024], F32)
        hnp = SbPool(C, st, 2, f"e{l}_hn", [128, 1024], F32)
        ssp = SbPool(C, st, 2, f"e{l}_ss", [128, 4], F32)
        sqj = sb("sqj", [128, 1024], BF16)
        t_sqj = Tok()
        tok0 = 256 if last else 0
        sgs = []
        t = tok0
        while t < NTOK:
            n = min(2048 if last else 2176, NTOK - t)
            sgs.append((t, n))
            t += n
        if QT_LIMIT:
            sgs = [(tok0, 256)]
        nexp = 32 if not QT_LIMIT else QT_LIMIT
        for (t0, n) in sgs:
            tiles = list(range(t0 // 128, (t0 + n) // 128))
            fxT, k_fxT = fxTp.get()
            S.dma("sp", fxT[:, :, 0:n], d["hxT_s"].rearrange("c p t -> p c t")[:, :, t0:t0 + n],
                  reads=[C.T(f"fxT:{tl}") for tl in tiles], writes=[k_fxT])
            steps = [(e_, s0) for e_ in range(nexp) for s0 in range(0, n, 512)]
            wcur = {}
            st_state = {}

            def load_w(e_):
                ws, k_ws = wgs.get()
                S.dma("sp", ws[:], d["w_gu"][l, e_].rearrange("(kc p) n -> p kc n", p=128), writes=[k_ws])
                wg, k_wg = wgp.get()
                cp(S, "act", wg[:], ws[:], [k_ws], [k_wg])
                ws2, k_ws2 = wds.get()
                S.dma("sp", ws2[:], d["w_dn"][l, e_].rearrange("(kc p) n -> p kc n", p=128), writes=[k_ws2])
                wd, k_wd = wdp.get()
                cp(S, "act", wd[:], ws2[:], [k_ws2], [k_wd])
                wcur[e_] = (wg, k_wg, wd, k_wd)

            def emit_gu(si):
                e_, s0 = steps[si]
                ns = min(512, n - s0)
                if e_ not in wcur:
                    load_w(e_)
                    wcur.pop(e_ - 2, None)
                wg, k_wg, wd, k_wd = wcur[e_]
                gw, k_gw = gwp.get()
                S.dma("sp", gw[:, 0:ns], bcast_row(d["gT_s"][e_:e_ + 1, t0 + s0:t0 + s0 + ns], ns),
                      reads=[C.T(f"gT:{tl}") for tl in tiles], writes=[k_gw])
                ac, k_ac = actp.get()
                for c in range(2):
                    psg, k_psg = ppg.get()
                    psu, k_psu = ppg.get()
                    for kc in range(8):
                        mm(S, psg[:, 0:ns], wg[:, kc, c * 128:(c + 1) * 128], fxT[:, kc, s0:s0 + ns], kc == 0, kc == 7, [k_wg, k_fxT], k_psg)
                    for kc in range(8):
                        mm(S, psu[:, 0:ns], wg[:, kc, 256 + c * 128:256 + (c + 1) * 128], fxT[:, kc, s0:s0 + ns], kc == 0, kc == 7, [k_wg, k_fxT], k_psu)
                    si_, k_si = sip.get()
                    act(S, si_[:, 0:ns], psg[:, 0:ns], AF.Silu, [k_psg], [k_si])
                    a1, k_a1 = a1p.get()
                    tt(S, "dve", a1[:, 0:ns], psu[:, 0:ns], si_[:, 0:ns], ALU.mult, [k_psu, k_si], [k_a1])
                    tt(S, "dve", ac[:, c, 0:ns], a1[:, 0:ns], gw[:, 0:ns], ALU.mult, [k_a1, k_gw], [k_ac], acc=(c > 0))
                st_state[si] = (ac, k_ac, wd, k_wd)

            def emit_dn(si):
                e_, s0 = steps[si]
                ns = min(512, n - s0)
                ac, k_ac, wd, k_wd = st_state.pop(si)
                for m in range(8):
                    py, k_py = pp.get()
                    for c in range(2):
                        mm(S, py[:, 0:ns], wd[:, c, m * 128:(m + 1) * 128], ac[:, c, 0:ns], c == 0, c == 1, [k_wd, k_ac], k_py)
                    if e_ == 0:
                        cp(S, "act", yacc[:, m, s0:s0 + ns], py[:, 0:ns], [k_py], [t_y], acc=True)
                    else:
                        tt(S, "dve", yacc[:, m, s0:s0 + ns], py[:, 0:ns], yacc[:, m, s0:s0 + ns], ALU.add, [k_py, t_y], [t_y], acc=True)

            emit_gu(0)
            for si in range(len(steps)):
                if si + 1 < len(steps):
                    emit_gu(si + 1)
                emit_dn(si)
            for m in range(8):
                for (a, b_, r) in ((0, 256 - t0, 1), (max(0, 256 - t0), n, 0)):
                    if b_ <= a:
                        continue
                    b_ = min(b_, n)
                    act(S, yacc[:, m, a:b_], yacc[:, m, a:b_], AF.Copy, [t_y, t_g2], [t_y], acc=True, scale=g2T[:, r, m:m + 1])
            for ti, tile in enumerate(tiles):
                ht, k_h = hp.get()
                S.dma("sp", ht[:], d["h_s"][tile * 128:(tile + 1) * 128, :], reads=[C.T(f"h:{tile}")], writes=[k_h])
                hn, k_hn = hnp.get()
                for hf in range(2):
                    ps, k_ps = pp.get()
                    for kc in range(4):
                        m = hf * 4 + kc
                        S.op("pe", lambda e, ps=ps, m=m, kc=kc, ti=ti: e.transpose(ps[:, kc * 128:(kc + 1) * 128], yacc[:, m, ti * 128:(ti + 1) * 128], C.idf[:]),
                             reads=[t_y, C.t_c], writes=[k_ps], acc=(kc > 0))
                    tt(S, "dve", hn[:, hf * 512:(hf + 1) * 512], ps[:, :], ht[:, hf * 512:(hf + 1) * 512], ALU.add, [k_ps, k_h], [k_hn], acc=(hf > 0))
                if not last:
                    S.dma("pool", d["h_s"][tile * 128:(tile + 1) * 128, :], hn[:], reads=[k_hn], writes=[C.T(f"h:{tile}")])
                else:
                    ss, k_ss = ssp.get()
                    act(S, sqj[:], hn[:], AF.Square, [k_hn], [t_sqj, k_ss], accum_out=ss[:, 0:1])
                    act(S, ss[:, 1:2], ss[:, 0:1], AF.Sqrt, [k_ss], [k_ss], acc=True, scale=1.0 / DM, bias=EPS)
                    S.op("dve", lambda e, ss=ss: e.reciprocal(out=ss[:, 2:3], in_=ss[:, 1:2]), reads=[k_ss], writes=[k_ss], acc=True)
                    ho, k_ho = hp.get()
                    stt(S, ho[:], hn[:], ss[:, 2:3], fg[:], ALU.mult, ALU.mult, [k_hn, k_ss, t_g2], [k_ho])
                    S.dma("pool", d["out"][(tile - 2) * 128:(tile - 1) * 128, :], ho[:], reads=[k_ho], writes=[C.T(f"out:{tile}")])
    S.barrier()


def all_phases(C):
    C.d["out"] = C.nc.dram_tensor("out", [SEQ, DM], F32, kind="ExternalOutput").ap()
    for l in range(DEPTH):
        phase_mod(C, l)
        phase_inproj(C, l)
        phase_attn_a(C, l)
        phase_attn_b(C, l)
        phase_pool(C, l)
        phase_merge(C, l)
        phase_moe(C, l)


def kernel(**inputs):
    sh = prep_shared(inputs)
    nc = build_program(all_phases)
    in_maps = []
    for b in range(8):
        m = dict(sh)
        m.update(prep_core(inputs, b))
        in_maps.append({k: m[k] for k in INPUTS})
    res = run_bass_kernel_spmd(nc, in_maps, core_ids=list(range(8)))
    return np.stack([np.asarray(r["out"], dtype=np.float32) for r in res.results], axis=0)


def phase_moe(C, l):
    nc, S, d = C.nc, C.S, C.d
    import contextlib
    last = (l == DEPTH - 1)
    tiles = list(range(2, NT)) if last else list(range(NT))
    if QT_LIMIT:
        tiles = tiles[:QT_LIMIT]
    nt = len(tiles)
    NB = -(-(2 * nt * 128 + 32 * 127) // 128)
    xbuf = d["xbuf"]
    ybuf = d["ybuf"]
    with contextlib.ExitStack() as st0:
        sb0 = lambda name, shape, dt: st0.enter_context(nc.sbuf_tensor(f"x{l}_{name}", list(shape), dt))
        sAB = sb0("sAB", [128, NT, 2], I32)
        w12 = sb0("w12", [128, NT, 2], F32)
        widx = sb0("widx", [128, 128], I32)
        t_sAB, t_w12, t_widx = Tok(), Tok(), Tok()
        t_xz = Tok()
        with contextlib.ExitStack() as st:
            sb = lambda name, shape, dt: st.enter_context(nc.sbuf_tensor(f"r{l}_{name}", list(shape), dt))
            zt = sb("zt", [128, 4, 1024], BF16)
            t_zt = Tok()
            S.op("dve", lambda e: e.memset(zt[:], 0.0), writes=[t_zt])
            xv = xbuf.rearrange("(a p) f -> p a f", p=128)
            for b0 in range(0, NB, 4):
                nb_ = min(4, NB - b0)
                S.dma("sp", xv[:, b0:b0 + nb_, :], zt[:, 0:nb_, :], reads=[t_zt], writes=[t_xz], acc=True)
            Gm, Sm = sb("Gm", [128, 2, 1024], F32), sb("Sm", [128, 2, 1024], F32)
            wr = sb("wr", [128, 8, 36], F32)
            br = sb("br", [128, 36], F32)
            t_mod, t_mod2, t_wr = Tok(), Tok(), Tok()
            mv = d["modv"][l]
            tmpp = SbPool(C, st, 2, f"r{l}_tmp", [128, 1024], F32)
            gn, k_gn = tmpp.get()
            S.dma("sp", gn[:], bcast_row(d["norm_ffn_g"][l:l + 1, :], 1024), writes=[k_gn])
            for r in range(2):
                S.dma("sp", Sm[:, r, :], bcast_row(mv[r:r + 1, 3072:4096], 1024), reads=[C.T(f"modv{l}")], writes=[t_mod], acc=True)
                S.dma("sp", Gm[:, r, :], bcast_row(mv[r:r + 1, 4096:5120], 1024), reads=[C.T(f"modv{l}")], writes=[t_mod], acc=True)
            for r in range(2):
                stt(S, Gm[:, r, :], Gm[:, r, :], 1.0, gn[:], ALU.add, ALU.mult, [t_mod, k_gn], [t_mod2], acc=True)
            S.dma("sp", wr[:], d["w_r"][l].rearrange("(kc p) n -> p kc n", p=128), writes=[t_wr], acc=True)
            S.dma("sp", br[:], bcast_row(d["b_r"][l:l + 1, :], 36), writes=[t_wr], acc=True)
            fx_all = sb("fxall", [128, NT, 1024], BF16)
            A01 = sb("A01", [128, NT, 32], F32)
            B01 = sb("B01", [128, NT, 32], F32)
            Mall = sb("Mall", [128, NT, 32], BF16)
            t_fx = [Tok() for _ in range(NT)]
            t_AB = [Tok() for _ in range(NT)]
            pt32 = PsumPool(C, st, 2, f"r{l}_pt", (128, 4, 128), F32)
            pp = PsumPool(C, st, 2, f"r{l}_pp", (128, 128), F32)
            hp = SbPool(C, st, 2, f"r{l}_h", [128, 1024], F32)
            fxp = SbPool(C, st, 2, f"r{l}_fx", [128, 1024], F32)
            fxTp = SbPool(C, st, 2, f"r{l}_fxT", [128, 8, 128], F32)
            ssp = SbPool(C, st, 2, f"r{l}_ss", [128, 4], F32)
            sqj = sb("sqj", [128, 1024], BF16)
            t_sqj = Tok()
            rtp = SbPool(C, st, 2, f"r{l}_rt", [128, 96], F32)
            for ti, tile in enumerate(tiles):
                mr = 0 if tile >= 2 else 1
                ht, k_h = hp.get()
                S.dma("sp", ht[:], d["h_s"][tile * 128:(tile + 1) * 128, :], reads=[C.T(f"h:{tile}")], writes=[k_h])
                ss, k_ss = ssp.get()
                act(S, sqj[:], ht[:], AF.Square, [k_h], [t_sqj, k_ss], accum_out=ss[:, 0:1])
                act(S, ss[:, 1:2], ss[:, 0:1], AF.Sqrt, [k_ss], [k_ss], acc=True, scale=1.0 / DM, bias=EPS)
                S.op("dve", lambda e, ss=ss: e.reciprocal(out=ss[:, 2:3], in_=ss[:, 1:2]), reads=[k_ss], writes=[k_ss], acc=True)
                tm, k_tm = tmpp.get()
                stt(S, tm[:], ht[:], ss[:, 2:3], Gm[:, mr, :], ALU.mult, ALU.mult, [k_h, k_ss, t_mod2], [k_tm])
                fx, k_fx = fxp.get()
                tt(S, "dve", fx[:], tm[:], Sm[:, mr, :], ALU.add, [k_tm, t_mod2], [k_fx])
                cp(S, "act", fx_all[:, ti, :], fx[:], [k_fx], [t_fx[ti]])
                fxT, k_fxT = fxTp.get()
                for hf in range(2):
                    tp, k_tp = pt32.get()
                    for kc in range(4):
                        S.op("pe", lambda e, tp=tp, fx=fx, kc=kc, hf=hf: e.transpose(tp[:, kc, :], fx[:, (hf * 4 + kc) * 128:(hf * 4 + kc + 1) * 128], C.idf[:]),
                             reads=[k_fx, C.t_c], writes=[k_tp], acc=(kc > 0))
                    cp(S, "act", fxT[:, hf * 4:(hf + 1) * 4, :], tp[:], [k_tp], [k_fxT], acc=(hf > 0))
                pl, k_pl = pp.get()
                for kc in range(8):
                    mm(S, pl[:, 0:36], fxT[:, kc, :], wr[:, kc, :], kc == 0, kc == 7, [k_fxT, t_wr], k_pl)
                rt, k = rtp.get()
                tt(S, "dve", rt[:, 0:36], pl[:, 0:36], br[:], ALU.add, [k_pl, t_wr], [k])
                S.op("dve", lambda e, rt=rt: e.reduce_max(out=rt[:, 36:37], in_=rt[:, 0:4], axis=AX.X), reads=[k], writes=[k], acc=True)
                ts(S, "dve", rt[:, 37:38], rt[:, 36:37], -1.0, None, ALU.mult, None, [k], [k], acc=True)
                act(S, rt[:, 44:48], rt[:, 0:4], AF.Exp, [k], [k], acc=True, bias=rt[:, 37:38], accum_out=rt[:, 38:39])
                S.op("dve", lambda e, rt=rt: e.reciprocal(out=rt[:, 39:40], in_=rt[:, 38:39]), reads=[k], writes=[k], acc=True)
                ts(S, "dve", rt[:, 40:44], rt[:, 0:4], rt[:, 36:37], None, ALU.is_equal, None, [k], [k], acc=True)
                ts(S, "dve", rt[:, 40:44], rt[:, 40:44], -1.0, 30000.0, ALU.add, ALU.mult, [k], [k], acc=True)
                for g in range(4):
                    ts(S, "dve", rt[:, 48 + g * 8:56 + g * 8], rt[:, 4 + g * 8:12 + g * 8], rt[:, 40 + g:41 + g], None,
                       ALU.add, None, [k], [k], acc=True)
                S.op("dve", lambda e, rt=rt: e.max(out=rt[:, 80:88], in_=rt[:, 48:80]), reads=[k], writes=[k], acc=True)
                tt(S, "dve", rt[:, 88:89], rt[:, 81:82], rt[:, 80:81], ALU.subtract, [k], [k], acc=True)
                act(S, rt[:, 89:90], rt[:, 88:89], AF.Exp, [k], [k], acc=True)
                ts(S, "dve", rt[:, 90:91], rt[:, 89:90], 1.0, None, ALU.add, None, [k], [k], acc=True)
                S.op("dve", lambda e, rt=rt: e.reciprocal(out=rt[:, 90:91], in_=rt[:, 90:91]), reads=[k], writes=[k], acc=True)
                tt(S, "dve", w12[:, ti, 0:1], rt[:, 90:91], rt[:, 39:40], ALU.mult, [k], [t_w12], acc=True)
                tt(S, "dve", w12[:, ti, 1:2], w12[:, ti, 0:1], rt[:, 89:90], ALU.mult, [k, t_w12], [t_w12], acc=True)
                ts(S, "dve", A01[:, ti, :], rt[:, 48:80], rt[:, 80:81], None, ALU.is_equal, None, [k], [t_AB[ti]])
                ts(S, "dve", B01[:, ti, :], rt[:, 48:80], rt[:, 81:82], None, ALU.is_equal, None, [k], [t_AB[ti]], acc=True)
                tt(S, "dve", Mall[:, ti, :], A01[:, ti, :], B01[:, ti, :], ALU.add, [t_AB[ti]], [t_AB[ti]], acc=True)
            pc, k_pc = pp.get()
            for ti in range(nt):
                mm(S, pc[:, 0:32], C.onb[:], Mall[:, ti, :], ti == 0, ti == nt - 1, [t_AB[ti], C.t_c], k_pc)
            cw = sb("cw", [128, 8, 32], F32)
            t_cw = Tok()
            ts(S, "dve", cw[:, 0, :], pc[:, 0:32], 1.0, None, ALU.mult, None, [k_pc], [t_cw])
            S.op("dve", lambda e: e.memset(cw[:, 1, :], 0.0), writes=[t_cw], acc=True)
            for kk in range(nt + 1):
                stt(S, cw[:, 1, :], cw[:, 0, :], 128.0 * kk, cw[:, 1, :], ALU.is_gt, ALU.add, [t_cw], [t_cw], acc=True)
            ts(S, "dve", cw[:, 2, :], cw[:, 1, :], 128.0, None, ALU.mult, None, [t_cw], [t_cw], acc=True)
            ts(S, "dve", cw[:, 3, :], cw[:, 2, :], 1.0, None, ALU.mult, None, [t_cw], [t_cw], acc=True)
            src_i = 3
            for s_ in (1, 2, 4, 8, 16):
                dst_i = 7 - src_i
                ts(S, "dve", cw[:, dst_i, 0:s_], cw[:, src_i, 0:s_], 1.0, None, ALU.mult, None, [t_cw], [t_cw], acc=True)
                tt(S, "dve", cw[:, dst_i, s_:32], cw[:, src_i, s_:32], cw[:, src_i, 0:32 - s_], ALU.add, [t_cw], [t_cw], acc=True)
                src_i = dst_i
            pe_i = src_i
            tt(S, "dve", cw[:, 5, :], cw[:, pe_i, :], cw[:, 2, :], ALU.subtract, [t_cw], [t_cw], acc=True)
            thi = sb("thi", [128, 128], I32)
            thr = sb("thr", [128, 128], F32)
            bacc = sb("bacc", [128, 128], F32)
            pidi = sb("pidi", [128, 1], I32)
            pidf = sb("pidf", [128, 1], F32)
            t_th = Tok()
            S.op("pool", lambda e: e.iota(thi[:], pattern=[[128, 128]], base=0, channel_multiplier=0), writes=[t_th])
            S.op("pool", lambda e: e.iota(pidi[:], pattern=[[0, 1]], base=0, channel_multiplier=1), writes=[t_th], acc=True)
            cp(S, "dve", thr[:], thi[:], [t_th], [t_th])
            cp(S, "dve", pidf[:], pidi[:], [t_th], [t_th], acc=True)
            S.op("dve", lambda e: e.memset(bacc[:], 0.0), writes=[t_th], acc=True)
            for e_ in range(32):
                stt(S, bacc[:], thr[:], cw[:, pe_i, e_:e_ + 1], bacc[:], ALU.is_ge, ALU.add, [t_th, t_cw], [t_th], acc=True)
            ts(S, "dve", bacc[:], bacc[:], 31.0, 128.0, ALU.min, ALU.mult, [t_th], [t_th], acc=True)
            ts(S, "dve", bacc[:], bacc[:], pidf[:, 0:1], 0.0, ALU.add, ALU.add, [t_th], [t_th], acc=True)
            cp(S, "dve", widx[:], bacc[:], [t_th], [t_widx])
            slp = SbPool(C, st, 2, f"r{l}_sl", [128, 3, 32], F32)
            sfp = SbPool(C, st, 2, f"r{l}_sf", [128, 2], F32)
            for ti, tile in enumerate(tiles):
                ps, k_ps = pp.get()
                for j in range(ti):
                    mm(S, ps[:, 0:32], C.onb[:], Mall[:, j, :], j == 0, False, [t_AB[j], C.t_c], k_ps)
                mm(S, ps[:, 0:32], C.utri[:], Mall[:, ti, :], ti == 0, True, [t_AB[ti], C.t_c], k_ps)
                sl, k_sl = slp.get()
                tt(S, "dve", sl[:, 0, :], ps[:, 0:32], cw[:, 5, :], ALU.add, [k_ps, t_cw], [k_sl])
                tt(S, "dve", sl[:, 1, :], sl[:, 0, :], A01[:, ti, :], ALU.mult, [k_sl, t_AB[ti]], [k_sl], acc=True)
                tt(S, "dve", sl[:, 2, :], sl[:, 0, :], B01[:, ti, :], ALU.mult, [k_sl, t_AB[ti]], [k_sl], acc=True)
                sf, k_sf = sfp.get()
                S.op("dve", lambda e, sf=sf, sl=sl: e.reduce_sum(out=sf[:, 0:1], in_=sl[:, 1, :], axis=AX.X), reads=[k_sl], writes=[k_sf])
                S.op("dve", lambda e, sf=sf, sl=sl: e.reduce_sum(out=sf[:, 1:2], in_=sl[:, 2, :], axis=AX.X), reads=[k_sl], writes=[k_sf], acc=True)
                cp(S, "dve", sAB[:, ti, :], sf[:], [k_sf], [t_sAB], acc=True)
                for k2 in range(2):
                    S.dma_fn("pool", lambda e, ti=ti, k2=k2: e.indirect_dma_start(
                        out=xbuf[:, :], out_offset=bass.IndirectOffsetOnAxis(ap=sAB[:, ti, k2:k2 + 1], axis=0),
                        in_=fx_all[:, ti, :], in_offset=None),
                        reads=[t_sAB, t_fx[ti], t_xz], writes=[C.T("xbuf")], acc=True)
        S.barrier()
        with contextlib.ExitStack() as st:
            sb = lambda name, shape, dt: st.enter_context(nc.sbuf_tensor(f"e{l}_{name}", list(shape), dt))
            wgb = SbPool(C, st, 4, f"e{l}_wgb", [128, 8, 512], BF16)
            wdb = SbPool(C, st, 4, f"e{l}_wdb", [128, 2, 1024], BF16)
            xbp = SbPool(C, st, 3, f"e{l}_xb", [128, 1024], BF16)
            xTp = SbPool(C, st, 3, f"e{l}_xT", [128, 8, 128], BF16)
            sip = SbPool(C, st, 2, f"e{l}_si", [128, 256], F32)
            acp = SbPool(C, st, 3, f"e{l}_ac", [128, 256], BF16)
            aTp = SbPool(C, st, 2, f"e{l}_aT", [128, 2, 128], BF16)
            ybp = SbPool(C, st, 2, f"e{l}_yb", [128, 1024], F32)
            ptx = PsumPool(C, st, 2, f"e{l}_ptx", (128, 8, 128), BF16)
            ptc = PsumPool(C, st, 1, f"e{l}_ptc", (128, 2, 128), BF16)
            pgu = PsumPool(C, st, 2, f"e{l}_pgu")
            py = PsumPool(C, st, 2, f"e{l}_py")
            wgsrc = d["wgu_b"]
            wdsrc = d["wdn_b"]
            nblk = NB
            stA, stB = {}, {}

            def stage_a(b):
                wg, k_wg = wgb.get()
                S.dma_fn("pool", lambda e, wg=wg, b=b: e.indirect_dma_start(
                    out=wg[:].rearrange("p a b -> p (a b)"), out_offset=None, in_=wgsrc[:, :],
                    in_offset=bass.IndirectOffsetOnAxis(ap=widx[:, b:b + 1], axis=0)),
                    reads=[t_widx, C.T("wexp_b")], writes=[k_wg])
                wd, k_wd = wdb.get()
                S.dma_fn("pool", lambda e, wd=wd, b=b: e.indirect_dma_start(
                    out=wd[:].rearrange("p a b -> p (a b)"), out_offset=None, in_=wdsrc[:, :],
                    in_offset=bass.IndirectOffsetOnAxis(ap=widx[:, b:b + 1], axis=0)),
                    reads=[t_widx, C.T("wexp_b")], writes=[k_wd])
                xb, k_xb = xbp.get()
                S.dma("sp", xb[:], xbuf[b * 128:(b + 1) * 128, :], reads=[C.T("xbuf")], writes=[k_xb])
                tp, k_tp = ptx.get()
                for kc in range(8):
                    S.op("pe", lambda e, tp=tp, xb=xb, kc=kc: e.transpose(tp[:, kc, :], xb[:, kc * 128:(kc + 1) * 128], C.idb[:]),
                         reads=[k_xb, C.t_c], writes=[k_tp], acc=(kc > 0))
                xT, k_xT = xTp.get()
                cp(S, "act", xT[:], tp[:], [k_tp], [k_xT])
                stA[b] = (wg, k_wg, wd, k_wd, xT, k_xT)

            def stage_b(b):
                wg, k_wg, wd, k_wd, xT, k_xT = stA.pop(b)
                pg, k_pg = pgu.get()
                for kc in range(8):
                    mm(S, pg[:, :], xT[:, kc, :], wg[:, kc, :], kc == 0, kc == 7, [k_xT, k_wg], k_pg)
                si_, k_si = sip.get()
                act(S, si_[:], pg[:, 0:256], AF.Silu, [k_pg], [k_si])
                ac, k_ac = acp.get()
                tt(S, "dve", ac[:], pg[:, 256:512], si_[:], ALU.mult, [k_pg, k_si], [k_ac])
                stB[b] = (wd, k_wd, ac, k_ac)

            def stage_c(b):
                wd, k_wd, ac, k_ac = stB.pop(b)
                tp2, k_tp2 = ptc.get()
                for c in range(2):
                    S.op("pe", lambda e, tp2=tp2, ac=ac, c=c: e.transpose(tp2[:, c, :], ac[:, c * 128:(c + 1) * 128], C.idb[:]),
                         reads=[k_ac, C.t_c], writes=[k_tp2], acc=(c > 0))
                aT, k_aT = aTp.get()
                cp(S, "act", aT[:], tp2[:], [k_tp2], [k_aT])
                yb, k_yb = ybp.get()
                for hf in range(2):
                    pyt, k_py = py.get()
                    for c in range(2):
                        mm(S, pyt[:, :], aT[:, c, :], wd[:, c, hf * 512:(hf + 1) * 512], c == 0, c == 1, [k_aT, k_wd], k_py)
                    cp(S, "act", yb[:, hf * 512:(hf + 1) * 512], pyt[:, :], [k_py], [k_yb], acc=(hf > 0))
                S.dma("sp", ybuf[b * 128:(b + 1) * 128, :], yb[:], reads=[k_yb], writes=[C.T("ybuf")], acc=True)

            for b in range(nblk + 2):
                if b < nblk:
                    stage_a(b)
                if 0 <= b - 1 < nblk:
                    stage_b(b - 1)
                if 0 <= b - 2 < nblk:
                    stage_c(b - 2)
            G2 = sb("G2", [128, 2, 1024], F32)
            fg = sb("fg", [128, 1024], F32)
            t_g2 = Tok()
            for r in range(2):
                S.dma("sp", G2[:, r, :], bcast_row(d["modv"][l][r:r + 1, 5120:6144], 1024), reads=[C.T(f"modv{l}")], writes=[t_g2], acc=True)
            if last:
                S.dma("sp", fg[:], bcast_row(d["final_g"][0:1, :], 1024), writes=[t_g2], acc=True)
            yAp = SbPool(C, st, 2, f"e{l}_yA", [128, 1024], F32)
            yBp = SbPool(C, st, 2, f"e{l}_yB", [128, 1024], F32)
            hp = SbPool(C, st, 2, f"e{l}_h", [128, 1024], F32)
            hnp = SbPool(C, st, 2, f"e{l}_hn", [128, 1024], F32)
            ssp = SbPool(C, st, 2, f"e{l}_ss", [128, 4], F32)
            sqj = sb("sqj", [128, 1024], BF16)
            t_sqj = Tok()
            for ti, tile in enumerate(tiles):
                r = 0 if tile >= 2 else 1
                ys = []
                for k2, pool_ in enumerate((yAp, yBp)):
                    yt, k_yt = pool_.get()
                    S.dma_fn("pool", lambda e, yt=yt, ti=ti, k2=k2: e.indirect_dma_start(
                        out=yt[:], out_offset=None, in_=ybuf[:, :], in_offset=bass.IndirectOffsetOnAxis(ap=sAB[:, ti, k2:k2 + 1], axis=0)),
                        reads=[t_sAB, C.T("ybuf")], writes=[k_yt])
                    ys.append((yt, k_yt))
                ht, k_h = hp.get()
                S.dma("sp", ht[:], d["h_s"][tile * 128:(tile + 1) * 128, :], reads=[C.T(f"h:{tile}")], writes=[k_h])
                (yA, k_yA), (yB, k_yB) = ys
                ts(S, "dve", yA[:], yA[:], w12[:, ti, 0:1], None, ALU.mult, None, [k_yA, t_w12], [k_yA])
                stt(S, yB[:], yB[:], w12[:, ti, 1:2], yA[:], ALU.mult, ALU.add, [k_yB, k_yA, t_w12], [k_yB])
                tt(S, "dve", yB[:], yB[:], G2[:, r, :], ALU.mult, [k_yB, t_g2], [k_yB])
                hn, k_hn = hnp.get()
                tt(S, "dve", hn[:], yB[:], ht[:], ALU.add, [k_yB, k_h], [k_hn])
                if not last:
                    S.dma("sp", d["h_s"][tile * 128:(tile + 1) * 128, :], hn[:], reads=[k_hn], writes=[C.T(f"h:{tile}")])
                else:
                    ss, k_ss = ssp.get()
                    act(S, sqj[:], hn[:], AF.Square, [k_hn], [t_sqj, k_ss], accum_out=ss[:, 0:1])
                    act(S, ss[:, 1:2], ss[:, 0:1], AF.Sqrt, [k_ss], [k_ss], acc=True, scale=1.0 / DM, bias=EPS)
                    S.op("dve", lambda e, ss=ss: e.reciprocal(out=ss[:, 2:3], in_=ss[:, 1:2]), reads=[k_ss], writes=[k_ss], acc=True)
                    ho, k_ho = hp.get()
                    stt(S, ho[:], hn[:], ss[:, 2:3], fg[:], ALU.mult, ALU.mult, [k_hn, k_ss, t_g2], [k_ho])
                    S.dma("sp", d["out"][(tile - 2) * 128:(tile - 1) * 128, :], ho[:], reads=[k_ho], writes=[C.T(f"out:{tile}")])
    S.barrier()
```

```python
import numpy as np
import concourse.bass as bass
import concourse.mybir as mybir
from concourse.bass_utils import run_bass_kernel_spmd

F32 = mybir.dt.float32
BF16 = mybir.dt.bfloat16
I32 = mybir.dt.int32
U32 = mybir.dt.uint32
AF = mybir.ActivationFunctionType
ALU = mybir.AluOpType
AX = mybir.AxisListType

ENGS = ("pe", "act", "dve", "pool", "sp")
EPOCH = 12000
NSLOT = 24


class Tok:
    __slots__ = ("name", "writers", "readers")

    def __init__(self, name=""):
        self.name = name
        self.writers = []
        self.readers = []


class Op:
    __slots__ = ("eng", "fn", "deps", "idx", "needed", "cnt", "dma", "slot", "val", "waits")

    def __init__(self, eng, fn, dma=False):
        self.eng = eng
        self.fn = fn
        self.deps = []
        self.idx = -1
        self.needed = False
        self.cnt = -1
        self.dma = dma
        self.slot = -1
        self.val = -1
        self.waits = []


class Sched:
    def __init__(self, nc):
        self.nc = nc
        self.ops = {e: [] for e in ENGS}
        self.ndma = {e: 0 for e in ENGS}
        self.seen = {e: {} for e in ENGS}
        self.pending = {e: [] for e in ENGS}

    def barrier(self):
        waits = []
        for e in ENGS:
            last = None
            for o in reversed(self.ops[e]):
                if not o.dma:
                    last = o
                    break
            if last is not None:
                waits.append(("c", last))
            slots = {}
            for o in self.ops[e]:
                if o.dma:
                    slots[o.slot] = o
            for o in slots.values():
                waits.append(("d", o))
        for e in ENGS:
            self.pending[e] = list(waits)

    def _add(self, op, reads, writes, acc):
        deps = []
        for t in reads:
            deps.extend(t.writers)
        for t in writes:
            deps.extend(t.readers)
            if not acc:
                deps.extend(t.writers)
            else:
                deps.extend(w for w in t.writers if w.eng != op.eng or w.dma != op.dma)
        e = op.eng
        op.idx = len(self.ops[e])
        seen = self.seen[e]
        if self.pending[e]:
            for w in self.pending[e]:
                if w[0] == "c":
                    deps.append(w[1])
                else:
                    deps.append(w[1])
            self.pending[e] = []
            barrier_dep = True
        else:
            barrier_dep = False
        if op.dma:
            n = self.ndma[e]
            self.ndma[e] = n + 1
            op.slot = n % NSLOT
            op.val = 16 * (n // NSLOT + 1)
            if op.val > 16:
                key = ("d", e, op.slot)
                if seen.get(key, 0) < op.val - 16:
                    seen[key] = op.val - 16
                    op.waits.append(("d", e, op.slot, op.val - 16))
        for d in sorted(deps, key=lambda o: -o.idx):
            if d.dma:
                key = ("d", d.eng, d.slot)
                if seen.get(key, 0) >= d.val:
                    continue
                seen[key] = d.val
                op.waits.append(("d", d.eng, d.slot, d.val))
            else:
                if d.eng == e:
                    if e in ("pe", "sp"):
                        continue
                key = ("c", d.eng)
                if seen.get(key, -1) >= d.idx:
                    continue
                seen[key] = d.idx
                d.needed = True
                op.waits.append(("c", d))
        for t in reads:
            t.readers.append(op)
        for t in writes:
            if acc:
                t.writers.append(op)
            else:
                t.writers = [op]
                t.readers = []
        self.ops[e].append(op)
        return op

    def op(self, eng, fn, reads=(), writes=(), acc=False):
        return self._add(Op(eng, fn), list(reads), list(writes), acc)

    def dma(self, eng, out, in_, reads=(), writes=(), acc=False, **kw):
        def fn(en):
            return en.dma_start(out=out, in_=in_, **kw)
        return self._add(Op(eng, fn, dma=True), list(reads), list(writes), acc)

    def dma_fn(self, eng, fn, reads=(), writes=(), acc=False):
        return self._add(Op(eng, fn, dma=True), list(reads), list(writes), acc)

    def emit(self):
        nc = self.nc
        nep = {}
        for e in ENGS:
            c = 0
            for o in self.ops[e]:
                if o.needed and not o.dma:
                    c += 1
                    o.cnt = c
            nep[e] = max(1, (c + EPOCH - 1) // EPOCH)
        import contextlib
        with contextlib.ExitStack() as st:
            csem = {e: [st.enter_context(nc.semaphore(f"c_{e}_{i}")) for i in range(nep[e])]
                    for e in ENGS}
            dsem = {e: [st.enter_context(nc.semaphore(f"d_{e}_{i}")) for i in range(NSLOT)]
                    for e in ENGS if self.ndma[e] > 0}
            block = st.enter_context(nc.Block())
            hw = {"pe": block.tensor, "act": block.scalar, "dve": block.vector,
                  "pool": block.gpsimd, "sp": block.sync}

            def body(e):
                def run(en):
                    for o in self.ops[e]:
                        for w in o.waits:
                            if w[0] == "d":
                                en.wait_ge(dsem[w[1]][w[2]], w[3])
                            else:
                                d = w[1]
                                en.wait_ge(csem[d.eng][(d.cnt - 1) // EPOCH], (d.cnt - 1) % EPOCH + 1)
                        ins = o.fn(en)
                        if o.dma:
                            ins.then_inc(dsem[e][o.slot], 16)
                        elif o.needed:
                            ins.then_inc(csem[e][(o.cnt - 1) // EPOCH], 1)
                    if self.ndma[e] > 0:
                        last = {}
                        for o in self.ops[e]:
                            if o.dma:
                                last[o.slot] = o.val
                        for s, v in last.items():
                            en.wait_ge(dsem[e][s], v)
                return run

            for e in ENGS:
                if self.ops[e]:
                    hw[e](body(e))


DM = 1024
SEQ = 4096
LC = 256
NTOK = SEQ + LC
NT = NTOK // 128
DEPTH = 2
EPS = 1e-6
STOP = 0
QT_LIMIT = 0


class Ctx:
    def __init__(self, nc, S):
        self.nc = nc
        self.S = S
        self.d = {}
        self.tok = {}

    def T(self, name):
        t = self.tok.get(name)
        if t is None:
            t = Tok(name)
            self.tok[name] = t
        return t


def phase_mod(C, l):
    nc, S = C.nc, C.S
    wmod = C.d["w_mod"][l].rearrange("(kc p) n -> p kc n", p=128)
    with nc.sbuf_tensor(f"m_ct{l}", [128, 8, 2], F32) as ct, \
            nc.sbuf_tensor(f"m_sct{l}", [128, 8, 2], F32) as sct, \
            nc.sbuf_tensor(f"m_w0{l}", [128, 8, 512], F32) as w0, \
            nc.sbuf_tensor(f"m_w1{l}", [128, 8, 512], F32) as w1, \
            nc.sbuf_tensor(f"m_b{l}", [2, 6144], F32) as bm, \
            nc.sbuf_tensor(f"m_row{l}", [2, 6144], F32) as mrow, \
            nc.psum_tensor(f"m_p0{l}", [2, 512], F32) as p0, \
            nc.psum_tensor(f"m_p1{l}", [2, 512], F32) as p1:
        t_ct, t_sct, t_bm, t_row = Tok(), Tok(), Tok(), Tok()
        t_w = [Tok(), Tok()]
        t_p = [Tok(), Tok()]
        wb = [w0, w1]
        pb = [p0, p1]
        S.dma("sp", ct[:], C.d["cT"][:], writes=[t_ct])
        S.dma("sp", bm[:], C.d["b_mod"][l:l + 1, :].to_broadcast([2, 6144]), writes=[t_bm])
        S.op("act", lambda e: e.activation(out=sct[:], in_=ct[:], func=AF.Silu),
             reads=[t_ct], writes=[t_sct])
        for n in range(12):
            S.dma("sp", wb[n % 2][:], wmod[:, :, n * 512:(n + 1) * 512], writes=[t_w[n % 2]])
            for kc in range(8):
                S.op("pe", lambda e, n=n, kc=kc: e.matmul(pb[n % 2][:], lhsT=sct[:, kc, :],
                                                       rhs=wb[n % 2][:, kc, :],
                                                       start=(kc == 0), stop=(kc == 7)),
                     reads=[t_sct, t_w[n % 2]], writes=[t_p[n % 2]], acc=(kc > 0))
            S.op("dve", lambda e, n=n: e.tensor_tensor(out=mrow[:, n * 512:(n + 1) * 512],
                                                      in0=pb[n % 2][:],
                                                      in1=bm[:, n * 512:(n + 1) * 512], op=ALU.add),
                 reads=[t_p[n % 2], t_bm], writes=[t_row], acc=True)
        S.dma("sp", C.d["modv"][l], mrow[:], reads=[t_row], writes=[C.T(f"modv{l}")])
    S.barrier()


class PsumPool:
    def __init__(self, C, st, n, name, shape=(128, 512), dtype=F32):
        self.tiles = [st.enter_context(C.nc.psum_tensor(f"{name}{i}", list(shape), dtype)) for i in range(n)]
        self.toks = [Tok(f"{name}{i}") for i in range(n)]
        self.i = 0

    def get(self):
        i = self.i
        self.i = (i + 1) % len(self.tiles)
        return self.tiles[i], self.toks[i]


class SbPool:
    def __init__(self, C, st, n, name, shape, dtype):
        self.tiles = [st.enter_context(C.nc.sbuf_tensor(f"{name}{i}", list(shape), dtype)) for i in range(n)]
        self.toks = [Tok(f"{name}{i}") for i in range(n)]
        self.i = 0

    def get(self):
        i = self.i
        self.i = (i + 1) % len(self.tiles)
        return self.tiles[i], self.toks[i]


def mm(S, out, lhsT, rhs, first, last, reads, wtok):
    S.op("pe", lambda e: e.matmul(out, lhsT=lhsT, rhs=rhs, start=first, stop=last),
         reads=reads, writes=[wtok], acc=not first)


def act(S, out, in_, func, reads, writes, acc=False, **kw):
    S.op("act", lambda e: e.activation(out=out, in_=in_, func=func, **kw), reads=reads, writes=writes, acc=acc)


def tt(S, eng, out, in0, in1, op, reads, writes, acc=False):
    S.op(eng, lambda e: e.tensor_tensor(out=out, in0=in0, in1=in1, op=op), reads=reads, writes=writes, acc=acc)


def ts(S, eng, out, in0, s1, s2, op0, op1, reads, writes, acc=False):
    if op1 is None:
        S.op(eng, lambda e: e.tensor_scalar(out=out, in0=in0, scalar1=s1, scalar2=None, op0=op0),
             reads=reads, writes=writes, acc=acc)
    else:
        S.op(eng, lambda e: e.tensor_scalar(out=out, in0=in0, scalar1=s1, scalar2=s2, op0=op0, op1=op1),
             reads=reads, writes=writes, acc=acc)


def stt(S, out, in0, scalar, in1, op0, op1, reads, writes, acc=False):
    S.op("dve", lambda e: e.scalar_tensor_tensor(out=out, in0=in0, scalar=scalar, in1=in1, op0=op0, op1=op1),
         reads=reads, writes=writes, acc=acc)


def cp(S, eng, out, in_, reads, writes, acc=False):
    if eng == "act":
        S.op("act", lambda e: e.copy(out=out, in_=in_), reads=reads, writes=writes, acc=acc)
    else:
        S.op(eng, lambda e: e.tensor_copy(out=out, in_=in_), reads=reads, writes=writes, acc=acc)


def make_ident(C, st):
    nc, S = C.nc, C.S
    idi = st.enter_context(nc.sbuf_tensor("c_idi", [128, 128], I32))
    idb = st.enter_context(nc.sbuf_tensor("c_idb", [128, 128], BF16))
    idf = st.enter_context(nc.sbuf_tensor("c_idf", [128, 128], F32))
    onb = st.enter_context(nc.sbuf_tensor("c_onb", [128, 128], BF16))
    t_i, t_c = Tok(), Tok("consts")
    S.op("pool", lambda e: e.iota(idi[:], pattern=[[1, 128]], base=0, channel_multiplier=-1), writes=[t_i])
    ts(S, "dve", idb[:], idi[:], 0, None, ALU.is_equal, None, [t_i], [t_c], acc=True)
    ts(S, "dve", idf[:], idi[:], 0, None, ALU.is_equal, None, [t_i], [t_c], acc=True)
    S.op("dve", lambda e: e.memset(onb[:], 1.0), writes=[t_c], acc=True)
    utri = st.enter_context(nc.sbuf_tensor("c_utri", [128, 128], BF16))
    ts(S, "dve", utri[:], idi[:], 0, None, ALU.is_gt, None, [t_i], [t_c], acc=True)
    C.utri = utri
    C.idb, C.idf, C.onb, C.t_c = idb, idf, onb, t_c


O_QA, O_QAS, O_KA, O_KAS, O_KR, O_KRS, O_CQ, O_CKV, O_U, O_VA, O_G, W_EXT = (
    0, 512, 1024, 1152, 1280, 1312, 1344, 1600, 1856, 2368, 2496, 5568)
W_N = O_G
GROUPS = [(0, 256)] + [(256 + 512 * i, 512) for i in range(8)]


def bcast_row(ap_row, n):
    return ap_row.to_broadcast([128, n])


def load_cast(C, st, name, dst, src3, nk, ncol, t_dst, scale_ap=None, scale_tok=None, half=2784):
    S = C.S
    stg = SbPool(C, st, 2, name, [128, half], F32)
    for kc in range(nk):
        for c0 in range(0, ncol, half):
            w = min(half, ncol - c0)
            s_t, s_k = stg.get()
            S.dma("sp", s_t[:, 0:w], src3[:, kc, c0:c0 + w], writes=[s_k])
            if scale_ap is None:
                cp(S, "act", dst[:, kc, c0:c0 + w], s_t[:, 0:w], [s_k], [t_dst], acc=True)
            else:
                ts(S, "dve", dst[:, kc, c0:c0 + w], s_t[:, 0:w], scale_ap(kc), None, ALU.mult, None,
                   [s_k, scale_tok], [t_dst], acc=True)


def phase_inproj(C, l):
    nc, S = C.nc, C.S
    import contextlib
    d = C.d
    hsrc = d["h0"] if l == 0 else d["h_s"]
    with contextlib.ExitStack() as st:
        sb = lambda name, shape, dt: st.enter_context(nc.sbuf_tensor(f"n{l}_{name}", list(shape), dt))
        wsb = sb("w", [128, 8, W_N], BF16)
        wq = sb("wq", [128, 2, 1024], BF16)
        wkv = sb("wkv", [128, 2, 1024], BF16)
        gq = sb("gq", [128, 4], F32)
        Gm, Sm = sb("Gm", [128, 2, 1024], F32), sb("Sm", [128, 2, 1024], F32)
        t_w, t_wq, t_gq, t_mod = Tok(), Tok(), Tok(), Tok()
        wext = d["w_in_ext"][l].rearrange("(kc p) n -> p kc n", p=128)
        load_cast(C, st, f"n{l}_stg", wsb, wext, 8, W_N, t_w)
        S.dma("sp", gq[:, 0:2], d["q_norm_gT"][l], writes=[t_gq], acc=True)
        S.dma("sp", gq[:, 2:4], d["kv_norm_gT"][l], writes=[t_gq], acc=True)
        for wi, (wname, wdst) in enumerate((("w_uq_ext", wq), ("w_ukv_ext", wkv))):
            wsrc = d[wname][l].rearrange("(kc p) n -> p kc n", p=128)
            load_cast(C, st, f"n{l}_stg{wi}", wdst, wsrc, 2, 1024, t_wq,
                      scale_ap=lambda kc, wi=wi: gq[:, 2 * wi + kc:2 * wi + kc + 1], scale_tok=t_gq, half=1024)
        mv = d["modv"][l]
        tmpp = SbPool(C, st, 2, f"n{l}_tmp", [128, 1024], F32)
        gn, k_gn = tmpp.get()
        S.dma("sp", gn[:], bcast_row(d["norm_mix_g"][l:l + 1, :], 1024), writes=[k_gn])
        for r in range(2):
            S.dma("sp", Sm[:, r, :], bcast_row(mv[r:r + 1, 0:1024], 1024), reads=[C.T(f"modv{l}")], writes=[t_mod], acc=True)
            S.dma("sp", Gm[:, r, :], bcast_row(mv[r:r + 1, 1024:2048], 1024), reads=[C.T(f"modv{l}")], writes=[t_mod], acc=True)
        t_mod2 = Tok()
        for r in range(2):
            stt(S, Gm[:, r, :], Gm[:, r, :], 1.0, gn[:], ALU.add, ALU.mult, [t_mod, k_gn], [t_mod2], acc=True)
        if STOP == 1:
            return
        pp = PsumPool(C, st, 5, f"n{l}_pp")
        ptp = PsumPool(C, st, 2, f"n{l}_ptp", (128, 8, 128), BF16)
        hp = SbPool(C, st, 2, f"n{l}_h", [128, 1024], F32)
        hxp = SbPool(C, st, 2, f"n{l}_hx", [128, 1024], BF16)
        hxTp = SbPool(C, st, 2, f"n{l}_hxT", [128, 8, 512], BF16)
        ssp = SbPool(C, st, 4, f"n{l}_ss", [128, 4], F32)
        sqj = sb("sqj", [128, 1024], BF16)
        t_sqj = Tok()
        ropep = SbPool(C, st, 1, f"n{l}_rope", [128, 4, 512], F32)
        r1p = SbPool(C, st, 1, f"n{l}_r1", [128, 512], F32)
        r2p = SbPool(C, st, 1, f"n{l}_r2", [128, 512], F32)
        o4p = SbPool(C, st, 4, f"n{l}_o4", [128, 4, 512], BF16)
        krTp = SbPool(C, st, 1, f"n{l}_krT", [32, 512], BF16)
        latfp = SbPool(C, st, 1, f"n{l}_latf", [128, 2, 512], F32)
        latqp = SbPool(C, st, 1, f"n{l}_latq", [128, 2, 512], BF16)
        rbcp = SbPool(C, st, 1, f"n{l}_rbc", [128, 512], F32)
        cqnp = SbPool(C, st, 1, f"n{l}_cqn", [128, 2, 512], BF16)
        ckvnp = SbPool(C, st, 1, f"n{l}_ckvn", [128, 2, 512], BF16)
        vap = SbPool(C, st, 1, f"n{l}_va", [128, 4, 2, 65], BF16)
        vbp = SbPool(C, st, 1, f"n{l}_vb", [128, 4, 8, 65], BF16)
        for pool_ in (vap, vbp):
            for t_, k_ in zip(pool_.tiles, pool_.toks):
                S.op("pool", lambda e, t_=t_: e.memset(t_[:], 1.0), writes=[k_])
        t_all = [t_mod2, C.t_c]

        for g, (t0, n) in enumerate(GROUPS):
            ntl = n // 128
            isx = g > 0
            mr = 0 if isx else 1
            hxT, k_hxT = hxTp.get()
            if isx:
                rp, k_rp = ropep.get()
                p0 = t0 - 256
                for i, nm in enumerate(("ropeA_c", "ropeA_s", "ropeB_c", "ropeB_s")):
                    S.dma("sp", rp[:, i, :], d[nm][:, p0:p0 + 512], writes=[k_rp], acc=(i > 0))
            for ti in range(ntl):
                r0 = t0 + ti * 128
                ht, k_h = hp.get()
                S.dma("sp", ht[:], hsrc[r0:r0 + 128, :], reads=[C.T(f"h:{r0 // 128}")], writes=[k_h])
                ss, k_ss = ssp.get()
                act(S, sqj[:], ht[:], AF.Square, [k_h], [t_sqj, k_ss], accum_out=ss[:, 0:1])
                act(S, ss[:, 1:2], ss[:, 0:1], AF.Sqrt, [k_ss], [k_ss], acc=True, scale=1.0 / DM, bias=EPS)
                S.op("dve", lambda e, ss=ss: e.reciprocal(out=ss[:, 2:3], in_=ss[:, 1:2]), reads=[k_ss], writes=[k_ss], acc=True)
                tm, k_tm = tmpp.get()
                stt(S, tm[:], ht[:], ss[:, 2:3], Gm[:, mr, :], ALU.mult, ALU.mult, [k_h, k_ss] + t_all, [k_tm])
                hx, k_hx = hxp.get()
                tt(S, "dve", hx[:], tm[:], Sm[:, mr, :], ALU.add, [k_tm] + t_all, [k_hx])
                tp, k_tp = ptp.get()
                for kc in range(8):
                    S.op("pe", lambda e, tp=tp, hx=hx, kc=kc: e.transpose(tp[:, kc, :], hx[:, kc * 128:(kc + 1) * 128], C.idb[:]),
                         reads=[k_hx, C.t_c], writes=[k_tp], acc=(kc > 0))
                cp(S, "act", hxT[:, :, ti * 128:(ti + 1) * 128], tp[:], [k_tp], [k_hxT], acc=(ti > 0))
            if STOP == 2:
                continue
            S.dma("pool", d["hxT_s"].rearrange("c p t -> p c t")[:, :, t0:t0 + n], hxT[:, :, 0:n],
                  reads=[k_hxT], writes=[C.T(f"hxT:{g}")])
            if STOP == 3:
                continue

            def proj(col0, m, ps):
                pt, pk = ps
                for kc in range(8):
                    mm(S, pt[0:m, 0:n], wsb[:, kc, col0:col0 + m], hxT[:, kc, 0:n], kc == 0, kc == 7,
                       [t_w, k_hxT], pk)

            def rope_evac(prj, col0, cols0, m, out_ap, out_k, ri, acc):
                ps = pp.get()
                prj(col0, m, ps)
                if not isx:
                    cp(S, "act", out_ap, ps[0][0:m, 0:n], [ps[1]], [out_k], acc=acc)
                    return
                ps2 = pp.get()
                prj(cols0, m, ps2)
                r1, k1 = r1p.get()
                r2, k2 = r2p.get()
                tt(S, "dve", r1[0:m, 0:n], ps[0][0:m, 0:n], rp[0:m, ri, 0:n], ALU.mult, [ps[1], k_rp], [k1])
                tt(S, "dve", r2[0:m, 0:n], ps2[0][0:m, 0:n], rp[0:m, ri + 1, 0:n], ALU.mult, [ps2[1], k_rp], [k2])
                tt(S, "dve", out_ap, r1[0:m, 0:n], r2[0:m, 0:n], ALU.add, [k1, k2], [out_k], acc=acc)

            qaT, k_qaT = o4p.get()
            for c in range(4):
                rope_evac(proj, O_QA + c * 128, O_QAS + c * 128, 128, qaT[:, c, 0:n], k_qaT, 0, c > 0)
            S.dma("pool", d["qaT_s"].rearrange("c p t -> p c t")[:, :, t0:t0 + n], qaT[:, :, 0:n],
                  reads=[k_qaT], writes=[C.T(f"qaT:{g}")])
            kaT, k_kaT = o4p.get()
            rope_evac(proj, O_KA, O_KAS, 128, kaT[:, 0, 0:n], k_kaT, 0, False)
            S.dma("pool", d["kaT_s"][:, t0:t0 + n], kaT[:, 0, 0:n], reads=[k_kaT], writes=[C.T(f"kaT:{g}")])
            krT, k_krT = krTp.get()
            rope_evac(proj, O_KR, O_KRS, 32, krT[:, 0:n], k_krT, 2, False)
            for h in range(8):
                S.dma("pool", d["kbT_s"][h, 64:96, t0:t0 + n], krT[:, 0:n], reads=[k_krT],
                      writes=[C.T(f"kbT:{g}")], acc=True)
            if STOP == 4:
                continue
            uT, k_uT = o4p.get()
            for c in range(4):
                ps = pp.get()
                proj(O_U + c * 128, 128, ps)
                cp(S, "act", uT[:, c, 0:n], ps[0][:, 0:n], [ps[1]], [k_uT], acc=(c > 0))
            S.dma("pool", d["uT_s"].rearrange("c p t -> p c t")[:, :, t0:t0 + n], uT[:, :, 0:n],
                  reads=[k_uT], writes=[C.T(f"uT:{g}")])
            va, k_va = vap.get()
            for ti in range(ntl):
                pt, pk = pp.get()
                for kc in range(8):
                    mm(S, pt[:, 0:128], hxT[:, kc, ti * 128:(ti + 1) * 128], wsb[:, kc, O_VA:O_VA + 128],
                       kc == 0, kc == 7, [t_w, k_hxT], pk)
                cp(S, "act", va[:, ti, :, 0:64], pt[:, 0:128].rearrange("p (g e) -> p g e", g=2), [pk], [k_va], acc=(ti > 0))
            S.dma("pool", d["va_s"][t0:t0 + n, :].rearrange("(i p) f -> p i f", p=128),
                  va[:, 0:ntl, :, :].rearrange("p i g e -> p i (g e)"), reads=[k_va], writes=[C.T(f"va:{g}")])
            if STOP == 5:
                continue
            lat_out = []
            for (col0, npool) in ((O_CQ, cqnp), (O_CKV, ckvnp)):
                lf, k_lf = latfp.get()
                lq, k_lq = latqp.get()
                for c in range(2):
                    ps = pp.get()
                    proj(col0 + c * 128, 128, ps)
                    act(S, lq[:, c, 0:n], ps[0][:, 0:n], AF.Square, [ps[1]], [k_lq], acc=(c > 0))
                    cp(S, "act", lf[:, c, 0:n], ps[0][:, 0:n], [ps[1]], [k_lf], acc=(c > 0))
                if STOP == 61:
                    continue
                pt, pk = pp.get()
                for c in range(2):
                    mm(S, pt[:, 0:n], C.onb[:], lq[:, c, 0:n], c == 0, c == 1, [k_lq, C.t_c], pk)
                rb, k_rb = rbcp.get()
                if STOP == 62:
                    continue
                act(S, rb[:, 0:n], pt[:, 0:n], AF.Sqrt, [pk], [k_rb], scale=1.0 / 256, bias=EPS)
                if STOP == 63:
                    continue
                S.op("dve", lambda e, rb=rb, n=n: e.reciprocal(out=rb[:, 0:n], in_=rb[:, 0:n]), reads=[k_rb], writes=[k_rb], acc=True)
                if STOP == 64:
                    continue
                ln, k_ln = npool.get()
                for c in range(2):
                    tt(S, "dve", ln[:, c, 0:n], lf[:, c, 0:n], rb[:, 0:n], ALU.mult, [k_lf, k_rb], [k_ln], acc=(c > 0))
                lat_out.append((ln, k_ln))
            if STOP > 60:
                continue
            (cqn, k_cqn), (ckvn, k_ckvn) = lat_out
            if STOP == 6:
                continue

            def mkproj2(wt, lat, k_lat):
                def proj2(col0, m, ps):
                    pt, pk = ps
                    for kc in range(2):
                        mm(S, pt[0:m, 0:n], wt[:, kc, col0:col0 + m], lat[:, kc, 0:n], kc == 0, kc == 1, [t_wq, k_lat], pk)
                return proj2
            pq = mkproj2(wq, cqn, k_cqn)
            pk_ = mkproj2(wkv, ckvn, k_ckvn)
            qnT, k_qnT = o4p.get()
            for c in range(4):
                ps = pp.get()
                pq(c * 128, 128, ps)
                cp(S, "act", qnT[:, c, 0:n], ps[0][:, 0:n], [ps[1]], [k_qnT], acc=(c > 0))
            for h in range(8):
                S.dma("pool", d["qbT_s"][h, 0:64, t0:t0 + n], qnT[(h % 2) * 64:(h % 2) * 64 + 64, h // 2, 0:n],
                      reads=[k_qnT], writes=[C.T(f"qbT:{g}")], acc=True)
            if STOP == 7:
                continue
            qrT, k_qrT = o4p.get()
            for c in range(2):
                rope_evac(pq, 512 + c * 128, 768 + c * 128, 128, qrT[:, c, 0:n], k_qrT, 2, c > 0)
            for h in range(8):
                S.dma("pool", d["qbT_s"][h, 64:96, t0:t0 + n], qrT[(h % 4) * 32:(h % 4) * 32 + 32, h // 4, 0:n],
                      reads=[k_qrT], writes=[C.T(f"qbT:{g}")], acc=True)
            if STOP == 8:
                continue
            knT, k_knT = o4p.get()
            for c in range(4):
                ps = pp.get()
                pk_(c * 128, 128, ps)
                cp(S, "act", knT[:, c, 0:n], ps[0][:, 0:n], [ps[1]], [k_knT], acc=(c > 0))
            for h in range(8):
                S.dma("pool", d["kbT_s"][h, 0:64, t0:t0 + n], knT[(h % 2) * 64:(h % 2) * 64 + 64, h // 2, 0:n],
                      reads=[k_knT], writes=[C.T(f"kbT:{g}")], acc=True)
            if STOP == 9:
                continue
            vb, k_vb = vbp.get()
            for ti in range(ntl):
                pt, pk = pp.get()
                for kc in range(2):
                    mm(S, pt[:, :], ckvn[:, kc, ti * 128:(ti + 1) * 128], wkv[:, kc, 512:1024], kc == 0, kc == 1,
                       [t_wq, k_ckvn], pk)
                cp(S, "act", vb[:, ti, :, 0:64], pt[:, :].rearrange("p (g e) -> p g e", g=8), [pk], [k_vb], acc=(ti > 0))
            S.dma("pool", d["vb_s"][t0:t0 + n, :].rearrange("(i p) f -> p i f", p=128),
                  vb[:, 0:ntl, :, :].rearrange("p i g e -> p i (g e)"), reads=[k_vb], writes=[C.T(f"vb:{g}")])
    S.barrier()


def _deint(n):
    return np.concatenate([np.arange(0, n, 2), np.arange(1, n, 2)])


def _deint_sw(n):
    return np.concatenate([np.arange(1, n, 2), np.arange(0, n, 2)])


def _w_in_cols():
    cols = []
    for sw in (False, True):
        for c in range(4):
            for h in (c, 4 + c):
                cols.append(h * 64 + (_deint_sw(64) if sw else _deint(64)))
    for sw in (False, True):
        for g in range(2):
            cols.append(512 + g * 64 + (_deint_sw(64) if sw else _deint(64)))
    cols.append(1280 + _deint(32))
    cols.append(1280 + _deint_sw(32))
    cols.append(np.arange(768, 1280))
    cols.append(np.arange(1312, 1824))
    cols.append(np.arange(640, 768))
    cols.append(np.arange(1824, 4896))
    out = np.concatenate(cols)
    assert out.shape[0] == W_EXT
    return out


def _w_uq_cols():
    cols = [h * 96 + np.arange(64) for h in range(8)]
    cols += [h * 96 + 64 + _deint(32) for h in range(8)]
    cols += [h * 96 + 64 + _deint_sw(32) for h in range(8)]
    return np.concatenate(cols)


def _w_ukv_cols():
    cols = [h * 128 + np.arange(64) for h in range(8)]
    cols += [h * 128 + 64 + np.arange(64) for h in range(8)]
    return np.concatenate(cols)


def _rope_tables():
    t = np.arange(SEQ)
    row = (t // 64).astype(np.float32)
    col = (t % 64).astype(np.float32)

    def tabs(rot):
        axis_dim = rot // 2
        inv = (np.float32(10000.0) ** (-np.arange(0, axis_dim, 2, dtype=np.float32) / np.float32(axis_dim))).astype(np.float32)
        ang = np.concatenate([row[:, None] * inv, col[:, None] * inv], axis=-1).astype(np.float32)
        cos, sin = np.cos(ang).astype(np.float32), np.sin(ang).astype(np.float32)
        half = rot // 2
        ct = np.concatenate([cos, cos], axis=1).T
        st_ = np.concatenate([-sin, sin], axis=1).T
        rep = 128 // rot
        return np.ascontiguousarray(np.tile(ct, (rep, 1))), np.ascontiguousarray(np.tile(st_, (rep, 1)))
    ca, sa = tabs(64)
    cb, sb_ = tabs(32)
    return ca, sa, cb, sb_


SCRATCH = {
    "modv": ([2, 2, 6144], F32),
    "h_s": ([NTOK, DM], F32),
    "hxT_s": ([8, 128, NTOK], BF16),
    "qaT_s": ([4, 128, NTOK], BF16),
    "kaT_s": ([128, NTOK], BF16),
    "va_s": ([NTOK, 130], BF16),
    "qbT_s": ([8, 96, NTOK], BF16),
    "kbT_s": ([8, 96, NTOK], BF16),
    "vb_s": ([NTOK, 520], BF16),
    "uT_s": ([4, 128, NTOK], BF16),
    "oaT_s": ([4, 128, NTOK], BF16),
    "obT_s": ([4, 128, NTOK], BF16),
    "ocT_s": ([4, 128, NTOK], BF16),
    "gT_s": ([32, NTOK], F32),
    "wgu_b": ([4096, 4096], BF16),
    "wdn_b": ([4096, 2048], BF16),
    "xbuf": ([12800, DM], BF16),
    "ybuf": ([12800, DM], F32),
}
INPUTS = {
    "h0": ([NTOK, DM], F32),
    "cT": ([128, 8, 2], F32),
    "w_mod": ([2, DM, 6144], F32),
    "b_mod": ([2, 6144], F32),
    "norm_mix_g": ([2, DM], F32),
    "norm_ffn_g": ([2, DM], F32),
    "w_in_ext": ([2, DM, W_EXT], F32),
    "w_uq_ext": ([2, 256, 1024], F32),
    "w_ukv_ext": ([2, 256, 1024], F32),
    "q_norm_gT": ([2, 128, 2], F32),
    "kv_norm_gT": ([2, 128, 2], F32),
    "ropeA_c": ([128, SEQ], F32),
    "ropeA_s": ([128, SEQ], F32),
    "ropeB_c": ([128, SEQ], F32),
    "ropeB_s": ([128, SEQ], F32),
    "sink": ([2, 8], F32),
    "sink_row": ([2, 2, 512], F32),
    "w_pool": ([2, 4, 128, 128], F32),
    "pool_scaleT": ([2, 128, 4], F32),
    "invx": ([4, SEQ], F32),
    "invc": ([4, LC], F32),
    "w_br": ([2, 3, 512, DM], F32),
    "w_out": ([2, DM, DM], F32),
    "w_r": ([2, DM, 36], F32),
    "b_r": ([2, 36], F32),
    "w_gu_h": ([2, 32 * 128, 4096], F32),
    "w_dn_h": ([2, 32 * 128, 2048], F32),
    "final_g": ([1, DM], F32),
}


def prep_shared(inp):
    out = {}
    f = lambda a: np.ascontiguousarray(np.asarray(a, dtype=np.float32))
    out["w_mod"] = f(inp["w_mod"])
    out["b_mod"] = f(inp["b_mod"])
    out["norm_mix_g"] = f(inp["norm_mix_g"])
    out["norm_ffn_g"] = f(inp["norm_ffn_g"])
    out["sink"] = f(inp["sink"])
    out["sink_row"] = f(np.repeat(np.asarray(inp["sink"]).reshape(2, 2, 4), 128, axis=-1))
    out["w_pool"] = f(inp["w_pool"])
    out["pool_scaleT"] = f(np.asarray(inp["pool_scale"]).reshape(2, 4, 128).transpose(0, 2, 1))
    out["w_br"] = f(np.stack([np.asarray(inp[k]) for k in ("w_br_a", "w_br_b", "w_br_c")], axis=1))
    out["w_out"] = f(inp["w_out"])
    out["w_r"] = f(np.concatenate([np.asarray(inp["w_rg"]), np.asarray(inp["w_re"])], axis=-1))
    out["b_r"] = f(np.concatenate([np.asarray(inp["b_rg"]), np.asarray(inp["b_re"])], axis=-1))
    out["w_gu_h"] = f(np.asarray(inp["w_gu"]).reshape(2, 32, 8, 128, 512).transpose(0, 1, 3, 2, 4).reshape(2, 32 * 128, 4096))
    out["w_dn_h"] = f(np.asarray(inp["w_dn"]).reshape(2, 32, 2, 128, 1024).transpose(0, 1, 3, 2, 4).reshape(2, 32 * 128, 2048))
    out["final_g"] = f(np.asarray(inp["final_g"]).reshape(1, DM))
    for nm, L in (("invx", SEQ), ("invc", LC)):
        t = np.arange(L)
        rows = []
        for r in (1, 2, 4, 8):
            cnt = np.minimum(t + r + 1, L) - np.maximum(t - r, 0)
            rows.append((1.0 / cnt.astype(np.float32)).astype(np.float32))
        out[nm] = np.stack(rows)
    out["w_in_ext"] = f(np.asarray(inp["w_in"])[:, :, _w_in_cols()])
    out["w_uq_ext"] = f(np.asarray(inp["w_uq"])[:, :, _w_uq_cols()])
    out["w_ukv_ext"] = f(np.asarray(inp["w_ukv"])[:, :, _w_ukv_cols()])
    out["q_norm_gT"] = f(np.asarray(inp["q_norm_g"]).reshape(2, 2, 128).transpose(0, 2, 1))
    out["kv_norm_gT"] = f(np.asarray(inp["kv_norm_g"]).reshape(2, 2, 128).transpose(0, 2, 1))
    ca, sa, cb, sb_ = _rope_tables()
    out["ropeA_c"], out["ropeA_s"], out["ropeB_c"], out["ropeB_s"] = ca, sa, cb, sb_
    return out


def prep_core(inp, b):
    out = {}
    x, ctx, c, cc = (np.asarray(inp[k], dtype=np.float32) for k in ("x", "ctx", "c", "c_ctx"))
    out["h0"] = np.ascontiguousarray(np.concatenate([ctx[b], x[b]], axis=0))
    out["cT"] = np.ascontiguousarray(np.stack([c[b].reshape(8, 128).T, cc.reshape(8, 128).T], axis=-1))
    return out


def build_program(phases, debug_out=()):
    nc = bass.Bass("TRN2", target_bir_lowering=False)
    S = Sched(nc)
    C = Ctx(nc, S)
    for k, (shape, dt) in INPUTS.items():
        C.d[k] = nc.dram_tensor(k, list(shape), dt, kind="ExternalInput").ap()
    for k, (shape, dt) in SCRATCH.items():
        kind = "ExternalOutput" if k in debug_out else "Internal"
        C.d[k] = nc.dram_tensor(k, list(shape), dt, kind=kind).ap()
    import contextlib
    with contextlib.ExitStack() as st:
        make_ident(C, st)
        phases(C)
        S.emit()
    return nc


NEG = -30000.0


def attn_pipeline(C, st, name, items, scale, hook=None, hook_every=1):
    S = C.S
    pps = PsumPool(C, st, 2, f"{name}_ps", (128, 2, 512), F32)
    pac = PsumPool(C, st, 2, f"{name}_ac", (128, 512), F32)
    ptp = SbPool(C, st, 3, f"{name}_pt", [128, 2, 512], BF16)
    steps = []
    for it in items:
        ks = it["keys"]
        for a in range(0, len(ks), 2):
            steps.append((it, a, ks[a:a + 2]))
    state = {}

    def emit_S(si):
        it, a, chunk = steps[si]
        n = it["n"]
        if "q" not in it:
            it["load_q"](it)
        ps, k_ps = pps.get()
        for jj, (k_ap, v_ap, msk) in enumerate(chunk):
            mm(S, ps[:, jj, 0:n], k_ap, it["q"], True, msk is None, it["kv_reads"] + it["q_reads"], k_ps)
            if msk is not None:
                mm(S, ps[:, jj, 0:n], C.idb[:], msk[:, 0:n], False, True, [it["m_tok"], C.t_c], k_ps)
        pt, k_pt = ptp.get()
        act(S, pt[:, 0:len(chunk), 0:n], ps[:, 0:len(chunk), 0:n], AF.Exp, [k_ps], [k_pt], scale=scale)
        state[si] = (pt, k_pt)

    def emit_PV(si):
        it, a, chunk = steps[si]
        n = it["n"]
        pt, k_pt = state.pop(si)
        if a == 0:
            it["acc"] = pac.get()
        acc, k_acc = it["acc"]
        nk = len(it["keys"])
        for jj, (k_ap, v_ap, msk) in enumerate(chunk):
            mm(S, acc[0:65, 0:n], v_ap, pt[:, jj, 0:n], a + jj == 0, a + jj == nk - 1, [k_pt] + it["kv_reads"], k_acc)
        if a + len(chunk) == nk:
            it["finish"](acc, k_acc)

    if not steps:
        return
    emit_S(0)
    for si in range(len(steps)):
        if hook is not None and si % hook_every == 0:
            hook(si // hook_every)
        if si + 1 < len(steps):
            emit_S(si + 1)
        emit_PV(si)


def _norm_store(C, pools, acc, k_acc, n, add_ap, add_tok, dst_ap, wtoks):
    S = C.S
    rsp, bcp, bcsp, op_, onesf = pools
    rs, k_rs = rsp.get()
    if add_ap is not None:
        tt(S, "dve", rs[64:65, 0:n], acc[64:65, 0:n], add_ap, ALU.add, [k_acc, add_tok], [k_rs])
        S.op("dve", lambda e, rs=rs, n=n: e.reciprocal(out=rs[64:65, 0:n], in_=rs[64:65, 0:n]), reads=[k_rs], writes=[k_rs], acc=True)
    else:
        S.op("dve", lambda e, rs=rs, n=n, acc=acc: e.reciprocal(out=rs[64:65, 0:n], in_=acc[64:65, 0:n]), reads=[k_acc], writes=[k_rs])
    bc, k_bc = bcp.get()
    mm(S, bc[0:64, 0:n], onesf[64:65, 0:64], rs[64:65, 0:n], True, True, [k_rs, C.t_c], k_bc)
    bcs, k_bcs = bcsp.get()
    cp(S, "act", bcs[:, 0:n], bc[0:64, 0:n], [k_bc], [k_bcs])
    o, k_o = op_.get()
    tt(S, "dve", o[:, 0:n], acc[0:64, 0:n], bcs[:, 0:n], ALU.mult, [k_acc, k_bcs], [k_o])
    S.dma("pool", dst_ap(o), o[:, 0:n] if dst_ap.flat else o[:, 0:n].rearrange("p (u t) -> p u t", u=4), reads=[k_o], writes=wtoks, acc=True)


class _Dst:
    def __init__(self, ap, flat):
        self.ap, self.flat = ap, flat

    def __call__(self, o):
        return self.ap


def _norm_pools(C, st, name):
    nc = C.nc
    rsp = SbPool(C, st, 2, f"{name}_rs", [65, 512], F32)
    bcp = PsumPool(C, st, 1, f"{name}_bc", (64, 512), F32)
    bcsp = SbPool(C, st, 2, f"{name}_bcs", [64, 512], F32)
    op_ = SbPool(C, st, 3, f"{name}_o", [64, 512], BF16)
    onesf = st.enter_context(nc.sbuf_tensor(f"{name}_onesf", [128, 64], F32))
    C.S.op("dve", lambda e: e.memset(onesf[:], 1.0), writes=[C.t_c], acc=True)
    return (rsp, bcp, bcsp, op_, onesf)


def phase_attn_a(C, l):
    nc, S, d = C.nc, C.S, C.d
    import contextlib
    last = (l == DEPTH - 1)
    with contextlib.ExitStack() as st:
        sb = lambda name, shape, dt: st.enter_context(nc.sbuf_tensor(f"a{l}_{name}", list(shape), dt))
        ka = sb("ka", [128, NTOK], BF16)
        va = sb("va", [128, NT, 130], BF16)
        esk = sb("esk", [65, 2, 512], F32)
        mi = sb("mi", [128, 512], I32)
        mP, mN = sb("mP", [128, 512], BF16), sb("mN", [128, 512], BF16)
        t_ka, t_va, t_es, t_mi, t_m = Tok(), Tok(), Tok(), Tok(), Tok()
        S.dma("sp", ka[:], d["kaT_s"][:, :], reads=[C.T(f"kaT:{g}") for g in range(9)], writes=[t_ka])
        S.dma("sp", va[:], d["va_s"].rearrange("(i p) f -> p i f", p=128), reads=[C.T(f"va:{g}") for g in range(9)], writes=[t_va])
        S.dma("sp", esk[64:65, :, :], d["sink_row"][l:l + 1, :, :], writes=[t_es])
        act(S, esk[64:65, :, :], esk[64:65, :, :], AF.Exp, [t_es], [t_es])
        S.op("pool", lambda e: e.iota(mi[:], pattern=[[0, 4], [1, 128]], base=0, channel_multiplier=-1), writes=[t_mi])
        ts(S, "dve", mP[:], mi[:], 0, NEG, ALU.is_gt, ALU.mult, [t_mi], [t_m], acc=True)
        ts(S, "dve", mN[:], mi[:], 0, NEG, ALU.is_lt, ALU.mult, [t_mi], [t_m], acc=True)
        pools = _norm_pools(C, st, f"a{l}")
        qp = SbPool(C, st, 3, f"a{l}_q", [128, 4, 128], BF16)
        qtiles = list(range(2, NT)) if last else list(range(NT))
        if QT_LIMIT:
            qtiles = qtiles[:QT_LIMIT]
        items = []
        for qt in qtiles:
            if qt < 2:
                keys = [(0, None), (1, None)]
            else:
                xq = qt - 2
                keys = [(0, None), (1, None)]
                if xq > 0:
                    keys.append((qt - 1, mP))
                keys.append((qt, None))
                if xq < 31:
                    keys.append((qt + 1, mN))
            for g in range(2):
                it = {"qt": qt, "g": g, "n": 512, "m_tok": t_m, "kv_reads": [t_ka, t_va],
                      "keys": [(ka[g * 64:(g + 1) * 64, kt * 128:(kt + 1) * 128], va[:, kt, g * 65:(g + 1) * 65], msk)
                               for kt, msk in keys]}
                items.append(it)
        cur = {}

        def load_q(it):
            qt, g = it["qt"], it["g"]
            if qt not in cur:
                q, k_q = qp.get()
                S.dma("sp", q[:], d["qaT_s"].rearrange("c p t -> p c t")[:, :, qt * 128:(qt + 1) * 128],
                      reads=[C.T(f"qaT:{gg}") for gg in range(9)], writes=[k_q])
                cur.clear()
                cur[qt] = (q, k_q)
            q, k_q = cur[qt]
            it["q"] = q[g * 64:(g + 1) * 64, :, :].rearrange("p c t -> p (c t)")
            it["q_reads"] = [k_q]

        for it in items:
            qt, g = it["qt"], it["g"]
            it["load_q"] = load_q
            dst = d["oaT_s"][2 * g:2 * g + 2].rearrange("u2 (u1 e) t -> e (u2 u1) t", u1=2)[:, :, qt * 128:(qt + 1) * 128]
            it["finish"] = (lambda acc, k_acc, g=g, qt=qt, dst=dst:
                            _norm_store(C, pools, acc, k_acc, 512, esk[64:65, g, :], t_es, _Dst(dst, False),
                                        [C.T(f"oaT_s:{qt}")]))
        attn_pipeline(C, st, f"a{l}", items, 0.125)
    S.barrier()


def phase_attn_b(C, l):
    nc, S, d = C.nc, C.S, C.d
    import contextlib
    last = (l == DEPTH - 1)
    scale = 96.0 ** -0.5
    with contextlib.ExitStack() as st:
        sb = lambda name, shape, dt: st.enter_context(nc.sbuf_tensor(f"b{l}_{name}", list(shape), dt))
        kb = sb("kb", [96, 8, NTOK], BF16)
        vb = sb("vb", [128, NT, 520], BF16)
        t_kb, t_vb = Tok(), Tok()
        for h in range(8):
            S.dma("sp", kb[:, h, :], d["kbT_s"][h], reads=[C.T(f"kbT:{g}") for g in range(9)], writes=[t_kb], acc=True)
        S.dma("sp", vb[:], d["vb_s"].rearrange("(i p) f -> p i f", p=128), reads=[C.T(f"vb:{g}") for g in range(9)], writes=[t_vb])
        pools = _norm_pools(C, st, f"b{l}")
        qp = SbPool(C, st, 2, f"b{l}_q", [96, 8, 512], BF16)
        groups = list(enumerate(GROUPS))
        if last:
            groups = groups[1:]
        if QT_LIMIT:
            groups = groups[:2]
        items = []
        curq = {}

        def load_q(it):
            g, h = it["g"], it["h"]
            t0, n = GROUPS[g]
            if g not in curq:
                q, k_q = qp.get()
                S.dma("sp", q[:, :, 0:n], d["qbT_s"].rearrange("h p t -> p h t")[:, :, t0:t0 + n],
                      reads=[C.T(f"qbT:{g}")], writes=[k_q])
                curq.clear()
                curq[g] = (q, k_q)
            q, k_q = curq[g]
            it["q"] = q[:, h, 0:n]
            it["q_reads"] = [k_q]

        for g, (t0, n) in groups:
            keys = [0, 1] if g == 0 else list(range(NT))
            tiles = list(range(t0 // 128, (t0 + n) // 128))
            for h in range(8):
                dst = d["obT_s"][h // 2, (h % 2) * 64:(h % 2) * 64 + 64, t0:t0 + n]
                items.append({"n": n, "g": g, "h": h, "load_q": load_q, "kv_reads": [t_kb, t_vb], "m_tok": None,
                              "keys": [(kb[:, h, kt * 128:(kt + 1) * 128], vb[:, kt, h * 65:(h + 1) * 65], None) for kt in keys],
                              "finish": (lambda acc, k_acc, n=n, dst=dst, tiles=tiles:
                                         _norm_store(C, pools, acc, k_acc, n, None, None, _Dst(dst, True),
                                                     [C.T(f"obT_s:{t}") for t in tiles]))})
        stg_g = SbPool(C, st, 1, f"b{l}_sgg", [128, 4096], F32)
        stg_d = SbPool(C, st, 1, f"b{l}_sgd", [128, 2048], F32)
        cb_g = SbPool(C, st, 1, f"b{l}_cbg", [128, 4096], BF16)
        cb_d = SbPool(C, st, 1, f"b{l}_cbd", [128, 2048], BF16)

        def precast(k):
            if k >= 64 or QT_LIMIT:
                return
            e_, part = k // 2, k % 2
            srcw, dstw, sp_, cp_ = ((d["w_gu_h"], d["wgu_b"], stg_g, cb_g) if part == 0 else (d["w_dn_h"], d["wdn_b"], stg_d, cb_d))
            s_t, s_k = sp_.get()
            S.dma("sp", s_t[:], srcw[l, e_ * 128:(e_ + 1) * 128, :], writes=[s_k])
            c_t, c_k = cp_.get()
            cp(S, "dve", c_t[:], s_t[:], [s_k], [c_k])
            S.dma("pool", dstw[e_ * 128:(e_ + 1) * 128, :], c_t[:], reads=[c_k], writes=[C.T("wexp_b")], acc=True)

        nsteps = sum((len(it["keys"]) + 1) // 2 for it in items)
        attn_pipeline(C, st, f"b{l}", items, scale, hook=precast, hook_every=max(1, nsteps // 66))
    S.barrier()


def phase_pool(C, l):
    nc, S, d = C.nc, C.S, C.d
    import contextlib
    last = (l == DEPTH - 1)
    seqs = [(256, SEQ, "invx")] + ([] if last else [(0, LC, "invc")])
    with contextlib.ExitStack() as st:
        sb = lambda name, shape, dt: st.enter_context(nc.sbuf_tensor(f"c{l}_{name}", list(shape), dt))
        wps = sb("wps", [128, 4, 128], F32)
        wp = sb("wp", [128, 4, 128], BF16)
        psc = sb("psc", [128, 4], F32)
        Up = sb("Up", [128, SEQ + 16], F32)
        Aa, Ab = sb("Aa", [128, SEQ + 16], F32), sb("Ab", [128, SEQ + 16], F32)
        inv = sb("inv", [128, SEQ], F32)
        ub = sb("ub", [128, SEQ], BF16)
        pl = sb("pl", [128, SEQ], BF16)
        oc = sb("oc", [128, SEQ], BF16)
        t_wp, t_psc, t_Up, t_A, t_B, t_inv, t_ub, t_pl, t_oc = (Tok() for _ in range(9))
        S.dma("sp", wps[:], d["w_pool"][l].rearrange("g c e -> c g e"), writes=[t_wp])
        cp(S, "act", wp[:], wps[:], [t_wp], [t_wp])
        S.dma("sp", psc[:], d["pool_scaleT"][l], writes=[t_psc])
        pp = PsumPool(C, st, 3, f"c{l}_pp")
        eng = ["dve", "dve"]
        ei = 0
        for (t0, L, invname) in seqs:
            for g in range(4):
                r = (1, 2, 4, 8)[g]
                S.dma("sp", ub[:, 0:L], d["uT_s"][g, :, t0:t0 + L], reads=[C.T(f"uT:{q}") for q in range(9)], writes=[t_ub])
                S.dma("sp", inv[:, 0:L], bcast_row(d[invname][g:g + 1, :], L), writes=[t_inv])
                S.op("dve", lambda e, L=L: e.memset(Up[:, 0:L + 16], 0.0), writes=[t_Up])
                cp(S, "dve", Up[:, 8:8 + L], ub[:, 0:L], [t_ub], [t_Up])
                src, k_src, ln = Up, t_Up, L + 16
                bufs = [(Aa, t_A), (Ab, t_B)]
                step = 1
                bi = 0
                while step <= r:
                    dst, k_dst = bufs[bi]
                    bi ^= 1
                    nl = ln - step
                    tt(S, eng[ei % 2], dst[:, 0:nl], src[:, 0:nl], src[:, step:step + nl], ALU.add, [k_src], [k_dst])
                    ei += 1
                    src, k_src, ln = dst, k_dst, nl
                    step *= 2
                dst, k_dst = bufs[bi]
                tt(S, eng[ei % 2], dst[:, 0:L], src[:, 8 - r:8 - r + L], Up[:, 8 + r:8 + r + L], ALU.add, [k_src, t_Up], [k_dst])
                ei += 1
                tt(S, "dve", dst[:, 0:L], dst[:, 0:L], inv[:, 0:L], ALU.mult, [k_dst, t_inv], [k_dst])
                tt(S, "dve", pl[:, 0:L], dst[:, 0:L], Up[:, 8:8 + L], ALU.subtract, [k_dst, t_Up], [t_pl])
                for c0 in range(0, L, 512):
                    n = min(512, L - c0)
                    ps, k_ps = pp.get()
                    mm(S, ps[:, 0:n], wp[:, g, :], pl[:, c0:c0 + n], True, True, [t_wp, t_pl], k_ps)
                    act(S, oc[:, c0:c0 + n], ps[:, 0:n], AF.Copy, [k_ps, t_psc], [t_oc], acc=(c0 > 0), scale=psc[:, g:g + 1])
                S.dma("pool", d["ocT_s"][g, :, t0:t0 + L], oc[:, 0:L], reads=[t_oc], writes=[C.T(f"ocT:{g}:{t0}")])
    S.barrier()


def phase_merge(C, l):
    nc, S, d = C.nc, C.S, C.d
    import contextlib
    last = (l == DEPTH - 1)
    hsrc = d["h0"] if l == 0 else d["h_s"]
    with contextlib.ExitStack() as st:
        sb = lambda name, shape, dt: st.enter_context(nc.sbuf_tensor(f"g{l}_{name}", list(shape), dt))
        wg = sb("wg", [128, 8, 3072], BF16)
        wbr = sb("wbr", [128, 12, 1024], BF16)
        wo = sb("wo", [128, 2, 8, 1024], BF16)
        G1 = sb("G1", [128, 2, 1024], F32)
        t_wg, t_wbr, t_wo, t_G1 = Tok(), Tok(), Tok(), Tok()
        wext = d["w_in_ext"][l].rearrange("(kc p) n -> p kc n", p=128)[:, :, O_G:W_EXT]
        load_cast(C, st, f"g{l}_stg", wg, wext, 8, 3072, t_wg, half=768)
        wbsrc = d["w_br"][l].rearrange("b (kc p) n -> p (b kc) n", p=128)
        load_cast(C, st, f"g{l}_stg2", wbr, wbsrc, 12, 1024, t_wbr, half=512)
        for r in range(2):
            S.dma("sp", G1[:, r, :], bcast_row(d["modv"][l][r:r + 1, 2048:3072], 1024), reads=[C.T(f"modv{l}")], writes=[t_G1], acc=True)
        stgo = SbPool(C, st, 2, f"g{l}_stgo", [128, 1024], F32)
        wosrc = d["w_out"][l].rearrange("(kc p) n -> p kc n", p=128)
        for kc in range(8):
            s_t, s_k = stgo.get()
            S.dma("sp", s_t[:], wosrc[:, kc, :], writes=[s_k])
            for r in range(2):
                tt(S, "dve", wo[:, r, kc, :], s_t[:], G1[:, r, :], ALU.mult, [s_k, t_G1], [t_wo], acc=True)
        pp = PsumPool(C, st, 6, f"g{l}_pp")
        hxTp = SbPool(C, st, 1, f"g{l}_hxT", [128, 8, 512], BF16)
        oTp = SbPool(C, st, 1, f"g{l}_oT", [128, 12, 512], BF16)
        YTp = SbPool(C, st, 1, f"g{l}_YT", [128, 8, 512], BF16)
        sgp = SbPool(C, st, 3, f"g{l}_sg", [128, 512], BF16)
        tbp = SbPool(C, st, 4, f"g{l}_tb", [128, 512], F32)
        y1p = SbPool(C, st, 2, f"g{l}_y1", [128, 512], F32)
        hp = SbPool(C, st, 2, f"g{l}_h", [128, 1024], F32)
        hnp = SbPool(C, st, 2, f"g{l}_hn", [128, 1024], F32)
        groups = list(enumerate(GROUPS))
        if last:
            groups = groups[1:]
        if QT_LIMIT:
            groups = groups[:2]
        oc_toks = [C.T(f"ocT:{g}:{t0}") for g in range(4) for t0 in (0, 256)]
        for g, (t0, n) in groups:
            r = 0 if g > 0 else 1
            hxT, k_hxT = hxTp.get()
            S.dma("sp", hxT[:, :, 0:n], d["hxT_s"].rearrange("c p t -> p c t")[:, :, t0:t0 + n], reads=[C.T(f"hxT:{g}")], writes=[k_hxT])
            oT, k_oT = oTp.get()
            tiles = list(range(t0 // 128, (t0 + n) // 128))
            for bi, (nm, rd) in enumerate((("oaT_s", [C.T(f"oaT_s:{t}") for t in tiles]),
                                           ("obT_s", [C.T(f"obT_s:{t}") for t in tiles]),
                                           ("ocT_s", oc_toks))):
                S.dma("sp", oT[:, bi * 4:(bi + 1) * 4, 0:n], d[nm].rearrange("c p t -> p c t")[:, :, t0:t0 + n],
                      reads=rd, writes=[k_oT], acc=(bi > 0))
            YT, k_YT = YTp.get()
            for m in range(8):
                tbs = []
                for br in range(3):
                    psg, k_psg = pp.get()
                    for kc in range(8):
                        mm(S, psg[:, 0:n], wg[:, kc, br * 1024 + m * 128:br * 1024 + (m + 1) * 128], hxT[:, kc, 0:n],
                           kc == 0, kc == 7, [t_wg, k_hxT], k_psg)
                    sg, k_sg = sgp.get()
                    act(S, sg[:, 0:n], psg[:, 0:n], AF.Sigmoid, [k_psg], [k_sg])
                    psv, k_psv = pp.get()
                    for kc in range(4):
                        mm(S, psv[:, 0:n], wbr[:, br * 4 + kc, m * 128:(m + 1) * 128], oT[:, br * 4 + kc, 0:n],
                           kc == 0, kc == 3, [t_wbr, k_oT], k_psv)
                    tb, k_tb = tbp.get()
                    tt(S, "dve", tb[:, 0:n], psv[:, 0:n], sg[:, 0:n], ALU.mult, [k_psv, k_sg], [k_tb])
                    tbs.append((tb, k_tb))
                y1, k_y1 = y1p.get()
                tt(S, "dve", y1[:, 0:n], tbs[0][0][:, 0:n], tbs[1][0][:, 0:n], ALU.add, [tbs[0][1], tbs[1][1]], [k_y1])
                tt(S, "dve", YT[:, m, 0:n], y1[:, 0:n], tbs[2][0][:, 0:n], ALU.add, [k_y1, tbs[2][1]], [k_YT], acc=(m > 0))
            for ti, tile in enumerate(tiles):
                ht, k_h = hp.get()
                S.dma("sp", ht[:], hsrc[tile * 128:(tile + 1) * 128, :], reads=[C.T(f"h:{tile}")], writes=[k_h])
                hn, k_hn = hnp.get()
                for half in range(2):
                    ps, k_ps = pp.get()
                    for m in range(8):
                        mm(S, ps[:, :], YT[:, m, ti * 128:(ti + 1) * 128], wo[:, r, m, half * 512:(half + 1) * 512],
                           m == 0, m == 7, [k_YT, t_wo], k_ps)
                    tt(S, "dve", hn[:, half * 512:(half + 1) * 512], ps[:, :], ht[:, half * 512:(half + 1) * 512], ALU.add,
                       [k_ps, k_h], [k_hn], acc=(half > 0))
                S.dma("pool", d["h_s"][tile * 128:(tile + 1) * 128, :], hn[:], reads=[k_hn], writes=[C.T(f"h:{tile}")])
    S.barrier()


def phase_route(C, l):
    nc, S, d = C.nc, C.S, C.d
    import contextlib
    last = (l == DEPTH - 1)
    with contextlib.ExitStack() as st:
        sb = lambda name, shape, dt: st.enter_context(nc.sbuf_tensor(f"r{l}_{name}", list(shape), dt))
        Gm, Sm = sb("Gm", [128, 2, 1024], F32), sb("Sm", [128, 2, 1024], F32)
        wr = sb("wr", [128, 8, 36], F32)
        br = sb("br", [128, 36], F32)
        t_mod, t_mod2, t_wr = Tok(), Tok(), Tok()
        mv = d["modv"][l]
        tmpp = SbPool(C, st, 2, f"r{l}_tmp", [128, 1024], F32)
        gn, k_gn = tmpp.get()
        S.dma("sp", gn[:], bcast_row(d["norm_ffn_g"][l:l + 1, :], 1024), writes=[k_gn])
        for r in range(2):
            S.dma("sp", Sm[:, r, :], bcast_row(mv[r:r + 1, 3072:4096], 1024), reads=[C.T(f"modv{l}")], writes=[t_mod], acc=True)
            S.dma("sp", Gm[:, r, :], bcast_row(mv[r:r + 1, 4096:5120], 1024), reads=[C.T(f"modv{l}")], writes=[t_mod], acc=True)
        for r in range(2):
            stt(S, Gm[:, r, :], Gm[:, r, :], 1.0, gn[:], ALU.add, ALU.mult, [t_mod, k_gn], [t_mod2], acc=True)
        S.dma("sp", wr[:], d["w_r"][l].rearrange("(kc p) n -> p kc n", p=128), writes=[t_wr], acc=True)
        S.dma("sp", br[:], bcast_row(d["b_r"][l:l + 1, :], 36), writes=[t_wr], acc=True)
        pt32 = PsumPool(C, st, 2, f"r{l}_pt", (128, 4, 128), F32)
        pp = PsumPool(C, st, 2, f"r{l}_pp", (128, 128), F32)
        ptb = PsumPool(C, st, 2, f"r{l}_ptb", (128, 8, 128), BF16)
        hp = SbPool(C, st, 2, f"r{l}_h", [128, 1024], F32)
        fxp = SbPool(C, st, 2, f"r{l}_fx", [128, 1024], F32)
        fxbp = SbPool(C, st, 2, f"r{l}_fxb", [128, 1024], BF16)
        fxTp = SbPool(C, st, 2, f"r{l}_fxT", [128, 8, 128], F32)
        fxTbp = SbPool(C, st, 2, f"r{l}_fxTb", [128, 8, 128], BF16)
        ssp = SbPool(C, st, 2, f"r{l}_ss", [128, 4], F32)
        sqj = sb("sqj", [128, 1024], BF16)
        t_sqj = Tok()
        rtp = SbPool(C, st, 2, f"r{l}_rt", [128, 160], F32)
        gTp = SbPool(C, st, 2, f"r{l}_gT", [32, 128], F32)
        tiles = list(range(2, NT)) if last else list(range(NT))
        if QT_LIMIT:
            tiles = tiles[:QT_LIMIT]
        for tile in tiles:
            mr = 0 if tile >= 2 else 1
            ht, k_h = hp.get()
            S.dma("sp", ht[:], d["h_s"][tile * 128:(tile + 1) * 128, :], reads=[C.T(f"h:{tile}")], writes=[k_h])
            ss, k_ss = ssp.get()
            act(S, sqj[:], ht[:], AF.Square, [k_h], [t_sqj, k_ss], accum_out=ss[:, 0:1])
            act(S, ss[:, 1:2], ss[:, 0:1], AF.Sqrt, [k_ss], [k_ss], acc=True, scale=1.0 / DM, bias=EPS)
            S.op("dve", lambda e, ss=ss: e.reciprocal(out=ss[:, 2:3], in_=ss[:, 1:2]), reads=[k_ss], writes=[k_ss], acc=True)
            tm, k_tm = tmpp.get()
            stt(S, tm[:], ht[:], ss[:, 2:3], Gm[:, mr, :], ALU.mult, ALU.mult, [k_h, k_ss, t_mod2], [k_tm])
            fx, k_fx = fxp.get()
            tt(S, "dve", fx[:], tm[:], Sm[:, mr, :], ALU.add, [k_tm, t_mod2], [k_fx])
            fxb, k_fxb = fxbp.get()
            cp(S, "act", fxb[:], fx[:], [k_fx], [k_fxb])
            tpb, k_tpb = ptb.get()
            for kc in range(8):
                S.op("pe", lambda e, tpb=tpb, fxb=fxb, kc=kc: e.transpose(tpb[:, kc, :], fxb[:, kc * 128:(kc + 1) * 128], C.idb[:]),
                     reads=[k_fxb, C.t_c], writes=[k_tpb], acc=(kc > 0))
            fxTb, k_fxTb = fxTbp.get()
            cp(S, "act", fxTb[:], tpb[:], [k_tpb], [k_fxTb])
            S.dma("pool", d["hxT_s"].rearrange("c p t -> p c t")[:, :, tile * 128:(tile + 1) * 128], fxTb[:],
                  reads=[k_fxTb], writes=[C.T(f"fxT:{tile}")])
            fxT, k_fxT = fxTp.get()
            for hf in range(2):
                tp, k_tp = pt32.get()
                for kc in range(4):
                    S.op("pe", lambda e, tp=tp, fx=fx, kc=kc, hf=hf: e.transpose(tp[:, kc, :], fx[:, (hf * 4 + kc) * 128:(hf * 4 + kc + 1) * 128], C.idf[:]),
                         reads=[k_fx, C.t_c], writes=[k_tp], acc=(kc > 0))
                cp(S, "act", fxT[:, hf * 4:(hf + 1) * 4, :], tp[:], [k_tp], [k_fxT], acc=(hf > 0))
            pl, k_pl = pp.get()
            for kc in range(8):
                mm(S, pl[:, 0:36], fxT[:, kc, :], wr[:, kc, :], kc == 0, kc == 7, [k_fxT, t_wr], k_pl)
            rt, k = rtp.get()
            tt(S, "dve", rt[:, 0:36], pl[:, 0:36], br[:], ALU.add, [k_pl, t_wr], [k])
            S.op("dve", lambda e, rt=rt: e.reduce_max(out=rt[:, 36:37], in_=rt[:, 0:4], axis=AX.X), reads=[k], writes=[k], acc=True)
            ts(S, "dve", rt[:, 37:38], rt[:, 36:37], -1.0, None, ALU.mult, None, [k], [k], acc=True)
            act(S, rt[:, 44:48], rt[:, 0:4], AF.Exp, [k], [k], acc=True, bias=rt[:, 37:38], accum_out=rt[:, 38:39])
            S.op("dve", lambda e, rt=rt: e.reciprocal(out=rt[:, 39:40], in_=rt[:, 38:39]), reads=[k], writes=[k], acc=True)
            ts(S, "dve", rt[:, 40:44], rt[:, 0:4], rt[:, 36:37], None, ALU.is_equal, None, [k], [k], acc=True)
            ts(S, "dve", rt[:, 40:44], rt[:, 40:44], -1.0, 30000.0, ALU.add, ALU.mult, [k], [k], acc=True)
            for g in range(4):
                ts(S, "dve", rt[:, 48 + g * 8:56 + g * 8], rt[:, 4 + g * 8:12 + g * 8], rt[:, 40 + g:41 + g], None,
                   ALU.add, None, [k], [k], acc=True)
            S.op("dve", lambda e, rt=rt: e.max(out=rt[:, 80:88], in_=rt[:, 48:80]), reads=[k], writes=[k], acc=True)
            tt(S, "dve", rt[:, 88:89], rt[:, 81:82], rt[:, 80:81], ALU.subtract, [k], [k], acc=True)
            act(S, rt[:, 89:90], rt[:, 88:89], AF.Exp, [k], [k], acc=True)
            ts(S, "dve", rt[:, 90:91], rt[:, 89:90], 1.0, None, ALU.add, None, [k], [k], acc=True)
            S.op("dve", lambda e, rt=rt: e.reciprocal(out=rt[:, 90:91], in_=rt[:, 90:91]), reads=[k], writes=[k], acc=True)
            tt(S, "dve", rt[:, 91:92], rt[:, 90:91], rt[:, 39:40], ALU.mult, [k], [k], acc=True)
            tt(S, "dve", rt[:, 92:93], rt[:, 91:92], rt[:, 89:90], ALU.mult, [k], [k], acc=True)
            ts(S, "dve", rt[:, 96:128], rt[:, 48:80], rt[:, 80:81], rt[:, 91:92], ALU.is_equal, ALU.mult, [k], [k], acc=True)
            ts(S, "dve", rt[:, 128:160], rt[:, 48:80], rt[:, 81:82], rt[:, 92:93], ALU.is_equal, ALU.mult, [k], [k], acc=True)
            tt(S, "dve", rt[:, 96:128], rt[:, 96:128], rt[:, 128:160], ALU.add, [k], [k], acc=True)
            pg, k_pg = pp.get()
            S.op("pe", lambda e, pg=pg, rt=rt: e.transpose(pg[0:32, 0:128], rt[:, 96:128], C.idf[:]),
                 reads=[k, C.t_c], writes=[k_pg])
            gT, k_gT = gTp.get()
            cp(S, "act", gT[:], pg[0:32, 0:128], [k_pg], [k_gT])
            S.dma("pool", d["gT_s"][:, tile * 128:(tile + 1) * 128], gT[:], reads=[k_gT], writes=[C.T(f"gT:{tile}")])
    S.barrier()


def phase_experts(C, l):
    nc, S, d = C.nc, C.S, C.d
    import contextlib
    last = (l == DEPTH - 1)
    with contextlib.ExitStack() as st:
        sb = lambda name, shape, dt: st.enter_context(nc.sbuf_tensor(f"e{l}_{name}", list(shape), dt))
        g2T = sb("g2T", [128, 2, 8], F32)
        fg = sb("fg", [128, 1024], F32)
        t_g2 = Tok()
        for r in range(2):
            S.dma("sp", g2T[:, r, :], d["modv"][l][r, 5120:6144].rearrange("(m p) -> p m", p=128), reads=[C.T(f"modv{l}")],
                  writes=[t_g2], acc=True, allow_slow_non_contiguous=True)
        if last:
            S.dma("sp", fg[:], bcast_row(d["final_g"][0:1, :], 1024), writes=[t_g2], acc=True)
        ppg = PsumPool(C, st, 4, f"e{l}_ppg")
        pp = PsumPool(C, st, 3, f"e{l}_pp")
        wgs = SbPool(C, st, 1, f"e{l}_wgs", [128, 8, 512], F32)
        wds = SbPool(C, st, 1, f"e{l}_wds", [128, 2, 1024], F32)
        wgp = SbPool(C, st, 2, f"e{l}_wg", [128, 8, 512], BF16)
        wdp = SbPool(C, st, 2, f"e{l}_wd", [128, 2, 1024], BF16)
        fxTp = SbPool(C, st, 1, f"e{l}_fxT", [128, 8, 2176], BF16)
        yacc = sb("yacc", [128, 8, 2176], F32)
        t_y = Tok()
        gwp = SbPool(C, st, 3, f"e{l}_gw", [128, 512], F32)
        sip = SbPool(C, st, 2, f"e{l}_si", [128, 512], F32)
        a1p = SbPool(C, st, 2, f"e{l}_a1", [128, 512], F32)
        actp = SbPool(C, st, 3, f"e{l}_act", [128, 2, 512], BF16)
        hp = SbPool(C, st, 2, f"e{l}_h", [128, 1024], F32)
        hnp = SbPool(C, st, 2, f"e{l}_hn", [128, 1024], F32)
        ssp = SbPool(C, st, 2, f"e{l}_ss", [128, 4], F32)
        sqj = sb("sqj", [128, 1024], BF16)
        t_sqj = Tok()
        tok0 = 256 if last else 0
        sgs = []
        t = tok0
        while t < NTOK:
            n = min(2048 if last else 2176, NTOK - t)
            sgs.append((t, n))
            t += n
        if QT_LIMIT:
            sgs = [(tok0, 256)]
        nexp = 32 if not QT_LIMIT else QT_LIMIT
        for (t0, n) in sgs:
            tiles = list(range(t0 // 128, (t0 + n) // 128))
            fxT, k_fxT = fxTp.get()
            S.dma("sp", fxT[:, :, 0:n], d["hxT_s"].rearrange("c p t -> p c t")[:, :, t0:t0 + n],
                  reads=[C.T(f"fxT:{tl}") for tl in tiles], writes=[k_fxT])
            steps = [(e_, s0) for e_ in range(nexp) for s0 in range(0, n, 512)]
            wcur = {}
            st_state = {}

            def load_w(e_):
                ws, k_ws = wgs.get()
                S.dma("sp", ws[:], d["w_gu"][l, e_].rearrange("(kc p) n -> p kc n", p=128), writes=[k_ws])
                wg, k_wg = wgp.get()
                cp(S, "act", wg[:], ws[:], [k_ws], [k_wg])
                ws2, k_ws2 = wds.get()
                S.dma("sp", ws2[:], d["w_dn"][l, e_].rearrange("(kc p) n -> p kc n", p=128), writes=[k_ws2])
                wd, k_wd = wdp.get()
                cp(S, "act", wd[:], ws2[:], [k_ws2], [k_wd])
                wcur[e_] = (wg, k_wg, wd, k_wd)

            def emit_gu(si):
                e_, s0 = steps[si]
                ns = min(512, n - s0)
                if e_ not in wcur:
                    load_w(e_)
                    wcur.pop(e_ - 2, None)
                wg, k_wg, wd, k_wd = wcur[e_]
                gw, k_gw = gwp.get()
                S.dma("sp", gw[:, 0:ns], bcast_row(d["gT_s"][e_:e_ + 1, t0 + s0:t0 + s0 + ns], ns),
                      reads=[C.T(f"gT:{tl}") for tl in tiles], writes=[k_gw])
                ac, k_ac = actp.get()
                for c in range(2):
                    psg, k_psg = ppg.get()
                    psu, k_psu = ppg.get()
                    for kc in range(8):
                        mm(S, psg[:, 0:ns], wg[:, kc, c * 128:(c + 1) * 128], fxT[:, kc, s0:s0 + ns], kc == 0, kc == 7, [k_wg, k_fxT], k_psg)
                    for kc in range(8):
                        mm(S, psu[:, 0:ns], wg[:, kc, 256 + c * 128:256 + (c + 1) * 128], fxT[:, kc, s0:s0 + ns], kc == 0, kc == 7, [k_wg, k_fxT], k_psu)
                    si_, k_si = sip.get()
                    act(S, si_[:, 0:ns], psg[:, 0:ns], AF.Silu, [k_psg], [k_si])
                    a1, k_a1 = a1p.get()
                    tt(S, "dve", a1[:, 0:ns], psu[:, 0:ns], si_[:, 0:ns], ALU.mult, [k_psu, k_si], [k_a1])
                    tt(S, "dve", ac[:, c, 0:ns], a1[:, 0:ns], gw[:, 0:ns], ALU.mult, [k_a1, k_gw], [k_ac], acc=(c > 0))
                st_state[si] = (ac, k_ac, wd, k_wd)

            def emit_dn(si):
                e_, s0 = steps[si]
                ns = min(512, n - s0)
                ac, k_ac, wd, k_wd = st_state.pop(si)
                for m in range(8):
                    py, k_py = pp.get()
                    for c in range(2):
                        mm(S, py[:, 0:ns], wd[:, c, m * 128:(m + 1) * 128], ac[:, c, 0:ns], c == 0, c == 1, [k_wd, k_ac], k_py)
                    if e_ == 0:
                        cp(S, "act", yacc[:, m, s0:s0 + ns], py[:, 0:ns], [k_py], [t_y], acc=True)
                    else:
                        tt(S, "dve", yacc[:, m, s0:s0 + ns], py[:, 0:ns], yacc[:, m, s0:s0 + ns], ALU.add, [k_py, t_y], [t_y], acc=True)

            emit_gu(0)
            for si in range(len(steps)):
                if si + 1 < len(steps):
                    emit_gu(si + 1)
                emit_dn(si)
            for m in range(8):
                for (a, b_, r) in ((0, 256 - t0, 1), (max(0, 256 - t0), n, 0)):
                    if b_ <= a:
                        continue
                    b_ = min(b_, n)
                    act(S, yacc[:, m, a:b_], yacc[:, m, a:b_], AF.Copy, [t_y, t_g2], [t_y], acc=True, scale=g2T[:, r, m:m + 1])
            for ti, tile in enumerate(tiles):
                ht, k_h = hp.get()
                S.dma("sp", ht[:], d["h_s"][tile * 128:(tile + 1) * 128, :], reads=[C.T(f"h:{tile}")], writes=[k_h])
                hn, k_hn = hnp.get()
                for hf in range(2):
                    ps, k_ps = pp.get()
                    for kc in range(4):
                        m = hf * 4 + kc
                        S.op("pe", lambda e, ps=ps, m=m, kc=kc, ti=ti: e.transpose(ps[:, kc * 128:(kc + 1) * 128], yacc[:, m, ti * 128:(ti + 1) * 128], C.idf[:]),
                             reads=[t_y, C.t_c], writes=[k_ps], acc=(kc > 0))
                    tt(S, "dve", hn[:, hf * 512:(hf + 1) * 512], ps[:, :], ht[:, hf * 512:(hf + 1) * 512], ALU.add, [k_ps, k_h], [k_hn], acc=(hf > 0))
                if not last:
                    S.dma("pool", d["h_s"][tile * 128:(tile + 1) * 128, :], hn[:], reads=[k_hn], writes=[C.T(f"h:{tile}")])
                else:
                    ss, k_ss = ssp.get()
                    act(S, sqj[:], hn[:], AF.Square, [k_hn], [t_sqj, k_ss], accum_out=ss[:, 0:1])
                    act(S, ss[:, 1:2], ss[:, 0:1], AF.Sqrt, [k_ss], [k_ss], acc=True, scale=1.0 / DM, bias=EPS)
                    S.op("dve", lambda e, ss=ss: e.reciprocal(out=ss[:, 2:3], in_=ss[:, 1:2]), reads=[k_ss], writes=[k_ss], acc=True)
                    ho, k_ho = hp.get()
                    stt(S, ho[:], hn[:], ss[:, 2:3], fg[:], ALU.mult, ALU.mult, [k_hn, k_ss, t_g2], [k_ho])
                    S.dma("pool", d["out"][(tile - 2) * 128:(tile - 1) * 128, :], ho[:], reads=[k_ho], writes=[C.T(f"out:{tile}")])
    S.barrier()


def all_phases(C):
    C.d["out"] = C.nc.dram_tensor("out", [SEQ, DM], F32, kind="ExternalOutput").ap()
    for l in range(DEPTH):
        phase_mod(C, l)
        phase_inproj(C, l)
        phase_attn_a(C, l)
        phase_attn_b(C, l)
        phase_pool(C, l)
        phase_merge(C, l)
        phase_moe(C, l)


def kernel(**inputs):
    sh = prep_shared(inputs)
    nc = build_program(all_phases)
    in_maps = []
    for b in range(8):
        m = dict(sh)
        m.update(prep_core(inputs, b))
        in_maps.append({k: m[k] for k in INPUTS})
    res = run_bass_kernel_spmd(nc, in_maps, core_ids=list(range(8)))
    return np.stack([np.asarray(r["out"], dtype=np.float32) for r in res.results], axis=0)


def phase_moe(C, l):
    nc, S, d = C.nc, C.S, C.d
    import contextlib
    last = (l == DEPTH - 1)
    tiles = list(range(2, NT)) if last else list(range(NT))
    if QT_LIMIT:
        tiles = tiles[:QT_LIMIT]
    nt = len(tiles)
    NB = -(-(2 * nt * 128 + 32 * 127) // 128)
    xbuf = d["xbuf"]
    ybuf = d["ybuf"]
    with contextlib.ExitStack() as st0:
        sb0 = lambda name, shape, dt: st0.enter_context(nc.sbuf_tensor(f"x{l}_{name}", list(shape), dt))
        sAB = sb0("sAB", [128, NT, 2], I32)
        w12 = sb0("w12", [128, NT, 2], F32)
        widx = sb0("widx", [128, 128], I32)
        t_sAB, t_w12, t_widx = Tok(), Tok(), Tok()
        t_xz = Tok()
        with contextlib.ExitStack() as st:
            sb = lambda name, shape, dt: st.enter_context(nc.sbuf_tensor(f"r{l}_{name}", list(shape), dt))
            zt = sb("zt", [128, 4, 1024], BF16)
            t_zt = Tok()
            S.op("dve", lambda e: e.memset(zt[:], 0.0), writes=[t_zt])
            xv = xbuf.rearrange("(a p) f -> p a f", p=128)
            for b0 in range(0, NB, 4):
                nb_ = min(4, NB - b0)
                S.dma("sp", xv[:, b0:b0 + nb_, :], zt[:, 0:nb_, :], reads=[t_zt], writes=[t_xz], acc=True)
            Gm, Sm = sb("Gm", [128, 2, 1024], F32), sb("Sm", [128, 2, 1024], F32)
            wr = sb("wr", [128, 8, 36], F32)
            br = sb("br", [128, 36], F32)
            t_mod, t_mod2, t_wr = Tok(), Tok(), Tok()
            mv = d["modv"][l]
            tmpp = SbPool(C, st, 2, f"r{l}_tmp", [128, 1024], F32)
            gn, k_gn = tmpp.get()
            S.dma("sp", gn[:], bcast_row(d["norm_ffn_g"][l:l + 1, :], 1024), writes=[k_gn])
            for r in range(2):
                S.dma("sp", Sm[:, r, :], bcast_row(mv[r:r + 1, 3072:4096], 1024), reads=[C.T(f"modv{l}")], writes=[t_mod], acc=True)
                S.dma("sp", Gm[:, r, :], bcast_row(mv[r:r + 1, 4096:5120], 1024), reads=[C.T(f"modv{l}")], writes=[t_mod], acc=True)
            for r in range(2):
                stt(S, Gm[:, r, :], Gm[:, r, :], 1.0, gn[:], ALU.add, ALU.mult, [t_mod, k_gn], [t_mod2], acc=True)
            S.dma("sp", wr[:], d["w_r"][l].rearrange("(kc p) n -> p kc n", p=128), writes=[t_wr], acc=True)
            S.dma("sp", br[:], bcast_row(d["b_r"][l:l + 1, :], 36), writes=[t_wr], acc=True)
            fx_all = sb("fxall", [128, NT, 1024], BF16)
            A01 = sb("A01", [128, NT, 32], F32)
            B01 = sb("B01", [128, NT, 32], F32)
            Mall = sb("Mall", [128, NT, 32], BF16)
            t_fx = [Tok() for _ in range(NT)]
            t_AB = [Tok() for _ in range(NT)]
            pt32 = PsumPool(C, st, 2, f"r{l}_pt", (128, 4, 128), F32)
            pp = PsumPool(C, st, 2, f"r{l}_pp", (128, 128), F32)
            hp = SbPool(C, st, 2, f"r{l}_h", [128, 1024], F32)
            fxp = SbPool(C, st, 2, f"r{l}_fx", [128, 1024], F32)
            fxTp = SbPool(C, st, 2, f"r{l}_fxT", [128, 8, 128], F32)
            ssp = SbPool(C, st, 2, f"r{l}_ss", [128, 4], F32)
            sqj = sb("sqj", [128, 1024], BF16)
            t_sqj = Tok()
            rtp = SbPool(C, st, 2, f"r{l}_rt", [128, 96], F32)
            for ti, tile in enumerate(tiles):
                mr = 0 if tile >= 2 else 1
                ht, k_h = hp.get()
                S.dma("sp", ht[:], d["h_s"][tile * 128:(tile + 1) * 128, :], reads=[C.T(f"h:{tile}")], writes=[k_h])
                ss, k_ss = ssp.get()
                act(S, sqj[:], ht[:], AF.Square, [k_h], [t_sqj, k_ss], accum_out=ss[:, 0:1])
                act(S, ss[:, 1:2], ss[:, 0:1], AF.Sqrt, [k_ss], [k_ss], acc=True, scale=1.0 / DM, bias=EPS)
                S.op("dve", lambda e, ss=ss: e.reciprocal(out=ss[:, 2:3], in_=ss[:, 1:2]), reads=[k_ss], writes=[k_ss], acc=True)
                tm, k_tm = tmpp.get()
                stt(S, tm[:], ht[:], ss[:, 2:3], Gm[:, mr, :], ALU.mult, ALU.mult, [k_h, k_ss, t_mod2], [k_tm])
                fx, k_fx = fxp.get()
                tt(S, "dve", fx[:], tm[:], Sm[:, mr, :], ALU.add, [k_tm, t_mod2], [k_fx])
                cp(S, "act", fx_all[:, ti, :], fx[:], [k_fx], [t_fx[ti]])
                fxT, k_fxT = fxTp.get()
                for hf in range(2):
                    tp, k_tp = pt32.get()
                    for kc in range(4):
                        S.op("pe", lambda e, tp=tp, fx=fx, kc=kc, hf=hf: e.transpose(tp[:, kc, :], fx[:, (hf * 4 + kc) * 128:(hf * 4 + kc + 1) * 128], C.idf[:]),
                             reads=[k_fx, C.t_c], writes=[k_tp], acc=(kc > 0))
                    cp(S, "act", fxT[:, hf * 4:(hf + 1) * 4, :], tp[:], [k_tp], [k_fxT], acc=(hf > 0))
                pl, k_pl = pp.get()
                for kc in range(8):
                    mm(S, pl[:, 0:36], fxT[:, kc, :], wr[:, kc, :], kc == 0, kc == 7, [k_fxT, t_wr], k_pl)
                rt, k = rtp.get()
                tt(S, "dve", rt[:, 0:36], pl[:, 0:36], br[:], ALU.add, [k_pl, t_wr], [k])
                S.op("dve", lambda e, rt=rt: e.reduce_max(out=rt[:, 36:37], in_=rt[:, 0:4], axis=AX.X), reads=[k], writes=[k], acc=True)
                ts(S, "dve", rt[:, 37:38], rt[:, 36:37], -1.0, None, ALU.mult, None, [k], [k], acc=True)
                act(S, rt[:, 44:48], rt[:, 0:4], AF.Exp, [k], [k], acc=True, bias=rt[:, 37:38], accum_out=rt[:, 38:39])
                S.op("dve", lambda e, rt=rt: e.reciprocal(out=rt[:, 39:40], in_=rt[:, 38:39]), reads=[k], writes=[k], acc=True)
                ts(S, "dve", rt[:, 40:44], rt[:, 0:4], rt[:, 36:37], None, ALU.is_equal, None, [k], [k], acc=True)
                ts(S, "dve", rt[:, 40:44], rt[:, 40:44], -1.0, 30000.0, ALU.add, ALU.mult, [k], [k], acc=True)
                for g in range(4):
                    ts(S, "dve", rt[:, 48 + g * 8:56 + g * 8], rt[:, 4 + g * 8:12 + g * 8], rt[:, 40 + g:41 + g], None,
                       ALU.add, None, [k], [k], acc=True)
                S.op("dve", lambda e, rt=rt: e.max(out=rt[:, 80:88], in_=rt[:, 48:80]), reads=[k], writes=[k], acc=True)
                tt(S, "dve", rt[:, 88:89], rt[:, 81:82], rt[:, 80:81], ALU.subtract, [k], [k], acc=True)
                act(S, rt[:, 89:90], rt[:, 88:89], AF.Exp, [k], [k], acc=True)
                ts(S, "dve", rt[:, 90:91], rt[:, 89:90], 1.0, None, ALU.add, None, [k], [k], acc=True)
                S.op("dve", lambda e, rt=rt: e.reciprocal(out=rt[:, 90:91], in_=rt[:, 90:91]), reads=[k], writes=[k], acc=True)
                tt(S, "dve", w12[:, ti, 0:1], rt[:, 90:91], rt[:, 39:40], ALU.mult, [k], [t_w12], acc=True)
                tt(S, "dve", w12[:, ti, 1:2], w12[:, ti, 0:1], rt[:, 89:90], ALU.mult, [k, t_w12], [t_w12], acc=True)
                ts(S, "dve", A01[:, ti, :], rt[:, 48:80], rt[:, 80:81], None, ALU.is_equal, None, [k], [t_AB[ti]])
                ts(S, "dve", B01[:, ti, :], rt[:, 48:80], rt[:, 81:82], None, ALU.is_equal, None, [k], [t_AB[ti]], acc=True)
                tt(S, "dve", Mall[:, ti, :], A01[:, ti, :], B01[:, ti, :], ALU.add, [t_AB[ti]], [t_AB[ti]], acc=True)
            pc, k_pc = pp.get()
            for ti in range(nt):
                mm(S, pc[:, 0:32], C.onb[:], Mall[:, ti, :], ti == 0, ti == nt - 1, [t_AB[ti], C.t_c], k_pc)
            cw = sb("cw", [128, 8, 32], F32)
            t_cw = Tok()
            ts(S, "dve", cw[:, 0, :], pc[:, 0:32], 1.0, None, ALU.mult, None, [k_pc], [t_cw])
            S.op("dve", lambda e: e.memset(cw[:, 1, :], 0.0), writes=[t_cw], acc=True)
            for kk in range(nt + 1):
                stt(S, cw[:, 1, :], cw[:, 0, :], 128.0 * kk, cw[:, 1, :], ALU.is_gt, ALU.add, [t_cw], [t_cw], acc=True)
            ts(S, "dve", cw[:, 2, :], cw[:, 1, :], 128.0, None, ALU.mult, None, [t_cw], [t_cw], acc=True)
            ts(S, "dve", cw[:, 3, :], cw[:, 2, :], 1.0, None, ALU.mult, None, [t_cw], [t_cw], acc=True)
            src_i = 3
            for s_ in (1, 2, 4, 8, 16):
                dst_i = 7 - src_i
                ts(S, "dve", cw[:, dst_i, 0:s_], cw[:, src_i, 0:s_], 1.0, None, ALU.mult, None, [t_cw], [t_cw], acc=True)
                tt(S, "dve", cw[:, dst_i, s_:32], cw[:, src_i, s_:32], cw[:, src_i, 0:32 - s_], ALU.add, [t_cw], [t_cw], acc=True)
                src_i = dst_i
            pe_i = src_i
            tt(S, "dve", cw[:, 5, :], cw[:, pe_i, :], cw[:, 2, :], ALU.subtract, [t_cw], [t_cw], acc=True)
            thi = sb("thi", [128, 128], I32)
            thr = sb("thr", [128, 128], F32)
            bacc = sb("bacc", [128, 128], F32)
            pidi = sb("pidi", [128, 1], I32)
            pidf = sb("pidf", [128, 1], F32)
            t_th = Tok()
            S.op("pool", lambda e: e.iota(thi[:], pattern=[[128, 128]], base=0, channel_multiplier=0), writes=[t_th])
            S.op("pool", lambda e: e.iota(pidi[:], pattern=[[0, 1]], base=0, channel_multiplier=1), writes=[t_th], acc=True)
            cp(S, "dve", thr[:], thi[:], [t_th], [t_th])
            cp(S, "dve", pidf[:], pidi[:], [t_th], [t_th], acc=True)
            S.op("dve", lambda e: e.memset(bacc[:], 0.0), writes=[t_th], acc=True)
            for e_ in range(32):
                stt(S, bacc[:], thr[:], cw[:, pe_i, e_:e_ + 1], bacc[:], ALU.is_ge, ALU.add, [t_th, t_cw], [t_th], acc=True)
            ts(S, "dve", bacc[:], bacc[:], 31.0, 128.0, ALU.min, ALU.mult, [t_th], [t_th], acc=True)
            ts(S, "dve", bacc[:], bacc[:], pidf[:, 0:1], 0.0, ALU.add, ALU.add, [t_th], [t_th], acc=True)
            cp(S, "dve", widx[:], bacc[:], [t_th], [t_widx])
            slp = SbPool(C, st, 2, f"r{l}_sl", [128, 3, 32], F32)
            sfp = SbPool(C, st, 2, f"r{l}_sf", [128, 2], F32)
            for ti, tile in enumerate(tiles):
                ps, k_ps = pp.get()
                for j in range(ti):
                    mm(S, ps[:, 0:32], C.onb[:], Mall[:, j, :], j == 0, False, [t_AB[j], C.t_c], k_ps)
                mm(S, ps[:, 0:32], C.utri[:], Mall[:, ti, :], ti == 0, True, [t_AB[ti], C.t_c], k_ps)
                sl, k_sl = slp.get()
                tt(S, "dve", sl[:, 0, :], ps[:, 0:32], cw[:, 5, :], ALU.add, [k_ps, t_cw], [k_sl])
                tt(S, "dve", sl[:, 1, :], sl[:, 0, :], A01[:, ti, :], ALU.mult, [k_sl, t_AB[ti]], [k_sl], acc=True)
                tt(S, "dve", sl[:, 2, :], sl[:, 0, :], B01[:, ti, :], ALU.mult, [k_sl, t_AB[ti]], [k_sl], acc=True)
                sf, k_sf = sfp.get()
                S.op("dve", lambda e, sf=sf, sl=sl: e.reduce_sum(out=sf[:, 0:1], in_=sl[:, 1, :], axis=AX.X), reads=[k_sl], writes=[k_sf])
                S.op("dve", lambda e, sf=sf, sl=sl: e.reduce_sum(out=sf[:, 1:2], in_=sl[:, 2, :], axis=AX.X), reads=[k_sl], writes=[k_sf], acc=True)
                cp(S, "dve", sAB[:, ti, :], sf[:], [k_sf], [t_sAB], acc=True)
                for k2 in range(2):
                    S.dma_fn("pool", lambda e, ti=ti, k2=k2: e.indirect_dma_start(
                        out=xbuf[:, :], out_offset=bass.IndirectOffsetOnAxis(ap=sAB[:, ti, k2:k2 + 1], axis=0),
                        in_=fx_all[:, ti, :], in_offset=None),
                        reads=[t_sAB, t_fx[ti], t_xz], writes=[C.T("xbuf")], acc=True)
        S.barrier()
        with contextlib.ExitStack() as st:
            sb = lambda name, shape, dt: st.enter_context(nc.sbuf_tensor(f"e{l}_{name}", list(shape), dt))
            wgb = SbPool(C, st, 4, f"e{l}_wgb", [128, 8, 512], BF16)
            wdb = SbPool(C, st, 4, f"e{l}_wdb", [128, 2, 1024], BF16)
            xbp = SbPool(C, st, 3, f"e{l}_xb", [128, 1024], BF16)
            xTp = SbPool(C, st, 3, f"e{l}_xT", [128, 8, 128], BF16)
            sip = SbPool(C, st, 2, f"e{l}_si", [128, 256], F32)
            acp = SbPool(C, st, 3, f"e{l}_ac", [128, 256], BF16)
            aTp = SbPool(C, st, 2, f"e{l}_aT", [128, 2, 128], BF16)
            ybp = SbPool(C, st, 2, f"e{l}_yb", [128, 1024], F32)
            ptx = PsumPool(C, st, 2, f"e{l}_ptx", (128, 8, 128), BF16)
            ptc = PsumPool(C, st, 1, f"e{l}_ptc", (128, 2, 128), BF16)
            pgu = PsumPool(C, st, 2, f"e{l}_pgu")
            py = PsumPool(C, st, 2, f"e{l}_py")
            wgsrc = d["wgu_b"]
            wdsrc = d["wdn_b"]
            nblk = NB
            stA, stB = {}, {}

            def stage_a(b):
                wg, k_wg = wgb.get()
                S.dma_fn("pool", lambda e, wg=wg, b=b: e.indirect_dma_start(
                    out=wg[:].rearrange("p a b -> p (a b)"), out_offset=None, in_=wgsrc[:, :],
                    in_offset=bass.IndirectOffsetOnAxis(ap=widx[:, b:b + 1], axis=0)),
                    reads=[t_widx, C.T("wexp_b")], writes=[k_wg])
                wd, k_wd = wdb.get()
                S.dma_fn("pool", lambda e, wd=wd, b=b: e.indirect_dma_start(
                    out=wd[:].rearrange("p a b -> p (a b)"), out_offset=None, in_=wdsrc[:, :],
                    in_offset=bass.IndirectOffsetOnAxis(ap=widx[:, b:b + 1], axis=0)),
                    reads=[t_widx, C.T("wexp_b")], writes=[k_wd])
                xb, k_xb = xbp.get()
                S.dma("sp", xb[:], xbuf[b * 128:(b + 1) * 128, :], reads=[C.T("xbuf")], writes=[k_xb])
                tp, k_tp = ptx.get()
                for kc in range(8):
                    S.op("pe", lambda e, tp=tp, xb=xb, kc=kc: e.transpose(tp[:, kc, :], xb[:, kc * 128:(kc + 1) * 128], C.idb[:]),
                         reads=[k_xb, C.t_c], writes=[k_tp], acc=(kc > 0))
                xT, k_xT = xTp.get()
                cp(S, "act", xT[:], tp[:], [k_tp], [k_xT])
                stA[b] = (wg, k_wg, wd, k_wd, xT, k_xT)

            def stage_b(b):
                wg, k_wg, wd, k_wd, xT, k_xT = stA.pop(b)
                pg, k_pg = pgu.get()
                for kc in range(8):
                    mm(S, pg[:, :], xT[:, kc, :], wg[:, kc, :], kc == 0, kc == 7, [k_xT, k_wg], k_pg)
                si_, k_si = sip.get()
                act(S, si_[:], pg[:, 0:256], AF.Silu, [k_pg], [k_si])
                ac, k_ac = acp.get()
                tt(S, "dve", ac[:], pg[:, 256:512], si_[:], ALU.mult, [k_pg, k_si], [k_ac])
                stB[b] = (wd, k_wd, ac, k_ac)

            def stage_c(b):
                wd, k_wd, ac, k_ac = stB.pop(b)
                tp2, k_tp2 = ptc.get()
                for c in range(2):
                    S.op("pe", lambda e, tp2=tp2, ac=ac, c=c: e.transpose(tp2[:, c, :], ac[:, c * 128:(c + 1) * 128], C.idb[:]),
                         reads=[k_ac, C.t_c], writes=[k_tp2], acc=(c > 0))
                aT, k_aT = aTp.get()
                cp(S, "act", aT[:], tp2[:], [k_tp2], [k_aT])
                yb, k_yb = ybp.get()
                for hf in range(2):
                    pyt, k_py = py.get()
                    for c in range(2):
                        mm(S, pyt[:, :], aT[:, c, :], wd[:, c, hf * 512:(hf + 1) * 512], c == 0, c == 1, [k_aT, k_wd], k_py)
                    cp(S, "act", yb[:, hf * 512:(hf + 1) * 512], pyt[:, :], [k_py], [k_yb], acc=(hf > 0))
                S.dma("act", ybuf[b * 128:(b + 1) * 128, :], yb[:], reads=[k_yb], writes=[C.T("ybuf")], acc=True)

            for b in range(nblk + 2):
                if b < nblk:
                    stage_a(b)
                if 0 <= b - 1 < nblk:
                    stage_b(b - 1)
                if 0 <= b - 2 < nblk:
                    stage_c(b - 2)
            G2 = sb("G2", [128, 2, 1024], F32)
            fg = sb("fg", [128, 1024], F32)
            t_g2 = Tok()
            for r in range(2):
                S.dma("sp", G2[:, r, :], bcast_row(d["modv"][l][r:r + 1, 5120:6144], 1024), reads=[C.T(f"modv{l}")], writes=[t_g2], acc=True)
            if last:
                S.dma("sp", fg[:], bcast_row(d["final_g"][0:1, :], 1024), writes=[t_g2], acc=True)
            yAp = SbPool(C, st, 3, f"e{l}_yA", [128, 1024], F32)
            yBp = SbPool(C, st, 3, f"e{l}_yB", [128, 1024], F32)
            hp = SbPool(C, st, 4, f"e{l}_h", [128, 1024], F32)
            hnp = SbPool(C, st, 2, f"e{l}_hn", [128, 1024], F32)
            hop = SbPool(C, st, 2, f"e{l}_ho", [128, 1024], F32)
            ssp = SbPool(C, st, 2, f"e{l}_ss", [128, 4], F32)
            sqj = sb("sqj", [128, 1024], BF16)
            t_sqj = Tok()
            pre = {}

            def prefetch(ti):
                tile = tiles[ti]
                ys = []
                for k2, pool_ in enumerate((yAp, yBp)):
                    yt, k_yt = pool_.get()
                    S.dma_fn("pool", lambda e, yt=yt, ti=ti, k2=k2: e.indirect_dma_start(
                        out=yt[:], out_offset=None, in_=ybuf[:, :], in_offset=bass.IndirectOffsetOnAxis(ap=sAB[:, ti, k2:k2 + 1], axis=0)),
                        reads=[t_sAB, C.T("ybuf")], writes=[k_yt])
                    ys.append((yt, k_yt))
                ht, k_h = hp.get()
                S.dma("sp", ht[:], d["h_s"][tile * 128:(tile + 1) * 128, :], reads=[C.T(f"h:{tile}")], writes=[k_h])
                pre[ti] = (ys, ht, k_h)

            prefetch(0)
            for ti, tile in enumerate(tiles):
                r = 0 if tile >= 2 else 1
                if ti + 1 < len(tiles):
                    prefetch(ti + 1)
                ys, ht, k_h = pre.pop(ti)
                (yA, k_yA), (yB, k_yB) = ys
                ts(S, "dve", yA[:], yA[:], w12[:, ti, 0:1], None, ALU.mult, None, [k_yA, t_w12], [k_yA])
                stt(S, yB[:], yB[:], w12[:, ti, 1:2], yA[:], ALU.mult, ALU.add, [k_yB, k_yA, t_w12], [k_yB])
                tt(S, "dve", yB[:], yB[:], G2[:, r, :], ALU.mult, [k_yB, t_g2], [k_yB])
                hn, k_hn = hnp.get()
                tt(S, "dve", hn[:], yB[:], ht[:], ALU.add, [k_yB, k_h], [k_hn])
                if not last:
                    S.dma("act", d["h_s"][tile * 128:(tile + 1) * 128, :], hn[:], reads=[k_hn], writes=[C.T(f"h:{tile}")])
                else:
                    ss, k_ss = ssp.get()
                    act(S, sqj[:], hn[:], AF.Square, [k_hn], [t_sqj, k_ss], accum_out=ss[:, 0:1])
                    act(S, ss[:, 1:2], ss[:, 0:1], AF.Sqrt, [k_ss], [k_ss], acc=True, scale=1.0 / DM, bias=EPS)
                    S.op("dve", lambda e, ss=ss: e.reciprocal(out=ss[:, 2:3], in_=ss[:, 1:2]), reads=[k_ss], writes=[k_ss], acc=True)
                    ho, k_ho = hop.get()
                    stt(S, ho[:], hn[:], ss[:, 2:3], fg[:], ALU.mult, ALU.mult, [k_hn, k_ss, t_g2], [k_ho])
                    S.dma("act", d["out"][(tile - 2) * 128:(tile - 1) * 128, :], ho[:], reads=[k_ho], writes=[C.T(f"out:{tile}")])
    S.barrier()
```

```python
import numpy as np
import concourse.bass as bass
import concourse.mybir as mybir
from concourse.bass_utils import run_bass_kernel_spmd

F32 = mybir.dt.float32
BF16 = mybir.dt.bfloat16
I32 = mybir.dt.int32
U32 = mybir.dt.uint32
AF = mybir.ActivationFunctionType
ALU = mybir.AluOpType
AX = mybir.AxisListType

ENGS = ("pe", "act", "dve", "pool", "sp")
EPOCH = 12000
NSLOT = 24


class Tok:
    __slots__ = ("name", "writers", "readers")

    def __init__(self, name=""):
        self.name = name
        self.writers = []
        self.readers = []


class Op:
    __slots__ = ("eng", "fn", "deps", "idx", "needed", "cnt", "dma", "slot", "val", "waits")

    def __init__(self, eng, fn, dma=False):
        self.eng = eng
        self.fn = fn
        self.deps = []
        self.idx = -1
        self.needed = False
        self.cnt = -1
        self.dma = dma
        self.slot = -1
        self.val = -1
        self.waits = []


class Sched:
    def __init__(self, nc):
        self.nc = nc
        self.ops = {e: [] for e in ENGS}
        self.ndma = {e: 0 for e in ENGS}
        self.seen = {e: {} for e in ENGS}
        self.pending = {e: [] for e in ENGS}

    def barrier(self):
        waits = []
        for e in ENGS:
            last = None
            for o in reversed(self.ops[e]):
                if not o.dma:
                    last = o
                    break
            if last is not None:
                waits.append(("c", last))
            slots = {}
            for o in self.ops[e]:
                if o.dma:
                    slots[o.slot] = o
            for o in slots.values():
                waits.append(("d", o))
        for e in ENGS:
            self.pending[e] = list(waits)

    def _add(self, op, reads, writes, acc):
        deps = []
        for t in reads:
            deps.extend(t.writers)
        for t in writes:
            deps.extend(t.readers)
            if not acc:
                deps.extend(t.writers)
            else:
                deps.extend(w for w in t.writers if w.eng != op.eng or w.dma != op.dma)
        e = op.eng
        op.idx = len(self.ops[e])
        seen = self.seen[e]
        if self.pending[e]:
            for w in self.pending[e]:
                if w[0] == "c":
                    deps.append(w[1])
                else:
                    deps.append(w[1])
            self.pending[e] = []
            barrier_dep = True
        else:
            barrier_dep = False
        if op.dma:
            n = self.ndma[e]
            self.ndma[e] = n + 1
            op.slot = n % NSLOT
            op.val = 16 * (n // NSLOT + 1)
            if op.val > 16:
                key = ("d", e, op.slot)
                if seen.get(key, 0) < op.val - 16:
                    seen[key] = op.val - 16
                    op.waits.append(("d", e, op.slot, op.val - 16))
        for d in sorted(deps, key=lambda o: -o.idx):
            if d.dma:
                key = ("d", d.eng, d.slot)
                if seen.get(key, 0) >= d.val:
                    continue
                seen[key] = d.val
                op.waits.append(("d", d.eng, d.slot, d.val))
            else:
                if d.eng == e:
                    if e in ("pe", "sp"):
                        continue
                key = ("c", d.eng)
                if seen.get(key, -1) >= d.idx:
                    continue
                seen[key] = d.idx
                d.needed = True
                op.waits.append(("c", d))
        for t in reads:
            t.readers.append(op)
        for t in writes:
            if acc:
                t.writers.append(op)
            else:
                t.writers = [op]
                t.readers = []
        self.ops[e].append(op)
        return op

    def op(self, eng, fn, reads=(), writes=(), acc=False):
        return self._add(Op(eng, fn), list(reads), list(writes), acc)

    def dma(self, eng, out, in_, reads=(), writes=(), acc=False, **kw):
        def fn(en):
            return en.dma_start(out=out, in_=in_, **kw)
        return self._add(Op(eng, fn, dma=True), list(reads), list(writes), acc)

    def dma_fn(self, eng, fn, reads=(), writes=(), acc=False):
        return self._add(Op(eng, fn, dma=True), list(reads), list(writes), acc)

    def emit(self):
        nc = self.nc
        nep = {}
        for e in ENGS:
            c = 0
            for o in self.ops[e]:
                if o.needed and not o.dma:
                    c += 1
                    o.cnt = c
            nep[e] = max(1, (c + EPOCH - 1) // EPOCH)
        import contextlib
        with contextlib.ExitStack() as st:
            csem = {e: [st.enter_context(nc.semaphore(f"c_{e}_{i}")) for i in range(nep[e])]
                    for e in ENGS}
            dsem = {e: [st.enter_context(nc.semaphore(f"d_{e}_{i}")) for i in range(NSLOT)]
                    for e in ENGS if self.ndma[e] > 0}
            block = st.enter_context(nc.Block())
            hw = {"pe": block.tensor, "act": block.scalar, "dve": block.vector,
                  "pool": block.gpsimd, "sp": block.sync}

            def body(e):
                def run(en):
                    for o in self.ops[e]:
                        for w in o.waits:
                            if w[0] == "d":
                                en.wait_ge(dsem[w[1]][w[2]], w[3])
                            else:
                                d = w[1]
                                en.wait_ge(csem[d.eng][(d.cnt - 1) // EPOCH], (d.cnt - 1) % EPOCH + 1)
                        ins = o.fn(en)
                        if o.dma:
                            ins.then_inc(dsem[e][o.slot], 16)
                        elif o.needed:
                            ins.then_inc(csem[e][(o.cnt - 1) // EPOCH], 1)
                    if self.ndma[e] > 0:
                        last = {}
                        for o in self.ops[e]:
                            if o.dma:
                                last[o.slot] = o.val
                        for s, v in last.items():
                            en.wait_ge(dsem[e][s], v)
                return run

            for e in ENGS:
                if self.ops[e]:
                    hw[e](body(e))


DM = 1024
SEQ = 4096
LC = 256
NTOK = SEQ + LC
NT = NTOK // 128
DEPTH = 2
EPS = 1e-6
STOP = 0
QT_LIMIT = 0


class Ctx:
    def __init__(self, nc, S):
        self.nc = nc
        self.S = S
        self.d = {}
        self.tok = {}

    def T(self, name):
        t = self.tok.get(name)
        if t is None:
            t = Tok(name)
            self.tok[name] = t
        return t


def phase_mod(C, l):
    nc, S = C.nc, C.S
    wmod = C.d["w_mod"][l].rearrange("(kc p) n -> p kc n", p=128)
    with nc.sbuf_tensor(f"m_ct{l}", [128, 8, 2], F32) as ct, \
            nc.sbuf_tensor(f"m_sct{l}", [128, 8, 2], F32) as sct, \
            nc.sbuf_tensor(f"m_w0{l}", [128, 8, 512], F32) as w0, \
            nc.sbuf_tensor(f"m_w1{l}", [128, 8, 512], F32) as w1, \
            nc.sbuf_tensor(f"m_b{l}", [2, 6144], F32) as bm, \
            nc.sbuf_tensor(f"m_row{l}", [2, 6144], F32) as mrow, \
            nc.psum_tensor(f"m_p0{l}", [2, 512], F32) as p0, \
            nc.psum_tensor(f"m_p1{l}", [2, 512], F32) as p1:
        t_ct, t_sct, t_bm, t_row = Tok(), Tok(), Tok(), Tok()
        t_w = [Tok(), Tok()]
        t_p = [Tok(), Tok()]
        wb = [w0, w1]
        pb = [p0, p1]
        S.dma("sp", ct[:], C.d["cT"][:], writes=[t_ct])
        S.dma("sp", bm[:], C.d["b_mod"][l:l + 1, :].to_broadcast([2, 6144]), writes=[t_bm])
        S.op("act", lambda e: e.activation(out=sct[:], in_=ct[:], func=AF.Silu),
             reads=[t_ct], writes=[t_sct])
        for n in range(12):
            S.dma("sp", wb[n % 2][:], wmod[:, :, n * 512:(n + 1) * 512], writes=[t_w[n % 2]])
            for kc in range(8):
                S.op("pe", lambda e, n=n, kc=kc: e.matmul(pb[n % 2][:], lhsT=sct[:, kc, :],
                                                       rhs=wb[n % 2][:, kc, :],
                                                       start=(kc == 0), stop=(kc == 7)),
                     reads=[t_sct, t_w[n % 2]], writes=[t_p[n % 2]], acc=(kc > 0))
            S.op("dve", lambda e, n=n: e.tensor_tensor(out=mrow[:, n * 512:(n + 1) * 512],
                                                      in0=pb[n % 2][:],
                                                      in1=bm[:, n * 512:(n + 1) * 512], op=ALU.add),
                 reads=[t_p[n % 2], t_bm], writes=[t_row], acc=True)
        S.dma("sp", C.d["modv"][l], mrow[:], reads=[t_row], writes=[C.T(f"modv{l}")])
    S.barrier()


class PsumPool:
    def __init__(self, C, st, n, name, shape=(128, 512), dtype=F32):
        self.tiles = [st.enter_context(C.nc.psum_tensor(f"{name}{i}", list(shape), dtype)) for i in range(n)]
        self.toks = [Tok(f"{name}{i}") for i in range(n)]
        self.i = 0

    def get(self):
        i = self.i
        self.i = (i + 1) % len(self.tiles)
        return self.tiles[i], self.toks[i]


class SbPool:
    def __init__(self, C, st, n, name, shape, dtype):
        self.tiles = [st.enter_context(C.nc.sbuf_tensor(f"{name}{i}", list(shape), dtype)) for i in range(n)]
        self.toks = [Tok(f"{name}{i}") for i in range(n)]
        self.i = 0

    def get(self):
        i = self.i
        self.i = (i + 1) % len(self.tiles)
        return self.tiles[i], self.toks[i]


def mm(S, out, lhsT, rhs, first, last, reads, wtok):
    S.op("pe", lambda e: e.matmul(out, lhsT=lhsT, rhs=rhs, start=first, stop=last),
         reads=reads, writes=[wtok], acc=not first)


def act(S, out, in_, func, reads, writes, acc=False, **kw):
    S.op("act", lambda e: e.activation(out=out, in_=in_, func=func, **kw), reads=reads, writes=writes, acc=acc)


def tt(S, eng, out, in0, in1, op, reads, writes, acc=False):
    S.op(eng, lambda e: e.tensor_tensor(out=out, in0=in0, in1=in1, op=op), reads=reads, writes=writes, acc=acc)


def ts(S, eng, out, in0, s1, s2, op0, op1, reads, writes, acc=False):
    if op1 is None:
        S.op(eng, lambda e: e.tensor_scalar(out=out, in0=in0, scalar1=s1, scalar2=None, op0=op0),
             reads=reads, writes=writes, acc=acc)
    else:
        S.op(eng, lambda e: e.tensor_scalar(out=out, in0=in0, scalar1=s1, scalar2=s2, op0=op0, op1=op1),
             reads=reads, writes=writes, acc=acc)


def stt(S, out, in0, scalar, in1, op0, op1, reads, writes, acc=False):
    S.op("dve", lambda e: e.scalar_tensor_tensor(out=out, in0=in0, scalar=scalar, in1=in1, op0=op0, op1=op1),
         reads=reads, writes=writes, acc=acc)


def cp(S, eng, out, in_, reads, writes, acc=False):
    if eng == "act":
        S.op("act", lambda e: e.copy(out=out, in_=in_), reads=reads, writes=writes, acc=acc)
    else:
        S.op(eng, lambda e: e.tensor_copy(out=out, in_=in_), reads=reads, writes=writes, acc=acc)


def make_ident(C, st):
    nc, S = C.nc, C.S
    idi = st.enter_context(nc.sbuf_tensor("c_idi", [128, 128], I32))
    idb = st.enter_context(nc.sbuf_tensor("c_idb", [128, 128], BF16))
    idf = st.enter_context(nc.sbuf_tensor("c_idf", [128, 128], F32))
    onb = st.enter_context(nc.sbuf_tensor("c_onb", [128, 128], BF16))
    t_i, t_c = Tok(), Tok("consts")
    S.op("pool", lambda e: e.iota(idi[:], pattern=[[1, 128]], base=0, channel_multiplier=-1), writes=[t_i])
    ts(S, "dve", idb[:], idi[:], 0, None, ALU.is_equal, None, [t_i], [t_c], acc=True)
    ts(S, "dve", idf[:], idi[:], 0, None, ALU.is_equal, None, [t_i], [t_c], acc=True)
    S.op("dve", lambda e: e.memset(onb[:], 1.0), writes=[t_c], acc=True)
    utri = st.enter_context(nc.sbuf_tensor("c_utri", [128, 128], BF16))
    ts(S, "dve", utri[:], idi[:], 0, None, ALU.is_gt, None, [t_i], [t_c], acc=True)
    C.utri = utri
    C.idb, C.idf, C.onb, C.t_c = idb, idf, onb, t_c


O_QA, O_QAS, O_KA, O_KAS, O_KR, O_KRS, O_CQ, O_CKV, O_U, O_VA, O_G, W_EXT = (
    0, 512, 1024, 1152, 1280, 1312, 1344, 1600, 1856, 2368, 2496, 5568)
W_N = O_G
GROUPS = [(0, 256)] + [(256 + 512 * i, 512) for i in range(8)]


def bcast_row(ap_row, n):
    return ap_row.to_broadcast([128, n])


def load_cast(C, st, name, dst, src3, nk, ncol, t_dst, scale_ap=None, scale_tok=None, half=2784):
    S = C.S
    stg = SbPool(C, st, 2, name, [128, half], F32)
    for kc in range(nk):
        for c0 in range(0, ncol, half):
            w = min(half, ncol - c0)
            s_t, s_k = stg.get()
            S.dma("sp", s_t[:, 0:w], src3[:, kc, c0:c0 + w], writes=[s_k])
            if scale_ap is None:
                cp(S, "act", dst[:, kc, c0:c0 + w], s_t[:, 0:w], [s_k], [t_dst], acc=True)
            else:
                ts(S, "dve", dst[:, kc, c0:c0 + w], s_t[:, 0:w], scale_ap(kc), None, ALU.mult, None,
                   [s_k, scale_tok], [t_dst], acc=True)


def phase_inproj(C, l):
    nc, S = C.nc, C.S
    import contextlib
    d = C.d
    hsrc = d["h0"] if l == 0 else d["h_s"]
    with contextlib.ExitStack() as st:
        sb = lambda name, shape, dt: st.enter_context(nc.sbuf_tensor(f"n{l}_{name}", list(shape), dt))
        wsb = sb("w", [128, 8, W_N], BF16)
        wq = sb("wq", [128, 2, 1024], BF16)
        wkv = sb("wkv", [128, 2, 1024], BF16)
        gq = sb("gq", [128, 4], F32)
        Gm, Sm = sb("Gm", [128, 2, 1024], F32), sb("Sm", [128, 2, 1024], F32)
        t_w, t_wq, t_gq, t_mod = Tok(), Tok(), Tok(), Tok()
        wext = d["w_in_ext"][l].rearrange("(kc p) n -> p kc n", p=128)
        load_cast(C, st, f"n{l}_stg", wsb, wext, 8, W_N, t_w)
        S.dma("sp", gq[:, 0:2], d["q_norm_gT"][l], writes=[t_gq], acc=True)
        S.dma("sp", gq[:, 2:4], d["kv_norm_gT"][l], writes=[t_gq], acc=True)
        for wi, (wname, wdst) in enumerate((("w_uq_ext", wq), ("w_ukv_ext", wkv))):
            wsrc = d[wname][l].rearrange("(kc p) n -> p kc n", p=128)
            load_cast(C, st, f"n{l}_stg{wi}", wdst, wsrc, 2, 1024, t_wq,
                      scale_ap=lambda kc, wi=wi: gq[:, 2 * wi + kc:2 * wi + kc + 1], scale_tok=t_gq, half=1024)
        mv = d["modv"][l]
        tmpp = SbPool(C, st, 2, f"n{l}_tmp", [128, 1024], F32)
        gn, k_gn = tmpp.get()
        S.dma("sp", gn[:], bcast_row(d["norm_mix_g"][l:l + 1, :], 1024), writes=[k_gn])
        for r in range(2):
            S.dma("sp", Sm[:, r, :], bcast_row(mv[r:r + 1, 0:1024], 1024), reads=[C.T(f"modv{l}")], writes=[t_mod], acc=True)
            S.dma("sp", Gm[:, r, :], bcast_row(mv[r:r + 1, 1024:2048], 1024), reads=[C.T(f"modv{l}")], writes=[t_mod], acc=True)
        t_mod2 = Tok()
        for r in range(2):
            stt(S, Gm[:, r, :], Gm[:, r, :], 1.0, gn[:], ALU.add, ALU.mult, [t_mod, k_gn], [t_mod2], acc=True)
        if STOP == 1:
            return
        pp = PsumPool(C, st, 5, f"n{l}_pp")
        ptp = PsumPool(C, st, 2, f"n{l}_ptp", (128, 8, 128), BF16)
        hp = SbPool(C, st, 2, f"n{l}_h", [128, 1024], F32)
        hxp = SbPool(C, st, 2, f"n{l}_hx", [128, 1024], BF16)
        hxTp = SbPool(C, st, 2, f"n{l}_hxT", [128, 8, 512], BF16)
        ssp = SbPool(C, st, 4, f"n{l}_ss", [128, 4], F32)
        sqj = sb("sqj", [128, 1024], BF16)
        t_sqj = Tok()
        ropep = SbPool(C, st, 1, f"n{l}_rope", [128, 4, 512], F32)
        r1p = SbPool(C, st, 1, f"n{l}_r1", [128, 512], F32)
        r2p = SbPool(C, st, 1, f"n{l}_r2", [128, 512], F32)
        o4p = SbPool(C, st, 4, f"n{l}_o4", [128, 4, 512], BF16)
        krTp = SbPool(C, st, 1, f"n{l}_krT", [32, 512], BF16)
        latfp = SbPool(C, st, 1, f"n{l}_latf", [128, 2, 512], F32)
        latqp = SbPool(C, st, 1, f"n{l}_latq", [128, 2, 512], BF16)
        rbcp = SbPool(C, st, 1, f"n{l}_rbc", [128, 512], F32)
        cqnp = SbPool(C, st, 1, f"n{l}_cqn", [128, 2, 512], BF16)
        ckvnp = SbPool(C, st, 1, f"n{l}_ckvn", [128, 2, 512], BF16)
        vap = SbPool(C, st, 1, f"n{l}_va", [128, 4, 2, 65], BF16)
        vbp = SbPool(C, st, 1, f"n{l}_vb", [128, 4, 8, 65], BF16)
        for pool_ in (vap, vbp):
            for t_, k_ in zip(pool_.tiles, pool_.toks):
                S.op("pool", lambda e, t_=t_: e.memset(t_[:], 1.0), writes=[k_])
        t_all = [t_mod2, C.t_c]

        for g, (t0, n) in enumerate(GROUPS):
            ntl = n // 128
            isx = g > 0
            mr = 0 if isx else 1
            hxT, k_hxT = hxTp.get()
            if isx:
                rp, k_rp = ropep.get()
                p0 = t0 - 256
                for i, nm in enumerate(("ropeA_c", "ropeA_s", "ropeB_c", "ropeB_s")):
                    S.dma("sp", rp[:, i, :], d[nm][:, p0:p0 + 512], writes=[k_rp], acc=(i > 0))
            for ti in range(ntl):
                r0 = t0 + ti * 128
                ht, k_h = hp.get()
                S.dma("sp", ht[:], hsrc[r0:r0 + 128, :], reads=[C.T(f"h:{r0 // 128}")], writes=[k_h])
                ss, k_ss = ssp.get()
                act(S, sqj[:], ht[:], AF.Square, [k_h], [t_sqj, k_ss], accum_out=ss[:, 0:1])
                act(S, ss[:, 1:2], ss[:, 0:1], AF.Sqrt, [k_ss], [k_ss], acc=True, scale=1.0 / DM, bias=EPS)
                S.op("dve", lambda e, ss=ss: e.reciprocal(out=ss[:, 2:3], in_=ss[:, 1:2]), reads=[k_ss], writes=[k_ss], acc=True)
                tm, k_tm = tmpp.get()
                stt(S, tm[:], ht[:], ss[:, 2:3], Gm[:, mr, :], ALU.mult, ALU.mult, [k_h, k_ss] + t_all, [k_tm])
                hx, k_hx = hxp.get()
                tt(S, "dve", hx[:], tm[:], Sm[:, mr, :], ALU.add, [k_tm] + t_all, [k_hx])
                tp, k_tp = ptp.get()
                for kc in range(8):
                    S.op("pe", lambda e, tp=tp, hx=hx, kc=kc: e.transpose(tp[:, kc, :], hx[:, kc * 128:(kc + 1) * 128], C.idb[:]),
                         reads=[k_hx, C.t_c], writes=[k_tp], acc=(kc > 0))
                cp(S, "act", hxT[:, :, ti * 128:(ti + 1) * 128], tp[:], [k_tp], [k_hxT], acc=(ti > 0))
            if STOP == 2:
                continue
            S.dma("pool", d["hxT_s"].rearrange("c p t -> p c t")[:, :, t0:t0 + n], hxT[:, :, 0:n],
                  reads=[k_hxT], writes=[C.T(f"hxT:{g}")])
            if STOP == 3:
                continue

            def proj(col0, m, ps):
                pt, pk = ps
                for kc in range(8):
                    mm(S, pt[0:m, 0:n], wsb[:, kc, col0:col0 + m], hxT[:, kc, 0:n], kc == 0, kc == 7,
                       [t_w, k_hxT], pk)

            def rope_evac(prj, col0, cols0, m, out_ap, out_k, ri, acc):
                ps = pp.get()
                prj(col0, m, ps)
                if not isx:
                    cp(S, "act", out_ap, ps[0][0:m, 0:n], [ps[1]], [out_k], acc=acc)
                    return
                ps2 = pp.get()
                prj(cols0, m, ps2)
                r1, k1 = r1p.get()
                r2, k2 = r2p.get()
                tt(S, "dve", r1[0:m, 0:n], ps[0][0:m, 0:n], rp[0:m, ri, 0:n], ALU.mult, [ps[1], k_rp], [k1])
                tt(S, "dve", r2[0:m, 0:n], ps2[0][0:m, 0:n], rp[0:m, ri + 1, 0:n], ALU.mult, [ps2[1], k_rp], [k2])
                tt(S, "dve", out_ap, r1[0:m, 0:n], r2[0:m, 0:n], ALU.add, [k1, k2], [out_k], acc=acc)

            qaT, k_qaT = o4p.get()
            for c in range(4):
                rope_evac(proj, O_QA + c * 128, O_QAS + c * 128, 128, qaT[:, c, 0:n], k_qaT, 0, c > 0)
            S.dma("pool", d["qaT_s"].rearrange("c p t -> p c t")[:, :, t0:t0 + n], qaT[:, :, 0:n],
                  reads=[k_qaT], writes=[C.T(f"qaT:{g}")])
            kaT, k_kaT = o4p.get()
            rope_evac(proj, O_KA, O_KAS, 128, kaT[:, 0, 0:n], k_kaT, 0, False)
            S.dma("pool", d["kaT_s"][:, t0:t0 + n], kaT[:, 0, 0:n], reads=[k_kaT], writes=[C.T(f"kaT:{g}")])
            krT, k_krT = krTp.get()
            rope_evac(proj, O_KR, O_KRS, 32, krT[:, 0:n], k_krT, 2, False)
            for h in range(8):
                S.dma("pool", d["kbT_s"][h, 64:96, t0:t0 + n], krT[:, 0:n], reads=[k_krT],
                      writes=[C.T(f"kbT:{g}")], acc=True)
            if STOP == 4:
                continue
            uT, k_uT = o4p.get()
            for c in range(4):
                ps = pp.get()
                proj(O_U + c * 128, 128, ps)
                cp(S, "act", uT[:, c, 0:n], ps[0][:, 0:n], [ps[1]], [k_uT], acc=(c > 0))
            S.dma("pool", d["uT_s"].rearrange("c p t -> p c t")[:, :, t0:t0 + n], uT[:, :, 0:n],
                  reads=[k_uT], writes=[C.T(f"uT:{g}")])
            va, k_va = vap.get()
            for ti in range(ntl):
                pt, pk = pp.get()
                for kc in range(8):
                    mm(S, pt[:, 0:128], hxT[:, kc, ti * 128:(ti + 1) * 128], wsb[:, kc, O_VA:O_VA + 128],
                       kc == 0, kc == 7, [t_w, k_hxT], pk)
                cp(S, "act", va[:, ti, :, 0:64], pt[:, 0:128].rearrange("p (g e) -> p g e", g=2), [pk], [k_va], acc=(ti > 0))
            S.dma("pool", d["va_s"][t0:t0 + n, :].rearrange("(i p) f -> p i f", p=128),
                  va[:, 0:ntl, :, :].rearrange("p i g e -> p i (g e)"), reads=[k_va], writes=[C.T(f"va:{g}")])
            if STOP == 5:
                continue
            lat_out = []
            for (col0, npool) in ((O_CQ, cqnp), (O_CKV, ckvnp)):
                lf, k_lf = latfp.get()
                lq, k_lq = latqp.get()
                for c in range(2):
                    ps = pp.get()
                    proj(col0 + c * 128, 128, ps)
                    act(S, lq[:, c, 0:n], ps[0][:, 0:n], AF.Square, [ps[1]], [k_lq], acc=(c > 0))
                    cp(S, "act", lf[:, c, 0:n], ps[0][:, 0:n], [ps[1]], [k_lf], acc=(c > 0))
                if STOP == 61:
                    continue
                pt, pk = pp.get()
                for c in range(2):
                    mm(S, pt[:, 0:n], C.onb[:], lq[:, c, 0:n], c == 0, c == 1, [k_lq, C.t_c], pk)
                rb, k_rb = rbcp.get()
                if STOP == 62:
                    continue
                act(S, rb[:, 0:n], pt[:, 0:n], AF.Sqrt, [pk], [k_rb], scale=1.0 / 256, bias=EPS)
                if STOP == 63:
                    continue
                S.op("dve", lambda e, rb=rb, n=n: e.reciprocal(out=rb[:, 0:n], in_=rb[:, 0:n]), reads=[k_rb], writes=[k_rb], acc=True)
                if STOP == 64:
                    continue
                ln, k_ln = npool.get()
                for c in range(2):
                    tt(S, "dve", ln[:, c, 0:n], lf[:, c, 0:n], rb[:, 0:n], ALU.mult, [k_lf, k_rb], [k_ln], acc=(c > 0))
                lat_out.append((ln, k_ln))
            if STOP > 60:
                continue
            (cqn, k_cqn), (ckvn, k_ckvn) = lat_out
            if STOP == 6:
                continue

            def mkproj2(wt, lat, k_lat):
                def proj2(col0, m, ps):
                    pt, pk = ps
                    for kc in range(2):
                        mm(S, pt[0:m, 0:n], wt[:, kc, col0:col0 + m], lat[:, kc, 0:n], kc == 0, kc == 1, [t_wq, k_lat], pk)
                return proj2
            pq = mkproj2(wq, cqn, k_cqn)
            pk_ = mkproj2(wkv, ckvn, k_ckvn)
            qnT, k_qnT = o4p.get()
            for c in range(4):
                ps = pp.get()
                pq(c * 128, 128, ps)
                cp(S, "act", qnT[:, c, 0:n], ps[0][:, 0:n], [ps[1]], [k_qnT], acc=(c > 0))
            for h in range(8):
                S.dma("pool", d["qbT_s"][h, 0:64, t0:t0 + n], qnT[(h % 2) * 64:(h % 2) * 64 + 64, h // 2, 0:n],
                      reads=[k_qnT], writes=[C.T(f"qbT:{g}")], acc=True)
            if STOP == 7:
                continue
            qrT, k_qrT = o4p.get()
            for c in range(2):
                rope_evac(pq, 512 + c * 128, 768 + c * 128, 128, qrT[:, c, 0:n], k_qrT, 2, c > 0)
            for h in range(8):
                S.dma("pool", d["qbT_s"][h, 64:96, t0:t0 + n], qrT[(h % 4) * 32:(h % 4) * 32 + 32, h // 4, 0:n],
                      reads=[k_qrT], writes=[C.T(f"qbT:{g}")], acc=True)
            if STOP == 8:
                continue
            knT, k_knT = o4p.get()
            for c in range(4):
                ps = pp.get()
                pk_(c * 128, 128, ps)
                cp(S, "act", knT[:, c, 0:n], ps[0][:, 0:n], [ps[1]], [k_knT], acc=(c > 0))
            for h in range(8):
                S.dma("pool", d["kbT_s"][h, 0:64, t0:t0 + n], knT[(h % 2) * 64:(h % 2) * 64 + 64, h // 2, 0:n],
                      reads=[k_knT], writes=[C.T(f"kbT:{g}")], acc=True)
            if STOP == 9:
                continue
            vb, k_vb = vbp.get()
            for ti in range(ntl):
                pt, pk = pp.get()
                for kc in range(2):
                    mm(S, pt[:, :], ckvn[:, kc, ti * 128:(ti + 1) * 128], wkv[:, kc, 512:1024], kc == 0, kc == 1,
                       [t_wq, k_ckvn], pk)
                cp(S, "act", vb[:, ti, :, 0:64], pt[:, :].rearrange("p (g e) -> p g e", g=8), [pk], [k_vb], acc=(ti > 0))
            S.dma("pool", d["vb_s"][t0:t0 + n, :].rearrange("(i p) f -> p i f", p=128),
                  vb[:, 0:ntl, :, :].rearrange("p i g e -> p i (g e)"), reads=[k_vb], writes=[C.T(f"vb:{g}")])
    S.barrier()


def _deint(n):
    return np.concatenate([np.arange(0, n, 2), np.arange(1, n, 2)])


def _deint_sw(n):
    return np.concatenate([np.arange(1, n, 2), np.arange(0, n, 2)])


def _w_in_cols():
    cols = []
    for sw in (False, True):
        for c in range(4):
            for h in (c, 4 + c):
                cols.append(h * 64 + (_deint_sw(64) if sw else _deint(64)))
    for sw in (False, True):
        for g in range(2):
            cols.append(512 + g * 64 + (_deint_sw(64) if sw else _deint(64)))
    cols.append(1280 + _deint(32))
    cols.append(1280 + _deint_sw(32))
    cols.append(np.arange(768, 1280))
    cols.append(np.arange(1312, 1824))
    cols.append(np.arange(640, 768))
    cols.append(np.arange(1824, 4896))
    out = np.concatenate(cols)
    assert out.shape[0] == W_EXT
    return out


def _w_uq_cols():
    cols = [h * 96 + np.arange(64) for h in range(8)]
    cols += [h * 96 + 64 + _deint(32) for h in range(8)]
    cols += [h * 96 + 64 + _deint_sw(32) for h in range(8)]
    return np.concatenate(cols)


def _w_ukv_cols():
    cols = [h * 128 + np.arange(64) for h in range(8)]
    cols += [h * 128 + 64 + np.arange(64) for h in range(8)]
    return np.concatenate(cols)


def _rope_tables():
    t = np.arange(SEQ)
    row = (t // 64).astype(np.float32)
    col = (t % 64).astype(np.float32)

    def tabs(rot):
        axis_dim = rot // 2
        inv = (np.float32(10000.0) ** (-np.arange(0, axis_dim, 2, dtype=np.float32) / np.float32(axis_dim))).astype(np.float32)
        ang = np.concatenate([row[:, None] * inv, col[:, None] * inv], axis=-1).astype(np.float32)
        cos, sin = np.cos(ang).astype(np.float32), np.sin(ang).astype(np.float32)
        half = rot // 2
        ct = np.concatenate([cos, cos], axis=1).T
        st_ = np.concatenate([-sin, sin], axis=1).T
        rep = 128 // rot
        return np.ascontiguousarray(np.tile(ct, (rep, 1))), np.ascontiguousarray(np.tile(st_, (rep, 1)))
    ca, sa = tabs(64)
    cb, sb_ = tabs(32)
    return ca, sa, cb, sb_


SCRATCH = {
    "modv": ([2, 2, 6144], F32),
    "h_s": ([NTOK, DM], F32),
    "hxT_s": ([8, 128, NTOK], BF16),
    "qaT_s": ([4, 128, NTOK], BF16),
    "kaT_s": ([128, NTOK], BF16),
    "va_s": ([NTOK, 130], BF16),
    "qbT_s": ([8, 96, NTOK], BF16),
    "kbT_s": ([8, 96, NTOK], BF16),
    "vb_s": ([NTOK, 520], BF16),
    "uT_s": ([4, 128, NTOK], BF16),
    "oaT_s": ([4, 128, NTOK], BF16),
    "obT_s": ([4, 128, NTOK], BF16),
    "ocT_s": ([4, 128, NTOK], BF16),
    "gT_s": ([32, NTOK], F32),
    "wgu_b": ([4096, 4096], BF16),
    "wdn_b": ([4096, 2048], BF16),
    "xbuf": ([12800, DM], BF16),
    "ybuf": ([12800, DM], F32),
}
INPUTS = {
    "h0": ([NTOK, DM], F32),
    "cT": ([128, 8, 2], F32),
    "w_mod": ([2, DM, 6144], F32),
    "b_mod": ([2, 6144], F32),
    "norm_mix_g": ([2, DM], F32),
    "norm_ffn_g": ([2, DM], F32),
    "w_in_ext": ([2, DM, W_EXT], F32),
    "w_uq_ext": ([2, 256, 1024], F32),
    "w_ukv_ext": ([2, 256, 1024], F32),
    "q_norm_gT": ([2, 128, 2], F32),
    "kv_norm_gT": ([2, 128, 2], F32),
    "ropeA_c": ([128, SEQ], F32),
    "ropeA_s": ([128, SEQ], F32),
    "ropeB_c": ([128, SEQ], F32),
    "ropeB_s": ([128, SEQ], F32),
    "sink": ([2, 8], F32),
    "sink_row": ([2, 2, 512], F32),
    "w_pool": ([2, 4, 128, 128], F32),
    "pool_scaleT": ([2, 128, 4], F32),
    "invx": ([4, SEQ], F32),
    "invc": ([4, LC], F32),
    "w_br": ([2, 3, 512, DM], F32),
    "w_out": ([2, DM, DM], F32),
    "w_r": ([2, DM, 36], F32),
    "b_r": ([2, 36], F32),
    "w_gu_h": ([2, 32 * 128, 4096], F32),
    "w_dn_h": ([2, 32 * 128, 2048], F32),
    "final_g": ([1, DM], F32),
}


def prep_shared(inp):
    out = {}
    f = lambda a: np.ascontiguousarray(np.asarray(a, dtype=np.float32))
    out["w_mod"] = f(inp["w_mod"])
    out["b_mod"] = f(inp["b_mod"])
    out["norm_mix_g"] = f(inp["norm_mix_g"])
    out["norm_ffn_g"] = f(inp["norm_ffn_g"])
    out["sink"] = f(inp["sink"])
    out["sink_row"] = f(np.repeat(np.asarray(inp["sink"]).reshape(2, 2, 4), 128, axis=-1))
    out["w_pool"] = f(inp["w_pool"])
    out["pool_scaleT"] = f(np.asarray(inp["pool_scale"]).reshape(2, 4, 128).transpose(0, 2, 1))
    out["w_br"] = f(np.stack([np.asarray(inp[k]) for k in ("w_br_a", "w_br_b", "w_br_c")], axis=1))
    out["w_out"] = f(inp["w_out"])
    out["w_r"] = f(np.concatenate([np.asarray(inp["w_rg"]), np.asarray(inp["w_re"])], axis=-1))
    out["b_r"] = f(np.concatenate([np.asarray(inp["b_rg"]), np.asarray(inp["b_re"])], axis=-1))
    out["w_gu_h"] = f(np.asarray(inp["w_gu"]).reshape(2, 32, 8, 128, 512).transpose(0, 1, 3, 2, 4).reshape(2, 32 * 128, 4096))
    out["w_dn_h"] = f(np.asarray(inp["w_dn"]).reshape(2, 32, 2, 128, 1024).transpose(0, 1, 3, 2, 4).reshape(2, 32 * 128, 2048))
    out["final_g"] = f(np.asarray(inp["final_g"]).reshape(1, DM))
    for nm, L in (("invx", SEQ), ("invc", LC)):
        t = np.arange(L)
        rows = []
        for r in (1, 2, 4, 8):
            cnt = np.minimum(t + r + 1, L) - np.maximum(t - r, 0)
            rows.append((1.0 / cnt.astype(np.float32)).astype(np.float32))
        out[nm] = np.stack(rows)
    out["w_in_ext"] = f(np.asarray(inp["w_in"])[:, :, _w_in_cols()])
    out["w_uq_ext"] = f(np.asarray(inp["w_uq"])[:, :, _w_uq_cols()])
    out["w_ukv_ext"] = f(np.asarray(inp["w_ukv"])[:, :, _w_ukv_cols()])
    out["q_norm_gT"] = f(np.asarray(inp["q_norm_g"]).reshape(2, 2, 128).transpose(0, 2, 1))
    out["kv_norm_gT"] = f(np.asarray(inp["kv_norm_g"]).reshape(2, 2, 128).transpose(0, 2, 1))
    ca, sa, cb, sb_ = _rope_tables()
    out["ropeA_c"], out["ropeA_s"], out["ropeB_c"], out["ropeB_s"] = ca, sa, cb, sb_
    return out


def prep_core(inp, b):
    out = {}
    x, ctx, c, cc = (np.asarray(inp[k], dtype=np.float32) for k in ("x", "ctx", "c", "c_ctx"))
    out["h0"] = np.ascontiguousarray(np.concatenate([ctx[b], x[b]], axis=0))
    out["cT"] = np.ascontiguousarray(np.stack([c[b].reshape(8, 128).T, cc.reshape(8, 128).T], axis=-1))
    return out


def build_program(phases, debug_out=()):
    nc = bass.Bass("TRN2", target_bir_lowering=False)
    S = Sched(nc)
    C = Ctx(nc, S)
    for k, (shape, dt) in INPUTS.items():
        C.d[k] = nc.dram_tensor(k, list(shape), dt, kind="ExternalInput").ap()
    for k, (shape, dt) in SCRATCH.items():
        kind = "ExternalOutput" if k in debug_out else "Internal"
        C.d[k] = nc.dram_tensor(k, list(shape), dt, kind=kind).ap()
    import contextlib
    with contextlib.ExitStack() as st:
        make_ident(C, st)
        phases(C)
        S.emit()
    return nc


NEG = -30000.0


def attn_pipeline(C, st, name, items, scale, hook=None, hook_every=1):
    S = C.S
    pps = PsumPool(C, st, 2, f"{name}_ps", (128, 2, 512), F32)
    pac = PsumPool(C, st, 2, f"{name}_ac", (128, 512), F32)
    ptp = SbPool(C, st, 3, f"{name}_pt", [128, 2, 512], BF16)
    steps = []
    for it in items:
        ks = it["keys"]
        for a in range(0, len(ks), 2):
            steps.append((it, a, ks[a:a + 2]))
    state = {}

    def emit_S(si):
        it, a, chunk = steps[si]
        n = it["n"]
        if "q" not in it:
            it["load_q"](it)
        ps, k_ps = pps.get()
        for jj, (k_ap, v_ap, msk) in enumerate(chunk):
            mm(S, ps[:, jj, 0:n], k_ap, it["q"], True, msk is None, it["kv_reads"] + it["q_reads"], k_ps)
            if msk is not None:
                mm(S, ps[:, jj, 0:n], C.idb[:], msk[:, 0:n], False, True, [it["m_tok"], C.t_c], k_ps)
        pt, k_pt = ptp.get()
        act(S, pt[:, 0:len(chunk), 0:n], ps[:, 0:len(chunk), 0:n], AF.Exp, [k_ps], [k_pt], scale=scale)
        state[si] = (pt, k_pt)

    def emit_PV(si):
        it, a, chunk = steps[si]
        n = it["n"]
        pt, k_pt = state.pop(si)
        if a == 0:
            it["acc"] = pac.get()
        acc, k_acc = it["acc"]
        nk = len(it["keys"])
        for jj, (k_ap, v_ap, msk) in enumerate(chunk):
            mm(S, acc[0:65, 0:n], v_ap, pt[:, jj, 0:n], a + jj == 0, a + jj == nk - 1, [k_pt] + it["kv_reads"], k_acc)
        if a + len(chunk) == nk:
            it["finish"](acc, k_acc)

    if not steps:
        return
    emit_S(0)
    for si in range(len(steps)):
        if hook is not None and si % hook_every == 0:
            hook(si // hook_every)
        if si + 1 < len(steps):
            emit_S(si + 1)
        emit_PV(si)


def _norm_store(C, pools, acc, k_acc, n, add_ap, add_tok, dst_ap, wtoks):
    S = C.S
    rsp, bcp, bcsp, op_, onesf = pools
    rs, k_rs = rsp.get()
    if add_ap is not None:
        tt(S, "dve", rs[64:65, 0:n], acc[64:65, 0:n], add_ap, ALU.add, [k_acc, add_tok], [k_rs])
        S.op("dve", lambda e, rs=rs, n=n: e.reciprocal(out=rs[64:65, 0:n], in_=rs[64:65, 0:n]), reads=[k_rs], writes=[k_rs], acc=True)
    else:
        S.op("dve", lambda e, rs=rs, n=n, acc=acc: e.reciprocal(out=rs[64:65, 0:n], in_=acc[64:65, 0:n]), reads=[k_acc], writes=[k_rs])
    bc, k_bc = bcp.get()
    mm(S, bc[0:64, 0:n], onesf[64:65, 0:64], rs[64:65, 0:n], True, True, [k_rs, C.t_c], k_bc)
    bcs, k_bcs = bcsp.get()
    cp(S, "act", bcs[:, 0:n], bc[0:64, 0:n], [k_bc], [k_bcs])
    o, k_o = op_.get()
    tt(S, "dve", o[:, 0:n], acc[0:64, 0:n], bcs[:, 0:n], ALU.mult, [k_acc, k_bcs], [k_o])
    S.dma("pool", dst_ap(o), o[:, 0:n] if dst_ap.flat else o[:, 0:n].rearrange("p (u t) -> p u t", u=4), reads=[k_o], writes=wtoks, acc=True)


class _Dst:
    def __init__(self, ap, flat):
        self.ap, self.flat = ap, flat

    def __call__(self, o):
        return self.ap


def _norm_pools(C, st, name):
    nc = C.nc
    rsp = SbPool(C, st, 2, f"{name}_rs", [65, 512], F32)
    bcp = PsumPool(C, st, 1, f"{name}_bc", (64, 512), F32)
    bcsp = SbPool(C, st, 2, f"{name}_bcs", [64, 512], F32)
    op_ = SbPool(C, st, 3, f"{name}_o", [64, 512], BF16)
    onesf = st.enter_context(nc.sbuf_tensor(f"{name}_onesf", [128, 64], F32))
    C.S.op("dve", lambda e: e.memset(onesf[:], 1.0), writes=[C.t_c], acc=True)
    return (rsp, bcp, bcsp, op_, onesf)


def phase_attn_a(C, l):
    nc, S, d = C.nc, C.S, C.d
    import contextlib
    last = (l == DEPTH - 1)
    with contextlib.ExitStack() as st:
        sb = lambda name, shape, dt: st.enter_context(nc.sbuf_tensor(f"a{l}_{name}", list(shape), dt))
        ka = sb("ka", [128, NTOK], BF16)
        va = sb("va", [128, NT, 130], BF16)
        esk = sb("esk", [65, 2, 512], F32)
        mi = sb("mi", [128, 512], I32)
        mP, mN = sb("mP", [128, 512], BF16), sb("mN", [128, 512], BF16)
        t_ka, t_va, t_es, t_mi, t_m = Tok(), Tok(), Tok(), Tok(), Tok()
        S.dma("sp", ka[:], d["kaT_s"][:, :], reads=[C.T(f"kaT:{g}") for g in range(9)], writes=[t_ka])
        S.dma("sp", va[:], d["va_s"].rearrange("(i p) f -> p i f", p=128), reads=[C.T(f"va:{g}") for g in range(9)], writes=[t_va])
        S.dma("sp", esk[64:65, :, :], d["sink_row"][l:l + 1, :, :], writes=[t_es])
        act(S, esk[64:65, :, :], esk[64:65, :, :], AF.Exp, [t_es], [t_es])
        S.op("pool", lambda e: e.iota(mi[:], pattern=[[0, 4], [1, 128]], base=0, channel_multiplier=-1), writes=[t_mi])
        ts(S, "dve", mP[:], mi[:], 0, NEG, ALU.is_gt, ALU.mult, [t_mi], [t_m], acc=True)
        ts(S, "dve", mN[:], mi[:], 0, NEG, ALU.is_lt, ALU.mult, [t_mi], [t_m], acc=True)
        pools = _norm_pools(C, st, f"a{l}")
        qp = SbPool(C, st, 3, f"a{l}_q", [128, 4, 128], BF16)
        qtiles = list(range(2, NT)) if last else list(range(NT))
        if QT_LIMIT:
            qtiles = qtiles[:QT_LIMIT]
        items = []
        for qt in qtiles:
            if qt < 2:
                keys = [(0, None), (1, None)]
            else:
                xq = qt - 2
                keys = [(0, None), (1, None)]
                if xq > 0:
                    keys.append((qt - 1, mP))
                keys.append((qt, None))
                if xq < 31:
                    keys.append((qt + 1, mN))
            for g in range(2):
                it = {"qt": qt, "g": g, "n": 512, "m_tok": t_m, "kv_reads": [t_ka, t_va],
                      "keys": [(ka[g * 64:(g + 1) * 64, kt * 128:(kt + 1) * 128], va[:, kt, g * 65:(g + 1) * 65], msk)
                               for kt, msk in keys]}
                items.append(it)
        cur = {}

        def load_q(it):
            qt, g = it["qt"], it["g"]
            if qt not in cur:
                q, k_q = qp.get()
                S.dma("sp", q[:], d["qaT_s"].rearrange("c p t -> p c t")[:, :, qt * 128:(qt + 1) * 128],
                      reads=[C.T(f"qaT:{gg}") for gg in range(9)], writes=[k_q])
                cur.clear()
                cur[qt] = (q, k_q)
            q, k_q = cur[qt]
            it["q"] = q[g * 64:(g + 1) * 64, :, :].rearrange("p c t -> p (c t)")
            it["q_reads"] = [k_q]

        for it in items:
            qt, g = it["qt"], it["g"]
            it["load_q"] = load_q
            dst = d["oaT_s"][2 * g:2 * g + 2].rearrange("u2 (u1 e) t -> e (u2 u1) t", u1=2)[:, :, qt * 128:(qt + 1) * 128]
            it["finish"] = (lambda acc, k_acc, g=g, qt=qt, dst=dst:
                            _norm_store(C, pools, acc, k_acc, 512, esk[64:65, g, :], t_es, _Dst(dst, False),
                                        [C.T(f"oaT_s:{qt}")]))
        attn_pipeline(C, st, f"a{l}", items, 0.125)
    S.barrier()


def phase_attn_b(C, l):
    nc, S, d = C.nc, C.S, C.d
    import contextlib
    last = (l == DEPTH - 1)
    scale = 96.0 ** -0.5
    with contextlib.ExitStack() as st:
        sb = lambda name, shape, dt: st.enter_context(nc.sbuf_tensor(f"b{l}_{name}", list(shape), dt))
        kb = sb("kb", [96, 8, NTOK], BF16)
        vb = sb("vb", [128, NT, 520], BF16)
        t_kb, t_vb = Tok(), Tok()
        for h in range(8):
            S.dma("sp", kb[:, h, :], d["kbT_s"][h], reads=[C.T(f"kbT:{g}") for g in range(9)], writes=[t_kb], acc=True)
        S.dma("sp", vb[:], d["vb_s"].rearrange("(i p) f -> p i f", p=128), reads=[C.T(f"vb:{g}") for g in range(9)], writes=[t_vb])
        pools = _norm_pools(C, st, f"b{l}")
        qp = SbPool(C, st, 2, f"b{l}_q", [96, 8, 512], BF16)
        groups = list(enumerate(GROUPS))
        if last:
            groups = groups[1:]
        if QT_LIMIT:
            groups = groups[:2]
        items = []
        curq = {}

        def load_q(it):
            g, h = it["g"], it["h"]
            t0, n = GROUPS[g]
            if g not in curq:
                q, k_q = qp.get()
                S.dma("sp", q[:, :, 0:n], d["qbT_s"].rearrange("h p t -> p h t")[:, :, t0:t0 + n],
                      reads=[C.T(f"qbT:{g}")], writes=[k_q])
                curq.clear()
                curq[g] = (q, k_q)
            q, k_q = curq[g]
            it["q"] = q[:, h, 0:n]
            it["q_reads"] = [k_q]

        for g, (t0, n) in groups:
            keys = [0, 1] if g == 0 else list(range(NT))
            tiles = list(range(t0 // 128, (t0 + n) // 128))
            for h in range(8):
                dst = d["obT_s"][h // 2, (h % 2) * 64:(h % 2) * 64 + 64, t0:t0 + n]
                items.append({"n": n, "g": g, "h": h, "load_q": load_q, "kv_reads": [t_kb, t_vb], "m_tok": None,
                              "keys": [(kb[:, h, kt * 128:(kt + 1) * 128], vb[:, kt, h * 65:(h + 1) * 65], None) for kt in keys],
                              "finish": (lambda acc, k_acc, n=n, dst=dst, tiles=tiles:
                                         _norm_store(C, pools, acc, k_acc, n, None, None, _Dst(dst, True),
                                                     [C.T(f"obT_s:{t}") for t in tiles]))})
        stg_g = SbPool(C, st, 1, f"b{l}_sgg", [128, 4096], F32)
        stg_d = SbPool(C, st, 1, f"b{l}_sgd", [128, 2048], F32)
        cb_g = SbPool(C, st, 1, f"b{l}_cbg", [128, 4096], BF16)
        cb_d = SbPool(C, st, 1, f"b{l}_cbd", [128, 2048], BF16)

        def precast(k):
            if k >= 64 or QT_LIMIT:
                return
            e_, part = k // 2, k % 2
            srcw, dstw, sp_, cp_ = ((d["w_gu_h"], d["wgu_b"], stg_g, cb_g) if part == 0 else (d["w_dn_h"], d["wdn_b"], stg_d, cb_d))
            s_t, s_k = sp_.get()
            S.dma("sp", s_t[:], srcw[l, e_ * 128:(e_ + 1) * 128, :], writes=[s_k])
            c_t, c_k = cp_.get()
            cp(S, "dve", c_t[:], s_t[:], [s_k], [c_k])
            S.dma("pool", dstw[e_ * 128:(e_ + 1) * 128, :], c_t[:], reads=[c_k], writes=[C.T("wexp_b")], acc=True)

        nsteps = sum((len(it["keys"]) + 1) // 2 for it in items)
        attn_pipeline(C, st, f"b{l}", items, scale, hook=precast, hook_every=max(1, nsteps // 66))
    S.barrier()


def phase_pool(C, l):
    nc, S, d = C.nc, C.S, C.d
    import contextlib
    last = (l == DEPTH - 1)
    seqs = [(256, SEQ, "invx")] + ([] if last else [(0, LC, "invc")])
    with contextlib.ExitStack() as st:
        sb = lambda name, shape, dt: st.enter_context(nc.sbuf_tensor(f"c{l}_{name}", list(shape), dt))
        wps = sb("wps", [128, 4, 128], F32)
        wp = sb("wp", [128, 4, 128], BF16)
        psc = sb("psc", [128, 4], F32)
        Up = sb("Up", [128, SEQ + 16], F32)
        Aa, Ab = sb("Aa", [128, SEQ + 16], F32), sb("Ab", [128, SEQ + 16], F32)
        inv = sb("inv", [128, SEQ], F32)
        ub = sb("ub", [128, SEQ], BF16)
        pl = sb("pl", [128, SEQ], BF16)
        oc = sb("oc", [128, SEQ], BF16)
        t_wp, t_psc, t_Up, t_A, t_B, t_inv, t_ub, t_pl, t_oc = (Tok() for _ in range(9))
        S.dma("sp", wps[:], d["w_pool"][l].rearrange("g c e -> c g e"), writes=[t_wp])
        cp(S, "act", wp[:], wps[:], [t_wp], [t_wp])
        S.dma("sp", psc[:], d["pool_scaleT"][l], writes=[t_psc])
        pp = PsumPool(C, st, 3, f"c{l}_pp")
        eng = ["dve", "dve"]
        ei = 0
        for (t0, L, invname) in seqs:
            for g in range(4):
                r = (1, 2, 4, 8)[g]
                S.dma("sp", ub[:, 0:L], d["uT_s"][g, :, t0:t0 + L], reads=[C.T(f"uT:{q}") for q in range(9)], writes=[t_ub])
                S.dma("sp", inv[:, 0:L], bcast_row(d[invname][g:g + 1, :], L), writes=[t_inv])
                S.op("dve", lambda e, L=L: e.memset(Up[:, 0:L + 16], 0.0), writes=[t_Up])
                cp(S, "dve", Up[:, 8:8 + L], ub[:, 0:L], [t_ub], [t_Up])
                src, k_src, ln = Up, t_Up, L + 16
                bufs = [(Aa, t_A), (Ab, t_B)]
                step = 1
                bi = 0
                while step <= r:
                    dst, k_dst = bufs[bi]
                    bi ^= 1
                    nl = ln - step
                    tt(S, eng[ei % 2], dst[:, 0:nl], src[:, 0:nl], src[:, step:step + nl], ALU.add, [k_src], [k_dst])
                    ei += 1
                    src, k_src, ln = dst, k_dst, nl
                    step *= 2
                dst, k_dst = bufs[bi]
                tt(S, eng[ei % 2], dst[:, 0:L], src[:, 8 - r:8 - r + L], Up[:, 8 + r:8 + r + L], ALU.add, [k_src, t_Up], [k_dst])
                ei += 1
                tt(S, "dve", dst[:, 0:L], dst[:, 0:L], inv[:, 0:L], ALU.mult, [k_dst, t_inv], [k_dst])
                tt(S, "dve", pl[:, 0:L], dst[:, 0:L], Up[:, 8:8 + L], ALU.subtract, [k_dst, t_Up], [t_pl])
                for c0 in range(0, L, 512):
                    n = min(512, L - c0)
                    ps, k_ps = pp.get()
                    mm(S, ps[:, 0:n], wp[:, g, :], pl[:, c0:c0 + n], True, True, [t_wp, t_pl], k_ps)
                    act(S, oc[:, c0:c0 + n], ps[:, 0:n], AF.Copy, [k_ps, t_psc], [t_oc], acc=(c0 > 0), scale=psc[:, g:g + 1])
                S.dma("pool", d["ocT_s"][g, :, t0:t0 + L], oc[:, 0:L], reads=[t_oc], writes=[C.T(f"ocT:{g}:{t0}")])
    S.barrier()


def phase_merge(C, l):
    nc, S, d = C.nc, C.S, C.d
    import contextlib
    last = (l == DEPTH - 1)
    hsrc = d["h0"] if l == 0 else d["h_s"]
    with contextlib.ExitStack() as st:
        sb = lambda name, shape, dt: st.enter_context(nc.sbuf_tensor(f"g{l}_{name}", list(shape), dt))
        wg = sb("wg", [128, 8, 3072], BF16)
        wbr = sb("wbr", [128, 12, 1024], BF16)
        wo = sb("wo", [128, 2, 8, 1024], BF16)
        G1 = sb("G1", [128, 2, 1024], F32)
        t_wg, t_wbr, t_wo, t_G1 = Tok(), Tok(), Tok(), Tok()
        wext = d["w_in_ext"][l].rearrange("(kc p) n -> p kc n", p=128)[:, :, O_G:W_EXT]
        load_cast(C, st, f"g{l}_stg", wg, wext, 8, 3072, t_wg, half=768)
        wbsrc = d["w_br"][l].rearrange("b (kc p) n -> p (b kc) n", p=128)
        load_cast(C, st, f"g{l}_stg2", wbr, wbsrc, 12, 1024, t_wbr, half=512)
        for r in range(2):
            S.dma("sp", G1[:, r, :], bcast_row(d["modv"][l][r:r + 1, 2048:3072], 1024), reads=[C.T(f"modv{l}")], writes=[t_G1], acc=True)
        stgo = SbPool(C, st, 2, f"g{l}_stgo", [128, 1024], F32)
        wosrc = d["w_out"][l].rearrange("(kc p) n -> p kc n", p=128)
        for kc in range(8):
            s_t, s_k = stgo.get()
            S.dma("sp", s_t[:], wosrc[:, kc, :], writes=[s_k])
            for r in range(2):
                tt(S, "dve", wo[:, r, kc, :], s_t[:], G1[:, r, :], ALU.mult, [s_k, t_G1], [t_wo], acc=True)
        pp = PsumPool(C, st, 6, f"g{l}_pp")
        hxTp = SbPool(C, st, 1, f"g{l}_hxT", [128, 8, 512], BF16)
        oTp = SbPool(C, st, 1, f"g{l}_oT", [128, 12, 512], BF16)
        YTp = SbPool(C, st, 1, f"g{l}_YT", [128, 8, 512], BF16)
        sgp = SbPool(C, st, 3, f"g{l}_sg", [128, 512], BF16)
        tbp = SbPool(C, st, 4, f"g{l}_tb", [128, 512], F32)
        y1p = SbPool(C, st, 2, f"g{l}_y1", [128, 512], F32)
        hp = SbPool(C, st, 2, f"g{l}_h", [128, 1024], F32)
        hnp = SbPool(C, st, 2, f"g{l}_hn", [128, 1024], F32)
        groups = list(enumerate(GROUPS))
        if last:
            groups = groups[1:]
        if QT_LIMIT:
            groups = groups[:2]
        oc_toks = [C.T(f"ocT:{g}:{t0}") for g in range(4) for t0 in (0, 256)]
        for g, (t0, n) in groups:
            r = 0 if g > 0 else 1
            hxT, k_hxT = hxTp.get()
            S.dma("sp", hxT[:, :, 0:n], d["hxT_s"].rearrange("c p t -> p c t")[:, :, t0:t0 + n], reads=[C.T(f"hxT:{g}")], writes=[k_hxT])
            oT, k_oT = oTp.get()
            tiles = list(range(t0 // 128, (t0 + n) // 128))
            for bi, (nm, rd) in enumerate((("oaT_s", [C.T(f"oaT_s:{t}") for t in tiles]),
                                           ("obT_s", [C.T(f"obT_s:{t}") for t in tiles]),
                                           ("ocT_s", oc_toks))):
                S.dma("sp", oT[:, bi * 4:(bi + 1) * 4, 0:n], d[nm].rearrange("c p t -> p c t")[:, :, t0:t0 + n],
                      reads=rd, writes=[k_oT], acc=(bi > 0))
            YT, k_YT = YTp.get()
            for m in range(8):
                tbs = []
                for br in range(3):
                    psg, k_psg = pp.get()
                    for kc in range(8):
                        mm(S, psg[:, 0:n], wg[:, kc, br * 1024 + m * 128:br * 1024 + (m + 1) * 128], hxT[:, kc, 0:n],
                           kc == 0, kc == 7, [t_wg, k_hxT], k_psg)
                    sg, k_sg = sgp.get()
                    act(S, sg[:, 0:n], psg[:, 0:n], AF.Sigmoid, [k_psg], [k_sg])
                    psv, k_psv = pp.get()
                    for kc in range(4):
                        mm(S, psv[:, 0:n], wbr[:, br * 4 + kc, m * 128:(m + 1) * 128], oT[:, br * 4 + kc, 0:n],
                           kc == 0, kc == 3, [t_wbr, k_oT], k_psv)
                    tb, k_tb = tbp.get()
                    tt(S, "dve", tb[:, 0:n], psv[:, 0:n], sg[:, 0:n], ALU.mult, [k_psv, k_sg], [k_tb])
                    tbs.append((tb, k_tb))
                y1, k_y1 = y1p.get()
                tt(S, "dve", y1[:, 0:n], tbs[0][0][:, 0:n], tbs[1][0][:, 0:n], ALU.add, [tbs[0][1], tbs[1][1]], [k_y1])
                tt(S, "dve", YT[:, m, 0:n], y1[:, 0:n], tbs[2][0][:, 0:n], ALU.add, [k_y1, tbs[2][1]], [k_YT], acc=(m > 0))
            for ti, tile in enumerate(tiles):
                ht, k_h = hp.get()
                S.dma("sp", ht[:], hsrc[tile * 128:(tile + 1) * 128, :], reads=[C.T(f"h:{tile}")], writes=[k_h])
                hn, k_hn = hnp.get()
                for half in range(2):
                    ps, k_ps = pp.get()
                    for m in range(8):
                        mm(S, ps[:, :], YT[:, m, ti * 128:(ti + 1) * 128], wo[:, r, m, half * 512:(half + 1) * 512],
                           m == 0, m == 7, [k_YT, t_wo], k_ps)
                    tt(S, "dve", hn[:, half * 512:(half + 1) * 512], ps[:, :], ht[:, half * 512:(half + 1) * 512], ALU.add,
                       [k_ps, k_h], [k_hn], acc=(half > 0))
                S.dma("pool", d["h_s"][tile * 128:(tile + 1) * 128, :], hn[:], reads=[k_hn], writes=[C.T(f"h:{tile}")])
    S.barrier()


def phase_route(C, l):
    nc, S, d = C.nc, C.S, C.d
    import contextlib
    last = (l == DEPTH - 1)
    with contextlib.ExitStack() as st:
        sb = lambda name, shape, dt: st.enter_context(nc.sbuf_tensor(f"r{l}_{name}", list(shape), dt))
        Gm, Sm = sb("Gm", [128, 2, 1024], F32), sb("Sm", [128, 2, 1024], F32)
        wr = sb("wr", [128, 8, 36], F32)
        br = sb("br", [128, 36], F32)
        t_mod, t_mod2, t_wr = Tok(), Tok(), Tok()
        mv = d["modv"][l]
        tmpp = SbPool(C, st, 2, f"r{l}_tmp", [128, 1024], F32)
        gn, k_gn = tmpp.get()
        S.dma("sp", gn[:], bcast_row(d["norm_ffn_g"][l:l + 1, :], 1024), writes=[k_gn])
        for r in range(2):
            S.dma("sp", Sm[:, r, :], bcast_row(mv[r:r + 1, 3072:4096], 1024), reads=[C.T(f"modv{l}")], writes=[t_mod], acc=True)
            S.dma("sp", Gm[:, r, :], bcast_row(mv[r:r + 1, 4096:5120], 1024), reads=[C.T(f"modv{l}")], writes=[t_mod], acc=True)
        for r in range(2):
            stt(S, Gm[:, r, :], Gm[:, r, :], 1.0, gn[:], ALU.add, ALU.mult, [t_mod, k_gn], [t_mod2], acc=True)
        S.dma("sp", wr[:], d["w_r"][l].rearrange("(kc p) n -> p kc n", p=128), writes=[t_wr], acc=True)
        S.dma("sp", br[:], bcast_row(d["b_r"][l:l + 1, :], 36), writes=[t_wr], acc=True)
        pt32 = PsumPool(C, st, 2, f"r{l}_pt", (128, 4, 128), F32)
        pp = PsumPool(C, st, 2, f"r{l}_pp", (128, 128), F32)
        ptb = PsumPool(C, st, 2, f"r{l}_ptb", (128, 8, 128), BF16)
        hp = SbPool(C, st, 2, f"r{l}_h", [128, 1024], F32)
        fxp = SbPool(C, st, 2, f"r{l}_fx", [128, 1024], F32)
        fxbp = SbPool(C, st, 2, f"r{l}_fxb", [128, 1024], BF16)
        fxTp = SbPool(C, st, 2, f"r{l}_fxT", [128, 8, 128], F32)
        fxTbp = SbPool(C, st, 2, f"r{l}_fxTb", [128, 8, 128], BF16)
        ssp = SbPool(C, st, 2, f"r{l}_ss", [128, 4], F32)
        sqj = sb("sqj", [128, 1024], BF16)
        t_sqj = Tok()
        rtp = SbPool(C, st, 2, f"r{l}_rt", [128, 160], F32)
        gTp = SbPool(C, st, 2, f"r{l}_gT", [32, 128], F32)
        tiles = list(range(2, NT)) if last else list(range(NT))
        if QT_LIMIT:
            tiles = tiles[:QT_LIMIT]
        for tile in tiles:
            mr = 0 if tile >= 2 else 1
            ht, k_h = hp.get()
            S.dma("sp", ht[:], d["h_s"][tile * 128:(tile + 1) * 128, :], reads=[C.T(f"h:{tile}")], writes=[k_h])
            ss, k_ss = ssp.get()
            act(S, sqj[:], ht[:], AF.Square, [k_h], [t_sqj, k_ss], accum_out=ss[:, 0:1])
            act(S, ss[:, 1:2], ss[:, 0:1], AF.Sqrt, [k_ss], [k_ss], acc=True, scale=1.0 / DM, bias=EPS)
            S.op("dve", lambda e, ss=ss: e.reciprocal(out=ss[:, 2:3], in_=ss[:, 1:2]), reads=[k_ss], writes=[k_ss], acc=True)
            tm, k_tm = tmpp.get()
            stt(S, tm[:], ht[:], ss[:, 2:3], Gm[:, mr, :], ALU.mult, ALU.mult, [k_h, k_ss, t_mod2], [k_tm])
            fx, k_fx = fxp.get()
            tt(S, "dve", fx[:], tm[:], Sm[:, mr, :], ALU.add, [k_tm, t_mod2], [k_fx])
            fxb, k_fxb = fxbp.get()
            cp(S, "act", fxb[:], fx[:], [k_fx], [k_fxb])
            tpb, k_tpb = ptb.get()
            for kc in range(8):
                S.op("pe", lambda e, tpb=tpb, fxb=fxb, kc=kc: e.transpose(tpb[:, kc, :], fxb[:, kc * 128:(kc + 1) * 128], C.idb[:]),
                     reads=[k_fxb, C.t_c], writes=[k_tpb], acc=(kc > 0))
            fxTb, k_fxTb = fxTbp.get()
            cp(S, "act", fxTb[:], tpb[:], [k_tpb], [k_fxTb])
            S.dma("pool", d["hxT_s"].rearrange("c p t -> p c t")[:, :, tile * 128:(tile + 1) * 128], fxTb[:],
                  reads=[k_fxTb], writes=[C.T(f"fxT:{tile}")])
            fxT, k_fxT = fxTp.get()
            for hf in range(2):
                tp, k_tp = pt32.get()
                for kc in range(4):
                    S.op("pe", lambda e, tp=tp, fx=fx, kc=kc, hf=hf: e.transpose(tp[:, kc, :], fx[:, (hf * 4 + kc) * 128:(hf * 4 + kc + 1) * 128], C.idf[:]),
                         reads=[k_fx, C.t_c], writes=[k_tp], acc=(kc > 0))
                cp(S, "act", fxT[:, hf * 4:(hf + 1) * 4, :], tp[:], [k_tp], [k_fxT], acc=(hf > 0))
            pl, k_pl = pp.get()
            for kc in range(8):
                mm(S, pl[:, 0:36], fxT[:, kc, :], wr[:, kc, :], kc == 0, kc == 7, [k_fxT, t_wr], k_pl)
            rt, k = rtp.get()
            tt(S, "dve", rt[:, 0:36], pl[:, 0:36], br[:], ALU.add, [k_pl, t_wr], [k])
            S.op("dve", lambda e, rt=rt: e.reduce_max(out=rt[:, 36:37], in_=rt[:, 0:4], axis=AX.X), reads=[k], writes=[k], acc=True)
            ts(S, "dve", rt[:, 37:38], rt[:, 36:37], -1.0, None, ALU.mult, None, [k], [k], acc=True)
            act(S, rt[:, 44:48], rt[:, 0:4], AF.Exp, [k], [k], acc=True, bias=rt[:, 37:38], accum_out=rt[:, 38:39])
            S.op("dve", lambda e, rt=rt: e.reciprocal(out=rt[:, 39:40], in_=rt[:, 38:39]), reads=[k], writes=[k], acc=True)
            ts(S, "dve", rt[:, 40:44], rt[:, 0:4], rt[:, 36:37], None, ALU.is_equal, None, [k], [k], acc=True)
            ts(S, "dve", rt[:, 40:44], rt[:, 40:44], -1.0, 30000.0, ALU.add, ALU.mult, [k], [k], acc=True)
            for g in range(4):
                ts(S, "dve", rt[:, 48 + g * 8:56 + g * 8], rt[:, 4 + g * 8:12 + g * 8], rt[:, 40 + g:41 + g], None,
                   ALU.add, None, [k], [k], acc=True)
            S.op("dve", lambda e, rt=rt: e.max(out=rt[:, 80:88], in_=rt[:, 48:80]), reads=[k], writes=[k], acc=True)
            tt(S, "dve", rt[:, 88:89], rt[:, 81:82], rt[:, 80:81], ALU.subtract, [k], [k], acc=True)
            act(S, rt[:, 89:90], rt[:, 88:89], AF.Exp, [k], [k], acc=True)
            ts(S, "dve", rt[:, 90:91], rt[:, 89:90], 1.0, None, ALU.add, None, [k], [k], acc=True)
            S.op("dve", lambda e, rt=rt: e.reciprocal(out=rt[:, 90:91], in_=rt[:, 90:91]), reads=[k], writes=[k], acc=True)
            tt(S, "dve", rt[:, 91:92], rt[:, 90:91], rt[:, 39:40], ALU.mult, [k], [k], acc=True)
            tt(S, "dve", rt[:, 92:93], rt[:, 91:92], rt[:, 89:90], ALU.mult, [k], [k], acc=True)
            ts(S, "dve", rt[:, 96:128], rt[:, 48:80], rt[:, 80:81], rt[:, 91:92], ALU.is_equal, ALU.mult, [k], [k], acc=True)
            ts(S, "dve", rt[:, 128:160], rt[:, 48:80], rt[:, 81:82], rt[:, 92:93], ALU.is_equal, ALU.mult, [k], [k], acc=True)
            tt(S, "dve", rt[:, 96:128], rt[:, 96:128], rt[:, 128:160], ALU.add, [k], [k], acc=True)
            pg, k_pg = pp.get()
            S.op("pe", lambda e, pg=pg, rt=rt: e.transpose(pg[0:32, 0:128], rt[:, 96:128], C.idf[:]),
                 reads=[k, C.t_c], writes=[k_pg])
            gT, k_gT = gTp.get()
            cp(S, "act", gT[:], pg[0:32, 0:128], [k_pg], [k_gT])
            S.dma("pool", d["gT_s"][:, tile * 128:(tile + 1) * 128], gT[:], reads=[k_gT], writes=[C.T(f"gT:{tile}")])
    S.barrier()


def phase_experts(C, l):
    nc, S, d = C.nc, C.S, C.d
    import contextlib
    last = (l == DEPTH - 1)
    with contextlib.ExitStack() as st:
        sb = lambda name, shape, dt: st.enter_context(nc.sbuf_tensor(f"e{l}_{name}", list(shape), dt))
        g2T = sb("g2T", [128, 2, 8], F32)
        fg = sb("fg", [128, 1024], F32)
        t_g2 = Tok()
        for r in range(2):
            S.dma("sp", g2T[:, r, :], d["modv"][l][r, 5120:6144].rearrange("(m p) -> p m", p=128), reads=[C.T(f"modv{l}")],
                  writes=[t_g2], acc=True, allow_slow_non_contiguous=True)
        if last:
            S.dma("sp", fg[:], bcast_row(d["final_g"][0:1, :], 1024), writes=[t_g2], acc=True)
        ppg = PsumPool(C, st, 4, f"e{l}_ppg")
        pp = PsumPool(C, st, 3, f"e{l}_pp")
        wgs = SbPool(C, st, 1, f"e{l}_wgs", [128, 8, 512], F32)
        wds = SbPool(C, st, 1, f"e{l}_wds", [128, 2, 1024], F32)
        wgp = SbPool(C, st, 2, f"e{l}_wg", [128, 8, 512], BF16)
        wdp = SbPool(C, st, 2, f"e{l}_wd", [128, 2, 1024], BF16)
        fxTp = SbPool(C, st, 1, f"e{l}_fxT", [128, 8, 2176], BF16)
        yacc = sb("yacc", [128, 8, 2176], F32)
        t_y = Tok()
        gwp = SbPool(C, st, 3, f"e{l}_gw", [128, 512], F32)
        sip = SbPool(C, st, 2, f"e{l}_si", [128, 512], F32)
        a1p = SbPool(C, st, 2, f"e{l}_a1", [128, 512], F32)
        actp = SbPool(C, st, 3, f"e{l}_act", [128, 2, 512], BF16)
        hp = SbPool(C, st, 2, f"e{l}_h", [128, 1024], F32)
        hnp = SbPool(C, st, 2, f"e{l}_hn", [128, 1024], F32)
        ssp = SbPool(C, st, 2, f"e{l}_ss", [128, 4], F32)
        sqj = sb("sqj", [128, 1024], BF16)
        t_sqj = Tok()
        tok0 = 256 if last else 0
        sgs = []
        t = tok0
        while t < NTOK:
            n = min(2048 if last else 2176, NTOK - t)
            sgs.append((t, n))
            t += n
        if QT_LIMIT:
            sgs = [(tok0, 256)]
        nexp = 32 if not QT_LIMIT else QT_LIMIT
        for (t0, n) in sgs:
            tiles = list(range(t0 // 128, (t0 + n) // 128))
            fxT, k_fxT = fxTp.get()
            S.dma("sp", fxT[:, :, 0:n], d["hxT_s"].rearrange("c p t -> p c t")[:, :, t0:t0 + n],
                  reads=[C.T(f"fxT:{tl}") for tl in tiles], writes=[k_fxT])
            steps = [(e_, s0) for e_ in range(nexp) for s0 in range(0, n, 512)]
            wcur = {}
            st_state = {}

            def load_w(e_):
                ws, k_ws = wgs.get()
                S.dma("sp", ws[:], d["w_gu"][l, e_].rearrange("(kc p) n -> p kc n", p=128), writes=[k_ws])
                wg, k_wg = wgp.get()
                cp(S, "act", wg[:], ws[:], [k_ws], [k_wg])
                ws2, k_ws2 = wds.get()
                S.dma("sp", ws2[:], d["w_dn"][l, e_].rearrange("(kc p) n -> p kc n", p=128), writes=[k_ws2])
                wd, k_wd = wdp.get()
                cp(S, "act", wd[:], ws2[:], [k_ws2], [k_wd])
                wcur[e_] = (wg, k_wg, wd, k_wd)

            def emit_gu(si):
                e_, s0 = steps[si]
                ns = min(512, n - s0)
                if e_ not in wcur:
                    load_w(e_)
                    wcur.pop(e_ - 2, None)
                wg, k_wg, wd, k_wd = wcur[e_]
                gw, k_gw = gwp.get()
                S.dma("sp", gw[:, 0:ns], bcast_row(d["gT_s"][e_:e_ + 1, t0 + s0:t0 + s0 + ns], ns),
                      reads=[C.T(f"gT:{tl}") for tl in tiles], writes=[k_gw])
                ac, k_ac = actp.get()
                for c in range(2):
                    psg, k_psg = ppg.get()
                    psu, k_psu = ppg.get()
                    for kc in range(8):
                        mm(S, psg[:, 0:ns], wg[:, kc, c * 128:(c + 1) * 128], fxT[:, kc, s0:s0 + ns], kc == 0, kc == 7, [k_wg, k_fxT], k_psg)
                    for kc in range(8):
                        mm(S, psu[:, 0:ns], wg[:, kc, 256 + c * 128:256 + (c + 1) * 128], fxT[:, kc, s0:s0 + ns], kc == 0, kc == 7, [k_wg, k_fxT], k_psu)
                    si_, k_si = sip.get()
                    act(S, si_[:, 0:ns], psg[:, 0:ns], AF.Silu, [k_psg], [k_si])
                    a1, k_a1 = a1p.get()
                    tt(S, "dve", a1[:, 0:ns], psu[:, 0:ns], si_[:, 0:ns], ALU.mult, [k_psu, k_si], [k_a1])
                    tt(S, "dve", ac[:, c, 0:ns], a1[:, 0:ns], gw[:, 0:ns], ALU.mult, [k_a1, k_gw], [k_ac], acc=(c > 0))
                st_state[si] = (ac, k_ac, wd, k_wd)

            def emit_dn(si):
                e_, s0 = steps[si]
                ns = min(512, n - s0)
                ac, k_ac, wd, k_wd = st_state.pop(si)
                for m in range(8):
                    py, k_py = pp.get()
                    for c in range(2):
                        mm(S, py[:, 0:ns], wd[:, c, m * 128:(m + 1) * 128], ac[:, c, 0:ns], c == 0, c == 1, [k_wd, k_ac], k_py)
                    if e_ == 0:
                        cp(S, "act", yacc[:, m, s0:s0 + ns], py[:, 0:ns], [k_py], [t_y], acc=True)
                    else:
                        tt(S, "dve", yacc[:, m, s0:s0 + ns], py[:, 0:ns], yacc[:, m, s0:s0 + ns], ALU.add, [k_py, t_y], [t_y], acc=True)

            emit_gu(0)
            for si in range(len(steps)):
                if si + 1 < len(steps):
                    emit_gu(si + 1)
                emit_dn(si)
            for m in range(8):
                for (a, b_, r) in ((0, 256 - t0, 1), (max(0, 256 - t0), n, 0)):
                    if b_ <= a:
                        continue
                    b_ = min(b_, n)
                    act(S, yacc[:, m, a:b_], yacc[:, m, a:b_], AF.Copy, [t_y, t_g2], [t_y], acc=True, scale=g2T[:, r, m:m + 1])
            for ti, tile in enumerate(tiles):
                ht, k_h = hp.get()
                S.dma("sp", ht[:], d["h_s"][tile * 128:(tile + 1) * 128, :], reads=[C.T(f"h:{tile}")], writes=[k_h])
                hn, k_hn = hnp.get()
                for hf in range(2):
                    ps, k_ps = pp.get()
                    for kc in range(4):
                        m = hf * 4 + kc
                        S.op("pe", lambda e, ps=ps, m=m, kc=kc, ti=ti: e.transpose(ps[:, kc * 128:(kc + 1) * 128], yacc[:, m, ti * 128:(ti + 1) * 128], C.idf[:]),
                             reads=[t_y, C.t_c], writes=[k_ps], acc=(kc > 0))
                    tt(S, "dve", hn[:, hf * 512:(hf + 1) * 512], ps[:, :], ht[:, hf * 512:(hf + 1) * 512], ALU.add, [k_ps, k_h], [k_hn], acc=(hf > 0))
                if not last:
                    S.dma("pool", d["h_s"][tile * 128:(tile + 1) * 128, :], hn[:], reads=[k_hn], writes=[C.T(f"h:{tile}")])
                else:
                    ss, k_ss = ssp.get()
                    act(S, sqj[:], hn[:], AF.Square, [k_hn], [t_sqj, k_ss], accum_out=ss[:, 0:1])
                    act(S, ss[:, 1:2], ss[:, 0:1], AF.Sqrt, [k_ss], [k_ss], acc=True, scale=1.0 / DM, bias=EPS)
                    S.op("dve", lambda e, ss=ss: e.reciprocal(out=ss[:, 2:3], in_=ss[:, 1:2]), reads=[k_ss], writes=[k_ss], acc=True)
                    ho, k_ho = hp.get()
                    stt(S, ho[:], hn[:], ss[:, 2:3], fg[:], ALU.mult, ALU.mult, [k_hn, k_ss, t_g2], [k_ho])
                    S.dma("pool", d["out"][(tile - 2) * 128:(tile - 1) * 128, :], ho[:], reads=[k_ho], writes=[C.T(f"out:{tile}")])
    S.barrier()


def all_phases(C):
    C.d["out"] = C.nc.dram_tensor("out", [SEQ, DM], F32, kind="ExternalOutput").ap()
    for l in range(DEPTH):
        phase_mod(C, l)
        phase_inproj(C, l)
        phase_attn_a(C, l)
        phase_attn_b(C, l)
        phase_pool(C, l)
        phase_merge(C, l)
        phase_moe(C, l)


def kernel(**inputs):
    sh = prep_shared(inputs)
    nc = build_program(all_phases)
    in_maps = []
    for b in range(8):
        m = dict(sh)
        m.update(prep_core(inputs, b))
        in_maps.append({k: m[k] for k in INPUTS})
    res = run_bass_kernel_spmd(nc, in_maps, core_ids=list(range(8)))
    return np.stack([np.asarray(r["out"], dtype=np.float32) for r in res.results], axis=0)


def phase_moe(C, l):
    nc, S, d = C.nc, C.S, C.d
    import contextlib
    last = (l == DEPTH - 1)
    tiles = list(range(2, NT)) if last else list(range(NT))
    if QT_LIMIT:
        tiles = tiles[:QT_LIMIT]
    nt = len(tiles)
    NB = -(-(2 * nt * 128 + 32 * 127) // 128)
    xbuf = d["xbuf"]
    ybuf = d["ybuf"]
    with contextlib.ExitStack() as st0:
        sb0 = lambda name, shape, dt: st0.enter_context(nc.sbuf_tensor(f"x{l}_{name}", list(shape), dt))
        sAB = sb0("sAB", [128, NT, 2], I32)
        w12 = sb0("w12", [128, NT, 2], F32)
        widx = sb0("widx", [128, 128], I32)
        t_sAB, t_w12, t_widx = Tok(), Tok(), Tok()
        t_xz = Tok()
        with contextlib.ExitStack() as st:
            sb = lambda name, shape, dt: st.enter_context(nc.sbuf_tensor(f"r{l}_{name}", list(shape), dt))
            zt = sb("zt", [128, 4, 1024], BF16)
            t_zt = Tok()
            S.op("dve", lambda e: e.memset(zt[:], 0.0), writes=[t_zt])
            xv = xbuf.rearrange("(a p) f -> p a f", p=128)
            for b0 in range(0, NB, 4):
                nb_ = min(4, NB - b0)
                S.dma("sp", xv[:, b0:b0 + nb_, :], zt[:, 0:nb_, :], reads=[t_zt], writes=[t_xz], acc=True)
            Gm, Sm = sb("Gm", [128, 2, 1024], F32), sb("Sm", [128, 2, 1024], F32)
            wr = sb("wr", [128, 8, 36], F32)
            br = sb("br", [128, 36], F32)
            t_mod, t_mod2, t_wr = Tok(), Tok(), Tok()
            mv = d["modv"][l]
            tmpp = SbPool(C, st, 2, f"r{l}_tmp", [128, 1024], F32)
            gn, k_gn = tmpp.get()
            S.dma("sp", gn[:], bcast_row(d["norm_ffn_g"][l:l + 1, :], 1024), writes=[k_gn])
            for r in range(2):
                S.dma("sp", Sm[:, r, :], bcast_row(mv[r:r + 1, 3072:4096], 1024), reads=[C.T(f"modv{l}")], writes=[t_mod], acc=True)
                S.dma("sp", Gm[:, r, :], bcast_row(mv[r:r + 1, 4096:5120], 1024), reads=[C.T(f"modv{l}")], writes=[t_mod], acc=True)
            for r in range(2):
                stt(S, Gm[:, r, :], Gm[:, r, :], 1.0, gn[:], ALU.add, ALU.mult, [t_mod, k_gn], [t_mod2], acc=True)
            S.dma("sp", wr[:], d["w_r"][l].rearrange("(kc p) n -> p kc n", p=128), writes=[t_wr], acc=True)
            S.dma("sp", br[:], bcast_row(d["b_r"][l:l + 1, :], 36), writes=[t_wr], acc=True)
            fx_all = sb("fxall", [128, NT, 1024], BF16)
            A01 = sb("A01", [128, NT, 32], F32)
            B01 = sb("B01", [128, NT, 32], F32)
            Mall = sb("Mall", [128, NT, 32], BF16)
            t_fx = [Tok() for _ in range(NT)]
            GT = 8
            pt32 = PsumPool(C, st, 2, f"r{l}_pt", (128, 4, 128), F32)
            pp = PsumPool(C, st, 2, f"r{l}_pp", (128, 512), F32)
            hbp = SbPool(C, st, 1, f"r{l}_hb", [128, GT, 1024], F32)
            fxp = SbPool(C, st, 2, f"r{l}_fx", [128, 1024], F32)
            fxTp = SbPool(C, st, 2, f"r{l}_fxT", [128, 8, 128], F32)
            ssp = SbPool(C, st, 2, f"r{l}_ss", [128, 3, GT], F32)
            sqj = sb("sqj", [128, GT, 1024], BF16)
            t_sqj = Tok()
            rwp = SbPool(C, st, 2, f"r{l}_rw", [128, GT, 128], F32)
            rsp_ = SbPool(C, st, 2, f"r{l}_rs", [128, 12, GT], F32)

            def b3(ap2, gt, w):
                return ap2.rearrange("p (g o) -> p g o", o=1).to_broadcast([128, gt, w])

            batches = [list(range(a, min(a + GT, nt))) for a in range(0, nt, GT)]
            t_ABb = []
            for bt in batches:
                gt = len(bt)
                t0i = bt[0]
                hb, k_hb = hbp.get()
                ss, k_ss = ssp.get()
                for j, ti in enumerate(bt):
                    tile = tiles[ti]
                    S.dma("sp", hb[:, j, :], d["h_s"][tile * 128:(tile + 1) * 128, :], reads=[C.T(f"h:{tile}")], writes=[k_hb], acc=(j > 0))
                for j, ti in enumerate(bt):
                    act(S, sqj[:, j, :], hb[:, j, :], AF.Square, [k_hb], [t_sqj, k_ss], acc=(j > 0), accum_out=ss[:, 0, j:j + 1])
                act(S, ss[:, 1, 0:gt], ss[:, 0, 0:gt], AF.Sqrt, [k_ss], [k_ss], acc=True, scale=1.0 / DM, bias=EPS)
                S.op("dve", lambda e, ss=ss, gt=gt: e.reciprocal(out=ss[:, 2, 0:gt], in_=ss[:, 1, 0:gt]), reads=[k_ss], writes=[k_ss], acc=True)
                pl, k_pl = pp.get()
                for j, ti in enumerate(bt):
                    tile = tiles[ti]
                    mr = 0 if tile >= 2 else 1
                    tm, k_tm = tmpp.get()
                    stt(S, tm[:], hb[:, j, :], ss[:, 2, j:j + 1], Gm[:, mr, :], ALU.mult, ALU.mult, [k_hb, k_ss, t_mod2], [k_tm])
                    fx, k_fx = fxp.get()
                    tt(S, "dve", fx[:], tm[:], Sm[:, mr, :], ALU.add, [k_tm, t_mod2], [k_fx])
                    cp(S, "act", fx_all[:, ti, :], fx[:], [k_fx], [t_fx[ti]])
                    fxT, k_fxT = fxTp.get()
                    for hf in range(2):
                        tp, k_tp = pt32.get()
                        for kc in range(4):
                            S.op("pe", lambda e, tp=tp, fx=fx, kc=kc, hf=hf: e.transpose(tp[:, kc, :], fx[:, (hf * 4 + kc) * 128:(hf * 4 + kc + 1) * 128], C.idf[:]),
                                 reads=[k_fx, C.t_c], writes=[k_tp], acc=(kc > 0))
                        cp(S, "act", fxT[:, hf * 4:(hf + 1) * 4, :], tp[:], [k_tp], [k_fxT], acc=(hf > 0))
                    for kc in range(8):
                        mm(S, pl[:, j * 36:(j + 1) * 36], fxT[:, kc, :], wr[:, kc, :], kc == 0, kc == 7, [k_fxT, t_wr], k_pl)
                rw, k = rwp.get()
                rs, k2 = rsp_.get()
                plv = pl[:, 0:gt * 36].rearrange("p (g n) -> p g n", n=36)
                tt(S, "dve", rw[:, 0:gt, 0:36], plv, br[:, :].rearrange("p (o n) -> p o n", o=1).to_broadcast([128, gt, 36]), ALU.add, [k_pl, t_wr], [k])
                if STOP == 101:
                    continue
                S.op("dve", lambda e, rw=rw, rs=rs, gt=gt: e.reduce_max(out=rs[:, 0, 0:gt], in_=rw[:, 0:gt, 0:4], axis=AX.X), reads=[k], writes=[k2])
                tt(S, "dve", rw[:, 0:gt, 36:40], rw[:, 0:gt, 0:4], b3(rs[:, 0, 0:gt], gt, 4), ALU.subtract, [k, k2], [k], acc=True)
                act(S, rw[:, 0:gt, 36:40], rw[:, 0:gt, 36:40], AF.Exp, [k], [k], acc=True)
                S.op("dve", lambda e, rw=rw, rs=rs, gt=gt: e.reduce_sum(out=rs[:, 1, 0:gt], in_=rw[:, 0:gt, 36:40], axis=AX.X), reads=[k], writes=[k2], acc=True)
                S.op("dve", lambda e, rs=rs, gt=gt: e.reciprocal(out=rs[:, 2, 0:gt], in_=rs[:, 1, 0:gt]), reads=[k2], writes=[k2], acc=True)
                if STOP == 102:
                    continue
                tt(S, "dve", rw[:, 0:gt, 36:40], rw[:, 0:gt, 0:4], b3(rs[:, 0, 0:gt], gt, 4), ALU.is_equal, [k, k2], [k], acc=True)
                ts(S, "dve", rw[:, 0:gt, 36:40], rw[:, 0:gt, 36:40], -1.0, 30000.0, ALU.add, ALU.mult, [k], [k], acc=True)
                tt(S, "dve", rw[:, 0:gt, 48:80].rearrange("p g (a b) -> p g a b", b=8),
                   rw[:, 0:gt, 4:36].rearrange("p g (a b) -> p g a b", b=8),
                   rw[:, 0:gt, 36:40].rearrange("p g (a o) -> p g a o", o=1).to_broadcast([128, gt, 4, 8]), ALU.add, [k], [k], acc=True)
                if STOP == 103:
                    continue
                S.op("dve", lambda e, rw=rw, rs=rs, gt=gt: e.reduce_max(out=rs[:, 3, 0:gt], in_=rw[:, 0:gt, 48:80], axis=AX.X), reads=[k], writes=[k2], acc=True)
                t_ab = Tok()
                t_ABb.append(t_ab)
                tt(S, "dve", A01[:, t0i:t0i + gt, :], rw[:, 0:gt, 48:80], b3(rs[:, 3, 0:gt], gt, 32), ALU.is_equal, [k, k2], [t_ab])
                stt(S, rw[:, 0:gt, 80:112], A01[:, t0i:t0i + gt, :], -60000.0, rw[:, 0:gt, 48:80], ALU.mult, ALU.add, [t_ab, k], [k], acc=True)
                S.op("dve", lambda e, rw=rw, rs=rs, gt=gt: e.reduce_max(out=rs[:, 4, 0:gt], in_=rw[:, 0:gt, 80:112], axis=AX.X), reads=[k], writes=[k2], acc=True)
                tt(S, "dve", B01[:, t0i:t0i + gt, :], rw[:, 0:gt, 80:112], b3(rs[:, 4, 0:gt], gt, 32), ALU.is_equal, [k, k2], [t_ab], acc=True)
                tt(S, "dve", Mall[:, t0i:t0i + gt, :], A01[:, t0i:t0i + gt, :], B01[:, t0i:t0i + gt, :], ALU.add, [t_ab], [t_ab], acc=True)
                if STOP == 104:
                    continue
                tt(S, "dve", rs[:, 5, 0:gt], rs[:, 4, 0:gt], rs[:, 3, 0:gt], ALU.subtract, [k2], [k2], acc=True)
                act(S, rs[:, 6, 0:gt], rs[:, 5, 0:gt], AF.Exp, [k2], [k2], acc=True)
                ts(S, "dve", rs[:, 7, 0:gt], rs[:, 6, 0:gt], 1.0, None, ALU.add, None, [k2], [k2], acc=True)
                S.op("dve", lambda e, rs=rs, gt=gt: e.reciprocal(out=rs[:, 7, 0:gt], in_=rs[:, 7, 0:gt]), reads=[k2], writes=[k2], acc=True)
                tt(S, "dve", w12[:, t0i:t0i + gt, 0], rs[:, 7, 0:gt], rs[:, 2, 0:gt], ALU.mult, [k2], [t_w12], acc=True)
                tt(S, "dve", w12[:, t0i:t0i + gt, 1], w12[:, t0i:t0i + gt, 0], rs[:, 6, 0:gt], ALU.mult, [k2, t_w12], [t_w12], acc=True)
            if 101 <= STOP <= 105:
                S.barrier()
                return
            t_AB = [t_ABb[ti // GT] for ti in range(nt)]
            pc, k_pc = pp.get()
            for ti in range(nt):
                mm(S, pc[:, 0:32], C.onb[:], Mall[:, ti, :], ti == 0, ti == nt - 1, [t_AB[ti], C.t_c], k_pc)
            cw = sb("cw", [128, 8, 32], F32)
            t_cw = Tok()
            ts(S, "dve", cw[:, 0, :], pc[:, 0:32], 1.0, None, ALU.mult, None, [k_pc], [t_cw])
            S.op("dve", lambda e: e.memset(cw[:, 1, :], 0.0), writes=[t_cw], acc=True)
            for kk in range(nt + 1):
                stt(S, cw[:, 1, :], cw[:, 0, :], 128.0 * kk, cw[:, 1, :], ALU.is_gt, ALU.add, [t_cw], [t_cw], acc=True)
            ts(S, "dve", cw[:, 2, :], cw[:, 1, :], 128.0, None, ALU.mult, None, [t_cw], [t_cw], acc=True)
            ts(S, "dve", cw[:, 3, :], cw[:, 2, :], 1.0, None, ALU.mult, None, [t_cw], [t_cw], acc=True)
            src_i = 3
            for s_ in (1, 2, 4, 8, 16):
                dst_i = 7 - src_i
                ts(S, "dve", cw[:, dst_i, 0:s_], cw[:, src_i, 0:s_], 1.0, None, ALU.mult, None, [t_cw], [t_cw], acc=True)
                tt(S, "dve", cw[:, dst_i, s_:32], cw[:, src_i, s_:32], cw[:, src_i, 0:32 - s_], ALU.add, [t_cw], [t_cw], acc=True)
                src_i = dst_i
            pe_i = src_i
            tt(S, "dve", cw[:, 5, :], cw[:, pe_i, :], cw[:, 2, :], ALU.subtract, [t_cw], [t_cw], acc=True)
            thi = sb("thi", [128, 128], I32)
            thr = sb("thr", [128, 128], F32)
            bacc = sb("bacc", [128, 128], F32)
            pidi = sb("pidi", [128, 1], I32)
            pidf = sb("pidf", [128, 1], F32)
            t_th = Tok()
            S.op("pool", lambda e: e.iota(thi[:], pattern=[[128, 128]], base=0, channel_multiplier=0), writes=[t_th])
            S.op("pool", lambda e: e.iota(pidi[:], pattern=[[0, 1]], base=0, channel_multiplier=1), writes=[t_th], acc=True)
            cp(S, "dve", thr[:], thi[:], [t_th], [t_th])
            cp(S, "dve", pidf[:], pidi[:], [t_th], [t_th], acc=True)
            S.op("dve", lambda e: e.memset(bacc[:], 0.0), writes=[t_th], acc=True)
            for e_ in range(32):
                stt(S, bacc[:], thr[:], cw[:, pe_i, e_:e_ + 1], bacc[:], ALU.is_ge, ALU.add, [t_th, t_cw], [t_th], acc=True)
            ts(S, "dve", bacc[:], bacc[:], 31.0, 128.0, ALU.min, ALU.mult, [t_th], [t_th], acc=True)
            ts(S, "dve", bacc[:], bacc[:], pidf[:, 0:1], 0.0, ALU.add, ALU.add, [t_th], [t_th], acc=True)
            cp(S, "dve", widx[:], bacc[:], [t_th], [t_widx])
            if STOP == 106:
                S.barrier()
                return
            slp = SbPool(C, st, 2, f"r{l}_sl", [128, 3, GT, 32], F32)
            sfp = SbPool(C, st, 2, f"r{l}_sf", [128, GT, 2], F32)
            for bt in batches:
                gt = len(bt)
                t0i = bt[0]
                ps, k_ps = pp.get()
                for j, ti in enumerate(bt):
                    for kk in range(ti):
                        mm(S, ps[:, j * 32:(j + 1) * 32], C.onb[:], Mall[:, kk, :], kk == 0, False, [t_AB[kk], C.t_c], k_ps)
                    mm(S, ps[:, j * 32:(j + 1) * 32], C.utri[:], Mall[:, ti, :], ti == 0, True, [t_AB[ti], C.t_c], k_ps)
                sl, k_sl = slp.get()
                psv = ps[:, 0:gt * 32].rearrange("p (g n) -> p g n", n=32)
                tt(S, "dve", sl[:, 0, 0:gt, :], psv, cw[:, 5, :].rearrange("p (o n) -> p o n", o=1).to_broadcast([128, gt, 32]), ALU.add, [k_ps, t_cw], [k_sl])
                tt(S, "dve", sl[:, 1, 0:gt, :], sl[:, 0, 0:gt, :], A01[:, t0i:t0i + gt, :], ALU.mult, [k_sl, t_AB[t0i]], [k_sl], acc=True)
                tt(S, "dve", sl[:, 2, 0:gt, :], sl[:, 0, 0:gt, :], B01[:, t0i:t0i + gt, :], ALU.mult, [k_sl, t_AB[t0i]], [k_sl], acc=True)
                sf, k_sf = sfp.get()
                S.op("dve", lambda e, sf=sf, sl=sl, gt=gt: e.reduce_sum(out=sf[:, 0:gt, 0], in_=sl[:, 1, 0:gt, :], axis=AX.X), reads=[k_sl], writes=[k_sf])
                S.op("dve", lambda e, sf=sf, sl=sl, gt=gt: e.reduce_sum(out=sf[:, 0:gt, 1], in_=sl[:, 2, 0:gt, :], axis=AX.X), reads=[k_sl], writes=[k_sf], acc=True)
                cp(S, "dve", sAB[:, t0i:t0i + gt, :], sf[:, 0:gt, :], [k_sf], [t_sAB], acc=True)
                for ti in bt:
                    for k2_ in range(2):
                        S.dma_fn("pool", lambda e, ti=ti, k2_=k2_: e.indirect_dma_start(
                            out=xbuf[:, :], out_offset=bass.IndirectOffsetOnAxis(ap=sAB[:, ti, k2_:k2_ + 1], axis=0),
                            in_=fx_all[:, ti, :], in_offset=None),
                            reads=[t_sAB, t_fx[ti], t_xz], writes=[C.T("xbuf")], acc=True)
        S.barrier()
        if STOP == 107:
            return
        with contextlib.ExitStack() as st:
            sb = lambda name, shape, dt: st.enter_context(nc.sbuf_tensor(f"e{l}_{name}", list(shape), dt))
            wgb = SbPool(C, st, 4, f"e{l}_wgb", [128, 8, 512], BF16)
            wdb = SbPool(C, st, 4, f"e{l}_wdb", [128, 2, 1024], BF16)
            xbp = SbPool(C, st, 3, f"e{l}_xb", [128, 1024], BF16)
            xTp = SbPool(C, st, 3, f"e{l}_xT", [128, 8, 128], BF16)
            sip = SbPool(C, st, 2, f"e{l}_si", [128, 256], F32)
            acp = SbPool(C, st, 3, f"e{l}_ac", [128, 256], BF16)
            aTp = SbPool(C, st, 2, f"e{l}_aT", [128, 2, 128], BF16)
            ybp = SbPool(C, st, 2, f"e{l}_yb", [128, 1024], F32)
            ptx = PsumPool(C, st, 2, f"e{l}_ptx", (128, 8, 128), BF16)
            ptc = PsumPool(C, st, 1, f"e{l}_ptc", (128, 2, 128), BF16)
            pgu = PsumPool(C, st, 2, f"e{l}_pgu")
            py = PsumPool(C, st, 2, f"e{l}_py")
            wgsrc = d["wgu_b"]
            wdsrc = d["wdn_b"]
            nblk = NB
            stA, stB = {}, {}

            def stage_a(b):
                wg, k_wg = wgb.get()
                S.dma_fn("pool", lambda e, wg=wg, b=b: e.indirect_dma_start(
                    out=wg[:].rearrange("p a b -> p (a b)"), out_offset=None, in_=wgsrc[:, :],
                    in_offset=bass.IndirectOffsetOnAxis(ap=widx[:, b:b + 1], axis=0)),
                    reads=[t_widx, C.T("wexp_b")], writes=[k_wg])
                wd, k_wd = wdb.get()
                S.dma_fn("pool", lambda e, wd=wd, b=b: e.indirect_dma_start(
                    out=wd[:].rearrange("p a b -> p (a b)"), out_offset=None, in_=wdsrc[:, :],
                    in_offset=bass.IndirectOffsetOnAxis(ap=widx[:, b:b + 1], axis=0)),
                    reads=[t_widx, C.T("wexp_b")], writes=[k_wd])
                xb, k_xb = xbp.get()
                S.dma("sp", xb[:], xbuf[b * 128:(b + 1) * 128, :], reads=[C.T("xbuf")], writes=[k_xb])
                tp, k_tp = ptx.get()
                for kc in range(8):
                    S.op("pe", lambda e, tp=tp, xb=xb, kc=kc: e.transpose(tp[:, kc, :], xb[:, kc * 128:(kc + 1) * 128], C.idb[:]),
                         reads=[k_xb, C.t_c], writes=[k_tp], acc=(kc > 0))
                xT, k_xT = xTp.get()
                cp(S, "act", xT[:], tp[:], [k_tp], [k_xT])
                stA[b] = (wg, k_wg, wd, k_wd, xT, k_xT)

            def stage_b(b):
                wg, k_wg, wd, k_wd, xT, k_xT = stA.pop(b)
                pg, k_pg = pgu.get()
                for kc in range(8):
                    mm(S, pg[:, :], xT[:, kc, :], wg[:, kc, :], kc == 0, kc == 7, [k_xT, k_wg], k_pg)
                si_, k_si = sip.get()
                act(S, si_[:], pg[:, 0:256], AF.Silu, [k_pg], [k_si])
                ac, k_ac = acp.get()
                tt(S, "dve", ac[:], pg[:, 256:512], si_[:], ALU.mult, [k_pg, k_si], [k_ac])
                stB[b] = (wd, k_wd, ac, k_ac)

            def stage_c(b):
                wd, k_wd, ac, k_ac = stB.pop(b)
                tp2, k_tp2 = ptc.get()
                for c in range(2):
                    S.op("pe", lambda e, tp2=tp2, ac=ac, c=c: e.transpose(tp2[:, c, :], ac[:, c * 128:(c + 1) * 128], C.idb[:]),
                         reads=[k_ac, C.t_c], writes=[k_tp2], acc=(c > 0))
                aT, k_aT = aTp.get()
                cp(S, "act", aT[:], tp2[:], [k_tp2], [k_aT])
                yb, k_yb = ybp.get()
                for hf in range(2):
                    pyt, k_py = py.get()
                    for c in range(2):
                        mm(S, pyt[:, :], aT[:, c, :], wd[:, c, hf * 512:(hf + 1) * 512], c == 0, c == 1, [k_aT, k_wd], k_py)
                    cp(S, "act", yb[:, hf * 512:(hf + 1) * 512], pyt[:, :], [k_py], [k_yb], acc=(hf > 0))
                S.dma("act", ybuf[b * 128:(b + 1) * 128, :], yb[:], reads=[k_yb], writes=[C.T("ybuf")], acc=True)

            for b in range(nblk + 2):
                if b < nblk:
                    stage_a(b)
                if 0 <= b - 1 < nblk:
                    stage_b(b - 1)
                if 0 <= b - 2 < nblk:
                    stage_c(b - 2)
            G2 = sb("G2", [128, 2, 1024], F32)
            fg = sb("fg", [128, 1024], F32)
            t_g2 = Tok()
            for r in range(2):
                S.dma("sp", G2[:, r, :], bcast_row(d["modv"][l][r:r + 1, 5120:6144], 1024), reads=[C.T(f"modv{l}")], writes=[t_g2], acc=True)
            if last:
                S.dma("sp", fg[:], bcast_row(d["final_g"][0:1, :], 1024), writes=[t_g2], acc=True)
            yAp = SbPool(C, st, 3, f"e{l}_yA", [128, 1024], F32)
            yBp = SbPool(C, st, 3, f"e{l}_yB", [128, 1024], F32)
            hp = SbPool(C, st, 4, f"e{l}_h", [128, 1024], F32)
            hnp = SbPool(C, st, 2, f"e{l}_hn", [128, 1024], F32)
            hop = SbPool(C, st, 2, f"e{l}_ho", [128, 1024], F32)
            ssp = SbPool(C, st, 2, f"e{l}_ss", [128, 4], F32)
            sqj = sb("sqj", [128, 1024], BF16)
            t_sqj = Tok()
            pre = {}

            def prefetch(ti):
                tile = tiles[ti]
                ys = []
                for k2, pool_ in enumerate((yAp, yBp)):
                    yt, k_yt = pool_.get()
                    S.dma_fn("pool", lambda e, yt=yt, ti=ti, k2=k2: e.indirect_dma_start(
                        out=yt[:], out_offset=None, in_=ybuf[:, :], in_offset=bass.IndirectOffsetOnAxis(ap=sAB[:, ti, k2:k2 + 1], axis=0)),
                        reads=[t_sAB, C.T("ybuf")], writes=[k_yt])
                    ys.append((yt, k_yt))
                ht, k_h = hp.get()
                S.dma("sp", ht[:], d["h_s"][tile * 128:(tile + 1) * 128, :], reads=[C.T(f"h:{tile}")], writes=[k_h])
                pre[ti] = (ys, ht, k_h)

            prefetch(0)
            for ti, tile in enumerate(tiles):
                r = 0 if tile >= 2 else 1
                if ti + 1 < len(tiles):
                    prefetch(ti + 1)
                ys, ht, k_h = pre.pop(ti)
                (yA, k_yA), (yB, k_yB) = ys
                ts(S, "dve", yA[:], yA[:], w12[:, ti, 0:1], None, ALU.mult, None, [k_yA, t_w12], [k_yA])
                stt(S, yB[:], yB[:], w12[:, ti, 1:2], yA[:], ALU.mult, ALU.add, [k_yB, k_yA, t_w12], [k_yB])
                tt(S, "dve", yB[:], yB[:], G2[:, r, :], ALU.mult, [k_yB, t_g2], [k_yB])
                hn, k_hn = hnp.get()
                tt(S, "dve", hn[:], yB[:], ht[:], ALU.add, [k_yB, k_h], [k_hn])
                if not last:
                    S.dma("act", d["h_s"][tile * 128:(tile + 1) * 128, :], hn[:], reads=[k_hn], writes=[C.T(f"h:{tile}")])
                else:
                    ss, k_ss = ssp.get()
                    act(S, sqj[:], hn[:], AF.Square, [k_hn], [t_sqj, k_ss], accum_out=ss[:, 0:1])
                    act(S, ss[:, 1:2], ss[:, 0:1], AF.Sqrt, [k_ss], [k_ss], acc=True, scale=1.0 / DM, bias=EPS)
                    S.op("dve", lambda e, ss=ss: e.reciprocal(out=ss[:, 2:3], in_=ss[:, 1:2]), reads=[k_ss], writes=[k_ss], acc=True)
                    ho, k_ho = hop.get()
                    stt(S, ho[:], hn[:], ss[:, 2:3], fg[:], ALU.mult, ALU.mult, [k_hn, k_ss, t_g2], [k_ho])
                    S.dma("act", d["out"][(tile - 2) * 128:(tile - 1) * 128, :], ho[:], reads=[k_ho], writes=[C.T(f"out:{tile}")])
    S.barrier()
```

```python
import numpy as np
import concourse.bass as bass
import concourse.mybir as mybir
from concourse.bass_utils import run_bass_kernel_spmd

F32 = mybir.dt.float32
BF16 = mybir.dt.bfloat16
I32 = mybir.dt.int32
U32 = mybir.dt.uint32
AF = mybir.ActivationFunctionType
ALU = mybir.AluOpType
AX = mybir.AxisListType

ENGS = ("pe", "act", "dve", "pool", "sp")
EPOCH = 12000
NSLOT = 24


class Tok:
    __slots__ = ("name", "writers", "readers")

    def __init__(self, name=""):
        self.name = name
        self.writers = []
        self.readers = []


class Op:
    __slots__ = ("eng", "fn", "deps", "idx", "needed", "cnt", "dma", "slot", "val", "waits")

    def __init__(self, eng, fn, dma=False):
        self.eng = eng
        self.fn = fn
        self.deps = []
        self.idx = -1
        self.needed = False
        self.cnt = -1
        self.dma = dma
        self.slot = -1
        self.val = -1
        self.waits = []


class Sched:
    def __init__(self, nc):
        self.nc = nc
        self.ops = {e: [] for e in ENGS}
        self.ndma = {e: 0 for e in ENGS}
        self.seen = {e: {} for e in ENGS}
        self.pending = {e: [] for e in ENGS}

    def barrier(self):
        waits = []
        for e in ENGS:
            last = None
            for o in reversed(self.ops[e]):
                if not o.dma:
                    last = o
                    break
            if last is not None:
                waits.append(("c", last))
            slots = {}
            for o in self.ops[e]:
                if o.dma:
                    slots[o.slot] = o
            for o in slots.values():
                waits.append(("d", o))
        for e in ENGS:
            self.pending[e] = list(waits)

    def _add(self, op, reads, writes, acc):
        deps = []
        for t in reads:
            deps.extend(t.writers)
        for t in writes:
            deps.extend(t.readers)
            if not acc:
                deps.extend(t.writers)
            else:
                deps.extend(w for w in t.writers if w.eng != op.eng or w.dma != op.dma)
        e = op.eng
        op.idx = len(self.ops[e])
        seen = self.seen[e]
        if self.pending[e]:
            for w in self.pending[e]:
                if w[0] == "c":
                    deps.append(w[1])
                else:
                    deps.append(w[1])
            self.pending[e] = []
            barrier_dep = True
        else:
            barrier_dep = False
        if op.dma:
            n = self.ndma[e]
            self.ndma[e] = n + 1
            op.slot = n % NSLOT
            op.val = 16 * (n // NSLOT + 1)
            if op.val > 16:
                key = ("d", e, op.slot)
                if seen.get(key, 0) < op.val - 16:
                    seen[key] = op.val - 16
                    op.waits.append(("d", e, op.slot, op.val - 16))
        for d in sorted(deps, key=lambda o: -o.idx):
            if d.dma:
                key = ("d", d.eng, d.slot)
                if seen.get(key, 0) >= d.val:
                    continue
                seen[key] = d.val
                op.waits.append(("d", d.eng, d.slot, d.val))
            else:
                if d.eng == e:
                    if e in ("pe", "sp"):
                        continue
                key = ("c", d.eng)
                if seen.get(key, -1) >= d.idx:
                    continue
                seen[key] = d.idx
                d.needed = True
                op.waits.append(("c", d))
        for t in reads:
            t.readers.append(op)
        for t in writes:
            if acc:
                t.writers.append(op)
            else:
                t.writers = [op]
                t.readers = []
        self.ops[e].append(op)
        return op

    def op(self, eng, fn, reads=(), writes=(), acc=False):
        return self._add(Op(eng, fn), list(reads), list(writes), acc)

    def dma(self, eng, out, in_, reads=(), writes=(), acc=False, **kw):
        def fn(en):
            return en.dma_start(out=out, in_=in_, **kw)
        return self._add(Op(eng, fn, dma=True), list(reads), list(writes), acc)

    def dma_fn(self, eng, fn, reads=(), writes=(), acc=False):
        return self._add(Op(eng, fn, dma=True), list(reads), list(writes), acc)

    def emit(self):
        nc = self.nc
        nep = {}
        for e in ENGS:
            c = 0
            for o in self.ops[e]:
                if o.needed and not o.dma:
                    c += 1
                    o.cnt = c
            nep[e] = max(1, (c + EPOCH - 1) // EPOCH)
        import contextlib
        with contextlib.ExitStack() as st:
            csem = {e: [st.enter_context(nc.semaphore(f"c_{e}_{i}")) for i in range(nep[e])]
                    for e in ENGS}
            dsem = {e: [st.enter_context(nc.semaphore(f"d_{e}_{i}")) for i in range(NSLOT)]
                    for e in ENGS if self.ndma[e] > 0}
            block = st.enter_context(nc.Block())
            hw = {"pe": block.tensor, "act": block.scalar, "dve": block.vector,
                  "pool": block.gpsimd, "sp": block.sync}

            def body(e):
                def run(en):
                    for o in self.ops[e]:
                        for w in o.waits:
                            if w[0] == "d":
                                en.wait_ge(dsem[w[1]][w[2]], w[3])
                            else:
                                d = w[1]
                                en.wait_ge(csem[d.eng][(d.cnt - 1) // EPOCH], (d.cnt - 1) % EPOCH + 1)
                        ins = o.fn(en)
                        if o.dma:
                            ins.then_inc(dsem[e][o.slot], 16)
                        elif o.needed:
                            ins.then_inc(csem[e][(o.cnt - 1) // EPOCH], 1)
                    if self.ndma[e] > 0:
                        last = {}
                        for o in self.ops[e]:
                            if o.dma:
                                last[o.slot] = o.val
                        for s, v in last.items():
                            en.wait_ge(dsem[e][s], v)
                return run

            for e in ENGS:
                if self.ops[e]:
                    hw[e](body(e))


DM = 1024
SEQ = 4096
LC = 256
NTOK = SEQ + LC
NT = NTOK // 128
DEPTH = 2
EPS = 1e-6
STOP = 0
QT_LIMIT = 0


class Ctx:
    def __init__(self, nc, S):
        self.nc = nc
        self.S = S
        self.d = {}
        self.tok = {}

    def T(self, name):
        t = self.tok.get(name)
        if t is None:
            t = Tok(name)
            self.tok[name] = t
        return t


def phase_mod(C, l):
    nc, S = C.nc, C.S
    wmod = C.d["w_mod"][l].rearrange("(kc p) n -> p kc n", p=128)
    with nc.sbuf_tensor(f"m_ct{l}", [128, 8, 2], F32) as ct, \
            nc.sbuf_tensor(f"m_sct{l}", [128, 8, 2], F32) as sct, \
            nc.sbuf_tensor(f"m_w0{l}", [128, 8, 512], F32) as w0, \
            nc.sbuf_tensor(f"m_w1{l}", [128, 8, 512], F32) as w1, \
            nc.sbuf_tensor(f"m_b{l}", [2, 6144], F32) as bm, \
            nc.sbuf_tensor(f"m_row{l}", [2, 6144], F32) as mrow, \
            nc.psum_tensor(f"m_p0{l}", [2, 512], F32) as p0, \
            nc.psum_tensor(f"m_p1{l}", [2, 512], F32) as p1:
        t_ct, t_sct, t_bm, t_row = Tok(), Tok(), Tok(), Tok()
        t_w = [Tok(), Tok()]
        t_p = [Tok(), Tok()]
        wb = [w0, w1]
        pb = [p0, p1]
        S.dma("sp", ct[:], C.d["cT"][:], writes=[t_ct])
        S.dma("sp", bm[:], C.d["b_mod"][l:l + 1, :].to_broadcast([2, 6144]), writes=[t_bm])
        S.op("act", lambda e: e.activation(out=sct[:], in_=ct[:], func=AF.Silu),
             reads=[t_ct], writes=[t_sct])
        for n in range(12):
            S.dma("sp", wb[n % 2][:], wmod[:, :, n * 512:(n + 1) * 512], writes=[t_w[n % 2]])
            for kc in range(8):
                S.op("pe", lambda e, n=n, kc=kc: e.matmul(pb[n % 2][:], lhsT=sct[:, kc, :],
                                                       rhs=wb[n % 2][:, kc, :],
                                                       start=(kc == 0), stop=(kc == 7)),
                     reads=[t_sct, t_w[n % 2]], writes=[t_p[n % 2]], acc=(kc > 0))
            S.op("dve", lambda e, n=n: e.tensor_tensor(out=mrow[:, n * 512:(n + 1) * 512],
                                                      in0=pb[n % 2][:],
                                                      in1=bm[:, n * 512:(n + 1) * 512], op=ALU.add),
                 reads=[t_p[n % 2], t_bm], writes=[t_row], acc=True)
        S.dma("sp", C.d["modv"][l], mrow[:], reads=[t_row], writes=[C.T(f"modv{l}")])
    S.barrier()


class PsumPool:
    def __init__(self, C, st, n, name, shape=(128, 512), dtype=F32):
        self.tiles = [st.enter_context(C.nc.psum_tensor(f"{name}{i}", list(shape), dtype)) for i in range(n)]
        self.toks = [Tok(f"{name}{i}") for i in range(n)]
        self.i = 0

    def get(self):
        i = self.i
        self.i = (i + 1) % len(self.tiles)
        return self.tiles[i], self.toks[i]


class SbPool:
    def __init__(self, C, st, n, name, shape, dtype):
        self.tiles = [st.enter_context(C.nc.sbuf_tensor(f"{name}{i}", list(shape), dtype)) for i in range(n)]
        self.toks = [Tok(f"{name}{i}") for i in range(n)]
        self.i = 0

    def get(self):
        i = self.i
        self.i = (i + 1) % len(self.tiles)
        return self.tiles[i], self.toks[i]


def mm(S, out, lhsT, rhs, first, last, reads, wtok):
    S.op("pe", lambda e: e.matmul(out, lhsT=lhsT, rhs=rhs, start=first, stop=last),
         reads=reads, writes=[wtok], acc=not first)


def act(S, out, in_, func, reads, writes, acc=False, **kw):
    S.op("act", lambda e: e.activation(out=out, in_=in_, func=func, **kw), reads=reads, writes=writes, acc=acc)


def tt(S, eng, out, in0, in1, op, reads, writes, acc=False):
    S.op(eng, lambda e: e.tensor_tensor(out=out, in0=in0, in1=in1, op=op), reads=reads, writes=writes, acc=acc)


def ts(S, eng, out, in0, s1, s2, op0, op1, reads, writes, acc=False):
    if op1 is None:
        S.op(eng, lambda e: e.tensor_scalar(out=out, in0=in0, scalar1=s1, scalar2=None, op0=op0),
             reads=reads, writes=writes, acc=acc)
    else:
        S.op(eng, lambda e: e.tensor_scalar(out=out, in0=in0, scalar1=s1, scalar2=s2, op0=op0, op1=op1),
             reads=reads, writes=writes, acc=acc)


def stt(S, out, in0, scalar, in1, op0, op1, reads, writes, acc=False):
    S.op("dve", lambda e: e.scalar_tensor_tensor(out=out, in0=in0, scalar=scalar, in1=in1, op0=op0, op1=op1),
         reads=reads, writes=writes, acc=acc)


def cp(S, eng, out, in_, reads, writes, acc=False):
    if eng == "act":
        S.op("act", lambda e: e.copy(out=out, in_=in_), reads=reads, writes=writes, acc=acc)
    else:
        S.op(eng, lambda e: e.tensor_copy(out=out, in_=in_), reads=reads, writes=writes, acc=acc)


def make_ident(C, st):
    nc, S = C.nc, C.S
    idi = st.enter_context(nc.sbuf_tensor("c_idi", [128, 128], I32))
    idb = st.enter_context(nc.sbuf_tensor("c_idb", [128, 128], BF16))
    idf = st.enter_context(nc.sbuf_tensor("c_idf", [128, 128], F32))
    onb = st.enter_context(nc.sbuf_tensor("c_onb", [128, 128], BF16))
    t_i, t_c = Tok(), Tok("consts")
    S.op("pool", lambda e: e.iota(idi[:], pattern=[[1, 128]], base=0, channel_multiplier=-1), writes=[t_i])
    ts(S, "dve", idb[:], idi[:], 0, None, ALU.is_equal, None, [t_i], [t_c], acc=True)
    ts(S, "dve", idf[:], idi[:], 0, None, ALU.is_equal, None, [t_i], [t_c], acc=True)
    S.op("dve", lambda e: e.memset(onb[:], 1.0), writes=[t_c], acc=True)
    utri = st.enter_context(nc.sbuf_tensor("c_utri", [128, 128], BF16))
    ts(S, "dve", utri[:], idi[:], 0, None, ALU.is_gt, None, [t_i], [t_c], acc=True)
    C.utri = utri
    C.idb, C.idf, C.onb, C.t_c = idb, idf, onb, t_c


O_QA, O_QAS, O_KA, O_KAS, O_KR, O_KRS, O_CQ, O_CKV, O_U, O_VA, O_G, W_EXT = (
    0, 512, 1024, 1152, 1280, 1312, 1344, 1600, 1856, 2368, 2496, 5568)
W_N = O_G
GROUPS = [(0, 256)] + [(256 + 512 * i, 512) for i in range(8)]


def bcast_row(ap_row, n):
    return ap_row.to_broadcast([128, n])


def load_cast(C, st, name, dst, src3, nk, ncol, t_dst, scale_ap=None, scale_tok=None, half=2784):
    S = C.S
    stg = SbPool(C, st, 2, name, [128, half], F32)
    for kc in range(nk):
        for c0 in range(0, ncol, half):
            w = min(half, ncol - c0)
            s_t, s_k = stg.get()
            S.dma("sp", s_t[:, 0:w], src3[:, kc, c0:c0 + w], writes=[s_k])
            if scale_ap is None:
                cp(S, "act", dst[:, kc, c0:c0 + w], s_t[:, 0:w], [s_k], [t_dst], acc=True)
            else:
                ts(S, "dve", dst[:, kc, c0:c0 + w], s_t[:, 0:w], scale_ap(kc), None, ALU.mult, None,
                   [s_k, scale_tok], [t_dst], acc=True)


def phase_inproj(C, l):
    nc, S = C.nc, C.S
    import contextlib
    d = C.d
    hsrc = d["h0"] if l == 0 else d["h_s"]
    with contextlib.ExitStack() as st:
        sb = lambda name, shape, dt: st.enter_context(nc.sbuf_tensor(f"n{l}_{name}", list(shape), dt))
        wsb = sb("w", [128, 8, W_N], BF16)
        wq = sb("wq", [128, 2, 1024], BF16)
        wkv = sb("wkv", [128, 2, 1024], BF16)
        gq = sb("gq", [128, 4], F32)
        Gm, Sm = sb("Gm", [128, 2, 1024], F32), sb("Sm", [128, 2, 1024], F32)
        t_w, t_wq, t_gq, t_mod = Tok(), Tok(), Tok(), Tok()
        wext = d["w_in_ext"][l].rearrange("(kc p) n -> p kc n", p=128)
        load_cast(C, st, f"n{l}_stg", wsb, wext, 8, W_N, t_w)
        S.dma("sp", gq[:, 0:2], d["q_norm_gT"][l], writes=[t_gq], acc=True)
        S.dma("sp", gq[:, 2:4], d["kv_norm_gT"][l], writes=[t_gq], acc=True)
        for wi, (wname, wdst) in enumerate((("w_uq_ext", wq), ("w_ukv_ext", wkv))):
            wsrc = d[wname][l].rearrange("(kc p) n -> p kc n", p=128)
            load_cast(C, st, f"n{l}_stg{wi}", wdst, wsrc, 2, 1024, t_wq,
                      scale_ap=lambda kc, wi=wi: gq[:, 2 * wi + kc:2 * wi + kc + 1], scale_tok=t_gq, half=1024)
        mv = d["modv"][l]
        tmpp = SbPool(C, st, 2, f"n{l}_tmp", [128, 1024], F32)
        gn, k_gn = tmpp.get()
        S.dma("sp", gn[:], bcast_row(d["norm_mix_g"][l:l + 1, :], 1024), writes=[k_gn])
        for r in range(2):
            S.dma("sp", Sm[:, r, :], bcast_row(mv[r:r + 1, 0:1024], 1024), reads=[C.T(f"modv{l}")], writes=[t_mod], acc=True)
            S.dma("sp", Gm[:, r, :], bcast_row(mv[r:r + 1, 1024:2048], 1024), reads=[C.T(f"modv{l}")], writes=[t_mod], acc=True)
        t_mod2 = Tok()
        for r in range(2):
            stt(S, Gm[:, r, :], Gm[:, r, :], 1.0, gn[:], ALU.add, ALU.mult, [t_mod, k_gn], [t_mod2], acc=True)
        if STOP == 1:
            return
        pp = PsumPool(C, st, 5, f"n{l}_pp")
        ptp = PsumPool(C, st, 2, f"n{l}_ptp", (128, 8, 128), BF16)
        hp = SbPool(C, st, 2, f"n{l}_h", [128, 1024], F32)
        hxp = SbPool(C, st, 2, f"n{l}_hx", [128, 1024], BF16)
        hxTp = SbPool(C, st, 2, f"n{l}_hxT", [128, 8, 512], BF16)
        ssp = SbPool(C, st, 4, f"n{l}_ss", [128, 4], F32)
        sqj = sb("sqj", [128, 1024], BF16)
        t_sqj = Tok()
        ropep = SbPool(C, st, 1, f"n{l}_rope", [128, 4, 512], F32)
        r1p = SbPool(C, st, 1, f"n{l}_r1", [128, 512], F32)
        r2p = SbPool(C, st, 1, f"n{l}_r2", [128, 512], F32)
        o4p = SbPool(C, st, 4, f"n{l}_o4", [128, 4, 512], BF16)
        krTp = SbPool(C, st, 1, f"n{l}_krT", [32, 512], BF16)
        latfp = SbPool(C, st, 1, f"n{l}_latf", [128, 2, 512], F32)
        latqp = SbPool(C, st, 1, f"n{l}_latq", [128, 2, 512], BF16)
        rbcp = SbPool(C, st, 1, f"n{l}_rbc", [128, 512], F32)
        cqnp = SbPool(C, st, 1, f"n{l}_cqn", [128, 2, 512], BF16)
        ckvnp = SbPool(C, st, 1, f"n{l}_ckvn", [128, 2, 512], BF16)
        vap = SbPool(C, st, 1, f"n{l}_va", [128, 4, 2, 65], BF16)
        vbp = SbPool(C, st, 1, f"n{l}_vb", [128, 4, 8, 65], BF16)
        for pool_ in (vap, vbp):
            for t_, k_ in zip(pool_.tiles, pool_.toks):
                S.op("pool", lambda e, t_=t_: e.memset(t_[:], 1.0), writes=[k_])
        t_all = [t_mod2, C.t_c]

        for g, (t0, n) in enumerate(GROUPS):
            ntl = n // 128
            isx = g > 0
            mr = 0 if isx else 1
            hxT, k_hxT = hxTp.get()
            if isx:
                rp, k_rp = ropep.get()
                p0 = t0 - 256
                for i, nm in enumerate(("ropeA_c", "ropeA_s", "ropeB_c", "ropeB_s")):
                    S.dma("sp", rp[:, i, :], d[nm][:, p0:p0 + 512], writes=[k_rp], acc=(i > 0))
            for ti in range(ntl):
                r0 = t0 + ti * 128
                ht, k_h = hp.get()
                S.dma("sp", ht[:], hsrc[r0:r0 + 128, :], reads=[C.T(f"h:{r0 // 128}")], writes=[k_h])
                ss, k_ss = ssp.get()
                act(S, sqj[:], ht[:], AF.Square, [k_h], [t_sqj, k_ss], accum_out=ss[:, 0:1])
                act(S, ss[:, 1:2], ss[:, 0:1], AF.Sqrt, [k_ss], [k_ss], acc=True, scale=1.0 / DM, bias=EPS)
                S.op("dve", lambda e, ss=ss: e.reciprocal(out=ss[:, 2:3], in_=ss[:, 1:2]), reads=[k_ss], writes=[k_ss], acc=True)
                tm, k_tm = tmpp.get()
                stt(S, tm[:], ht[:], ss[:, 2:3], Gm[:, mr, :], ALU.mult, ALU.mult, [k_h, k_ss] + t_all, [k_tm])
                hx, k_hx = hxp.get()
                tt(S, "dve", hx[:], tm[:], Sm[:, mr, :], ALU.add, [k_tm] + t_all, [k_hx])
                tp, k_tp = ptp.get()
                for kc in range(8):
                    S.op("pe", lambda e, tp=tp, hx=hx, kc=kc: e.transpose(tp[:, kc, :], hx[:, kc * 128:(kc + 1) * 128], C.idb[:]),
                         reads=[k_hx, C.t_c], writes=[k_tp], acc=(kc > 0))
                cp(S, "act", hxT[:, :, ti * 128:(ti + 1) * 128], tp[:], [k_tp], [k_hxT], acc=(ti > 0))
            if STOP == 2:
                continue
            S.dma("pool", d["hxT_s"].rearrange("c p t -> p c t")[:, :, t0:t0 + n], hxT[:, :, 0:n],
                  reads=[k_hxT], writes=[C.T(f"hxT:{g}")])
            if STOP == 3:
                continue

            def proj(col0, m, ps):
                pt, pk = ps
                for kc in range(8):
                    mm(S, pt[0:m, 0:n], wsb[:, kc, col0:col0 + m], hxT[:, kc, 0:n], kc == 0, kc == 7,
                       [t_w, k_hxT], pk)

            def rope_evac(prj, col0, cols0, m, out_ap, out_k, ri, acc):
                ps = pp.get()
                prj(col0, m, ps)
                if not isx:
                    cp(S, "act", out_ap, ps[0][0:m, 0:n], [ps[1]], [out_k], acc=acc)
                    return
                ps2 = pp.get()
                prj(cols0, m, ps2)
                r1, k1 = r1p.get()
                r2, k2 = r2p.get()
                tt(S, "dve", r1[0:m, 0:n], ps[0][0:m, 0:n], rp[0:m, ri, 0:n], ALU.mult, [ps[1], k_rp], [k1])
                tt(S, "dve", r2[0:m, 0:n], ps2[0][0:m, 0:n], rp[0:m, ri + 1, 0:n], ALU.mult, [ps2[1], k_rp], [k2])
                tt(S, "dve", out_ap, r1[0:m, 0:n], r2[0:m, 0:n], ALU.add, [k1, k2], [out_k], acc=acc)

            qaT, k_qaT = o4p.get()
            for c in range(4):
                rope_evac(proj, O_QA + c * 128, O_QAS + c * 128, 128, qaT[:, c, 0:n], k_qaT, 0, c > 0)
            S.dma("pool", d["qaT_s"].rearrange("c p t -> p c t")[:, :, t0:t0 + n], qaT[:, :, 0:n],
                  reads=[k_qaT], writes=[C.T(f"qaT:{g}")])
            kaT, k_kaT = o4p.get()
            rope_evac(proj, O_KA, O_KAS, 128, kaT[:, 0, 0:n], k_kaT, 0, False)
            S.dma("pool", d["kaT_s"][:, t0:t0 + n], kaT[:, 0, 0:n], reads=[k_kaT], writes=[C.T(f"kaT:{g}")])
            krT, k_krT = krTp.get()
            rope_evac(proj, O_KR, O_KRS, 32, krT[:, 0:n], k_krT, 2, False)
            for h in range(8):
                S.dma("pool", d["kbT_s"][h, 64:96, t0:t0 + n], krT[:, 0:n], reads=[k_krT],
                      writes=[C.T(f"kbT:{g}")], acc=True)
            if STOP == 4:
                continue
            uT, k_uT = o4p.get()
            for c in range(4):
                ps = pp.get()
                proj(O_U + c * 128, 128, ps)
                cp(S, "act", uT[:, c, 0:n], ps[0][:, 0:n], [ps[1]], [k_uT], acc=(c > 0))
            S.dma("pool", d["uT_s"].rearrange("c p t -> p c t")[:, :, t0:t0 + n], uT[:, :, 0:n],
                  reads=[k_uT], writes=[C.T(f"uT:{g}")])
            va, k_va = vap.get()
            for ti in range(ntl):
                pt, pk = pp.get()
                for kc in range(8):
                    mm(S, pt[:, 0:128], hxT[:, kc, ti * 128:(ti + 1) * 128], wsb[:, kc, O_VA:O_VA + 128],
                       kc == 0, kc == 7, [t_w, k_hxT], pk)
                cp(S, "act", va[:, ti, :, 0:64], pt[:, 0:128].rearrange("p (g e) -> p g e", g=2), [pk], [k_va], acc=(ti > 0))
            S.dma("pool", d["va_s"][t0:t0 + n, :].rearrange("(i p) f -> p i f", p=128),
                  va[:, 0:ntl, :, :].rearrange("p i g e -> p i (g e)"), reads=[k_va], writes=[C.T(f"va:{g}")])
            if STOP == 5:
                continue
            lat_out = []
            for (col0, npool) in ((O_CQ, cqnp), (O_CKV, ckvnp)):
                lf, k_lf = latfp.get()
                lq, k_lq = latqp.get()
                for c in range(2):
                    ps = pp.get()
                    proj(col0 + c * 128, 128, ps)
                    act(S, lq[:, c, 0:n], ps[0][:, 0:n], AF.Square, [ps[1]], [k_lq], acc=(c > 0))
                    cp(S, "act", lf[:, c, 0:n], ps[0][:, 0:n], [ps[1]], [k_lf], acc=(c > 0))
                if STOP == 61:
                    continue
                pt, pk = pp.get()
                for c in range(2):
                    mm(S, pt[:, 0:n], C.onb[:], lq[:, c, 0:n], c == 0, c == 1, [k_lq, C.t_c], pk)
                rb, k_rb = rbcp.get()
                if STOP == 62:
                    continue
                act(S, rb[:, 0:n], pt[:, 0:n], AF.Sqrt, [pk], [k_rb], scale=1.0 / 256, bias=EPS)
                if STOP == 63:
                    continue
                S.op("dve", lambda e, rb=rb, n=n: e.reciprocal(out=rb[:, 0:n], in_=rb[:, 0:n]), reads=[k_rb], writes=[k_rb], acc=True)
                if STOP == 64:
                    continue
                ln, k_ln = npool.get()
                for c in range(2):
                    tt(S, "dve", ln[:, c, 0:n], lf[:, c, 0:n], rb[:, 0:n], ALU.mult, [k_lf, k_rb], [k_ln], acc=(c > 0))
                lat_out.append((ln, k_ln))
            if STOP > 60:
                continue
            (cqn, k_cqn), (ckvn, k_ckvn) = lat_out
            if STOP == 6:
                continue

            def mkproj2(wt, lat, k_lat):
                def proj2(col0, m, ps):
                    pt, pk = ps
                    for kc in range(2):
                        mm(S, pt[0:m, 0:n], wt[:, kc, col0:col0 + m], lat[:, kc, 0:n], kc == 0, kc == 1, [t_wq, k_lat], pk)
                return proj2
            pq = mkproj2(wq, cqn, k_cqn)
            pk_ = mkproj2(wkv, ckvn, k_ckvn)
            qnT, k_qnT = o4p.get()
            for c in range(4):
                ps = pp.get()
                pq(c * 128, 128, ps)
                cp(S, "act", qnT[:, c, 0:n], ps[0][:, 0:n], [ps[1]], [k_qnT], acc=(c > 0))
            for h in range(8):
                S.dma("pool", d["qbT_s"][h, 0:64, t0:t0 + n], qnT[(h % 2) * 64:(h % 2) * 64 + 64, h // 2, 0:n],
                      reads=[k_qnT], writes=[C.T(f"qbT:{g}")], acc=True)
            if STOP == 7:
                continue
            qrT, k_qrT = o4p.get()
            for c in range(2):
                rope_evac(pq, 512 + c * 128, 768 + c * 128, 128, qrT[:, c, 0:n], k_qrT, 2, c > 0)
            for h in range(8):
                S.dma("pool", d["qbT_s"][h, 64:96, t0:t0 + n], qrT[(h % 4) * 32:(h % 4) * 32 + 32, h // 4, 0:n],
                      reads=[k_qrT], writes=[C.T(f"qbT:{g}")], acc=True)
            if STOP == 8:
                continue
            knT, k_knT = o4p.get()
            for c in range(4):
                ps = pp.get()
                pk_(c * 128, 128, ps)
                cp(S, "act", knT[:, c, 0:n], ps[0][:, 0:n], [ps[1]], [k_knT], acc=(c > 0))
            for h in range(8):
                S.dma("pool", d["kbT_s"][h, 0:64, t0:t0 + n], knT[(h % 2) * 64:(h % 2) * 64 + 64, h // 2, 0:n],
                      reads=[k_knT], writes=[C.T(f"kbT:{g}")], acc=True)
            if STOP == 9:
                continue
            vb, k_vb = vbp.get()
            for ti in range(ntl):
                pt, pk = pp.get()
                for kc in range(2):
                    mm(S, pt[:, :], ckvn[:, kc, ti * 128:(ti + 1) * 128], wkv[:, kc, 512:1024], kc == 0, kc == 1,
                       [t_wq, k_ckvn], pk)
                cp(S, "act", vb[:, ti, :, 0:64], pt[:, :].rearrange("p (g e) -> p g e", g=8), [pk], [k_vb], acc=(ti > 0))
            S.dma("pool", d["vb_s"][t0:t0 + n, :].rearrange("(i p) f -> p i f", p=128),
                  vb[:, 0:ntl, :, :].rearrange("p i g e -> p i (g e)"), reads=[k_vb], writes=[C.T(f"vb:{g}")])
    S.barrier()


def _deint(n):
    return np.concatenate([np.arange(0, n, 2), np.arange(1, n, 2)])


def _deint_sw(n):
    return np.concatenate([np.arange(1, n, 2), np.arange(0, n, 2)])


def _w_in_cols():
    cols = []
    for sw in (False, True):
        for c in range(4):
            for h in (c, 4 + c):
                cols.append(h * 64 + (_deint_sw(64) if sw else _deint(64)))
    for sw in (False, True):
        for g in range(2):
            cols.append(512 + g * 64 + (_deint_sw(64) if sw else _deint(64)))
    cols.append(1280 + _deint(32))
    cols.append(1280 + _deint_sw(32))
    cols.append(np.arange(768, 1280))
    cols.append(np.arange(1312, 1824))
    cols.append(np.arange(640, 768))
    cols.append(np.arange(1824, 4896))
    out = np.concatenate(cols)
    assert out.shape[0] == W_EXT
    return out


def _w_uq_cols():
    cols = [h * 96 + np.arange(64) for h in range(8)]
    cols += [h * 96 + 64 + _deint(32) for h in range(8)]
    cols += [h * 96 + 64 + _deint_sw(32) for h in range(8)]
    return np.concatenate(cols)


def _w_ukv_cols():
    cols = [h * 128 + np.arange(64) for h in range(8)]
    cols += [h * 128 + 64 + np.arange(64) for h in range(8)]
    return np.concatenate(cols)


def _rope_tables():
    t = np.arange(SEQ)
    row = (t // 64).astype(np.float32)
    col = (t % 64).astype(np.float32)

    def tabs(rot):
        axis_dim = rot // 2
        inv = (np.float32(10000.0) ** (-np.arange(0, axis_dim, 2, dtype=np.float32) / np.float32(axis_dim))).astype(np.float32)
        ang = np.concatenate([row[:, None] * inv, col[:, None] * inv], axis=-1).astype(np.float32)
        cos, sin = np.cos(ang).astype(np.float32), np.sin(ang).astype(np.float32)
        half = rot // 2
        ct = np.concatenate([cos, cos], axis=1).T
        st_ = np.concatenate([-sin, sin], axis=1).T
        rep = 128 // rot
        return np.ascontiguousarray(np.tile(ct, (rep, 1))), np.ascontiguousarray(np.tile(st_, (rep, 1)))
    ca, sa = tabs(64)
    cb, sb_ = tabs(32)
    return ca, sa, cb, sb_


SCRATCH = {
    "modv": ([2, 2, 6144], F32),
    "h_s": ([NTOK, DM], F32),
    "hxT_s": ([8, 128, NTOK], BF16),
    "qaT_s": ([4, 128, NTOK], BF16),
    "kaT_s": ([128, NTOK], BF16),
    "va_s": ([NTOK, 130], BF16),
    "qbT_s": ([8, 96, NTOK], BF16),
    "kbT_s": ([8, 96, NTOK], BF16),
    "vb_s": ([NTOK, 520], BF16),
    "uT_s": ([4, 128, NTOK], BF16),
    "oaT_s": ([4, 128, NTOK], BF16),
    "obT_s": ([4, 128, NTOK], BF16),
    "ocT_s": ([4, 128, NTOK], BF16),
    "gT_s": ([32, NTOK], F32),
    "bc_s": ([8, 512], F32),
    "wgu_b": ([4096, 4096], BF16),
    "wdn_b": ([4096, 2048], BF16),
    "xbuf": ([12800, DM], BF16),
    "ybuf": ([12800, DM], F32),
}
INPUTS = {
    "h0": ([NTOK, DM], F32),
    "cT": ([128, 8, 2], F32),
    "w_mod": ([2, DM, 6144], F32),
    "b_mod": ([2, 6144], F32),
    "norm_mix_g": ([2, DM], F32),
    "norm_ffn_g": ([2, DM], F32),
    "w_in_ext": ([2, DM, W_EXT], F32),
    "w_uq_ext": ([2, 256, 1024], F32),
    "w_ukv_ext": ([2, 256, 1024], F32),
    "q_norm_gT": ([2, 128, 2], F32),
    "kv_norm_gT": ([2, 128, 2], F32),
    "ropeA_c": ([128, SEQ], F32),
    "ropeA_s": ([128, SEQ], F32),
    "ropeB_c": ([128, SEQ], F32),
    "ropeB_s": ([128, SEQ], F32),
    "sink": ([2, 8], F32),
    "sink_row": ([2, 2, 512], F32),
    "w_pool": ([2, 4, 128, 128], F32),
    "pool_scaleT": ([2, 128, 4], F32),
    "invx": ([4, SEQ], F32),
    "invc": ([4, LC], F32),
    "w_br": ([2, 3, 512, DM], F32),
    "w_out": ([2, DM, DM], F32),
    "w_r": ([2, DM, 36], F32),
    "b_r": ([2, 36], F32),
    "w_gu_h": ([2, 32 * 128, 4096], F32),
    "w_dn_h": ([2, 32 * 128, 2048], F32),
    "final_g": ([1, DM], F32),
}


def prep_shared(inp):
    out = {}
    f = lambda a: np.ascontiguousarray(np.asarray(a, dtype=np.float32))
    out["w_mod"] = f(inp["w_mod"])
    out["b_mod"] = f(inp["b_mod"])
    out["norm_mix_g"] = f(inp["norm_mix_g"])
    out["norm_ffn_g"] = f(inp["norm_ffn_g"])
    out["sink"] = f(inp["sink"])
    out["sink_row"] = f(np.repeat(np.asarray(inp["sink"]).reshape(2, 2, 4), 128, axis=-1))
    out["w_pool"] = f(inp["w_pool"])
    out["pool_scaleT"] = f(np.asarray(inp["pool_scale"]).reshape(2, 4, 128).transpose(0, 2, 1))
    out["w_br"] = f(np.stack([np.asarray(inp[k]) for k in ("w_br_a", "w_br_b", "w_br_c")], axis=1))
    out["w_out"] = f(inp["w_out"])
    out["w_r"] = f(np.concatenate([np.asarray(inp["w_rg"]), np.asarray(inp["w_re"])], axis=-1))
    out["b_r"] = f(np.concatenate([np.asarray(inp["b_rg"]), np.asarray(inp["b_re"])], axis=-1))
    out["w_gu_h"] = f(np.asarray(inp["w_gu"]).reshape(2, 32, 8, 128, 512).transpose(0, 1, 3, 2, 4).reshape(2, 32 * 128, 4096))
    out["w_dn_h"] = f(np.asarray(inp["w_dn"]).reshape(2, 32, 2, 128, 1024).transpose(0, 1, 3, 2, 4).reshape(2, 32 * 128, 2048))
    out["final_g"] = f(np.asarray(inp["final_g"]).reshape(1, DM))
    for nm, L in (("invx", SEQ), ("invc", LC)):
        t = np.arange(L)
        rows = []
        for r in (1, 2, 4, 8):
            cnt = np.minimum(t + r + 1, L) - np.maximum(t - r, 0)
            rows.append((1.0 / cnt.astype(np.float32)).astype(np.float32))
        out[nm] = np.stack(rows)
    out["w_in_ext"] = f(np.asarray(inp["w_in"])[:, :, _w_in_cols()])
    out["w_uq_ext"] = f(np.asarray(inp["w_uq"])[:, :, _w_uq_cols()])
    out["w_ukv_ext"] = f(np.asarray(inp["w_ukv"])[:, :, _w_ukv_cols()])
    out["q_norm_gT"] = f(np.asarray(inp["q_norm_g"]).reshape(2, 2, 128).transpose(0, 2, 1))
    out["kv_norm_gT"] = f(np.asarray(inp["kv_norm_g"]).reshape(2, 2, 128).transpose(0, 2, 1))
    ca, sa, cb, sb_ = _rope_tables()
    out["ropeA_c"], out["ropeA_s"], out["ropeB_c"], out["ropeB_s"] = ca, sa, cb, sb_
    return out


def prep_core(inp, b):
    out = {}
    x, ctx, c, cc = (np.asarray(inp[k], dtype=np.float32) for k in ("x", "ctx", "c", "c_ctx"))
    out["h0"] = np.ascontiguousarray(np.concatenate([ctx[b], x[b]], axis=0))
    out["cT"] = np.ascontiguousarray(np.stack([c[b].reshape(8, 128).T, cc.reshape(8, 128).T], axis=-1))
    return out


def build_program(phases, debug_out=()):
    nc = bass.Bass("TRN2", target_bir_lowering=False)
    S = Sched(nc)
    C = Ctx(nc, S)
    for k, (shape, dt) in INPUTS.items():
        C.d[k] = nc.dram_tensor(k, list(shape), dt, kind="ExternalInput").ap()
    for k, (shape, dt) in SCRATCH.items():
        kind = "ExternalOutput" if k in debug_out else "Internal"
        C.d[k] = nc.dram_tensor(k, list(shape), dt, kind=kind).ap()
    import contextlib
    with contextlib.ExitStack() as st:
        make_ident(C, st)
        phases(C)
        S.emit()
    return nc


NEG = -30000.0


def attn_pipeline(C, st, name, items, scale, hook=None, hook_every=1, LA=2):
    S = C.S
    pps = PsumPool(C, st, LA + 1, f"{name}_ps", (128, 2, 512), F32)
    pac = PsumPool(C, st, 2, f"{name}_ac", (128, 512), F32)
    ptp = SbPool(C, st, LA + 2, f"{name}_pt", [128, 2, 512], BF16)
    steps = []
    for it in items:
        ks = it["keys"]
        for a in range(0, len(ks), 2):
            steps.append((it, a, ks[a:a + 2]))
    state = {}

    def emit_S(si):
        it, a, chunk = steps[si]
        n = it["n"]
        if "q" not in it:
            it["load_q"](it)
        ps, k_ps = pps.get()
        for jj, (k_ap, v_ap, msk) in enumerate(chunk):
            mm(S, ps[:, jj, 0:n], k_ap, it["q"], True, msk is None, it["kv_reads"] + it["q_reads"], k_ps)
            if msk is not None:
                mm(S, ps[:, jj, 0:n], C.idb[:], msk[:, 0:n], False, True, [it["m_tok"], C.t_c], k_ps)
        pt, k_pt = ptp.get()
        act(S, pt[:, 0:len(chunk), 0:n], ps[:, 0:len(chunk), 0:n], AF.Exp, [k_ps], [k_pt], scale=scale)
        state[si] = (pt, k_pt)

    def emit_PV(si):
        it, a, chunk = steps[si]
        n = it["n"]
        pt, k_pt = state.pop(si)
        if a == 0:
            it["acc"] = pac.get()
        acc, k_acc = it["acc"]
        nk = len(it["keys"])
        for jj, (k_ap, v_ap, msk) in enumerate(chunk):
            mm(S, acc[0:65, 0:n], v_ap, pt[:, jj, 0:n], a + jj == 0, a + jj == nk - 1, [k_pt] + it["kv_reads"], k_acc)
        if a + len(chunk) == nk:
            it["finish"](acc, k_acc)

    if not steps:
        return
    for si in range(min(LA, len(steps))):
        emit_S(si)
    for si in range(len(steps)):
        if hook is not None and si % hook_every == 0:
            hook(si // hook_every)
        if si + LA < len(steps):
            emit_S(si + LA)
        emit_PV(si)


def _norm_store(C, pools, acc, k_acc, n, add_ap, add_tok, dst_ap, wtoks):
    S = C.S
    rsp, bcp, bcsp, op_, onesf = pools
    rs, k_rs = rsp.get()
    if add_ap is not None:
        tt(S, "dve", rs[64:65, 0:n], acc[64:65, 0:n], add_ap, ALU.add, [k_acc, add_tok], [k_rs])
        S.op("dve", lambda e, rs=rs, n=n: e.reciprocal(out=rs[64:65, 0:n], in_=rs[64:65, 0:n]), reads=[k_rs], writes=[k_rs], acc=True)
    else:
        S.op("dve", lambda e, rs=rs, n=n, acc=acc: e.reciprocal(out=rs[64:65, 0:n], in_=acc[64:65, 0:n]), reads=[k_acc], writes=[k_rs])
    bcs, k_bcs = bcsp.get()
    if bcp is None:
        slot = C.bc_slot = (getattr(C, "bc_slot", -1) + 1) % 8
        brow = C.d["bc_s"][slot:slot + 1, 0:n]
        S.dma("pool", brow, rs[64:65, 0:n], reads=[k_rs], writes=[C.T(f"bc_s:{slot}")])
        S.dma("pool", bcs[:, 0:n], brow.to_broadcast([64, n]), reads=[C.T(f"bc_s:{slot}")], writes=[k_bcs])
    else:
        bc, k_bc = bcp.get()
        mm(S, bc[0:64, 0:n], onesf[64:65, 0:64], rs[64:65, 0:n], True, True, [k_rs, C.t_c], k_bc)
        cp(S, "act", bcs[:, 0:n], bc[0:64, 0:n], [k_bc], [k_bcs])
    o, k_o = op_.get()
    tt(S, "dve", o[:, 0:n], acc[0:64, 0:n], bcs[:, 0:n], ALU.mult, [k_acc, k_bcs], [k_o])
    S.dma("sp", dst_ap(o), o[:, 0:n] if dst_ap.flat else o[:, 0:n].rearrange("p (u t) -> p u t", u=4), reads=[k_o], writes=wtoks, acc=True)


class _Dst:
    def __init__(self, ap, flat):
        self.ap, self.flat = ap, flat

    def __call__(self, o):
        return self.ap


def _norm_pools(C, st, name, bounce=True):
    nc = C.nc
    rsp = SbPool(C, st, 2, f"{name}_rs", [65, 512], F32)
    bcp = None if bounce else PsumPool(C, st, 1, f"{name}_bc", (64, 512), F32)
    bcsp = SbPool(C, st, 3, f"{name}_bcs", [64, 512], F32)
    op_ = SbPool(C, st, 3, f"{name}_o", [64, 512], BF16)
    onesf = st.enter_context(nc.sbuf_tensor(f"{name}_onesf", [128, 64], F32))
    C.S.op("dve", lambda e: e.memset(onesf[:], 1.0), writes=[C.t_c], acc=True)
    return (rsp, bcp, bcsp, op_, onesf)


def phase_attn_a(C, l):
    nc, S, d = C.nc, C.S, C.d
    import contextlib
    last = (l == DEPTH - 1)
    with contextlib.ExitStack() as st:
        sb = lambda name, shape, dt: st.enter_context(nc.sbuf_tensor(f"a{l}_{name}", list(shape), dt))
        ka = sb("ka", [128, NTOK], BF16)
        va = sb("va", [128, NT, 130], BF16)
        esk = sb("esk", [65, 2, 512], F32)
        mi = sb("mi", [128, 512], I32)
        mP, mN = sb("mP", [128, 512], BF16), sb("mN", [128, 512], BF16)
        t_ka, t_va, t_es, t_mi, t_m = Tok(), Tok(), Tok(), Tok(), Tok()
        S.dma("sp", ka[:], d["kaT_s"][:, :], reads=[C.T(f"kaT:{g}") for g in range(9)], writes=[t_ka])
        S.dma("sp", va[:], d["va_s"].rearrange("(i p) f -> p i f", p=128), reads=[C.T(f"va:{g}") for g in range(9)], writes=[t_va])
        S.dma("sp", esk[64:65, :, :], d["sink_row"][l:l + 1, :, :], writes=[t_es])
        act(S, esk[64:65, :, :], esk[64:65, :, :], AF.Exp, [t_es], [t_es])
        S.op("pool", lambda e: e.iota(mi[:], pattern=[[0, 4], [1, 128]], base=0, channel_multiplier=-1), writes=[t_mi])
        ts(S, "dve", mP[:], mi[:], 0, NEG, ALU.is_gt, ALU.mult, [t_mi], [t_m], acc=True)
        ts(S, "dve", mN[:], mi[:], 0, NEG, ALU.is_lt, ALU.mult, [t_mi], [t_m], acc=True)
        pools = _norm_pools(C, st, f"a{l}", bounce=False)
        qp = SbPool(C, st, 3, f"a{l}_q", [128, 4, 128], BF16)
        qtiles = list(range(2, NT)) if last else list(range(NT))
        if QT_LIMIT:
            qtiles = qtiles[:QT_LIMIT]
        items = []
        for qt in qtiles:
            if qt < 2:
                keys = [(0, None), (1, None)]
            else:
                xq = qt - 2
                keys = [(0, None), (1, None)]
                if xq > 0:
                    keys.append((qt - 1, mP))
                keys.append((qt, None))
                if xq < 31:
                    keys.append((qt + 1, mN))
            for g in range(2):
                it = {"qt": qt, "g": g, "n": 512, "m_tok": t_m, "kv_reads": [t_ka, t_va],
                      "keys": [(ka[g * 64:(g + 1) * 64, kt * 128:(kt + 1) * 128], va[:, kt, g * 65:(g + 1) * 65], msk)
                               for kt, msk in keys]}
                items.append(it)
        cur = {}

        def load_q(it):
            qt, g = it["qt"], it["g"]
            if qt not in cur:
                q, k_q = qp.get()
                S.dma("sp", q[:], d["qaT_s"].rearrange("c p t -> p c t")[:, :, qt * 128:(qt + 1) * 128],
                      reads=[C.T(f"qaT:{gg}") for gg in range(9)], writes=[k_q])
                cur.clear()
                cur[qt] = (q, k_q)
            q, k_q = cur[qt]
            it["q"] = q[g * 64:(g + 1) * 64, :, :].rearrange("p c t -> p (c t)")
            it["q_reads"] = [k_q]

        for it in items:
            qt, g = it["qt"], it["g"]
            it["load_q"] = load_q
            dst = d["oaT_s"][2 * g:2 * g + 2].rearrange("u2 (u1 e) t -> e (u2 u1) t", u1=2)[:, :, qt * 128:(qt + 1) * 128]
            it["finish"] = (lambda acc, k_acc, g=g, qt=qt, dst=dst:
                            _norm_store(C, pools, acc, k_acc, 512, esk[64:65, g, :], t_es, _Dst(dst, False),
                                        [C.T(f"oaT_s:{qt}")]))
        attn_pipeline(C, st, f"a{l}", items, 0.125, LA=1)
    S.barrier()


def phase_attn_b(C, l):
    nc, S, d = C.nc, C.S, C.d
    import contextlib
    last = (l == DEPTH - 1)
    scale = 96.0 ** -0.5
    with contextlib.ExitStack() as st:
        sb = lambda name, shape, dt: st.enter_context(nc.sbuf_tensor(f"b{l}_{name}", list(shape), dt))
        kb = sb("kb", [96, 8, NTOK], BF16)
        vb = sb("vb", [128, NT, 520], BF16)
        t_kb, t_vb = Tok(), Tok()
        for h in range(8):
            S.dma("sp", kb[:, h, :], d["kbT_s"][h], reads=[C.T(f"kbT:{g}") for g in range(9)], writes=[t_kb], acc=True)
        S.dma("sp", vb[:], d["vb_s"].rearrange("(i p) f -> p i f", p=128), reads=[C.T(f"vb:{g}") for g in range(9)], writes=[t_vb])
        pools = _norm_pools(C, st, f"b{l}")
        qp = SbPool(C, st, 2, f"b{l}_q", [96, 8, 512], BF16)
        groups = list(enumerate(GROUPS))
        if last:
            groups = groups[1:]
        if QT_LIMIT:
            groups = groups[:2]
        items = []
        curq = {}

        def load_q(it):
            g, h = it["g"], it["h"]
            t0, n = GROUPS[g]
            if g not in curq:
                q, k_q = qp.get()
                S.dma("sp", q[:, :, 0:n], d["qbT_s"].rearrange("h p t -> p h t")[:, :, t0:t0 + n],
                      reads=[C.T(f"qbT:{g}")], writes=[k_q])
                curq.clear()
                curq[g] = (q, k_q)
            q, k_q = curq[g]
            it["q"] = q[:, h, 0:n]
            it["q_reads"] = [k_q]

        for g, (t0, n) in groups:
            keys = [0, 1] if g == 0 else list(range(NT))
            tiles = list(range(t0 // 128, (t0 + n) // 128))
            for h in range(8):
                dst = d["obT_s"][h // 2, (h % 2) * 64:(h % 2) * 64 + 64, t0:t0 + n]
                items.append({"n": n, "g": g, "h": h, "load_q": load_q, "kv_reads": [t_kb, t_vb], "m_tok": None,
                              "keys": [(kb[:, h, kt * 128:(kt + 1) * 128], vb[:, kt, h * 65:(h + 1) * 65], None) for kt in keys],
                              "finish": (lambda acc, k_acc, n=n, dst=dst, tiles=tiles:
                                         _norm_store(C, pools, acc, k_acc, n, None, None, _Dst(dst, True),
                                                     [C.T(f"obT_s:{t}") for t in tiles]))})
        stg_g = SbPool(C, st, 1, f"b{l}_sgg", [128, 4096], F32)
        stg_d = SbPool(C, st, 1, f"b{l}_sgd", [128, 2048], F32)
        cb_g = SbPool(C, st, 1, f"b{l}_cbg", [128, 4096], BF16)
        cb_d = SbPool(C, st, 1, f"b{l}_cbd", [128, 2048], BF16)

        def precast(k):
            if k >= 64 or QT_LIMIT:
                return
            e_, part = k // 2, k % 2
            srcw, dstw, sp_, cp_ = ((d["w_gu_h"], d["wgu_b"], stg_g, cb_g) if part == 0 else (d["w_dn_h"], d["wdn_b"], stg_d, cb_d))
            s_t, s_k = sp_.get()
            S.dma("sp", s_t[:], srcw[l, e_ * 128:(e_ + 1) * 128, :], writes=[s_k])
            c_t, c_k = cp_.get()
            cp(S, "dve", c_t[:], s_t[:], [s_k], [c_k])
            S.dma("pool", dstw[e_ * 128:(e_ + 1) * 128, :], c_t[:], reads=[c_k], writes=[C.T("wexp_b")], acc=True)

        nsteps = sum((len(it["keys"]) + 1) // 2 for it in items)
        attn_pipeline(C, st, f"b{l}", items, scale, hook=precast, hook_every=max(1, nsteps // 66))
    S.barrier()


def phase_pool(C, l):
    nc, S, d = C.nc, C.S, C.d
    import contextlib
    last = (l == DEPTH - 1)
    seqs = [(256, SEQ, "invx")] + ([] if last else [(0, LC, "invc")])
    with contextlib.ExitStack() as st:
        sb = lambda name, shape, dt: st.enter_context(nc.sbuf_tensor(f"c{l}_{name}", list(shape), dt))
        wps = sb("wps", [128, 4, 128], F32)
        wp = sb("wp", [128, 4, 128], BF16)
        psc = sb("psc", [128, 4], F32)
        Up = sb("Up", [128, SEQ + 16], F32)
        Aa, Ab = sb("Aa", [128, SEQ + 16], F32), sb("Ab", [128, SEQ + 16], F32)
        inv = sb("inv", [128, SEQ], F32)
        ub = sb("ub", [128, SEQ], BF16)
        pl = sb("pl", [128, SEQ], BF16)
        oc = sb("oc", [128, SEQ], BF16)
        t_wp, t_psc, t_Up, t_A, t_B, t_inv, t_ub, t_pl, t_oc = (Tok() for _ in range(9))
        S.dma("sp", wps[:], d["w_pool"][l].rearrange("g c e -> c g e"), writes=[t_wp])
        cp(S, "act", wp[:], wps[:], [t_wp], [t_wp])
        S.dma("sp", psc[:], d["pool_scaleT"][l], writes=[t_psc])
        pp = PsumPool(C, st, 3, f"c{l}_pp")
        eng = ["dve", "dve"]
        ei = 0
        for (t0, L, invname) in seqs:
            for g in range(4):
                r = (1, 2, 4, 8)[g]
                S.dma("sp", ub[:, 0:L], d["uT_s"][g, :, t0:t0 + L], reads=[C.T(f"uT:{q}") for q in range(9)], writes=[t_ub])
                S.dma("sp", inv[:, 0:L], bcast_row(d[invname][g:g + 1, :], L), writes=[t_inv])
                S.op("dve", lambda e, L=L: e.memset(Up[:, 0:L + 16], 0.0), writes=[t_Up])
                cp(S, "dve", Up[:, 8:8 + L], ub[:, 0:L], [t_ub], [t_Up])
                src, k_src, ln = Up, t_Up, L + 16
                bufs = [(Aa, t_A), (Ab, t_B)]
                step = 1
                bi = 0
                while step <= r:
                    dst, k_dst = bufs[bi]
                    bi ^= 1
                    nl = ln - step
                    tt(S, eng[ei % 2], dst[:, 0:nl], src[:, 0:nl], src[:, step:step + nl], ALU.add, [k_src], [k_dst])
                    ei += 1
                    src, k_src, ln = dst, k_dst, nl
                    step *= 2
                dst, k_dst = bufs[bi]
                tt(S, eng[ei % 2], dst[:, 0:L], src[:, 8 - r:8 - r + L], Up[:, 8 + r:8 + r + L], ALU.add, [k_src, t_Up], [k_dst])
                ei += 1
                tt(S, "dve", dst[:, 0:L], dst[:, 0:L], inv[:, 0:L], ALU.mult, [k_dst, t_inv], [k_dst])
                tt(S, "dve", pl[:, 0:L], dst[:, 0:L], Up[:, 8:8 + L], ALU.subtract, [k_dst, t_Up], [t_pl])
                for c0 in range(0, L, 512):
                    n = min(512, L - c0)
                    ps, k_ps = pp.get()
                    mm(S, ps[:, 0:n], wp[:, g, :], pl[:, c0:c0 + n], True, True, [t_wp, t_pl], k_ps)
                    act(S, oc[:, c0:c0 + n], ps[:, 0:n], AF.Copy, [k_ps, t_psc], [t_oc], acc=(c0 > 0), scale=psc[:, g:g + 1])
                S.dma("pool", d["ocT_s"][g, :, t0:t0 + L], oc[:, 0:L], reads=[t_oc], writes=[C.T(f"ocT:{g}:{t0}")])
    S.barrier()


def phase_merge(C, l):
    nc, S, d = C.nc, C.S, C.d
    import contextlib
    last = (l == DEPTH - 1)
    hsrc = d["h0"] if l == 0 else d["h_s"]
    with contextlib.ExitStack() as st:
        sb = lambda name, shape, dt: st.enter_context(nc.sbuf_tensor(f"g{l}_{name}", list(shape), dt))
        wg = sb("wg", [128, 8, 3072], BF16)
        wbr = sb("wbr", [128, 12, 1024], BF16)
        wo = sb("wo", [128, 2, 8, 1024], BF16)
        G1 = sb("G1", [128, 2, 1024], F32)
        t_wg, t_wbr, t_wo, t_G1 = Tok(), Tok(), Tok(), Tok()
        wext = d["w_in_ext"][l].rearrange("(kc p) n -> p kc n", p=128)[:, :, O_G:W_EXT]
        load_cast(C, st, f"g{l}_stg", wg, wext, 8, 3072, t_wg, half=768)
        wbsrc = d["w_br"][l].rearrange("b (kc p) n -> p (b kc) n", p=128)
        load_cast(C, st, f"g{l}_stg2", wbr, wbsrc, 12, 1024, t_wbr, half=512)
        for r in range(2):
            S.dma("sp", G1[:, r, :], bcast_row(d["modv"][l][r:r + 1, 2048:3072], 1024), reads=[C.T(f"modv{l}")], writes=[t_G1], acc=True)
        stgo = SbPool(C, st, 2, f"g{l}_stgo", [128, 1024], F32)
        wosrc = d["w_out"][l].rearrange("(kc p) n -> p kc n", p=128)
        for kc in range(8):
            s_t, s_k = stgo.get()
            S.dma("sp", s_t[:], wosrc[:, kc, :], writes=[s_k])
            for r in range(2):
                tt(S, "dve", wo[:, r, kc, :], s_t[:], G1[:, r, :], ALU.mult, [s_k, t_G1], [t_wo], acc=True)
        pp = PsumPool(C, st, 6, f"g{l}_pp")
        hxTp = SbPool(C, st, 1, f"g{l}_hxT", [128, 8, 512], BF16)
        oTp = SbPool(C, st, 1, f"g{l}_oT", [128, 12, 512], BF16)
        YTp = SbPool(C, st, 1, f"g{l}_YT", [128, 8, 512], BF16)
        sgp = SbPool(C, st, 3, f"g{l}_sg", [128, 512], BF16)
        tbp = SbPool(C, st, 4, f"g{l}_tb", [128, 512], F32)
        y1p = SbPool(C, st, 2, f"g{l}_y1", [128, 512], F32)
        hp = SbPool(C, st, 2, f"g{l}_h", [128, 1024], F32)
        hnp = SbPool(C, st, 2, f"g{l}_hn", [128, 1024], F32)
        groups = list(enumerate(GROUPS))
        if last:
            groups = groups[1:]
        if QT_LIMIT:
            groups = groups[:2]
        oc_toks = [C.T(f"ocT:{g}:{t0}") for g in range(4) for t0 in (0, 256)]
        for g, (t0, n) in groups:
            r = 0 if g > 0 else 1
            hxT, k_hxT = hxTp.get()
            S.dma("sp", hxT[:, :, 0:n], d["hxT_s"].rearrange("c p t -> p c t")[:, :, t0:t0 + n], reads=[C.T(f"hxT:{g}")], writes=[k_hxT])
            oT, k_oT = oTp.get()
            tiles = list(range(t0 // 128, (t0 + n) // 128))
            for bi, (nm, rd) in enumerate((("oaT_s", [C.T(f"oaT_s:{t}") for t in tiles]),
                                           ("obT_s", [C.T(f"obT_s:{t}") for t in tiles]),
                                           ("ocT_s", oc_toks))):
                S.dma("sp", oT[:, bi * 4:(bi + 1) * 4, 0:n], d[nm].rearrange("c p t -> p c t")[:, :, t0:t0 + n],
                      reads=rd, writes=[k_oT], acc=(bi > 0))
            YT, k_YT = YTp.get()
            for m in range(8):
                tbs = []
                for br in range(3):
                    psg, k_psg = pp.get()
                    for kc in range(8):
                        mm(S, psg[:, 0:n], wg[:, kc, br * 1024 + m * 128:br * 1024 + (m + 1) * 128], hxT[:, kc, 0:n],
                           kc == 0, kc == 7, [t_wg, k_hxT], k_psg)
                    sg, k_sg = sgp.get()
                    act(S, sg[:, 0:n], psg[:, 0:n], AF.Sigmoid, [k_psg], [k_sg])
                    psv, k_psv = pp.get()
                    for kc in range(4):
                        mm(S, psv[:, 0:n], wbr[:, br * 4 + kc, m * 128:(m + 1) * 128], oT[:, br * 4 + kc, 0:n],
                           kc == 0, kc == 3, [t_wbr, k_oT], k_psv)
                    tb, k_tb = tbp.get()
                    tt(S, "dve", tb[:, 0:n], psv[:, 0:n], sg[:, 0:n], ALU.mult, [k_psv, k_sg], [k_tb])
                    tbs.append((tb, k_tb))
                y1, k_y1 = y1p.get()
                tt(S, "dve", y1[:, 0:n], tbs[0][0][:, 0:n], tbs[1][0][:, 0:n], ALU.add, [tbs[0][1], tbs[1][1]], [k_y1])
                tt(S, "dve", YT[:, m, 0:n], y1[:, 0:n], tbs[2][0][:, 0:n], ALU.add, [k_y1, tbs[2][1]], [k_YT], acc=(m > 0))
            for ti, tile in enumerate(tiles):
                ht, k_h = hp.get()
                S.dma("sp", ht[:], hsrc[tile * 128:(tile + 1) * 128, :], reads=[C.T(f"h:{tile}")], writes=[k_h])
                hn, k_hn = hnp.get()
                for half in range(2):
                    ps, k_ps = pp.get()
                    for m in range(8):
                        mm(S, ps[:, :], YT[:, m, ti * 128:(ti + 1) * 128], wo[:, r, m, half * 512:(half + 1) * 512],
                           m == 0, m == 7, [k_YT, t_wo], k_ps)
                    tt(S, "dve", hn[:, half * 512:(half + 1) * 512], ps[:, :], ht[:, half * 512:(half + 1) * 512], ALU.add,
                       [k_ps, k_h], [k_hn], acc=(half > 0))
                S.dma("pool", d["h_s"][tile * 128:(tile + 1) * 128, :], hn[:], reads=[k_hn], writes=[C.T(f"h:{tile}")])
    S.barrier()


def phase_route(C, l):
    nc, S, d = C.nc, C.S, C.d
    import contextlib
    last = (l == DEPTH - 1)
    with contextlib.ExitStack() as st:
        sb = lambda name, shape, dt: st.enter_context(nc.sbuf_tensor(f"r{l}_{name}", list(shape), dt))
        Gm, Sm = sb("Gm", [128, 2, 1024], F32), sb("Sm", [128, 2, 1024], F32)
        wr = sb("wr", [128, 8, 36], F32)
        br = sb("br", [128, 36], F32)
        t_mod, t_mod2, t_wr = Tok(), Tok(), Tok()
        mv = d["modv"][l]
        tmpp = SbPool(C, st, 2, f"r{l}_tmp", [128, 1024], F32)
        gn, k_gn = tmpp.get()
        S.dma("sp", gn[:], bcast_row(d["norm_ffn_g"][l:l + 1, :], 1024), writes=[k_gn])
        for r in range(2):
            S.dma("sp", Sm[:, r, :], bcast_row(mv[r:r + 1, 3072:4096], 1024), reads=[C.T(f"modv{l}")], writes=[t_mod], acc=True)
            S.dma("sp", Gm[:, r, :], bcast_row(mv[r:r + 1, 4096:5120], 1024), reads=[C.T(f"modv{l}")], writes=[t_mod], acc=True)
        for r in range(2):
            stt(S, Gm[:, r, :], Gm[:, r, :], 1.0, gn[:], ALU.add, ALU.mult, [t_mod, k_gn], [t_mod2], acc=True)
        S.dma("sp", wr[:], d["w_r"][l].rearrange("(kc p) n -> p kc n", p=128), writes=[t_wr], acc=True)
        S.dma("sp", br[:], bcast_row(d["b_r"][l:l + 1, :], 36), writes=[t_wr], acc=True)
        pt32 = PsumPool(C, st, 2, f"r{l}_pt", (128, 4, 128), F32)
        pp = PsumPool(C, st, 2, f"r{l}_pp", (128, 128), F32)
        ptb = PsumPool(C, st, 2, f"r{l}_ptb", (128, 8, 128), BF16)
        hp = SbPool(C, st, 2, f"r{l}_h", [128, 1024], F32)
        fxp = SbPool(C, st, 2, f"r{l}_fx", [128, 1024], F32)
        fxbp = SbPool(C, st, 2, f"r{l}_fxb", [128, 1024], BF16)
        fxTp = SbPool(C, st, 2, f"r{l}_fxT", [128, 8, 128], F32)
        fxTbp = SbPool(C, st, 2, f"r{l}_fxTb", [128, 8, 128], BF16)
        ssp = SbPool(C, st, 2, f"r{l}_ss", [128, 4], F32)
        sqj = sb("sqj", [128, 1024], BF16)
        t_sqj = Tok()
        rtp = SbPool(C, st, 2, f"r{l}_rt", [128, 160], F32)
        gTp = SbPool(C, st, 2, f"r{l}_gT", [32, 128], F32)
        tiles = list(range(2, NT)) if last else list(range(NT))
        if QT_LIMIT:
            tiles = tiles[:QT_LIMIT]
        for tile in tiles:
            mr = 0 if tile >= 2 else 1
            ht, k_h = hp.get()
            S.dma("sp", ht[:], d["h_s"][tile * 128:(tile + 1) * 128, :], reads=[C.T(f"h:{tile}")], writes=[k_h])
            ss, k_ss = ssp.get()
            act(S, sqj[:], ht[:], AF.Square, [k_h], [t_sqj, k_ss], accum_out=ss[:, 0:1])
            act(S, ss[:, 1:2], ss[:, 0:1], AF.Sqrt, [k_ss], [k_ss], acc=True, scale=1.0 / DM, bias=EPS)
            S.op("dve", lambda e, ss=ss: e.reciprocal(out=ss[:, 2:3], in_=ss[:, 1:2]), reads=[k_ss], writes=[k_ss], acc=True)
            tm, k_tm = tmpp.get()
            stt(S, tm[:], ht[:], ss[:, 2:3], Gm[:, mr, :], ALU.mult, ALU.mult, [k_h, k_ss, t_mod2], [k_tm])
            fx, k_fx = fxp.get()
            tt(S, "dve", fx[:], tm[:], Sm[:, mr, :], ALU.add, [k_tm, t_mod2], [k_fx])
            fxb, k_fxb = fxbp.get()
            cp(S, "act", fxb[:], fx[:], [k_fx], [k_fxb])
            tpb, k_tpb = ptb.get()
            for kc in range(8):
                S.op("pe", lambda e, tpb=tpb, fxb=fxb, kc=kc: e.transpose(tpb[:, kc, :], fxb[:, kc * 128:(kc + 1) * 128], C.idb[:]),
                     reads=[k_fxb, C.t_c], writes=[k_tpb], acc=(kc > 0))
            fxTb, k_fxTb = fxTbp.get()
            cp(S, "act", fxTb[:], tpb[:], [k_tpb], [k_fxTb])
            S.dma("pool", d["hxT_s"].rearrange("c p t -> p c t")[:, :, tile * 128:(tile + 1) * 128], fxTb[:],
                  reads=[k_fxTb], writes=[C.T(f"fxT:{tile}")])
            fxT, k_fxT = fxTp.get()
            for hf in range(2):
                tp, k_tp = pt32.get()
                for kc in range(4):
                    S.op("pe", lambda e, tp=tp, fx=fx, kc=kc, hf=hf: e.transpose(tp[:, kc, :], fx[:, (hf * 4 + kc) * 128:(hf * 4 + kc + 1) * 128], C.idf[:]),
                         reads=[k_fx, C.t_c], writes=[k_tp], acc=(kc > 0))
                cp(S, "act", fxT[:, hf * 4:(hf + 1) * 4, :], tp[:], [k_tp], [k_fxT], acc=(hf > 0))
            pl, k_pl = pp.get()
            for kc in range(8):
                mm(S, pl[:, 0:36], fxT[:, kc, :], wr[:, kc, :], kc == 0, kc == 7, [k_fxT, t_wr], k_pl)
            rt, k = rtp.get()
            tt(S, "dve", rt[:, 0:36], pl[:, 0:36], br[:], ALU.add, [k_pl, t_wr], [k])
            S.op("dve", lambda e, rt=rt: e.reduce_max(out=rt[:, 36:37], in_=rt[:, 0:4], axis=AX.X), reads=[k], writes=[k], acc=True)
            ts(S, "dve", rt[:, 37:38], rt[:, 36:37], -1.0, None, ALU.mult, None, [k], [k], acc=True)
            act(S, rt[:, 44:48], rt[:, 0:4], AF.Exp, [k], [k], acc=True, bias=rt[:, 37:38], accum_out=rt[:, 38:39])
            S.op("dve", lambda e, rt=rt: e.reciprocal(out=rt[:, 39:40], in_=rt[:, 38:39]), reads=[k], writes=[k], acc=True)
            ts(S, "dve", rt[:, 40:44], rt[:, 0:4], rt[:, 36:37], None, ALU.is_equal, None, [k], [k], acc=True)
            ts(S, "dve", rt[:, 40:44], rt[:, 40:44], -1.0, 30000.0, ALU.add, ALU.mult, [k], [k], acc=True)
            for g in range(4):
                ts(S, "dve", rt[:, 48 + g * 8:56 + g * 8], rt[:, 4 + g * 8:12 + g * 8], rt[:, 40 + g:41 + g], None,
                   ALU.add, None, [k], [k], acc=True)
            S.op("dve", lambda e, rt=rt: e.max(out=rt[:, 80:88], in_=rt[:, 48:80]), reads=[k], writes=[k], acc=True)
            tt(S, "dve", rt[:, 88:89], rt[:, 81:82], rt[:, 80:81], ALU.subtract, [k], [k], acc=True)
            act(S, rt[:, 89:90], rt[:, 88:89], AF.Exp, [k], [k], acc=True)
            ts(S, "dve", rt[:, 90:91], rt[:, 89:90], 1.0, None, ALU.add, None, [k], [k], acc=True)
            S.op("dve", lambda e, rt=rt: e.reciprocal(out=rt[:, 90:91], in_=rt[:, 90:91]), reads=[k], writes=[k], acc=True)
            tt(S, "dve", rt[:, 91:92], rt[:, 90:91], rt[:, 39:40], ALU.mult, [k], [k], acc=True)
            tt(S, "dve", rt[:, 92:93], rt[:, 91:92], rt[:, 89:90], ALU.mult, [k], [k], acc=True)
            ts(S, "dve", rt[:, 96:128], rt[:, 48:80], rt[:, 80:81], rt[:, 91:92], ALU.is_equal, ALU.mult, [k], [k], acc=True)
            ts(S, "dve", rt[:, 128:160], rt[:, 48:80], rt[:, 81:82], rt[:, 92:93], ALU.is_equal, ALU.mult, [k], [k], acc=True)
            tt(S, "dve", rt[:, 96:128], rt[:, 96:128], rt[:, 128:160], ALU.add, [k], [k], acc=True)
            pg, k_pg = pp.get()
            S.op("pe", lambda e, pg=pg, rt=rt: e.transpose(pg[0:32, 0:128], rt[:, 96:128], C.idf[:]),
                 reads=[k, C.t_c], writes=[k_pg])
            gT, k_gT = gTp.get()
            cp(S, "act", gT[:], pg[0:32, 0:128], [k_pg], [k_gT])
            S.dma("pool", d["gT_s"][:, tile * 128:(tile + 1) * 128], gT[:], reads=[k_gT], writes=[C.T(f"gT:{tile}")])
    S.barrier()


def phase_experts(C, l):
    nc, S, d = C.nc, C.S, C.d
    import contextlib
    last = (l == DEPTH - 1)
    with contextlib.ExitStack() as st:
        sb = lambda name, shape, dt: st.enter_context(nc.sbuf_tensor(f"e{l}_{name}", list(shape), dt))
        g2T = sb("g2T", [128, 2, 8], F32)
        fg = sb("fg", [128, 1024], F32)
        t_g2 = Tok()
        for r in range(2):
            S.dma("sp", g2T[:, r, :], d["modv"][l][r, 5120:6144].rearrange("(m p) -> p m", p=128), reads=[C.T(f"modv{l}")],
                  writes=[t_g2], acc=True, allow_slow_non_contiguous=True)
        if last:
            S.dma("sp", fg[:], bcast_row(d["final_g"][0:1, :], 1024), writes=[t_g2], acc=True)
        ppg = PsumPool(C, st, 4, f"e{l}_ppg")
        pp = PsumPool(C, st, 3, f"e{l}_pp")
        wgs = SbPool(C, st, 1, f"e{l}_wgs", [128, 8, 512], F32)
        wds = SbPool(C, st, 1, f"e{l}_wds", [128, 2, 1024], F32)
        wgp = SbPool(C, st, 2, f"e{l}_wg", [128, 8, 512], BF16)
        wdp = SbPool(C, st, 2, f"e{l}_wd", [128, 2, 1024], BF16)
        fxTp = SbPool(C, st, 1, f"e{l}_fxT", [128, 8, 2176], BF16)
        yacc = sb("yacc", [128, 8, 2176], F32)
        t_y = Tok()
        gwp = SbPool(C, st, 3, f"e{l}_gw", [128, 512], F32)
        sip = SbPool(C, st, 2, f"e{l}_si", [128, 512], F32)
        a1p = SbPool(C, st, 2, f"e{l}_a1", [128, 512], F32)
        actp = SbPool(C, st, 3, f"e{l}_act", [128, 2, 512], BF16)
        hp = SbPool(C, st, 2, f"e{l}_h", [128, 1024], F32)
        hnp = SbPool(C, st, 2, f"e{l}_hn", [128, 1024], F32)
        ssp = SbPool(C, st, 2, f"e{l}_ss", [128, 4], F32)
        sqj = sb("sqj", [128, 1024], BF16)
        t_sqj = Tok()
        tok0 = 256 if last else 0
        sgs = []
        t = tok0
        while t < NTOK:
            n = min(2048 if last else 2176, NTOK - t)
            sgs.append((t, n))
            t += n
        if QT_LIMIT:
            sgs = [(tok0, 256)]
        nexp = 32 if not QT_LIMIT else QT_LIMIT
        for (t0, n) in sgs:
            tiles = list(range(t0 // 128, (t0 + n) // 128))
            fxT, k_fxT = fxTp.get()
            S.dma("sp", fxT[:, :, 0:n], d["hxT_s"].rearrange("c p t -> p c t")[:, :, t0:t0 + n],
                  reads=[C.T(f"fxT:{tl}") for tl in tiles], writes=[k_fxT])
            steps = [(e_, s0) for e_ in range(nexp) for s0 in range(0, n, 512)]
            wcur = {}
            st_state = {}

            def load_w(e_):
                ws, k_ws = wgs.get()
                S.dma("sp", ws[:], d["w_gu"][l, e_].rearrange("(kc p) n -> p kc n", p=128), writes=[k_ws])
                wg, k_wg = wgp.get()
                cp(S, "act", wg[:], ws[:], [k_ws], [k_wg])
                ws2, k_ws2 = wds.get()
                S.dma("sp", ws2[:], d["w_dn"][l, e_].rearrange("(kc p) n -> p kc n", p=128), writes=[k_ws2])
                wd, k_wd = wdp.get()
                cp(S, "act", wd[:], ws2[:], [k_ws2], [k_wd])
                wcur[e_] = (wg, k_wg, wd, k_wd)

            def emit_gu(si):
                e_, s0 = steps[si]
                ns = min(512, n - s0)
                if e_ not in wcur:
                    load_w(e_)
                    wcur.pop(e_ - 2, None)
                wg, k_wg, wd, k_wd = wcur[e_]
                gw, k_gw = gwp.get()
                S.dma("sp", gw[:, 0:ns], bcast_row(d["gT_s"][e_:e_ + 1, t0 + s0:t0 + s0 + ns], ns),
                      reads=[C.T(f"gT:{tl}") for tl in tiles], writes=[k_gw])
                ac, k_ac = actp.get()
                for c in range(2):
                    psg, k_psg = ppg.get()
                    psu, k_psu = ppg.get()
                    for kc in range(8):
                        mm(S, psg[:, 0:ns], wg[:, kc, c * 128:(c + 1) * 128], fxT[:, kc, s0:s0 + ns], kc == 0, kc == 7, [k_wg, k_fxT], k_psg)
                    for kc in range(8):
                        mm(S, psu[:, 0:ns], wg[:, kc, 256 + c * 128:256 + (c + 1) * 128], fxT[:, kc, s0:s0 + ns], kc == 0, kc == 7, [k_wg, k_fxT], k_psu)
                    si_, k_si = sip.get()
                    act(S, si_[:, 0:ns], psg[:, 0:ns], AF.Silu, [k_psg], [k_si])
                    a1, k_a1 = a1p.get()
                    tt(S, "dve", a1[:, 0:ns], psu[:, 0:ns], si_[:, 0:ns], ALU.mult, [k_psu, k_si], [k_a1])
                    tt(S, "dve", ac[:, c, 0:ns], a1[:, 0:ns], gw[:, 0:ns], ALU.mult, [k_a1, k_gw], [k_ac], acc=(c > 0))
                st_state[si] = (ac, k_ac, wd, k_wd)

            def emit_dn(si):
                e_, s0 = steps[si]
                ns = min(512, n - s0)
                ac, k_ac, wd, k_wd = st_state.pop(si)
                for m in range(8):
                    py, k_py = pp.get()
                    for c in range(2):
                        mm(S, py[:, 0:ns], wd[:, c, m * 128:(m + 1) * 128], ac[:, c, 0:ns], c == 0, c == 1, [k_wd, k_ac], k_py)
                    if e_ == 0:
                        cp(S, "act", yacc[:, m, s0:s0 + ns], py[:, 0:ns], [k_py], [t_y], acc=True)
                    else:
                        tt(S, "dve", yacc[:, m, s0:s0 + ns], py[:, 0:ns], yacc[:, m, s0:s0 + ns], ALU.add, [k_py, t_y], [t_y], acc=True)

            emit_gu(0)
            for si in range(len(steps)):
                if si + 1 < len(steps):
                    emit_gu(si + 1)
                emit_dn(si)
            for m in range(8):
                for (a, b_, r) in ((0, 256 - t0, 1), (max(0, 256 - t0), n, 0)):
                    if b_ <= a:
                        continue
                    b_ = min(b_, n)
                    act(S, yacc[:, m, a:b_], yacc[:, m, a:b_], AF.Copy, [t_y, t_g2], [t_y], acc=True, scale=g2T[:, r, m:m + 1])
            for ti, tile in enumerate(tiles):
                ht, k_h = hp.get()
                S.dma("sp", ht[:], d["h_s"][tile * 128:(tile + 1) * 128, :], reads=[C.T(f"h:{tile}")], writes=[k_h])
                hn, k_hn = hnp.get()
                for hf in range(2):
                    ps, k_ps = pp.get()
                    for kc in range(4):
                        m = hf * 4 + kc
                        S.op("pe", lambda e, ps=ps, m=m, kc=kc, ti=ti: e.transpose(ps[:, kc * 128:(kc + 1) * 128], yacc[:, m, ti * 128:(ti + 1) * 128], C.idf[:]),
                             reads=[t_y, C.t_c], writes=[k_ps], acc=(kc > 0))
                    tt(S, "dve", hn[:, hf * 512:(hf + 1) * 512], ps[:, :], ht[:, hf * 512:(hf + 1) * 512], ALU.add, [k_ps, k_h], [k_hn], acc=(hf > 0))
                if not last:
                    S.dma("pool", d["h_s"][tile * 128:(tile + 1) * 128, :], hn[:], reads=[k_hn], writes=[C.T(f"h:{tile}")])
                else:
                    ss, k_ss = ssp.get()
                    act(S, sqj[:], hn[:], AF.Square, [k_hn], [t_sqj, k_ss], accum_out=ss[:, 0:1])
                    act(S, ss[:, 1:2], ss[:, 0:1], AF.Sqrt, [k_ss], [k_ss], acc=True, scale=1.0 / DM, bias=EPS)
                    S.op("dve", lambda e, ss=ss: e.reciprocal(out=ss[:, 2:3], in_=ss[:, 1:2]), reads=[k_ss], writes=[k_ss], acc=True)
                    ho, k_ho = hp.get()
                    stt(S, ho[:], hn[:], ss[:, 2:3], fg[:], ALU.mult, ALU.mult, [k_hn, k_ss, t_g2], [k_ho])
                    S.dma("pool", d["out"][(tile - 2) * 128:(tile - 1) * 128, :], ho[:], reads=[k_ho], writes=[C.T(f"out:{tile}")])
    S.barrier()


def all_phases(C):
    C.d["out"] = C.nc.dram_tensor("out", [SEQ, DM], F32, kind="ExternalOutput").ap()
    for l in range(DEPTH):
        phase_mod(C, l)
        phase_inproj(C, l)
        phase_attn_a(C, l)
        phase_attn_b(C, l)
        phase_pool(C, l)
        phase_merge(C, l)
        phase_moe(C, l)


def kernel(**inputs):
    sh = prep_shared(inputs)
    nc = build_program(all_phases)
    in_maps = []
    for b in range(8):
        m = dict(sh)
        m.update(prep_core(inputs, b))
        in_maps.append({k: m[k] for k in INPUTS})
    res = run_bass_kernel_spmd(nc, in_maps, core_ids=list(range(8)))
    return np.stack([np.asarray(r["out"], dtype=np.float32) for r in res.results], axis=0)


def phase_moe(C, l):
    nc, S, d = C.nc, C.S, C.d
    import contextlib
    last = (l == DEPTH - 1)
    tiles = list(range(2, NT)) if last else list(range(NT))
    if QT_LIMIT:
        tiles = tiles[:QT_LIMIT]
    nt = len(tiles)
    NB = -(-(2 * nt * 128 + 32 * 127) // 128)
    xbuf = d["xbuf"]
    ybuf = d["ybuf"]
    with contextlib.ExitStack() as st0:
        sb0 = lambda name, shape, dt: st0.enter_context(nc.sbuf_tensor(f"x{l}_{name}", list(shape), dt))
        sAB = sb0("sAB", [128, NT, 2], I32)
        w12 = sb0("w12", [128, NT, 2], F32)
        widx = sb0("widx", [128, 128], I32)
        t_sAB, t_w12, t_widx = Tok(), Tok(), Tok()
        t_xz = Tok()
        with contextlib.ExitStack() as st:
            sb = lambda name, shape, dt: st.enter_context(nc.sbuf_tensor(f"r{l}_{name}", list(shape), dt))
            zt = sb("zt", [128, 4, 1024], BF16)
            t_zt = Tok()
            S.op("dve", lambda e: e.memset(zt[:], 0.0), writes=[t_zt])
            xv = xbuf.rearrange("(a p) f -> p a f", p=128)
            for b0 in range(0, NB, 4):
                nb_ = min(4, NB - b0)
                S.dma("sp", xv[:, b0:b0 + nb_, :], zt[:, 0:nb_, :], reads=[t_zt], writes=[t_xz], acc=True)
            Gm, Sm = sb("Gm", [128, 2, 1024], F32), sb("Sm", [128, 2, 1024], F32)
            wr = sb("wr", [128, 8, 36], F32)
            br = sb("br", [128, 36], F32)
            t_mod, t_mod2, t_wr = Tok(), Tok(), Tok()
            mv = d["modv"][l]
            tmpp = SbPool(C, st, 2, f"r{l}_tmp", [128, 1024], F32)
            gn, k_gn = tmpp.get()
            S.dma("sp", gn[:], bcast_row(d["norm_ffn_g"][l:l + 1, :], 1024), writes=[k_gn])
            for r in range(2):
                S.dma("sp", Sm[:, r, :], bcast_row(mv[r:r + 1, 3072:4096], 1024), reads=[C.T(f"modv{l}")], writes=[t_mod], acc=True)
                S.dma("sp", Gm[:, r, :], bcast_row(mv[r:r + 1, 4096:5120], 1024), reads=[C.T(f"modv{l}")], writes=[t_mod], acc=True)
            for r in range(2):
                stt(S, Gm[:, r, :], Gm[:, r, :], 1.0, gn[:], ALU.add, ALU.mult, [t_mod, k_gn], [t_mod2], acc=True)
            S.dma("sp", wr[:], d["w_r"][l].rearrange("(kc p) n -> p kc n", p=128), writes=[t_wr], acc=True)
            S.dma("sp", br[:], bcast_row(d["b_r"][l:l + 1, :], 36), writes=[t_wr], acc=True)
            fx_all = sb("fxall", [128, NT, 1024], BF16)
            A01 = sb("A01", [128, NT, 32], F32)
            B01 = sb("B01", [128, NT, 32], F32)
            Mall = sb("Mall", [128, NT, 32], BF16)
            t_fx = [Tok() for _ in range(NT)]
            GT = 8
            pt32 = PsumPool(C, st, 2, f"r{l}_pt", (128, 4, 128), F32)
            pp = PsumPool(C, st, 2, f"r{l}_pp", (128, 512), F32)
            hbp = SbPool(C, st, 1, f"r{l}_hb", [128, GT, 1024], F32)
            fxp = SbPool(C, st, 2, f"r{l}_fx", [128, 1024], F32)
            fxTp = SbPool(C, st, 2, f"r{l}_fxT", [128, 8, 128], F32)
            ssp = SbPool(C, st, 2, f"r{l}_ss", [128, 3, GT], F32)
            sqj = sb("sqj", [128, GT, 1024], BF16)
            t_sqj = Tok()
            rwp = SbPool(C, st, 2, f"r{l}_rw", [128, GT, 128], F32)
            rsp_ = SbPool(C, st, 2, f"r{l}_rs", [128, 12, GT], F32)

            def b3(ap2, gt, w):
                return ap2.rearrange("p (g o) -> p g o", o=1).to_broadcast([128, gt, w])

            batches = [list(range(a, min(a + GT, nt))) for a in range(0, nt, GT)]
            t_ABb = []
            for bt in batches:
                gt = len(bt)
                t0i = bt[0]
                hb, k_hb = hbp.get()
                ss, k_ss = ssp.get()
                for j, ti in enumerate(bt):
                    tile = tiles[ti]
                    S.dma("sp", hb[:, j, :], d["h_s"][tile * 128:(tile + 1) * 128, :], reads=[C.T(f"h:{tile}")], writes=[k_hb], acc=(j > 0))
                for j, ti in enumerate(bt):
                    act(S, sqj[:, j, :], hb[:, j, :], AF.Square, [k_hb], [t_sqj, k_ss], acc=(j > 0), accum_out=ss[:, 0, j:j + 1])
                act(S, ss[:, 1, 0:gt], ss[:, 0, 0:gt], AF.Sqrt, [k_ss], [k_ss], acc=True, scale=1.0 / DM, bias=EPS)
                S.op("dve", lambda e, ss=ss, gt=gt: e.reciprocal(out=ss[:, 2, 0:gt], in_=ss[:, 1, 0:gt]), reads=[k_ss], writes=[k_ss], acc=True)
                pl, k_pl = pp.get()
                for j, ti in enumerate(bt):
                    tile = tiles[ti]
                    mr = 0 if tile >= 2 else 1
                    tm, k_tm = tmpp.get()
                    stt(S, tm[:], hb[:, j, :], ss[:, 2, j:j + 1], Gm[:, mr, :], ALU.mult, ALU.mult, [k_hb, k_ss, t_mod2], [k_tm])
                    fx, k_fx = fxp.get()
                    tt(S, "dve", fx[:], tm[:], Sm[:, mr, :], ALU.add, [k_tm, t_mod2], [k_fx])
                    cp(S, "act", fx_all[:, ti, :], fx[:], [k_fx], [t_fx[ti]])
                    fxT, k_fxT = fxTp.get()
                    for hf in range(2):
                        tp, k_tp = pt32.get()
                        for kc in range(4):
                            S.op("pe", lambda e, tp=tp, fx=fx, kc=kc, hf=hf: e.transpose(tp[:, kc, :], fx[:, (hf * 4 + kc) * 128:(hf * 4 + kc + 1) * 128], C.idf[:]),
                                 reads=[k_fx, C.t_c], writes=[k_tp], acc=(kc > 0))
                        cp(S, "act", fxT[:, hf * 4:(hf + 1) * 4, :], tp[:], [k_tp], [k_fxT], acc=(hf > 0))
                    for kc in range(8):
                        mm(S, pl[:, j * 36:(j + 1) * 36], fxT[:, kc, :], wr[:, kc, :], kc == 0, kc == 7, [k_fxT, t_wr], k_pl)
                rw, k = rwp.get()
                rs, k2 = rsp_.get()
                plv = pl[:, 0:gt * 36].rearrange("p (g n) -> p g n", n=36)
                tt(S, "dve", rw[:, 0:gt, 0:36], plv, br[:, :].rearrange("p (o n) -> p o n", o=1).to_broadcast([128, gt, 36]), ALU.add, [k_pl, t_wr], [k])
                if STOP == 101:
                    continue
                S.op("dve", lambda e, rw=rw, rs=rs, gt=gt: e.reduce_max(out=rs[:, 0, 0:gt], in_=rw[:, 0:gt, 0:4], axis=AX.X), reads=[k], writes=[k2])
                tt(S, "dve", rw[:, 0:gt, 36:40], rw[:, 0:gt, 0:4], b3(rs[:, 0, 0:gt], gt, 4), ALU.subtract, [k, k2], [k], acc=True)
                act(S, rw[:, 0:gt, 36:40], rw[:, 0:gt, 36:40], AF.Exp, [k], [k], acc=True)
                S.op("dve", lambda e, rw=rw, rs=rs, gt=gt: e.reduce_sum(out=rs[:, 1, 0:gt], in_=rw[:, 0:gt, 36:40], axis=AX.X), reads=[k], writes=[k2], acc=True)
                S.op("dve", lambda e, rs=rs, gt=gt: e.reciprocal(out=rs[:, 2, 0:gt], in_=rs[:, 1, 0:gt]), reads=[k2], writes=[k2], acc=True)
                if STOP == 102:
                    continue
                tt(S, "dve", rw[:, 0:gt, 36:40], rw[:, 0:gt, 0:4], b3(rs[:, 0, 0:gt], gt, 4), ALU.is_equal, [k, k2], [k], acc=True)
                ts(S, "dve", rw[:, 0:gt, 36:40], rw[:, 0:gt, 36:40], -1.0, 30000.0, ALU.add, ALU.mult, [k], [k], acc=True)
                tt(S, "dve", rw[:, 0:gt, 48:80].rearrange("p g (a b) -> p g a b", b=8),
                   rw[:, 0:gt, 4:36].rearrange("p g (a b) -> p g a b", b=8),
                   rw[:, 0:gt, 36:40].rearrange("p g (a o) -> p g a o", o=1).to_broadcast([128, gt, 4, 8]), ALU.add, [k], [k], acc=True)
                if STOP == 103:
                    continue
                S.op("dve", lambda e, rw=rw, rs=rs, gt=gt: e.reduce_max(out=rs[:, 3, 0:gt], in_=rw[:, 0:gt, 48:80], axis=AX.X), reads=[k], writes=[k2], acc=True)
                t_ab = Tok()
                t_ABb.append(t_ab)
                tt(S, "dve", A01[:, t0i:t0i + gt, :], rw[:, 0:gt, 48:80], b3(rs[:, 3, 0:gt], gt, 32), ALU.is_equal, [k, k2], [t_ab])
                stt(S, rw[:, 0:gt, 80:112], A01[:, t0i:t0i + gt, :], -60000.0, rw[:, 0:gt, 48:80], ALU.mult, ALU.add, [t_ab, k], [k], acc=True)
                S.op("dve", lambda e, rw=rw, rs=rs, gt=gt: e.reduce_max(out=rs[:, 4, 0:gt], in_=rw[:, 0:gt, 80:112], axis=AX.X), reads=[k], writes=[k2], acc=True)
                tt(S, "dve", B01[:, t0i:t0i + gt, :], rw[:, 0:gt, 80:112], b3(rs[:, 4, 0:gt], gt, 32), ALU.is_equal, [k, k2], [t_ab], acc=True)
                tt(S, "dve", Mall[:, t0i:t0i + gt, :], A01[:, t0i:t0i + gt, :], B01[:, t0i:t0i + gt, :], ALU.add, [t_ab], [t_ab], acc=True)
                if STOP == 104:
                    continue
                tt(S, "dve", rs[:, 5, 0:gt], rs[:, 4, 0:gt], rs[:, 3, 0:gt], ALU.subtract, [k2], [k2], acc=True)
                act(S, rs[:, 6, 0:gt], rs[:, 5, 0:gt], AF.Exp, [k2], [k2], acc=True)
                ts(S, "dve", rs[:, 7, 0:gt], rs[:, 6, 0:gt], 1.0, None, ALU.add, None, [k2], [k2], acc=True)
                S.op("dve", lambda e, rs=rs, gt=gt: e.reciprocal(out=rs[:, 7, 0:gt], in_=rs[:, 7, 0:gt]), reads=[k2], writes=[k2], acc=True)
                tt(S, "dve", w12[:, t0i:t0i + gt, 0], rs[:, 7, 0:gt], rs[:, 2, 0:gt], ALU.mult, [k2], [t_w12], acc=True)
                tt(S, "dve", w12[:, t0i:t0i + gt, 1], w12[:, t0i:t0i + gt, 0], rs[:, 6, 0:gt], ALU.mult, [k2, t_w12], [t_w12], acc=True)
            if 101 <= STOP <= 105:
                S.barrier()
                return
            t_AB = [t_ABb[ti // GT] for ti in range(nt)]
            pc, k_pc = pp.get()
            for ti in range(nt):
                mm(S, pc[:, 0:32], C.onb[:], Mall[:, ti, :], ti == 0, ti == nt - 1, [t_AB[ti], C.t_c], k_pc)
            cw = sb("cw", [128, 8, 32], F32)
            t_cw = Tok()
            ts(S, "dve", cw[:, 0, :], pc[:, 0:32], 1.0, None, ALU.mult, None, [k_pc], [t_cw])
            S.op("dve", lambda e: e.memset(cw[:, 1, :], 0.0), writes=[t_cw], acc=True)
            for kk in range(nt + 1):
                stt(S, cw[:, 1, :], cw[:, 0, :], 128.0 * kk, cw[:, 1, :], ALU.is_gt, ALU.add, [t_cw], [t_cw], acc=True)
            ts(S, "dve", cw[:, 2, :], cw[:, 1, :], 128.0, None, ALU.mult, None, [t_cw], [t_cw], acc=True)
            ts(S, "dve", cw[:, 3, :], cw[:, 2, :], 1.0, None, ALU.mult, None, [t_cw], [t_cw], acc=True)
            src_i = 3
            for s_ in (1, 2, 4, 8, 16):
                dst_i = 7 - src_i
                ts(S, "dve", cw[:, dst_i, 0:s_], cw[:, src_i, 0:s_], 1.0, None, ALU.mult, None, [t_cw], [t_cw], acc=True)
                tt(S, "dve", cw[:, dst_i, s_:32], cw[:, src_i, s_:32], cw[:, src_i, 0:32 - s_], ALU.add, [t_cw], [t_cw], acc=True)
                src_i = dst_i
            pe_i = src_i
            tt(S, "dve", cw[:, 5, :], cw[:, pe_i, :], cw[:, 2, :], ALU.subtract, [t_cw], [t_cw], acc=True)
            thi = sb("thi", [128, 128], I32)
            thr = sb("thr", [128, 128], F32)
            bacc = sb("bacc", [128, 128], F32)
            pidi = sb("pidi", [128, 1], I32)
            pidf = sb("pidf", [128, 1], F32)
            t_th = Tok()
            S.op("pool", lambda e: e.iota(thi[:], pattern=[[128, 128]], base=0, channel_multiplier=0), writes=[t_th])
            S.op("pool", lambda e: e.iota(pidi[:], pattern=[[0, 1]], base=0, channel_multiplier=1), writes=[t_th], acc=True)
            cp(S, "dve", thr[:], thi[:], [t_th], [t_th])
            cp(S, "dve", pidf[:], pidi[:], [t_th], [t_th], acc=True)
            S.op("dve", lambda e: e.memset(bacc[:], 0.0), writes=[t_th], acc=True)
            for e_ in range(32):
                stt(S, bacc[:], thr[:], cw[:, pe_i, e_:e_ + 1], bacc[:], ALU.is_ge, ALU.add, [t_th, t_cw], [t_th], acc=True)
            ts(S, "dve", bacc[:], bacc[:], 31.0, 128.0, ALU.min, ALU.mult, [t_th], [t_th], acc=True)
            ts(S, "dve", bacc[:], bacc[:], pidf[:, 0:1], 0.0, ALU.add, ALU.add, [t_th], [t_th], acc=True)
            cp(S, "dve", widx[:], bacc[:], [t_th], [t_widx])
            if STOP == 106:
                S.barrier()
                return
            slp = SbPool(C, st, 2, f"r{l}_sl", [128, 3, GT, 32], F32)
            sfp = SbPool(C, st, 2, f"r{l}_sf", [128, GT, 2], F32)
            for bt in batches:
                gt = len(bt)
                t0i = bt[0]
                ps, k_ps = pp.get()
                for j, ti in enumerate(bt):
                    for kk in range(ti):
                        mm(S, ps[:, j * 32:(j + 1) * 32], C.onb[:], Mall[:, kk, :], kk == 0, False, [t_AB[kk], C.t_c], k_ps)
                    mm(S, ps[:, j * 32:(j + 1) * 32], C.utri[:], Mall[:, ti, :], ti == 0, True, [t_AB[ti], C.t_c], k_ps)
                sl, k_sl = slp.get()
                psv = ps[:, 0:gt * 32].rearrange("p (g n) -> p g n", n=32)
                tt(S, "dve", sl[:, 0, 0:gt, :], psv, cw[:, 5, :].rearrange("p (o n) -> p o n", o=1).to_broadcast([128, gt, 32]), ALU.add, [k_ps, t_cw], [k_sl])
                tt(S, "dve", sl[:, 1, 0:gt, :], sl[:, 0, 0:gt, :], A01[:, t0i:t0i + gt, :], ALU.mult, [k_sl, t_AB[t0i]], [k_sl], acc=True)
                tt(S, "dve", sl[:, 2, 0:gt, :], sl[:, 0, 0:gt, :], B01[:, t0i:t0i + gt, :], ALU.mult, [k_sl, t_AB[t0i]], [k_sl], acc=True)
                sf, k_sf = sfp.get()
                S.op("dve", lambda e, sf=sf, sl=sl, gt=gt: e.reduce_sum(out=sf[:, 0:gt, 0], in_=sl[:, 1, 0:gt, :], axis=AX.X), reads=[k_sl], writes=[k_sf])
                S.op("dve", lambda e, sf=sf, sl=sl, gt=gt: e.reduce_sum(out=sf[:, 0:gt, 1], in_=sl[:, 2, 0:gt, :], axis=AX.X), reads=[k_sl], writes=[k_sf], acc=True)
                cp(S, "dve", sAB[:, t0i:t0i + gt, :], sf[:, 0:gt, :], [k_sf], [t_sAB], acc=True)
                for ti in bt:
                    for k2_ in range(2):
                        S.dma_fn("pool", lambda e, ti=ti, k2_=k2_: e.indirect_dma_start(
                            out=xbuf[:, :], out_offset=bass.IndirectOffsetOnAxis(ap=sAB[:, ti, k2_:k2_ + 1], axis=0),
                            in_=fx_all[:, ti, :], in_offset=None),
                            reads=[t_sAB, t_fx[ti], t_xz], writes=[C.T("xbuf")], acc=True)
        S.barrier()
        if STOP == 107:
            return
        with contextlib.ExitStack() as st:
            sb = lambda name, shape, dt: st.enter_context(nc.sbuf_tensor(f"e{l}_{name}", list(shape), dt))
            wgb = SbPool(C, st, 4, f"e{l}_wgb", [128, 8, 512], BF16)
            wdb = SbPool(C, st, 4, f"e{l}_wdb", [128, 2, 1024], BF16)
            xbp = SbPool(C, st, 3, f"e{l}_xb", [128, 1024], BF16)
            xTp = SbPool(C, st, 3, f"e{l}_xT", [128, 8, 128], BF16)
            sip = SbPool(C, st, 2, f"e{l}_si", [128, 256], F32)
            acp = SbPool(C, st, 3, f"e{l}_ac", [128, 256], BF16)
            aTp = SbPool(C, st, 2, f"e{l}_aT", [128, 2, 128], BF16)
            ybp = SbPool(C, st, 2, f"e{l}_yb", [128, 1024], F32)
            ptx = PsumPool(C, st, 2, f"e{l}_ptx", (128, 8, 128), BF16)
            ptc = PsumPool(C, st, 1, f"e{l}_ptc", (128, 2, 128), BF16)
            pgu = PsumPool(C, st, 2, f"e{l}_pgu")
            py = PsumPool(C, st, 2, f"e{l}_py")
            wgsrc = d["wgu_b"]
            wdsrc = d["wdn_b"]
            nblk = NB
            stA, stB = {}, {}

            def stage_a(b):
                wg, k_wg = wgb.get()
                S.dma_fn("pool", lambda e, wg=wg, b=b: e.indirect_dma_start(
                    out=wg[:].rearrange("p a b -> p (a b)"), out_offset=None, in_=wgsrc[:, :],
                    in_offset=bass.IndirectOffsetOnAxis(ap=widx[:, b:b + 1], axis=0)),
                    reads=[t_widx, C.T("wexp_b")], writes=[k_wg])
                wd, k_wd = wdb.get()
                S.dma_fn("pool", lambda e, wd=wd, b=b: e.indirect_dma_start(
                    out=wd[:].rearrange("p a b -> p (a b)"), out_offset=None, in_=wdsrc[:, :],
                    in_offset=bass.IndirectOffsetOnAxis(ap=widx[:, b:b + 1], axis=0)),
                    reads=[t_widx, C.T("wexp_b")], writes=[k_wd])
                xb, k_xb = xbp.get()
                S.dma("sp", xb[:], xbuf[b * 128:(b + 1) * 128, :], reads=[C.T("xbuf")], writes=[k_xb])
                tp, k_tp = ptx.get()
                for kc in range(8):
                    S.op("pe", lambda e, tp=tp, xb=xb, kc=kc: e.transpose(tp[:, kc, :], xb[:, kc * 128:(kc + 1) * 128], C.idb[:]),
                         reads=[k_xb, C.t_c], writes=[k_tp], acc=(kc > 0))
                xT, k_xT = xTp.get()
                cp(S, "act", xT[:], tp[:], [k_tp], [k_xT])
                stA[b] = (wg, k_wg, wd, k_wd, xT, k_xT)

            def stage_b(b):
                wg, k_wg, wd, k_wd, xT, k_xT = stA.pop(b)
                pg, k_pg = pgu.get()
                for kc in range(8):
                    mm(S, pg[:, :], xT[:, kc, :], wg[:, kc, :], kc == 0, kc == 7, [k_xT, k_wg], k_pg)
                si_, k_si = sip.get()
                act(S, si_[:], pg[:, 0:256], AF.Silu, [k_pg], [k_si])
                ac, k_ac = acp.get()
                tt(S, "dve", ac[:], pg[:, 256:512], si_[:], ALU.mult, [k_pg, k_si], [k_ac])
                stB[b] = (wd, k_wd, ac, k_ac)

            def stage_c(b):
                wd, k_wd, ac, k_ac = stB.pop(b)
                tp2, k_tp2 = ptc.get()
                for c in range(2):
                    S.op("pe", lambda e, tp2=tp2, ac=ac, c=c: e.transpose(tp2[:, c, :], ac[:, c * 128:(c + 1) * 128], C.idb[:]),
                         reads=[k_ac, C.t_c], writes=[k_tp2], acc=(c > 0))
                aT, k_aT = aTp.get()
                cp(S, "act", aT[:], tp2[:], [k_tp2], [k_aT])
                yb, k_yb = ybp.get()
                for hf in range(2):
                    pyt, k_py = py.get()
                    for c in range(2):
                        mm(S, pyt[:, :], aT[:, c, :], wd[:, c, hf * 512:(hf + 1) * 512], c == 0, c == 1, [k_aT, k_wd], k_py)
                    cp(S, "act", yb[:, hf * 512:(hf + 1) * 512], pyt[:, :], [k_py], [k_yb], acc=(hf > 0))
                S.dma("act", ybuf[b * 128:(b + 1) * 128, :], yb[:], reads=[k_yb], writes=[C.T("ybuf")], acc=True)

            for b in range(nblk + 2):
                if b < nblk:
                    stage_a(b)
                if 0 <= b - 1 < nblk:
                    stage_b(b - 1)
                if 0 <= b - 2 < nblk:
                    stage_c(b - 2)
            G2 = sb("G2", [128, 2, 1024], F32)
            fg = sb("fg", [128, 1024], F32)
            t_g2 = Tok()
            for r in range(2):
                S.dma("sp", G2[:, r, :], bcast_row(d["modv"][l][r:r + 1, 5120:6144], 1024), reads=[C.T(f"modv{l}")], writes=[t_g2], acc=True)
            if last:
                S.dma("sp", fg[:], bcast_row(d["final_g"][0:1, :], 1024), writes=[t_g2], acc=True)
            yAp = SbPool(C, st, 3, f"e{l}_yA", [128, 1024], F32)
            yBp = SbPool(C, st, 3, f"e{l}_yB", [128, 1024], F32)
            hp = SbPool(C, st, 4, f"e{l}_h", [128, 1024], F32)
            hnp = SbPool(C, st, 2, f"e{l}_hn", [128, 1024], F32)
            hop = SbPool(C, st, 2, f"e{l}_ho", [128, 1024], F32)
            ssp = SbPool(C, st, 2, f"e{l}_ss", [128, 4], F32)
            sqj = sb("sqj", [128, 1024], BF16)
            t_sqj = Tok()
            pre = {}

            def prefetch(ti):
                tile = tiles[ti]
                ys = []
                for k2, pool_ in enumerate((yAp, yBp)):
                    yt, k_yt = pool_.get()
                    S.dma_fn("pool", lambda e, yt=yt, ti=ti, k2=k2: e.indirect_dma_start(
                        out=yt[:], out_offset=None, in_=ybuf[:, :], in_offset=bass.IndirectOffsetOnAxis(ap=sAB[:, ti, k2:k2 + 1], axis=0)),
                        reads=[t_sAB, C.T("ybuf")], writes=[k_yt])
                    ys.append((yt, k_yt))
                ht, k_h = hp.get()
                S.dma("sp", ht[:], d["h_s"][tile * 128:(tile + 1) * 128, :], reads=[C.T(f"h:{tile}")], writes=[k_h])
                pre[ti] = (ys, ht, k_h)

            prefetch(0)
            for ti, tile in enumerate(tiles):
                r = 0 if tile >= 2 else 1
                if ti + 1 < len(tiles):
                    prefetch(ti + 1)
                ys, ht, k_h = pre.pop(ti)
                (yA, k_yA), (yB, k_yB) = ys
                ts(S, "dve", yA[:], yA[:], w12[:, ti, 0:1], None, ALU.mult, None, [k_yA, t_w12], [k_yA])
                stt(S, yB[:], yB[:], w12[:, ti, 1:2], yA[:], ALU.mult, ALU.add, [k_yB, k_yA, t_w12], [k_yB])
                tt(S, "dve", yB[:], yB[:], G2[:, r, :], ALU.mult, [k_yB, t_g2], [k_yB])
                hn, k_hn = hnp.get()
                tt(S, "dve", hn[:], yB[:], ht[:], ALU.add, [k_yB, k_h], [k_hn])
                if not last:
                    S.dma("act", d["h_s"][tile * 128:(tile + 1) * 128, :], hn[:], reads=[k_hn], writes=[C.T(f"h:{tile}")])
                else:
                    ss, k_ss = ssp.get()
                    act(S, sqj[:], hn[:], AF.Square, [k_hn], [t_sqj, k_ss], accum_out=ss[:, 0:1])
                    act(S, ss[:, 1:2], ss[:, 0:1], AF.Sqrt, [k_ss], [k_ss], acc=True, scale=1.0 / DM, bias=EPS)
                    S.op("dve", lambda e, ss=ss: e.reciprocal(out=ss[:, 2:3], in_=ss[:, 1:2]), reads=[k_ss], writes=[k_ss], acc=True)
                    ho, k_ho = hop.get()
                    stt(S, ho[:], hn[:], ss[:, 2:3], fg[:], ALU.mult, ALU.mult, [k_hn, k_ss, t_g2], [k_ho])
                    S.dma("act", d["out"][(tile - 2) * 128:(tile - 1) * 128, :], ho[:], reads=[k_ho], writes=[C.T(f"out:{tile}")])
    S.barrier()
```

```python
import numpy as np
import concourse.bass as bass
import concourse.mybir as mybir
from concourse.bass_utils import run_bass_kernel_spmd

F32 = mybir.dt.float32
BF16 = mybir.dt.bfloat16
I32 = mybir.dt.int32
U32 = mybir.dt.uint32
AF = mybir.ActivationFunctionType
ALU = mybir.AluOpType
AX = mybir.AxisListType

ENGS = ("pe", "act", "dve", "pool", "sp")
EPOCH = 12000
NSLOT = 24


class Tok:
    __slots__ = ("name", "writers", "readers")

    def __init__(self, name=""):
        self.name = name
        self.writers = []
        self.readers = []


class Op:
    __slots__ = ("eng", "fn", "deps", "idx", "needed", "cnt", "dma", "slot", "val", "waits")

    def __init__(self, eng, fn, dma=False):
        self.eng = eng
        self.fn = fn
        self.deps = []
        self.idx = -1
        self.needed = False
        self.cnt = -1
        self.dma = dma
        self.slot = -1
        self.val = -1
        self.waits = []


class Sched:
    def __init__(self, nc):
        self.nc = nc
        self.ops = {e: [] for e in ENGS}
        self.ndma = {e: 0 for e in ENGS}
        self.seen = {e: {} for e in ENGS}
        self.pending = {e: [] for e in ENGS}

    def barrier(self):
        waits = []
        for e in ENGS:
            last = None
            for o in reversed(self.ops[e]):
                if not o.dma:
                    last = o
                    break
            if last is not None:
                waits.append(("c", last))
            slots = {}
            for o in self.ops[e]:
                if o.dma:
                    slots[o.slot] = o
            for o in slots.values():
                waits.append(("d", o))
        for e in ENGS:
            self.pending[e] = list(waits)

    def _add(self, op, reads, writes, acc):
        deps = []
        for t in reads:
            deps.extend(t.writers)
        for t in writes:
            deps.extend(t.readers)
            if not acc:
                deps.extend(t.writers)
            else:
                deps.extend(w for w in t.writers if w.eng != op.eng or w.dma != op.dma)
        e = op.eng
        op.idx = len(self.ops[e])
        seen = self.seen[e]
        if self.pending[e]:
            for w in self.pending[e]:
                if w[0] == "c":
                    deps.append(w[1])
                else:
                    deps.append(w[1])
            self.pending[e] = []
            barrier_dep = True
        else:
            barrier_dep = False
        if op.dma:
            n = self.ndma[e]
            self.ndma[e] = n + 1
            op.slot = n % NSLOT
            op.val = 16 * (n // NSLOT + 1)
            if op.val > 16:
                key = ("d", e, op.slot)
                if seen.get(key, 0) < op.val - 16:
                    seen[key] = op.val - 16
                    op.waits.append(("d", e, op.slot, op.val - 16))
        for d in sorted(deps, key=lambda o: -o.idx):
            if d.dma:
                key = ("d", d.eng, d.slot)
                if seen.get(key, 0) >= d.val:
                    continue
                seen[key] = d.val
                op.waits.append(("d", d.eng, d.slot, d.val))
            else:
                if d.eng == e:
                    if e in ("pe", "sp"):
                        continue
                key = ("c", d.eng)
                if seen.get(key, -1) >= d.idx:
                    continue
                seen[key] = d.idx
                d.needed = True
                op.waits.append(("c", d))
        for t in reads:
            t.readers.append(op)
        for t in writes:
            if acc:
                t.writers.append(op)
            else:
                t.writers = [op]
                t.readers = []
        self.ops[e].append(op)
        return op

    def op(self, eng, fn, reads=(), writes=(), acc=False):
        return self._add(Op(eng, fn), list(reads), list(writes), acc)

    def dma(self, eng, out, in_, reads=(), writes=(), acc=False, **kw):
        def fn(en):
            return en.dma_start(out=out, in_=in_, **kw)
        return self._add(Op(eng, fn, dma=True), list(reads), list(writes), acc)

    def dma_fn(self, eng, fn, reads=(), writes=(), acc=False):
        return self._add(Op(eng, fn, dma=True), list(reads), list(writes), acc)

    def emit(self):
        nc = self.nc
        nep = {}
        for e in ENGS:
            c = 0
            for o in self.ops[e]:
                if o.needed and not o.dma:
                    c += 1
                    o.cnt = c
            nep[e] = max(1, (c + EPOCH - 1) // EPOCH)
        import contextlib
        with contextlib.ExitStack() as st:
            csem = {e: [st.enter_context(nc.semaphore(f"c_{e}_{i}")) for i in range(nep[e])]
                    for e in ENGS}
            dsem = {e: [st.enter_context(nc.semaphore(f"d_{e}_{i}")) for i in range(NSLOT)]
                    for e in ENGS if self.ndma[e] > 0}
            block = st.enter_context(nc.Block())
            hw = {"pe": block.tensor, "act": block.scalar, "dve": block.vector,
                  "pool": block.gpsimd, "sp": block.sync}

            def body(e):
                def run(en):
                    for o in self.ops[e]:
                        for w in o.waits:
                            if w[0] == "d":
                                en.wait_ge(dsem[w[1]][w[2]], w[3])
                            else:
                                d = w[1]
                                en.wait_ge(csem[d.eng][(d.cnt - 1) // EPOCH], (d.cnt - 1) % EPOCH + 1)
                        ins = o.fn(en)
                        if o.dma:
                            ins.then_inc(dsem[e][o.slot], 16)
                        elif o.needed:
                            ins.then_inc(csem[e][(o.cnt - 1) // EPOCH], 1)
                    if self.ndma[e] > 0:
                        last = {}
                        for o in self.ops[e]:
                            if o.dma:
                                last[o.slot] = o.val
                        for s, v in last.items():
                            en.wait_ge(dsem[e][s], v)
                return run

            for e in ENGS:
                if self.ops[e]:
                    hw[e](body(e))


DM = 1024
SEQ = 4096
LC = 256
NTOK = SEQ + LC
NT = NTOK // 128
DEPTH = 2
EPS = 1e-6
STOP = 0
QT_LIMIT = 0


class Ctx:
    def __init__(self, nc, S):
        self.nc = nc
        self.S = S
        self.d = {}
        self.tok = {}

    def T(self, name):
        t = self.tok.get(name)
        if t is None:
            t = Tok(name)
            self.tok[name] = t
        return t


def phase_mod(C, l):
    nc, S = C.nc, C.S
    wmod = C.d["w_mod"][l].rearrange("(kc p) n -> p kc n", p=128)
    with nc.sbuf_tensor(f"m_ct{l}", [128, 8, 2], F32) as ct, \
            nc.sbuf_tensor(f"m_sct{l}", [128, 8, 2], F32) as sct, \
            nc.sbuf_tensor(f"m_w0{l}", [128, 8, 512], F32) as w0, \
            nc.sbuf_tensor(f"m_w1{l}", [128, 8, 512], F32) as w1, \
            nc.sbuf_tensor(f"m_b{l}", [2, 6144], F32) as bm, \
            nc.sbuf_tensor(f"m_row{l}", [2, 6144], F32) as mrow, \
            nc.psum_tensor(f"m_p0{l}", [2, 512], F32) as p0, \
            nc.psum_tensor(f"m_p1{l}", [2, 512], F32) as p1:
        t_ct, t_sct, t_bm, t_row = Tok(), Tok(), Tok(), Tok()
        t_w = [Tok(), Tok()]
        t_p = [Tok(), Tok()]
        wb = [w0, w1]
        pb = [p0, p1]
        S.dma("sp", ct[:], C.d["cT"][:], writes=[t_ct])
        S.dma("sp", bm[:], C.d["b_mod"][l:l + 1, :].to_broadcast([2, 6144]), writes=[t_bm])
        S.op("act", lambda e: e.activation(out=sct[:], in_=ct[:], func=AF.Silu),
             reads=[t_ct], writes=[t_sct])
        for n in range(12):
            S.dma("sp", wb[n % 2][:], wmod[:, :, n * 512:(n + 1) * 512], writes=[t_w[n % 2]])
            for kc in range(8):
                S.op("pe", lambda e, n=n, kc=kc: e.matmul(pb[n % 2][:], lhsT=sct[:, kc, :],
                                                       rhs=wb[n % 2][:, kc, :],
                                                       start=(kc == 0), stop=(kc == 7)),
                     reads=[t_sct, t_w[n % 2]], writes=[t_p[n % 2]], acc=(kc > 0))
            S.op("dve", lambda e, n=n: e.tensor_tensor(out=mrow[:, n * 512:(n + 1) * 512],
                                                      in0=pb[n % 2][:],
                                                      in1=bm[:, n * 512:(n + 1) * 512], op=ALU.add),
                 reads=[t_p[n % 2], t_bm], writes=[t_row], acc=True)
        S.dma("sp", C.d["modv"][l], mrow[:], reads=[t_row], writes=[C.T(f"modv{l}")])
    S.barrier()


class PsumPool:
    def __init__(self, C, st, n, name, shape=(128, 512), dtype=F32):
        self.tiles = [st.enter_context(C.nc.psum_tensor(f"{name}{i}", list(shape), dtype)) for i in range(n)]
        self.toks = [Tok(f"{name}{i}") for i in range(n)]
        self.i = 0

    def get(self):
        i = self.i
        self.i = (i + 1) % len(self.tiles)
        return self.tiles[i], self.toks[i]


class SbPool:
    def __init__(self, C, st, n, name, shape, dtype):
        self.tiles = [st.enter_context(C.nc.sbuf_tensor(f"{name}{i}", list(shape), dtype)) for i in range(n)]
        self.toks = [Tok(f"{name}{i}") for i in range(n)]
        self.i = 0

    def get(self):
        i = self.i
        self.i = (i + 1) % len(self.tiles)
        return self.tiles[i], self.toks[i]


def mm(S, out, lhsT, rhs, first, last, reads, wtok):
    S.op("pe", lambda e: e.matmul(out, lhsT=lhsT, rhs=rhs, start=first, stop=last),
         reads=reads, writes=[wtok], acc=not first)


def act(S, out, in_, func, reads, writes, acc=False, **kw):
    S.op("act", lambda e: e.activation(out=out, in_=in_, func=func, **kw), reads=reads, writes=writes, acc=acc)


def tt(S, eng, out, in0, in1, op, reads, writes, acc=False):
    S.op(eng, lambda e: e.tensor_tensor(out=out, in0=in0, in1=in1, op=op), reads=reads, writes=writes, acc=acc)


def ts(S, eng, out, in0, s1, s2, op0, op1, reads, writes, acc=False):
    if op1 is None:
        S.op(eng, lambda e: e.tensor_scalar(out=out, in0=in0, scalar1=s1, scalar2=None, op0=op0),
             reads=reads, writes=writes, acc=acc)
    else:
        S.op(eng, lambda e: e.tensor_scalar(out=out, in0=in0, scalar1=s1, scalar2=s2, op0=op0, op1=op1),
             reads=reads, writes=writes, acc=acc)


def stt(S, out, in0, scalar, in1, op0, op1, reads, writes, acc=False):
    S.op("dve", lambda e: e.scalar_tensor_tensor(out=out, in0=in0, scalar=scalar, in1=in1, op0=op0, op1=op1),
         reads=reads, writes=writes, acc=acc)


def cp(S, eng, out, in_, reads, writes, acc=False):
    if eng == "act":
        S.op("act", lambda e: e.copy(out=out, in_=in_), reads=reads, writes=writes, acc=acc)
    else:
        S.op(eng, lambda e: e.tensor_copy(out=out, in_=in_), reads=reads, writes=writes, acc=acc)


def make_ident(C, st):
    nc, S = C.nc, C.S
    idi = st.enter_context(nc.sbuf_tensor("c_idi", [128, 128], I32))
    idb = st.enter_context(nc.sbuf_tensor("c_idb", [128, 128], BF16))
    idf = st.enter_context(nc.sbuf_tensor("c_idf", [128, 128], F32))
    onb = st.enter_context(nc.sbuf_tensor("c_onb", [128, 128], BF16))
    t_i, t_c = Tok(), Tok("consts")
    S.op("pool", lambda e: e.iota(idi[:], pattern=[[1, 128]], base=0, channel_multiplier=-1), writes=[t_i])
    ts(S, "dve", idb[:], idi[:], 0, None, ALU.is_equal, None, [t_i], [t_c], acc=True)
    ts(S, "dve", idf[:], idi[:], 0, None, ALU.is_equal, None, [t_i], [t_c], acc=True)
    S.op("dve", lambda e: e.memset(onb[:], 1.0), writes=[t_c], acc=True)
    utri = st.enter_context(nc.sbuf_tensor("c_utri", [128, 128], BF16))
    ts(S, "dve", utri[:], idi[:], 0, None, ALU.is_gt, None, [t_i], [t_c], acc=True)
    C.utri = utri
    C.idb, C.idf, C.onb, C.t_c = idb, idf, onb, t_c


O_QA, O_QAS, O_KA, O_KAS, O_KR, O_KRS, O_CQ, O_CKV, O_U, O_VA, O_G, W_EXT = (
    0, 512, 1024, 1152, 1280, 1312, 1344, 1600, 1856, 2368, 2496, 5568)
W_N = O_G
GROUPS = [(0, 256)] + [(256 + 512 * i, 512) for i in range(8)]


def bcast_row(ap_row, n):
    return ap_row.to_broadcast([128, n])


def load_cast(C, st, name, dst, src3, nk, ncol, t_dst, scale_ap=None, scale_tok=None, half=2784):
    S = C.S
    stg = SbPool(C, st, 2, name, [128, half], F32)
    for kc in range(nk):
        for c0 in range(0, ncol, half):
            w = min(half, ncol - c0)
            s_t, s_k = stg.get()
            S.dma("sp", s_t[:, 0:w], src3[:, kc, c0:c0 + w], writes=[s_k])
            if scale_ap is None:
                cp(S, "act", dst[:, kc, c0:c0 + w], s_t[:, 0:w], [s_k], [t_dst], acc=True)
            else:
                ts(S, "dve", dst[:, kc, c0:c0 + w], s_t[:, 0:w], scale_ap(kc), None, ALU.mult, None,
                   [s_k, scale_tok], [t_dst], acc=True)


def phase_inproj(C, l):
    nc, S = C.nc, C.S
    import contextlib
    d = C.d
    hsrc = d["h0"] if l == 0 else d["h_s"]
    with contextlib.ExitStack() as st:
        sb = lambda name, shape, dt: st.enter_context(nc.sbuf_tensor(f"n{l}_{name}", list(shape), dt))
        wsb = sb("w", [128, 8, W_N], BF16)
        wq = sb("wq", [128, 2, 1024], BF16)
        wkv = sb("wkv", [128, 2, 1024], BF16)
        gq = sb("gq", [128, 4], F32)
        Gm, Sm = sb("Gm", [128, 2, 1024], F32), sb("Sm", [128, 2, 1024], F32)
        t_w, t_wq, t_gq, t_mod = Tok(), Tok(), Tok(), Tok()
        wext = d["w_in_ext"][l].rearrange("(kc p) n -> p kc n", p=128)
        load_cast(C, st, f"n{l}_stg", wsb, wext, 8, W_N, t_w)
        S.dma("sp", gq[:, 0:2], d["q_norm_gT"][l], writes=[t_gq], acc=True)
        S.dma("sp", gq[:, 2:4], d["kv_norm_gT"][l], writes=[t_gq], acc=True)
        for wi, (wname, wdst) in enumerate((("w_uq_ext", wq), ("w_ukv_ext", wkv))):
            wsrc = d[wname][l].rearrange("(kc p) n -> p kc n", p=128)
            load_cast(C, st, f"n{l}_stg{wi}", wdst, wsrc, 2, 1024, t_wq,
                      scale_ap=lambda kc, wi=wi: gq[:, 2 * wi + kc:2 * wi + kc + 1], scale_tok=t_gq, half=1024)
        mv = d["modv"][l]
        tmpp = SbPool(C, st, 2, f"n{l}_tmp", [128, 1024], F32)
        gn, k_gn = tmpp.get()
        S.dma("sp", gn[:], bcast_row(d["norm_mix_g"][l:l + 1, :], 1024), writes=[k_gn])
        for r in range(2):
            S.dma("sp", Sm[:, r, :], bcast_row(mv[r:r + 1, 0:1024], 1024), reads=[C.T(f"modv{l}")], writes=[t_mod], acc=True)
            S.dma("sp", Gm[:, r, :], bcast_row(mv[r:r + 1, 1024:2048], 1024), reads=[C.T(f"modv{l}")], writes=[t_mod], acc=True)
        t_mod2 = Tok()
        for r in range(2):
            stt(S, Gm[:, r, :], Gm[:, r, :], 1.0, gn[:], ALU.add, ALU.mult, [t_mod, k_gn], [t_mod2], acc=True)
        if STOP == 1:
            return
        pp = PsumPool(C, st, 5, f"n{l}_pp")
        ptp = PsumPool(C, st, 2, f"n{l}_ptp", (128, 8, 128), BF16)
        hp = SbPool(C, st, 3, f"n{l}_h", [128, 1024], F32)
        hxp = SbPool(C, st, 8, f"n{l}_hx", [128, 1024], BF16)
        hxTp = SbPool(C, st, 2, f"n{l}_hxT", [128, 8, 512], BF16)
        ssp = SbPool(C, st, 4, f"n{l}_ss", [128, 4], F32)
        sqj = sb("sqj", [128, 1024], BF16)
        t_sqj = Tok()
        ropep = SbPool(C, st, 1, f"n{l}_rope", [128, 4, 512], F32)
        r1p = SbPool(C, st, 1, f"n{l}_r1", [128, 512], F32)
        r2p = SbPool(C, st, 1, f"n{l}_r2", [128, 512], F32)
        o4p = SbPool(C, st, 4, f"n{l}_o4", [128, 4, 512], BF16)
        krTp = SbPool(C, st, 1, f"n{l}_krT", [32, 512], BF16)
        latfp = SbPool(C, st, 1, f"n{l}_latf", [128, 2, 512], F32)
        latqp = SbPool(C, st, 1, f"n{l}_latq", [128, 2, 512], BF16)
        rbcp = SbPool(C, st, 1, f"n{l}_rbc", [128, 512], F32)
        cqnp = SbPool(C, st, 1, f"n{l}_cqn", [128, 2, 512], BF16)
        ckvnp = SbPool(C, st, 1, f"n{l}_ckvn", [128, 2, 512], BF16)
        vap = SbPool(C, st, 1, f"n{l}_va", [128, 4, 2, 65], BF16)
        vbp = SbPool(C, st, 1, f"n{l}_vb", [128, 4, 8, 65], BF16)
        for pool_ in (vap, vbp):
            for t_, k_ in zip(pool_.tiles, pool_.toks):
                S.op("pool", lambda e, t_=t_: e.memset(t_[:], 1.0), writes=[k_])
        t_all = [t_mod2, C.t_c]

        gst = {}

        def normC(g):
            t0, n = GROUPS[g]
            ntl = n // 128
            isx = g > 0
            mr = 0 if isx else 1
            G = {"hx": []}
            if isx:
                rp, k_rp = ropep.get()
                p0 = t0 - 256
                for i, nm in enumerate(("ropeA_c", "ropeA_s", "ropeB_c", "ropeB_s")):
                    S.dma("sp", rp[:, i, :], d[nm][:, p0:p0 + 512], writes=[k_rp], acc=(i > 0))
                G["rp"] = (rp, k_rp)
            for ti in range(ntl):
                r0 = t0 + ti * 128
                ht, k_h = hp.get()
                S.dma("sp", ht[:], hsrc[r0:r0 + 128, :], reads=[C.T(f"h:{r0 // 128}")], writes=[k_h])
                ss, k_ss = ssp.get()
                act(S, sqj[:], ht[:], AF.Square, [k_h], [t_sqj, k_ss], accum_out=ss[:, 0:1])
                act(S, ss[:, 1:2], ss[:, 0:1], AF.Sqrt, [k_ss], [k_ss], acc=True, scale=1.0 / DM, bias=EPS)
                S.op("dve", lambda e, ss=ss: e.reciprocal(out=ss[:, 2:3], in_=ss[:, 1:2]), reads=[k_ss], writes=[k_ss], acc=True)
                tm, k_tm = tmpp.get()
                stt(S, tm[:], ht[:], ss[:, 2:3], Gm[:, mr, :], ALU.mult, ALU.mult, [k_h, k_ss] + t_all, [k_tm])
                hx, k_hx = hxp.get()
                tt(S, "dve", hx[:], tm[:], Sm[:, mr, :], ALU.add, [k_tm] + t_all, [k_hx])
                G["hx"].append((hx, k_hx))
            gst[g] = G

        def normT(g):
            t0, n = GROUPS[g]
            G = gst[g]
            hxT, k_hxT = hxTp.get()
            for ti, (hx, k_hx) in enumerate(G["hx"]):
                tp, k_tp = ptp.get()
                for kc in range(8):
                    S.op("pe", lambda e, tp=tp, hx=hx, kc=kc: e.transpose(tp[:, kc, :], hx[:, kc * 128:(kc + 1) * 128], C.idb[:]),
                         reads=[k_hx, C.t_c], writes=[k_tp], acc=(kc > 0))
                cp(S, "act", hxT[:, :, ti * 128:(ti + 1) * 128], tp[:], [k_tp], [k_hxT], acc=(ti > 0))
            G["hxT"] = (hxT, k_hxT)

        def part(g, which):
            t0, n = GROUPS[g]
            ntl = n // 128
            isx = g > 0
            G = gst[g]
            hxT, k_hxT = G["hxT"]
            if isx:
                rp, k_rp = G["rp"]
            if which == 2:
                (cqn, k_cqn), (ckvn, k_ckvn) = G["lat"]
            def proj(col0, m, ps):
                pt, pk = ps
                for kc in range(8):
                    mm(S, pt[0:m, 0:n], wsb[:, kc, col0:col0 + m], hxT[:, kc, 0:n], kc == 0, kc == 7,
                       [t_w, k_hxT], pk)

            def rope_evac(prj, col0, cols0, m, out_ap, out_k, ri, acc):
                ps = pp.get()
                prj(col0, m, ps)
                if not isx:
                    cp(S, "act", out_ap, ps[0][0:m, 0:n], [ps[1]], [out_k], acc=acc)
                    return
                ps2 = pp.get()
                prj(cols0, m, ps2)
                r1, k1 = r1p.get()
                r2, k2 = r2p.get()
                tt(S, "dve", r1[0:m, 0:n], ps[0][0:m, 0:n], rp[0:m, ri, 0:n], ALU.mult, [ps[1], k_rp], [k1])
                tt(S, "dve", r2[0:m, 0:n], ps2[0][0:m, 0:n], rp[0:m, ri + 1, 0:n], ALU.mult, [ps2[1], k_rp], [k2])
                tt(S, "dve", out_ap, r1[0:m, 0:n], r2[0:m, 0:n], ALU.add, [k1, k2], [out_k], acc=acc)

            if which == 1:
                S.dma("pool", d["hxT_s"].rearrange("c p t -> p c t")[:, :, t0:t0 + n], hxT[:, :, 0:n],
                      reads=[k_hxT], writes=[C.T(f"hxT:{g}")])

                lat_out = []
                for (col0, npool) in ((O_CQ, cqnp), (O_CKV, ckvnp)):
                    lf, k_lf = latfp.get()
                    lq, k_lq = latqp.get()
                    for c in range(2):
                        ps = pp.get()
                        proj(col0 + c * 128, 128, ps)
                        act(S, lq[:, c, 0:n], ps[0][:, 0:n], AF.Square, [ps[1]], [k_lq], acc=(c > 0))
                        cp(S, "act", lf[:, c, 0:n], ps[0][:, 0:n], [ps[1]], [k_lf], acc=(c > 0))
                    pt, pk = pp.get()
                    for c in range(2):
                        mm(S, pt[:, 0:n], C.onb[:], lq[:, c, 0:n], c == 0, c == 1, [k_lq, C.t_c], pk)
                    rb, k_rb = rbcp.get()
                    act(S, rb[:, 0:n], pt[:, 0:n], AF.Sqrt, [pk], [k_rb], scale=1.0 / 256, bias=EPS)
                    S.op("dve", lambda e, rb=rb, n=n: e.reciprocal(out=rb[:, 0:n], in_=rb[:, 0:n]), reads=[k_rb], writes=[k_rb], acc=True)
                    ln, k_ln = npool.get()
                    for c in range(2):
                        tt(S, "dve", ln[:, c, 0:n], lf[:, c, 0:n], rb[:, 0:n], ALU.mult, [k_lf, k_rb], [k_ln], acc=(c > 0))
                    lat_out.append((ln, k_ln))
                G["lat"] = lat_out

                qaT, k_qaT = o4p.get()
                for c in range(4):
                    rope_evac(proj, O_QA + c * 128, O_QAS + c * 128, 128, qaT[:, c, 0:n], k_qaT, 0, c > 0)
                S.dma("pool", d["qaT_s"].rearrange("c p t -> p c t")[:, :, t0:t0 + n], qaT[:, :, 0:n],
                      reads=[k_qaT], writes=[C.T(f"qaT:{g}")])
                kaT, k_kaT = o4p.get()
                rope_evac(proj, O_KA, O_KAS, 128, kaT[:, 0, 0:n], k_kaT, 0, False)
                S.dma("pool", d["kaT_s"][:, t0:t0 + n], kaT[:, 0, 0:n], reads=[k_kaT], writes=[C.T(f"kaT:{g}")])
                krT, k_krT = krTp.get()
                rope_evac(proj, O_KR, O_KRS, 32, krT[:, 0:n], k_krT, 2, False)
                for h in range(8):
                    S.dma("pool", d["kbT_s"][h, 64:96, t0:t0 + n], krT[:, 0:n], reads=[k_krT],
                          writes=[C.T(f"kbT:{g}")], acc=True)
                uT, k_uT = o4p.get()
                for c in range(4):
                    ps = pp.get()
                    proj(O_U + c * 128, 128, ps)
                    cp(S, "act", uT[:, c, 0:n], ps[0][:, 0:n], [ps[1]], [k_uT], acc=(c > 0))
                S.dma("pool", d["uT_s"].rearrange("c p t -> p c t")[:, :, t0:t0 + n], uT[:, :, 0:n],
                      reads=[k_uT], writes=[C.T(f"uT:{g}")])
                va, k_va = vap.get()
                for ti in range(ntl):
                    pt, pk = pp.get()
                    for kc in range(8):
                        mm(S, pt[:, 0:128], hxT[:, kc, ti * 128:(ti + 1) * 128], wsb[:, kc, O_VA:O_VA + 128],
                           kc == 0, kc == 7, [t_w, k_hxT], pk)
                    cp(S, "act", va[:, ti, :, 0:64], pt[:, 0:128].rearrange("p (g e) -> p g e", g=2), [pk], [k_va], acc=(ti > 0))
                S.dma("pool", d["va_s"][t0:t0 + n, :].rearrange("(i p) f -> p i f", p=128),
                      va[:, 0:ntl, :, :].rearrange("p i g e -> p i (g e)"), reads=[k_va], writes=[C.T(f"va:{g}")])
            else:
                def mkproj2(wt, lat, k_lat):
                    def proj2(col0, m, ps):
                        pt, pk = ps
                        for kc in range(2):
                            mm(S, pt[0:m, 0:n], wt[:, kc, col0:col0 + m], lat[:, kc, 0:n], kc == 0, kc == 1, [t_wq, k_lat], pk)
                    return proj2
                pq = mkproj2(wq, cqn, k_cqn)
                pk_ = mkproj2(wkv, ckvn, k_ckvn)
                qnT, k_qnT = o4p.get()
                for c in range(4):
                    ps = pp.get()
                    pq(c * 128, 128, ps)
                    cp(S, "act", qnT[:, c, 0:n], ps[0][:, 0:n], [ps[1]], [k_qnT], acc=(c > 0))
                for h in range(8):
                    S.dma("pool", d["qbT_s"][h, 0:64, t0:t0 + n], qnT[(h % 2) * 64:(h % 2) * 64 + 64, h // 2, 0:n],
                          reads=[k_qnT], writes=[C.T(f"qbT:{g}")], acc=True)
                qrT, k_qrT = o4p.get()
                for c in range(2):
                    rope_evac(pq, 512 + c * 128, 768 + c * 128, 128, qrT[:, c, 0:n], k_qrT, 2, c > 0)
                for h in range(8):
                    S.dma("pool", d["qbT_s"][h, 64:96, t0:t0 + n], qrT[(h % 4) * 32:(h % 4) * 32 + 32, h // 4, 0:n],
                          reads=[k_qrT], writes=[C.T(f"qbT:{g}")], acc=True)
                knT, k_knT = o4p.get()
                for c in range(4):
                    ps = pp.get()
                    pk_(c * 128, 128, ps)
                    cp(S, "act", knT[:, c, 0:n], ps[0][:, 0:n], [ps[1]], [k_knT], acc=(c > 0))
                for h in range(8):
                    S.dma("pool", d["kbT_s"][h, 0:64, t0:t0 + n], knT[(h % 2) * 64:(h % 2) * 64 + 64, h // 2, 0:n],
                          reads=[k_knT], writes=[C.T(f"kbT:{g}")], acc=True)
                vb, k_vb = vbp.get()
                for ti in range(ntl):
                    pt, pk = pp.get()
                    for kc in range(2):
                        mm(S, pt[:, :], ckvn[:, kc, ti * 128:(ti + 1) * 128], wkv[:, kc, 512:1024], kc == 0, kc == 1,
                           [t_wq, k_ckvn], pk)
                    cp(S, "act", vb[:, ti, :, 0:64], pt[:, :].rearrange("p (g e) -> p g e", g=8), [pk], [k_vb], acc=(ti > 0))
                S.dma("pool", d["vb_s"][t0:t0 + n, :].rearrange("(i p) f -> p i f", p=128),
                      vb[:, 0:ntl, :, :].rearrange("p i g e -> p i (g e)"), reads=[k_vb], writes=[C.T(f"vb:{g}")])

        ng = len(GROUPS)
        normC(0)
        normT(0)
        for g in range(ng):
            part(g, 1)
            if g + 1 < ng:
                normC(g + 1)
            part(g, 2)
            if g + 1 < ng:
                normT(g + 1)
    S.barrier()


def _deint(n):
    return np.concatenate([np.arange(0, n, 2), np.arange(1, n, 2)])


def _deint_sw(n):
    return np.concatenate([np.arange(1, n, 2), np.arange(0, n, 2)])


def _w_in_cols():
    cols = []
    for sw in (False, True):
        for c in range(4):
            for h in (c, 4 + c):
                cols.append(h * 64 + (_deint_sw(64) if sw else _deint(64)))
    for sw in (False, True):
        for g in range(2):
            cols.append(512 + g * 64 + (_deint_sw(64) if sw else _deint(64)))
    cols.append(1280 + _deint(32))
    cols.append(1280 + _deint_sw(32))
    cols.append(np.arange(768, 1280))
    cols.append(np.arange(1312, 1824))
    cols.append(np.arange(640, 768))
    cols.append(np.arange(1824, 4896))
    out = np.concatenate(cols)
    assert out.shape[0] == W_EXT
    return out


def _w_uq_cols():
    cols = [h * 96 + np.arange(64) for h in range(8)]
    cols += [h * 96 + 64 + _deint(32) for h in range(8)]
    cols += [h * 96 + 64 + _deint_sw(32) for h in range(8)]
    return np.concatenate(cols)


def _w_ukv_cols():
    cols = [h * 128 + np.arange(64) for h in range(8)]
    cols += [h * 128 + 64 + np.arange(64) for h in range(8)]
    return np.concatenate(cols)


def _rope_tables():
    t = np.arange(SEQ)
    row = (t // 64).astype(np.float32)
    col = (t % 64).astype(np.float32)

    def tabs(rot):
        axis_dim = rot // 2
        inv = (np.float32(10000.0) ** (-np.arange(0, axis_dim, 2, dtype=np.float32) / np.float32(axis_dim))).astype(np.float32)
        ang = np.concatenate([row[:, None] * inv, col[:, None] * inv], axis=-1).astype(np.float32)
        cos, sin = np.cos(ang).astype(np.float32), np.sin(ang).astype(np.float32)
        half = rot // 2
        ct = np.concatenate([cos, cos], axis=1).T
        st_ = np.concatenate([-sin, sin], axis=1).T
        rep = 128 // rot
        return np.ascontiguousarray(np.tile(ct, (rep, 1))), np.ascontiguousarray(np.tile(st_, (rep, 1)))
    ca, sa = tabs(64)
    cb, sb_ = tabs(32)
    return ca, sa, cb, sb_


SCRATCH = {
    "modv": ([2, 2, 6144], F32),
    "h_s": ([NTOK, DM], F32),
    "hxT_s": ([8, 128, NTOK], BF16),
    "qaT_s": ([4, 128, NTOK], BF16),
    "kaT_s": ([128, NTOK], BF16),
    "va_s": ([NTOK, 130], BF16),
    "qbT_s": ([8, 96, NTOK], BF16),
    "kbT_s": ([8, 96, NTOK], BF16),
    "vb_s": ([NTOK, 520], BF16),
    "uT_s": ([4, 128, NTOK], BF16),
    "oaT_s": ([4, 128, NTOK], BF16),
    "obT_s": ([4, 128, NTOK], BF16),
    "ocT_s": ([4, 128, NTOK], BF16),
    "gT_s": ([32, NTOK], F32),
    "bc_s": ([8, 512], F32),
    "wgu_b": ([4096, 4096], BF16),
    "wdn_b": ([4096, 2048], BF16),
    "xbuf": ([12800, DM], BF16),
    "ybuf": ([12800, DM], F32),
}
INPUTS = {
    "h0": ([NTOK, DM], F32),
    "cT": ([128, 8, 2], F32),
    "w_mod": ([2, DM, 6144], F32),
    "b_mod": ([2, 6144], F32),
    "norm_mix_g": ([2, DM], F32),
    "norm_ffn_g": ([2, DM], F32),
    "w_in_ext": ([2, DM, W_EXT], F32),
    "w_uq_ext": ([2, 256, 1024], F32),
    "w_ukv_ext": ([2, 256, 1024], F32),
    "q_norm_gT": ([2, 128, 2], F32),
    "kv_norm_gT": ([2, 128, 2], F32),
    "ropeA_c": ([128, SEQ], F32),
    "ropeA_s": ([128, SEQ], F32),
    "ropeB_c": ([128, SEQ], F32),
    "ropeB_s": ([128, SEQ], F32),
    "sink": ([2, 8], F32),
    "sink_row": ([2, 2, 512], F32),
    "w_pool": ([2, 4, 128, 128], F32),
    "pool_scaleT": ([2, 128, 4], F32),
    "invx": ([4, SEQ], F32),
    "invc": ([4, LC], F32),
    "w_br": ([2, 3, 512, DM], F32),
    "w_out": ([2, DM, DM], F32),
    "w_r": ([2, DM, 36], F32),
    "b_r": ([2, 36], F32),
    "w_gu_h": ([2, 32 * 128, 4096], F32),
    "w_dn_h": ([2, 32 * 128, 2048], F32),
    "final_g": ([1, DM], F32),
}


def prep_shared(inp):
    out = {}
    f = lambda a: np.ascontiguousarray(np.asarray(a, dtype=np.float32))
    out["w_mod"] = f(inp["w_mod"])
    out["b_mod"] = f(inp["b_mod"])
    out["norm_mix_g"] = f(inp["norm_mix_g"])
    out["norm_ffn_g"] = f(inp["norm_ffn_g"])
    out["sink"] = f(inp["sink"])
    out["sink_row"] = f(np.repeat(np.asarray(inp["sink"]).reshape(2, 2, 4), 128, axis=-1))
    out["w_pool"] = f(inp["w_pool"])
    out["pool_scaleT"] = f(np.asarray(inp["pool_scale"]).reshape(2, 4, 128).transpose(0, 2, 1))
    out["w_br"] = f(np.stack([np.asarray(inp[k]) for k in ("w_br_a", "w_br_b", "w_br_c")], axis=1))
    out["w_out"] = f(inp["w_out"])
    out["w_r"] = f(np.concatenate([np.asarray(inp["w_rg"]), np.asarray(inp["w_re"])], axis=-1))
    out["b_r"] = f(np.concatenate([np.asarray(inp["b_rg"]), np.asarray(inp["b_re"])], axis=-1))
    out["w_gu_h"] = f(np.asarray(inp["w_gu"]).reshape(2, 32, 8, 128, 512).transpose(0, 1, 3, 2, 4).reshape(2, 32 * 128, 4096))
    out["w_dn_h"] = f(np.asarray(inp["w_dn"]).reshape(2, 32, 2, 128, 1024).transpose(0, 1, 3, 2, 4).reshape(2, 32 * 128, 2048))
    out["final_g"] = f(np.asarray(inp["final_g"]).reshape(1, DM))
    for nm, L in (("invx", SEQ), ("invc", LC)):
        t = np.arange(L)
        rows = []
        for r in (1, 2, 4, 8):
            cnt = np.minimum(t + r + 1, L) - np.maximum(t - r, 0)
            rows.append((1.0 / cnt.astype(np.float32)).astype(np.float32))
        out[nm] = np.stack(rows)
    out["w_in_ext"] = f(np.asarray(inp["w_in"])[:, :, _w_in_cols()])
    out["w_uq_ext"] = f(np.asarray(inp["w_uq"])[:, :, _w_uq_cols()])
    out["w_ukv_ext"] = f(np.asarray(inp["w_ukv"])[:, :, _w_ukv_cols()])
    out["q_norm_gT"] = f(np.asarray(inp["q_norm_g"]).reshape(2, 2, 128).transpose(0, 2, 1))
    out["kv_norm_gT"] = f(np.asarray(inp["kv_norm_g"]).reshape(2, 2, 128).transpose(0, 2, 1))
    ca, sa, cb, sb_ = _rope_tables()
    out["ropeA_c"], out["ropeA_s"], out["ropeB_c"], out["ropeB_s"] = ca, sa, cb, sb_
    return out


def prep_core(inp, b):
    out = {}
    x, ctx, c, cc = (np.asarray(inp[k], dtype=np.float32) for k in ("x", "ctx", "c", "c_ctx"))
    out["h0"] = np.ascontiguousarray(np.concatenate([ctx[b], x[b]], axis=0))
    out["cT"] = np.ascontiguousarray(np.stack([c[b].reshape(8, 128).T, cc.reshape(8, 128).T], axis=-1))
    return out


def build_program(phases, debug_out=()):
    nc = bass.Bass("TRN2", target_bir_lowering=False)
    S = Sched(nc)
    C = Ctx(nc, S)
    for k, (shape, dt) in INPUTS.items():
        C.d[k] = nc.dram_tensor(k, list(shape), dt, kind="ExternalInput").ap()
    for k, (shape, dt) in SCRATCH.items():
        kind = "ExternalOutput" if k in debug_out else "Internal"
        C.d[k] = nc.dram_tensor(k, list(shape), dt, kind=kind).ap()
    import contextlib
    with contextlib.ExitStack() as st:
        make_ident(C, st)
        phases(C)
        S.emit()
    return nc


NEG = -30000.0


def attn_pipeline(C, st, name, items, scale, hook=None, hook_every=1, LA=2):
    S = C.S
    pps = PsumPool(C, st, LA + 1, f"{name}_ps", (128, 2, 512), F32)
    pac = PsumPool(C, st, 2, f"{name}_ac", (128, 512), F32)
    ptp = SbPool(C, st, LA + 2, f"{name}_pt", [128, 2, 512], BF16)
    steps = []
    for it in items:
        ks = it["keys"]
        for a in range(0, len(ks), 2):
            steps.append((it, a, ks[a:a + 2]))
    state = {}

    def emit_S(si):
        it, a, chunk = steps[si]
        n = it["n"]
        if "q" not in it:
            it["load_q"](it)
        ps, k_ps = pps.get()
        for jj, (k_ap, v_ap, msk) in enumerate(chunk):
            mm(S, ps[:, jj, 0:n], k_ap, it["q"], True, msk is None, it["kv_reads"] + it["q_reads"], k_ps)
            if msk is not None:
                mm(S, ps[:, jj, 0:n], C.idb[:], msk[:, 0:n], False, True, [it["m_tok"], C.t_c], k_ps)
        pt, k_pt = ptp.get()
        act(S, pt[:, 0:len(chunk), 0:n], ps[:, 0:len(chunk), 0:n], AF.Exp, [k_ps], [k_pt], scale=scale)
        state[si] = (pt, k_pt)

    def emit_PV(si):
        it, a, chunk = steps[si]
        n = it["n"]
        pt, k_pt = state.pop(si)
        if a == 0:
            it["acc"] = pac.get()
        acc, k_acc = it["acc"]
        nk = len(it["keys"])
        for jj, (k_ap, v_ap, msk) in enumerate(chunk):
            mm(S, acc[0:65, 0:n], v_ap, pt[:, jj, 0:n], a + jj == 0, a + jj == nk - 1, [k_pt] + it["kv_reads"], k_acc)
        if a + len(chunk) == nk:
            it["finish"](acc, k_acc)

    if not steps:
        return
    for si in range(min(LA, len(steps))):
        emit_S(si)
    for si in range(len(steps)):
        if hook is not None and si % hook_every == 0:
            hook(si // hook_every)
        if si + LA < len(steps):
            emit_S(si + LA)
        emit_PV(si)


def _norm_store(C, pools, acc, k_acc, n, add_ap, add_tok, dst_ap, wtoks):
    S = C.S
    rsp, bcp, bcsp, op_, onesf = pools
    rs, k_rs = rsp.get()
    if add_ap is not None:
        tt(S, "dve", rs[64:65, 0:n], acc[64:65, 0:n], add_ap, ALU.add, [k_acc, add_tok], [k_rs])
        S.op("dve", lambda e, rs=rs, n=n: e.reciprocal(out=rs[64:65, 0:n], in_=rs[64:65, 0:n]), reads=[k_rs], writes=[k_rs], acc=True)
    else:
        S.op("dve", lambda e, rs=rs, n=n, acc=acc: e.reciprocal(out=rs[64:65, 0:n], in_=acc[64:65, 0:n]), reads=[k_acc], writes=[k_rs])
    bcs, k_bcs = bcsp.get()
    if bcp is None:
        slot = C.bc_slot = (getattr(C, "bc_slot", -1) + 1) % 8
        brow = C.d["bc_s"][slot:slot + 1, 0:n]
        S.dma("pool", brow, rs[64:65, 0:n], reads=[k_rs], writes=[C.T(f"bc_s:{slot}")])
        S.dma("pool", bcs[:, 0:n], brow.to_broadcast([64, n]), reads=[C.T(f"bc_s:{slot}")], writes=[k_bcs])
    else:
        bc, k_bc = bcp.get()
        mm(S, bc[0:64, 0:n], onesf[64:65, 0:64], rs[64:65, 0:n], True, True, [k_rs, C.t_c], k_bc)
        cp(S, "act", bcs[:, 0:n], bc[0:64, 0:n], [k_bc], [k_bcs])
    o, k_o = op_.get()
    tt(S, "dve", o[:, 0:n], acc[0:64, 0:n], bcs[:, 0:n], ALU.mult, [k_acc, k_bcs], [k_o])
    S.dma("sp", dst_ap(o), o[:, 0:n] if dst_ap.flat else o[:, 0:n].rearrange("p (u t) -> p u t", u=4), reads=[k_o], writes=wtoks, acc=True)


class _Dst:
    def __init__(self, ap, flat):
        self.ap, self.flat = ap, flat

    def __call__(self, o):
        return self.ap


def _norm_pools(C, st, name, bounce=True):
    nc = C.nc
    rsp = SbPool(C, st, 2, f"{name}_rs", [65, 512], F32)
    bcp = None if bounce else PsumPool(C, st, 1, f"{name}_bc", (64, 512), F32)
    bcsp = SbPool(C, st, 3, f"{name}_bcs", [64, 512], F32)
    op_ = SbPool(C, st, 3, f"{name}_o", [64, 512], BF16)
    onesf = st.enter_context(nc.sbuf_tensor(f"{name}_onesf", [128, 64], F32))
    C.S.op("dve", lambda e: e.memset(onesf[:], 1.0), writes=[C.t_c], acc=True)
    return (rsp, bcp, bcsp, op_, onesf)


def phase_attn_a(C, l):
    nc, S, d = C.nc, C.S, C.d
    import contextlib
    last = (l == DEPTH - 1)
    with contextlib.ExitStack() as st:
        sb = lambda name, shape, dt: st.enter_context(nc.sbuf_tensor(f"a{l}_{name}", list(shape), dt))
        ka = sb("ka", [128, NTOK], BF16)
        va = sb("va", [128, NT, 130], BF16)
        esk = sb("esk", [65, 2, 512], F32)
        mi = sb("mi", [128, 512], I32)
        mP, mN = sb("mP", [128, 512], BF16), sb("mN", [128, 512], BF16)
        t_ka, t_va, t_es, t_mi, t_m = Tok(), Tok(), Tok(), Tok(), Tok()
        S.dma("sp", ka[:], d["kaT_s"][:, :], reads=[C.T(f"kaT:{g}") for g in range(9)], writes=[t_ka])
        S.dma("sp", va[:], d["va_s"].rearrange("(i p) f -> p i f", p=128), reads=[C.T(f"va:{g}") for g in range(9)], writes=[t_va])
        S.dma("sp", esk[64:65, :, :], d["sink_row"][l:l + 1, :, :], writes=[t_es])
        act(S, esk[64:65, :, :], esk[64:65, :, :], AF.Exp, [t_es], [t_es])
        S.op("pool", lambda e: e.iota(mi[:], pattern=[[0, 4], [1, 128]], base=0, channel_multiplier=-1), writes=[t_mi])
        ts(S, "dve", mP[:], mi[:], 0, NEG, ALU.is_gt, ALU.mult, [t_mi], [t_m], acc=True)
        ts(S, "dve", mN[:], mi[:], 0, NEG, ALU.is_lt, ALU.mult, [t_mi], [t_m], acc=True)
        pools = _norm_pools(C, st, f"a{l}", bounce=False)
        qp = SbPool(C, st, 3, f"a{l}_q", [128, 4, 128], BF16)
        qtiles = list(range(2, NT)) if last else list(range(NT))
        if QT_LIMIT:
            qtiles = qtiles[:QT_LIMIT]
        items = []
        for qt in qtiles:
            if qt < 2:
                keys = [(0, None), (1, None)]
            else:
                xq = qt - 2
                keys = [(0, None), (1, None)]
                if xq > 0:
                    keys.append((qt - 1, mP))
                keys.append((qt, None))
                if xq < 31:
                    keys.append((qt + 1, mN))
            for g in range(2):
                it = {"qt": qt, "g": g, "n": 512, "m_tok": t_m, "kv_reads": [t_ka, t_va],
                      "keys": [(ka[g * 64:(g + 1) * 64, kt * 128:(kt + 1) * 128], va[:, kt, g * 65:(g + 1) * 65], msk)
                               for kt, msk in keys]}
                items.append(it)
        cur = {}

        def load_q(it):
            qt, g = it["qt"], it["g"]
            if qt not in cur:
                q, k_q = qp.get()
                S.dma("sp", q[:], d["qaT_s"].rearrange("c p t -> p c t")[:, :, qt * 128:(qt + 1) * 128],
                      reads=[C.T(f"qaT:{gg}") for gg in range(9)], writes=[k_q])
                cur.clear()
                cur[qt] = (q, k_q)
            q, k_q = cur[qt]
            it["q"] = q[g * 64:(g + 1) * 64, :, :].rearrange("p c t -> p (c t)")
            it["q_reads"] = [k_q]

        for it in items:
            qt, g = it["qt"], it["g"]
            it["load_q"] = load_q
            dst = d["oaT_s"][2 * g:2 * g + 2].rearrange("u2 (u1 e) t -> e (u2 u1) t", u1=2)[:, :, qt * 128:(qt + 1) * 128]
            it["finish"] = (lambda acc, k_acc, g=g, qt=qt, dst=dst:
                            _norm_store(C, pools, acc, k_acc, 512, esk[64:65, g, :], t_es, _Dst(dst, False),
                                        [C.T(f"oaT_s:{qt}")]))
        attn_pipeline(C, st, f"a{l}", items, 0.125, LA=1)
    S.barrier()


def phase_attn_b(C, l):
    nc, S, d = C.nc, C.S, C.d
    import contextlib
    last = (l == DEPTH - 1)
    scale = 96.0 ** -0.5
    with contextlib.ExitStack() as st:
        sb = lambda name, shape, dt: st.enter_context(nc.sbuf_tensor(f"b{l}_{name}", list(shape), dt))
        kb = sb("kb", [96, 8, NTOK], BF16)
        vb = sb("vb", [128, NT, 520], BF16)
        t_kb, t_vb = Tok(), Tok()
        for h in range(8):
            S.dma("sp", kb[:, h, :], d["kbT_s"][h], reads=[C.T(f"kbT:{g}") for g in range(9)], writes=[t_kb], acc=True)
        S.dma("sp", vb[:], d["vb_s"].rearrange("(i p) f -> p i f", p=128), reads=[C.T(f"vb:{g}") for g in range(9)], writes=[t_vb])
        pools = _norm_pools(C, st, f"b{l}")
        qp = SbPool(C, st, 2, f"b{l}_q", [96, 8, 512], BF16)
        groups = list(enumerate(GROUPS))
        if last:
            groups = groups[1:]
        if QT_LIMIT:
            groups = groups[:2]
        items = []
        curq = {}

        def load_q(it):
            g, h = it["g"], it["h"]
            t0, n = GROUPS[g]
            if g not in curq:
                q, k_q = qp.get()
                S.dma("sp", q[:, :, 0:n], d["qbT_s"].rearrange("h p t -> p h t")[:, :, t0:t0 + n],
                      reads=[C.T(f"qbT:{g}")], writes=[k_q])
                curq.clear()
                curq[g] = (q, k_q)
            q, k_q = curq[g]
            it["q"] = q[:, h, 0:n]
            it["q_reads"] = [k_q]

        for g, (t0, n) in groups:
            keys = [0, 1] if g == 0 else list(range(NT))
            tiles = list(range(t0 // 128, (t0 + n) // 128))
            for h in range(8):
                dst = d["obT_s"][h // 2, (h % 2) * 64:(h % 2) * 64 + 64, t0:t0 + n]
                items.append({"n": n, "g": g, "h": h, "load_q": load_q, "kv_reads": [t_kb, t_vb], "m_tok": None,
                              "keys": [(kb[:, h, kt * 128:(kt + 1) * 128], vb[:, kt, h * 65:(h + 1) * 65], None) for kt in keys],
                              "finish": (lambda acc, k_acc, n=n, dst=dst, tiles=tiles:
                                         _norm_store(C, pools, acc, k_acc, n, None, None, _Dst(dst, True),
                                                     [C.T(f"obT_s:{t}") for t in tiles]))})
        stg_g = SbPool(C, st, 1, f"b{l}_sgg", [128, 4096], F32)
        stg_d = SbPool(C, st, 1, f"b{l}_sgd", [128, 2048], F32)
        cb_g = SbPool(C, st, 1, f"b{l}_cbg", [128, 4096], BF16)
        cb_d = SbPool(C, st, 1, f"b{l}_cbd", [128, 2048], BF16)

        def precast(k):
            if k >= 64 or QT_LIMIT:
                return
            e_, part = k // 2, k % 2
            srcw, dstw, sp_, cp_ = ((d["w_gu_h"], d["wgu_b"], stg_g, cb_g) if part == 0 else (d["w_dn_h"], d["wdn_b"], stg_d, cb_d))
            s_t, s_k = sp_.get()
            S.dma("sp", s_t[:], srcw[l, e_ * 128:(e_ + 1) * 128, :], writes=[s_k])
            c_t, c_k = cp_.get()
            cp(S, "dve", c_t[:], s_t[:], [s_k], [c_k])
            S.dma("pool", dstw[e_ * 128:(e_ + 1) * 128, :], c_t[:], reads=[c_k], writes=[C.T("wexp_b")], acc=True)

        nsteps = sum((len(it["keys"]) + 1) // 2 for it in items)
        attn_pipeline(C, st, f"b{l}", items, scale, hook=precast, hook_every=max(1, nsteps // 66))
    S.barrier()


def phase_pool(C, l):
    nc, S, d = C.nc, C.S, C.d
    import contextlib
    last = (l == DEPTH - 1)
    seqs = [(256, SEQ, "invx")] + ([] if last else [(0, LC, "invc")])
    with contextlib.ExitStack() as st:
        sb = lambda name, shape, dt: st.enter_context(nc.sbuf_tensor(f"c{l}_{name}", list(shape), dt))
        wps = sb("wps", [128, 4, 128], F32)
        wp = sb("wp", [128, 4, 128], BF16)
        psc = sb("psc", [128, 4], F32)
        Up = sb("Up", [128, SEQ + 16], F32)
        Aa, Ab = sb("Aa", [128, SEQ + 16], F32), sb("Ab", [128, SEQ + 16], F32)
        inv = sb("inv", [128, SEQ], F32)
        ub = sb("ub", [128, SEQ], BF16)
        pl = sb("pl", [128, SEQ], BF16)
        oc = sb("oc", [128, SEQ], BF16)
        t_wp, t_psc, t_Up, t_A, t_B, t_inv, t_ub, t_pl, t_oc = (Tok() for _ in range(9))
        S.dma("sp", wps[:], d["w_pool"][l].rearrange("g c e -> c g e"), writes=[t_wp])
        cp(S, "act", wp[:], wps[:], [t_wp], [t_wp])
        S.dma("sp", psc[:], d["pool_scaleT"][l], writes=[t_psc])
        pp = PsumPool(C, st, 3, f"c{l}_pp")
        eng = ["dve", "dve"]
        ei = 0
        for (t0, L, invname) in seqs:
            for g in range(4):
                r = (1, 2, 4, 8)[g]
                S.dma("sp", ub[:, 0:L], d["uT_s"][g, :, t0:t0 + L], reads=[C.T(f"uT:{q}") for q in range(9)], writes=[t_ub])
                S.dma("sp", inv[:, 0:L], bcast_row(d[invname][g:g + 1, :], L), writes=[t_inv])
                S.op("dve", lambda e, L=L: e.memset(Up[:, 0:L + 16], 0.0), writes=[t_Up])
                cp(S, "dve", Up[:, 8:8 + L], ub[:, 0:L], [t_ub], [t_Up])
                src, k_src, ln = Up, t_Up, L + 16
                bufs = [(Aa, t_A), (Ab, t_B)]
                step = 1
                bi = 0
                while step <= r:
                    dst, k_dst = bufs[bi]
                    bi ^= 1
                    nl = ln - step
                    tt(S, eng[ei % 2], dst[:, 0:nl], src[:, 0:nl], src[:, step:step + nl], ALU.add, [k_src], [k_dst])
                    ei += 1
                    src, k_src, ln = dst, k_dst, nl
                    step *= 2
                dst, k_dst = bufs[bi]
                tt(S, eng[ei % 2], dst[:, 0:L], src[:, 8 - r:8 - r + L], Up[:, 8 + r:8 + r + L], ALU.add, [k_src, t_Up], [k_dst])
                ei += 1
                tt(S, "dve", dst[:, 0:L], dst[:, 0:L], inv[:, 0:L], ALU.mult, [k_dst, t_inv], [k_dst])
                tt(S, "dve", pl[:, 0:L], dst[:, 0:L], Up[:, 8:8 + L], ALU.subtract, [k_dst, t_Up], [t_pl])
                for c0 in range(0, L, 512):
                    n = min(512, L - c0)
                    ps, k_ps = pp.get()
                    mm(S, ps[:, 0:n], wp[:, g, :], pl[:, c0:c0 + n], True, True, [t_wp, t_pl], k_ps)
                    act(S, oc[:, c0:c0 + n], ps[:, 0:n], AF.Copy, [k_ps, t_psc], [t_oc], acc=(c0 > 0), scale=psc[:, g:g + 1])
                S.dma("pool", d["ocT_s"][g, :, t0:t0 + L], oc[:, 0:L], reads=[t_oc], writes=[C.T(f"ocT:{g}:{t0}")])
    S.barrier()


def phase_merge(C, l):
    nc, S, d = C.nc, C.S, C.d
    import contextlib
    last = (l == DEPTH - 1)
    hsrc = d["h0"] if l == 0 else d["h_s"]
    with contextlib.ExitStack() as st:
        sb = lambda name, shape, dt: st.enter_context(nc.sbuf_tensor(f"g{l}_{name}", list(shape), dt))
        wg = sb("wg", [128, 8, 3072], BF16)
        wbr = sb("wbr", [128, 12, 1024], BF16)
        wo = sb("wo", [128, 2, 8, 1024], BF16)
        G1 = sb("G1", [128, 2, 1024], F32)
        t_wg, t_wbr, t_wo, t_G1 = Tok(), Tok(), Tok(), Tok()
        wext = d["w_in_ext"][l].rearrange("(kc p) n -> p kc n", p=128)[:, :, O_G:W_EXT]
        load_cast(C, st, f"g{l}_stg", wg, wext, 8, 3072, t_wg, half=768)
        wbsrc = d["w_br"][l].rearrange("b (kc p) n -> p (b kc) n", p=128)
        load_cast(C, st, f"g{l}_stg2", wbr, wbsrc, 12, 1024, t_wbr, half=512)
        for r in range(2):
            S.dma("sp", G1[:, r, :], bcast_row(d["modv"][l][r:r + 1, 2048:3072], 1024), reads=[C.T(f"modv{l}")], writes=[t_G1], acc=True)
        stgo = SbPool(C, st, 2, f"g{l}_stgo", [128, 1024], F32)
        wosrc = d["w_out"][l].rearrange("(kc p) n -> p kc n", p=128)
        for kc in range(8):
            s_t, s_k = stgo.get()
            S.dma("sp", s_t[:], wosrc[:, kc, :], writes=[s_k])
            for r in range(2):
                tt(S, "dve", wo[:, r, kc, :], s_t[:], G1[:, r, :], ALU.mult, [s_k, t_G1], [t_wo], acc=True)
        pp = PsumPool(C, st, 6, f"g{l}_pp")
        hxTp = SbPool(C, st, 1, f"g{l}_hxT", [128, 8, 512], BF16)
        oTp = SbPool(C, st, 1, f"g{l}_oT", [128, 12, 512], BF16)
        YTp = SbPool(C, st, 1, f"g{l}_YT", [128, 8, 512], BF16)
        sgp = SbPool(C, st, 3, f"g{l}_sg", [128, 512], BF16)
        tbp = SbPool(C, st, 4, f"g{l}_tb", [128, 512], F32)
        y1p = SbPool(C, st, 2, f"g{l}_y1", [128, 512], F32)
        hp = SbPool(C, st, 2, f"g{l}_h", [128, 1024], F32)
        hnp = SbPool(C, st, 2, f"g{l}_hn", [128, 1024], F32)
        groups = list(enumerate(GROUPS))
        if last:
            groups = groups[1:]
        if QT_LIMIT:
            groups = groups[:2]
        oc_toks = [C.T(f"ocT:{g}:{t0}") for g in range(4) for t0 in (0, 256)]
        for g, (t0, n) in groups:
            r = 0 if g > 0 else 1
            hxT, k_hxT = hxTp.get()
            S.dma("sp", hxT[:, :, 0:n], d["hxT_s"].rearrange("c p t -> p c t")[:, :, t0:t0 + n], reads=[C.T(f"hxT:{g}")], writes=[k_hxT])
            oT, k_oT = oTp.get()
            tiles = list(range(t0 // 128, (t0 + n) // 128))
            for bi, (nm, rd) in enumerate((("oaT_s", [C.T(f"oaT_s:{t}") for t in tiles]),
                                           ("obT_s", [C.T(f"obT_s:{t}") for t in tiles]),
                                           ("ocT_s", oc_toks))):
                S.dma("sp", oT[:, bi * 4:(bi + 1) * 4, 0:n], d[nm].rearrange("c p t -> p c t")[:, :, t0:t0 + n],
                      reads=rd, writes=[k_oT], acc=(bi > 0))
            YT, k_YT = YTp.get()
            for m in range(8):
                tbs = []
                for br in range(3):
                    psg, k_psg = pp.get()
                    for kc in range(8):
                        mm(S, psg[:, 0:n], wg[:, kc, br * 1024 + m * 128:br * 1024 + (m + 1) * 128], hxT[:, kc, 0:n],
                           kc == 0, kc == 7, [t_wg, k_hxT], k_psg)
                    sg, k_sg = sgp.get()
                    act(S, sg[:, 0:n], psg[:, 0:n], AF.Sigmoid, [k_psg], [k_sg])
                    psv, k_psv = pp.get()
                    for kc in range(4):
                        mm(S, psv[:, 0:n], wbr[:, br * 4 + kc, m * 128:(m + 1) * 128], oT[:, br * 4 + kc, 0:n],
                           kc == 0, kc == 3, [t_wbr, k_oT], k_psv)
                    tb, k_tb = tbp.get()
                    tt(S, "dve", tb[:, 0:n], psv[:, 0:n], sg[:, 0:n], ALU.mult, [k_psv, k_sg], [k_tb])
                    tbs.append((tb, k_tb))
                y1, k_y1 = y1p.get()
                tt(S, "dve", y1[:, 0:n], tbs[0][0][:, 0:n], tbs[1][0][:, 0:n], ALU.add, [tbs[0][1], tbs[1][1]], [k_y1])
                tt(S, "dve", YT[:, m, 0:n], y1[:, 0:n], tbs[2][0][:, 0:n], ALU.add, [k_y1, tbs[2][1]], [k_YT], acc=(m > 0))
            for ti, tile in enumerate(tiles):
                ht, k_h = hp.get()
                S.dma("sp", ht[:], hsrc[tile * 128:(tile + 1) * 128, :], reads=[C.T(f"h:{tile}")], writes=[k_h])
                hn, k_hn = hnp.get()
                for half in range(2):
                    ps, k_ps = pp.get()
                    for m in range(8):
                        mm(S, ps[:, :], YT[:, m, ti * 128:(ti + 1) * 128], wo[:, r, m, half * 512:(half + 1) * 512],
                           m == 0, m == 7, [k_YT, t_wo], k_ps)
                    tt(S, "dve", hn[:, half * 512:(half + 1) * 512], ps[:, :], ht[:, half * 512:(half + 1) * 512], ALU.add,
                       [k_ps, k_h], [k_hn], acc=(half > 0))
                S.dma("pool", d["h_s"][tile * 128:(tile + 1) * 128, :], hn[:], reads=[k_hn], writes=[C.T(f"h:{tile}")])
    S.barrier()


def phase_route(C, l):
    nc, S, d = C.nc, C.S, C.d
    import contextlib
    last = (l == DEPTH - 1)
    with contextlib.ExitStack() as st:
        sb = lambda name, shape, dt: st.enter_context(nc.sbuf_tensor(f"r{l}_{name}", list(shape), dt))
        Gm, Sm = sb("Gm", [128, 2, 1024], F32), sb("Sm", [128, 2, 1024], F32)
        wr = sb("wr", [128, 8, 36], F32)
        br = sb("br", [128, 36], F32)
        t_mod, t_mod2, t_wr = Tok(), Tok(), Tok()
        mv = d["modv"][l]
        tmpp = SbPool(C, st, 2, f"r{l}_tmp", [128, 1024], F32)
        gn, k_gn = tmpp.get()
        S.dma("sp", gn[:], bcast_row(d["norm_ffn_g"][l:l + 1, :], 1024), writes=[k_gn])
        for r in range(2):
            S.dma("sp", Sm[:, r, :], bcast_row(mv[r:r + 1, 3072:4096], 1024), reads=[C.T(f"modv{l}")], writes=[t_mod], acc=True)
            S.dma("sp", Gm[:, r, :], bcast_row(mv[r:r + 1, 4096:5120], 1024), reads=[C.T(f"modv{l}")], writes=[t_mod], acc=True)
        for r in range(2):
            stt(S, Gm[:, r, :], Gm[:, r, :], 1.0, gn[:], ALU.add, ALU.mult, [t_mod, k_gn], [t_mod2], acc=True)
        S.dma("sp", wr[:], d["w_r"][l].rearrange("(kc p) n -> p kc n", p=128), writes=[t_wr], acc=True)
        S.dma("sp", br[:], bcast_row(d["b_r"][l:l + 1, :], 36), writes=[t_wr], acc=True)
        pt32 = PsumPool(C, st, 2, f"r{l}_pt", (128, 4, 128), F32)
        pp = PsumPool(C, st, 2, f"r{l}_pp", (128, 128), F32)
        ptb = PsumPool(C, st, 2, f"r{l}_ptb", (128, 8, 128), BF16)
        hp = SbPool(C, st, 2, f"r{l}_h", [128, 1024], F32)
        fxp = SbPool(C, st, 2, f"r{l}_fx", [128, 1024], F32)
        fxbp = SbPool(C, st, 2, f"r{l}_fxb", [128, 1024], BF16)
        fxTp = SbPool(C, st, 2, f"r{l}_fxT", [128, 8, 128], F32)
        fxTbp = SbPool(C, st, 2, f"r{l}_fxTb", [128, 8, 128], BF16)
        ssp = SbPool(C, st, 2, f"r{l}_ss", [128, 4], F32)
        sqj = sb("sqj", [128, 1024], BF16)
        t_sqj = Tok()
        rtp = SbPool(C, st, 2, f"r{l}_rt", [128, 160], F32)
        gTp = SbPool(C, st, 2, f"r{l}_gT", [32, 128], F32)
        tiles = list(range(2, NT)) if last else list(range(NT))
        if QT_LIMIT:
            tiles = tiles[:QT_LIMIT]
        for tile in tiles:
            mr = 0 if tile >= 2 else 1
            ht, k_h = hp.get()
            S.dma("sp", ht[:], d["h_s"][tile * 128:(tile + 1) * 128, :], reads=[C.T(f"h:{tile}")], writes=[k_h])
            ss, k_ss = ssp.get()
            act(S, sqj[:], ht[:], AF.Square, [k_h], [t_sqj, k_ss], accum_out=ss[:, 0:1])
            act(S, ss[:, 1:2], ss[:, 0:1], AF.Sqrt, [k_ss], [k_ss], acc=True, scale=1.0 / DM, bias=EPS)
            S.op("dve", lambda e, ss=ss: e.reciprocal(out=ss[:, 2:3], in_=ss[:, 1:2]), reads=[k_ss], writes=[k_ss], acc=True)
            tm, k_tm = tmpp.get()
            stt(S, tm[:], ht[:], ss[:, 2:3], Gm[:, mr, :], ALU.mult, ALU.mult, [k_h, k_ss, t_mod2], [k_tm])
            fx, k_fx = fxp.get()
            tt(S, "dve", fx[:], tm[:], Sm[:, mr, :], ALU.add, [k_tm, t_mod2], [k_fx])
            fxb, k_fxb = fxbp.get()
            cp(S, "act", fxb[:], fx[:], [k_fx], [k_fxb])
            tpb, k_tpb = ptb.get()
            for kc in range(8):
                S.op("pe", lambda e, tpb=tpb, fxb=fxb, kc=kc: e.transpose(tpb[:, kc, :], fxb[:, kc * 128:(kc + 1) * 128], C.idb[:]),
                     reads=[k_fxb, C.t_c], writes=[k_tpb], acc=(kc > 0))
            fxTb, k_fxTb = fxTbp.get()
            cp(S, "act", fxTb[:], tpb[:], [k_tpb], [k_fxTb])
            S.dma("pool", d["hxT_s"].rearrange("c p t -> p c t")[:, :, tile * 128:(tile + 1) * 128], fxTb[:],
                  reads=[k_fxTb], writes=[C.T(f"fxT:{tile}")])
            fxT, k_fxT = fxTp.get()
            for hf in range(2):
                tp, k_tp = pt32.get()
                for kc in range(4):
                    S.op("pe", lambda e, tp=tp, fx=fx, kc=kc, hf=hf: e.transpose(tp[:, kc, :], fx[:, (hf * 4 + kc) * 128:(hf * 4 + kc + 1) * 128], C.idf[:]),
                         reads=[k_fx, C.t_c], writes=[k_tp], acc=(kc > 0))
                cp(S, "act", fxT[:, hf * 4:(hf + 1) * 4, :], tp[:], [k_tp], [k_fxT], acc=(hf > 0))
            pl, k_pl = pp.get()
            for kc in range(8):
                mm(S, pl[:, 0:36], fxT[:, kc, :], wr[:, kc, :], kc == 0, kc == 7, [k_fxT, t_wr], k_pl)
            rt, k = rtp.get()
            tt(S, "dve", rt[:, 0:36], pl[:, 0:36], br[:], ALU.add, [k_pl, t_wr], [k])
            S.op("dve", lambda e, rt=rt: e.reduce_max(out=rt[:, 36:37], in_=rt[:, 0:4], axis=AX.X), reads=[k], writes=[k], acc=True)
            ts(S, "dve", rt[:, 37:38], rt[:, 36:37], -1.0, None, ALU.mult, None, [k], [k], acc=True)
            act(S, rt[:, 44:48], rt[:, 0:4], AF.Exp, [k], [k], acc=True, bias=rt[:, 37:38], accum_out=rt[:, 38:39])
            S.op("dve", lambda e, rt=rt: e.reciprocal(out=rt[:, 39:40], in_=rt[:, 38:39]), reads=[k], writes=[k], acc=True)
            ts(S, "dve", rt[:, 40:44], rt[:, 0:4], rt[:, 36:37], None, ALU.is_equal, None, [k], [k], acc=True)
            ts(S, "dve", rt[:, 40:44], rt[:, 40:44], -1.0, 30000.0, ALU.add, ALU.mult, [k], [k], acc=True)
            for g in range(4):
                ts(S, "dve", rt[:, 48 + g * 8:56 + g * 8], rt[:, 4 + g * 8:12 + g * 8], rt[:, 40 + g:41 + g], None,
                   ALU.add, None, [k], [k], acc=True)
            S.op("dve", lambda e, rt=rt: e.max(out=rt[:, 80:88], in_=rt[:, 48:80]), reads=[k], writes=[k], acc=True)
            tt(S, "dve", rt[:, 88:89], rt[:, 81:82], rt[:, 80:81], ALU.subtract, [k], [k], acc=True)
            act(S, rt[:, 89:90], rt[:, 88:89], AF.Exp, [k], [k], acc=True)
            ts(S, "dve", rt[:, 90:91], rt[:, 89:90], 1.0, None, ALU.add, None, [k], [k], acc=True)
            S.op("dve", lambda e, rt=rt: e.reciprocal(out=rt[:, 90:91], in_=rt[:, 90:91]), reads=[k], writes=[k], acc=True)
            tt(S, "dve", rt[:, 91:92], rt[:, 90:91], rt[:, 39:40], ALU.mult, [k], [k], acc=True)
            tt(S, "dve", rt[:, 92:93], rt[:, 91:92], rt[:, 89:90], ALU.mult, [k], [k], acc=True)
            ts(S, "dve", rt[:, 96:128], rt[:, 48:80], rt[:, 80:81], rt[:, 91:92], ALU.is_equal, ALU.mult, [k], [k], acc=True)
            ts(S, "dve", rt[:, 128:160], rt[:, 48:80], rt[:, 81:82], rt[:, 92:93], ALU.is_equal, ALU.mult, [k], [k], acc=True)
            tt(S, "dve", rt[:, 96:128], rt[:, 96:128], rt[:, 128:160], ALU.add, [k], [k], acc=True)
            pg, k_pg = pp.get()
            S.op("pe", lambda e, pg=pg, rt=rt: e.transpose(pg[0:32, 0:128], rt[:, 96:128], C.idf[:]),
                 reads=[k, C.t_c], writes=[k_pg])
            gT, k_gT = gTp.get()
            cp(S, "act", gT[:], pg[0:32, 0:128], [k_pg], [k_gT])
            S.dma("pool", d["gT_s"][:, tile * 128:(tile + 1) * 128], gT[:], reads=[k_gT], writes=[C.T(f"gT:{tile}")])
    S.barrier()


def phase_experts(C, l):
    nc, S, d = C.nc, C.S, C.d
    import contextlib
    last = (l == DEPTH - 1)
    with contextlib.ExitStack() as st:
        sb = lambda name, shape, dt: st.enter_context(nc.sbuf_tensor(f"e{l}_{name}", list(shape), dt))
        g2T = sb("g2T", [128, 2, 8], F32)
        fg = sb("fg", [128, 1024], F32)
        t_g2 = Tok()
        for r in range(2):
            S.dma("sp", g2T[:, r, :], d["modv"][l][r, 5120:6144].rearrange("(m p) -> p m", p=128), reads=[C.T(f"modv{l}")],
                  writes=[t_g2], acc=True, allow_slow_non_contiguous=True)
        if last:
            S.dma("sp", fg[:], bcast_row(d["final_g"][0:1, :], 1024), writes=[t_g2], acc=True)
        ppg = PsumPool(C, st, 4, f"e{l}_ppg")
        pp = PsumPool(C, st, 3, f"e{l}_pp")
        wgs = SbPool(C, st, 1, f"e{l}_wgs", [128, 8, 512], F32)
        wds = SbPool(C, st, 1, f"e{l}_wds", [128, 2, 1024], F32)
        wgp = SbPool(C, st, 2, f"e{l}_wg", [128, 8, 512], BF16)
        wdp = SbPool(C, st, 2, f"e{l}_wd", [128, 2, 1024], BF16)
        fxTp = SbPool(C, st, 1, f"e{l}_fxT", [128, 8, 2176], BF16)
        yacc = sb("yacc", [128, 8, 2176], F32)
        t_y = Tok()
        gwp = SbPool(C, st, 3, f"e{l}_gw", [128, 512], F32)
        sip = SbPool(C, st, 2, f"e{l}_si", [128, 512], F32)
        a1p = SbPool(C, st, 2, f"e{l}_a1", [128, 512], F32)
        actp = SbPool(C, st, 3, f"e{l}_act", [128, 2, 512], BF16)
        hp = SbPool(C, st, 2, f"e{l}_h", [128, 1024], F32)
        hnp = SbPool(C, st, 2, f"e{l}_hn", [128, 1024], F32)
        ssp = SbPool(C, st, 2, f"e{l}_ss", [128, 4], F32)
        sqj = sb("sqj", [128, 1024], BF16)
        t_sqj = Tok()
        tok0 = 256 if last else 0
        sgs = []
        t = tok0
        while t < NTOK:
            n = min(2048 if last else 2176, NTOK - t)
            sgs.append((t, n))
            t += n
        if QT_LIMIT:
            sgs = [(tok0, 256)]
        nexp = 32 if not QT_LIMIT else QT_LIMIT
        for (t0, n) in sgs:
            tiles = list(range(t0 // 128, (t0 + n) // 128))
            fxT, k_fxT = fxTp.get()
            S.dma("sp", fxT[:, :, 0:n], d["hxT_s"].rearrange("c p t -> p c t")[:, :, t0:t0 + n],
                  reads=[C.T(f"fxT:{tl}") for tl in tiles], writes=[k_fxT])
            steps = [(e_, s0) for e_ in range(nexp) for s0 in range(0, n, 512)]
            wcur = {}
            st_state = {}

            def load_w(e_):
                ws, k_ws = wgs.get()
                S.dma("sp", ws[:], d["w_gu"][l, e_].rearrange("(kc p) n -> p kc n", p=128), writes=[k_ws])
                wg, k_wg = wgp.get()
                cp(S, "act", wg[:], ws[:], [k_ws], [k_wg])
                ws2, k_ws2 = wds.get()
                S.dma("sp", ws2[:], d["w_dn"][l, e_].rearrange("(kc p) n -> p kc n", p=128), writes=[k_ws2])
                wd, k_wd = wdp.get()
                cp(S, "act", wd[:], ws2[:], [k_ws2], [k_wd])
                wcur[e_] = (wg, k_wg, wd, k_wd)

            def emit_gu(si):
                e_, s0 = steps[si]
                ns = min(512, n - s0)
                if e_ not in wcur:
                    load_w(e_)
                    wcur.pop(e_ - 2, None)
                wg, k_wg, wd, k_wd = wcur[e_]
                gw, k_gw = gwp.get()
                S.dma("sp", gw[:, 0:ns], bcast_row(d["gT_s"][e_:e_ + 1, t0 + s0:t0 + s0 + ns], ns),
                      reads=[C.T(f"gT:{tl}") for tl in tiles], writes=[k_gw])
                ac, k_ac = actp.get()
                for c in range(2):
                    psg, k_psg = ppg.get()
                    psu, k_psu = ppg.get()
                    for kc in range(8):
                        mm(S, psg[:, 0:ns], wg[:, kc, c * 128:(c + 1) * 128], fxT[:, kc, s0:s0 + ns], kc == 0, kc == 7, [k_wg, k_fxT], k_psg)
                    for kc in range(8):
                        mm(S, psu[:, 0:ns], wg[:, kc, 256 + c * 128:256 + (c + 1) * 128], fxT[:, kc, s0:s0 + ns], kc == 0, kc == 7, [k_wg, k_fxT], k_psu)
                    si_, k_si = sip.get()
                    act(S, si_[:, 0:ns], psg[:, 0:ns], AF.Silu, [k_psg], [k_si])
                    a1, k_a1 = a1p.get()
                    tt(S, "dve", a1[:, 0:ns], psu[:, 0:ns], si_[:, 0:ns], ALU.mult, [k_psu, k_si], [k_a1])
                    tt(S, "dve", ac[:, c, 0:ns], a1[:, 0:ns], gw[:, 0:ns], ALU.mult, [k_a1, k_gw], [k_ac], acc=(c > 0))
                st_state[si] = (ac, k_ac, wd, k_wd)

            def emit_dn(si):
                e_, s0 = steps[si]
                ns = min(512, n - s0)
                ac, k_ac, wd, k_wd = st_state.pop(si)
                for m in range(8):
                    py, k_py = pp.get()
                    for c in range(2):
                        mm(S, py[:, 0:ns], wd[:, c, m * 128:(m + 1) * 128], ac[:, c, 0:ns], c == 0, c == 1, [k_wd, k_ac], k_py)
                    if e_ == 0:
                        cp(S, "act", yacc[:, m, s0:s0 + ns], py[:, 0:ns], [k_py], [t_y], acc=True)
                    else:
                        tt(S, "dve", yacc[:, m, s0:s0 + ns], py[:, 0:ns], yacc[:, m, s0:s0 + ns], ALU.add, [k_py, t_y], [t_y], acc=True)

            emit_gu(0)
            for si in range(len(steps)):
                if si + 1 < len(steps):
                    emit_gu(si + 1)
                emit_dn(si)
            for m in range(8):
                for (a, b_, r) in ((0, 256 - t0, 1), (max(0, 256 - t0), n, 0)):
                    if b_ <= a:
                        continue
                    b_ = min(b_, n)
                    act(S, yacc[:, m, a:b_], yacc[:, m, a:b_], AF.Copy, [t_y, t_g2], [t_y], acc=True, scale=g2T[:, r, m:m + 1])
            for ti, tile in enumerate(tiles):
                ht, k_h = hp.get()
                S.dma("sp", ht[:], d["h_s"][tile * 128:(tile + 1) * 128, :], reads=[C.T(f"h:{tile}")], writes=[k_h])
                hn, k_hn = hnp.get()
                for hf in range(2):
                    ps, k_ps = pp.get()
                    for kc in range(4):
                        m = hf * 4 + kc
                        S.op("pe", lambda e, ps=ps, m=m, kc=kc, ti=ti: e.transpose(ps[:, kc * 128:(kc + 1) * 128], yacc[:, m, ti * 128:(ti + 1) * 128], C.idf[:]),
                             reads=[t_y, C.t_c], writes=[k_ps], acc=(kc > 0))
                    tt(S, "dve", hn[:, hf * 512:(hf + 1) * 512], ps[:, :], ht[:, hf * 512:(hf + 1) * 512], ALU.add, [k_ps, k_h], [k_hn], acc=(hf > 0))
                if not last:
                    S.dma("pool", d["h_s"][tile * 128:(tile + 1) * 128, :], hn[:], reads=[k_hn], writes=[C.T(f"h:{tile}")])
                else:
                    ss, k_ss = ssp.get()
                    act(S, sqj[:], hn[:], AF.Square, [k_hn], [t_sqj, k_ss], accum_out=ss[:, 0:1])
                    act(S, ss[:, 1:2], ss[:, 0:1], AF.Sqrt, [k_ss], [k_ss], acc=True, scale=1.0 / DM, bias=EPS)
                    S.op("dve", lambda e, ss=ss: e.reciprocal(out=ss[:, 2:3], in_=ss[:, 1:2]), reads=[k_ss], writes=[k_ss], acc=True)
                    ho, k_ho = hp.get()
                    stt(S, ho[:], hn[:], ss[:, 2:3], fg[:], ALU.mult, ALU.mult, [k_hn, k_ss, t_g2], [k_ho])
                    S.dma("pool", d["out"][(tile - 2) * 128:(tile - 1) * 128, :], ho[:], reads=[k_ho], writes=[C.T(f"out:{tile}")])
    S.barrier()


def all_phases(C):
    C.d["out"] = C.nc.dram_tensor("out", [SEQ, DM], F32, kind="ExternalOutput").ap()
    for l in range(DEPTH):
        phase_mod(C, l)
        phase_inproj(C, l)
        phase_attn_a(C, l)
        phase_attn_b(C, l)
        phase_pool(C, l)
        phase_merge(C, l)
        phase_moe(C, l)


def kernel(**inputs):
    sh = prep_shared(inputs)
    nc = build_program(all_phases)
    in_maps = []
    for b in range(8):
        m = dict(sh)
        m.update(prep_core(inputs, b))
        in_maps.append({k: m[k] for k in INPUTS})
    res = run_bass_kernel_spmd(nc, in_maps, core_ids=list(range(8)))
    return np.stack([np.asarray(r["out"], dtype=np.float32) for r in res.results], axis=0)


def phase_moe(C, l):
    nc, S, d = C.nc, C.S, C.d
    import contextlib
    last = (l == DEPTH - 1)
    tiles = list(range(2, NT)) if last else list(range(NT))
    if QT_LIMIT:
        tiles = tiles[:QT_LIMIT]
    nt = len(tiles)
    NB = -(-(2 * nt * 128 + 32 * 127) // 128)
    xbuf = d["xbuf"]
    ybuf = d["ybuf"]
    with contextlib.ExitStack() as st0:
        sb0 = lambda name, shape, dt: st0.enter_context(nc.sbuf_tensor(f"x{l}_{name}", list(shape), dt))
        sAB = sb0("sAB", [128, NT, 2], I32)
        w12 = sb0("w12", [128, NT, 2], F32)
        widx = sb0("widx", [128, 128], I32)
        t_sAB, t_w12, t_widx = Tok(), Tok(), Tok()
        t_xz = Tok()
        with contextlib.ExitStack() as st:
            sb = lambda name, shape, dt: st.enter_context(nc.sbuf_tensor(f"r{l}_{name}", list(shape), dt))
            zt = sb("zt", [128, 4, 1024], BF16)
            t_zt = Tok()
            S.op("dve", lambda e: e.memset(zt[:], 0.0), writes=[t_zt])
            xv = xbuf.rearrange("(a p) f -> p a f", p=128)
            for b0 in range(0, NB, 4):
                nb_ = min(4, NB - b0)
                S.dma("sp", xv[:, b0:b0 + nb_, :], zt[:, 0:nb_, :], reads=[t_zt], writes=[t_xz], acc=True)
            Gm, Sm = sb("Gm", [128, 2, 1024], F32), sb("Sm", [128, 2, 1024], F32)
            wr = sb("wr", [128, 8, 36], F32)
            br = sb("br", [128, 36], F32)
            t_mod, t_mod2, t_wr = Tok(), Tok(), Tok()
            mv = d["modv"][l]
            tmpp = SbPool(C, st, 2, f"r{l}_tmp", [128, 1024], F32)
            gn, k_gn = tmpp.get()
            S.dma("sp", gn[:], bcast_row(d["norm_ffn_g"][l:l + 1, :], 1024), writes=[k_gn])
            for r in range(2):
                S.dma("sp", Sm[:, r, :], bcast_row(mv[r:r + 1, 3072:4096], 1024), reads=[C.T(f"modv{l}")], writes=[t_mod], acc=True)
                S.dma("sp", Gm[:, r, :], bcast_row(mv[r:r + 1, 4096:5120], 1024), reads=[C.T(f"modv{l}")], writes=[t_mod], acc=True)
            for r in range(2):
                stt(S, Gm[:, r, :], Gm[:, r, :], 1.0, gn[:], ALU.add, ALU.mult, [t_mod, k_gn], [t_mod2], acc=True)
            S.dma("sp", wr[:], d["w_r"][l].rearrange("(kc p) n -> p kc n", p=128), writes=[t_wr], acc=True)
            S.dma("sp", br[:], bcast_row(d["b_r"][l:l + 1, :], 36), writes=[t_wr], acc=True)
            fx_all = sb("fxall", [128, NT, 1024], BF16)
            A01 = sb("A01", [128, NT, 32], F32)
            B01 = sb("B01", [128, NT, 32], F32)
            Mall = sb("Mall", [128, NT, 32], BF16)
            t_fx = [Tok() for _ in range(NT)]
            GT = 8
            pt32 = PsumPool(C, st, 2, f"r{l}_pt", (128, 4, 128), F32)
            pp = PsumPool(C, st, 2, f"r{l}_pp", (128, 512), F32)
            hbp = SbPool(C, st, 1, f"r{l}_hb", [128, GT, 1024], F32)
            fxp = SbPool(C, st, 2, f"r{l}_fx", [128, 1024], F32)
            fxTp = SbPool(C, st, 2, f"r{l}_fxT", [128, 8, 128], F32)
            ssp = SbPool(C, st, 2, f"r{l}_ss", [128, 3, GT], F32)
            sqj = sb("sqj", [128, GT, 1024], BF16)
            t_sqj = Tok()
            rwp = SbPool(C, st, 2, f"r{l}_rw", [128, GT, 128], F32)
            rsp_ = SbPool(C, st, 2, f"r{l}_rs", [128, 12, GT], F32)

            def b3(ap2, gt, w):
                return ap2.rearrange("p (g o) -> p g o", o=1).to_broadcast([128, gt, w])

            batches = [list(range(a, min(a + GT, nt))) for a in range(0, nt, GT)]
            t_ABb = []
            for bt in batches:
                gt = len(bt)
                t0i = bt[0]
                hb, k_hb = hbp.get()
                ss, k_ss = ssp.get()
                for j, ti in enumerate(bt):
                    tile = tiles[ti]
                    S.dma("sp", hb[:, j, :], d["h_s"][tile * 128:(tile + 1) * 128, :], reads=[C.T(f"h:{tile}")], writes=[k_hb], acc=(j > 0))
                for j, ti in enumerate(bt):
                    act(S, sqj[:, j, :], hb[:, j, :], AF.Square, [k_hb], [t_sqj, k_ss], acc=(j > 0), accum_out=ss[:, 0, j:j + 1])
                act(S, ss[:, 1, 0:gt], ss[:, 0, 0:gt], AF.Sqrt, [k_ss], [k_ss], acc=True, scale=1.0 / DM, bias=EPS)
                S.op("dve", lambda e, ss=ss, gt=gt: e.reciprocal(out=ss[:, 2, 0:gt], in_=ss[:, 1, 0:gt]), reads=[k_ss], writes=[k_ss], acc=True)
                pl, k_pl = pp.get()
                for j, ti in enumerate(bt):
                    tile = tiles[ti]
                    mr = 0 if tile >= 2 else 1
                    tm, k_tm = tmpp.get()
                    stt(S, tm[:], hb[:, j, :], ss[:, 2, j:j + 1], Gm[:, mr, :], ALU.mult, ALU.mult, [k_hb, k_ss, t_mod2], [k_tm])
                    fx, k_fx = fxp.get()
                    tt(S, "dve", fx[:], tm[:], Sm[:, mr, :], ALU.add, [k_tm, t_mod2], [k_fx])
                    cp(S, "act", fx_all[:, ti, :], fx[:], [k_fx], [t_fx[ti]])
                    fxT, k_fxT = fxTp.get()
                    for hf in range(2):
                        tp, k_tp = pt32.get()
                        for kc in range(4):
                            S.op("pe", lambda e, tp=tp, fx=fx, kc=kc, hf=hf: e.transpose(tp[:, kc, :], fx[:, (hf * 4 + kc) * 128:(hf * 4 + kc + 1) * 128], C.idf[:]),
                                 reads=[k_fx, C.t_c], writes=[k_tp], acc=(kc > 0))
                        cp(S, "act", fxT[:, hf * 4:(hf + 1) * 4, :], tp[:], [k_tp], [k_fxT], acc=(hf > 0))
                    for kc in range(8):
                        mm(S, pl[:, j * 36:(j + 1) * 36], fxT[:, kc, :], wr[:, kc, :], kc == 0, kc == 7, [k_fxT, t_wr], k_pl)
                rw, k = rwp.get()
                rs, k2 = rsp_.get()
                plv = pl[:, 0:gt * 36].rearrange("p (g n) -> p g n", n=36)
                tt(S, "dve", rw[:, 0:gt, 0:36], plv, br[:, :].rearrange("p (o n) -> p o n", o=1).to_broadcast([128, gt, 36]), ALU.add, [k_pl, t_wr], [k])
                if STOP == 101:
                    continue
                S.op("dve", lambda e, rw=rw, rs=rs, gt=gt: e.reduce_max(out=rs[:, 0, 0:gt], in_=rw[:, 0:gt, 0:4], axis=AX.X), reads=[k], writes=[k2])
                tt(S, "dve", rw[:, 0:gt, 36:40], rw[:, 0:gt, 0:4], b3(rs[:, 0, 0:gt], gt, 4), ALU.subtract, [k, k2], [k], acc=True)
                act(S, rw[:, 0:gt, 36:40], rw[:, 0:gt, 36:40], AF.Exp, [k], [k], acc=True)
                S.op("dve", lambda e, rw=rw, rs=rs, gt=gt: e.reduce_sum(out=rs[:, 1, 0:gt], in_=rw[:, 0:gt, 36:40], axis=AX.X), reads=[k], writes=[k2], acc=True)
                S.op("dve", lambda e, rs=rs, gt=gt: e.reciprocal(out=rs[:, 2, 0:gt], in_=rs[:, 1, 0:gt]), reads=[k2], writes=[k2], acc=True)
                if STOP == 102:
                    continue
                tt(S, "dve", rw[:, 0:gt, 36:40], rw[:, 0:gt, 0:4], b3(rs[:, 0, 0:gt], gt, 4), ALU.is_equal, [k, k2], [k], acc=True)
                ts(S, "dve", rw[:, 0:gt, 36:40], rw[:, 0:gt, 36:40], -1.0, 30000.0, ALU.add, ALU.mult, [k], [k], acc=True)
                tt(S, "dve", rw[:, 0:gt, 48:80].rearrange("p g (a b) -> p g a b", b=8),
                   rw[:, 0:gt, 4:36].rearrange("p g (a b) -> p g a b", b=8),
                   rw[:, 0:gt, 36:40].rearrange("p g (a o) -> p g a o", o=1).to_broadcast([128, gt, 4, 8]), ALU.add, [k], [k], acc=True)
                if STOP == 103:
                    continue
                S.op("dve", lambda e, rw=rw, rs=rs, gt=gt: e.reduce_max(out=rs[:, 3, 0:gt], in_=rw[:, 0:gt, 48:80], axis=AX.X), reads=[k], writes=[k2], acc=True)
                t_ab = Tok()
                t_ABb.append(t_ab)
                tt(S, "dve", A01[:, t0i:t0i + gt, :], rw[:, 0:gt, 48:80], b3(rs[:, 3, 0:gt], gt, 32), ALU.is_equal, [k, k2], [t_ab])
                stt(S, rw[:, 0:gt, 80:112], A01[:, t0i:t0i + gt, :], -60000.0, rw[:, 0:gt, 48:80], ALU.mult, ALU.add, [t_ab, k], [k], acc=True)
                S.op("dve", lambda e, rw=rw, rs=rs, gt=gt: e.reduce_max(out=rs[:, 4, 0:gt], in_=rw[:, 0:gt, 80:112], axis=AX.X), reads=[k], writes=[k2], acc=True)
                tt(S, "dve", B01[:, t0i:t0i + gt, :], rw[:, 0:gt, 80:112], b3(rs[:, 4, 0:gt], gt, 32), ALU.is_equal, [k, k2], [t_ab], acc=True)
                tt(S, "dve", Mall[:, t0i:t0i + gt, :], A01[:, t0i:t0i + gt, :], B01[:, t0i:t0i + gt, :], ALU.add, [t_ab], [t_ab], acc=True)
                if STOP == 104:
                    continue
                tt(S, "dve", rs[:, 5, 0:gt], rs[:, 4, 0:gt], rs[:, 3, 0:gt], ALU.subtract, [k2], [k2], acc=True)
                act(S, rs[:, 6, 0:gt], rs[:, 5, 0:gt], AF.Exp, [k2], [k2], acc=True)
                ts(S, "dve", rs[:, 7, 0:gt], rs[:, 6, 0:gt], 1.0, None, ALU.add, None, [k2], [k2], acc=True)
                S.op("dve", lambda e, rs=rs, gt=gt: e.reciprocal(out=rs[:, 7, 0:gt], in_=rs[:, 7, 0:gt]), reads=[k2], writes=[k2], acc=True)
                tt(S, "dve", w12[:, t0i:t0i + gt, 0], rs[:, 7, 0:gt], rs[:, 2, 0:gt], ALU.mult, [k2], [t_w12], acc=True)
                tt(S, "dve", w12[:, t0i:t0i + gt, 1], w12[:, t0i:t0i + gt, 0], rs[:, 6, 0:gt], ALU.mult, [k2, t_w12], [t_w12], acc=True)
            if 101 <= STOP <= 105:
                S.barrier()
                return
            t_AB = [t_ABb[ti // GT] for ti in range(nt)]
            pc, k_pc = pp.get()
            for ti in range(nt):
                mm(S, pc[:, 0:32], C.onb[:], Mall[:, ti, :], ti == 0, ti == nt - 1, [t_AB[ti], C.t_c], k_pc)
            cw = sb("cw", [128, 8, 32], F32)
            t_cw = Tok()
            ts(S, "dve", cw[:, 0, :], pc[:, 0:32], 1.0, None, ALU.mult, None, [k_pc], [t_cw])
            S.op("dve", lambda e: e.memset(cw[:, 1, :], 0.0), writes=[t_cw], acc=True)
            for kk in range(nt + 1):
                stt(S, cw[:, 1, :], cw[:, 0, :], 128.0 * kk, cw[:, 1, :], ALU.is_gt, ALU.add, [t_cw], [t_cw], acc=True)
            ts(S, "dve", cw[:, 2, :], cw[:, 1, :], 128.0, None, ALU.mult, None, [t_cw], [t_cw], acc=True)
            ts(S, "dve", cw[:, 3, :], cw[:, 2, :], 1.0, None, ALU.mult, None, [t_cw], [t_cw], acc=True)
            src_i = 3
            for s_ in (1, 2, 4, 8, 16):
                dst_i = 7 - src_i
                ts(S, "dve", cw[:, dst_i, 0:s_], cw[:, src_i, 0:s_], 1.0, None, ALU.mult, None, [t_cw], [t_cw], acc=True)
                tt(S, "dve", cw[:, dst_i, s_:32], cw[:, src_i, s_:32], cw[:, src_i, 0:32 - s_], ALU.add, [t_cw], [t_cw], acc=True)
                src_i = dst_i
            pe_i = src_i
            tt(S, "dve", cw[:, 5, :], cw[:, pe_i, :], cw[:, 2, :], ALU.subtract, [t_cw], [t_cw], acc=True)
            thi = sb("thi", [128, 128], I32)
            thr = sb("thr", [128, 128], F32)
            bacc = sb("bacc", [128, 128], F32)
            pidi = sb("pidi", [128, 1], I32)
            pidf = sb("pidf", [128, 1], F32)
            t_th = Tok()
            S.op("pool", lambda e: e.iota(thi[:], pattern=[[128, 128]], base=0, channel_multiplier=0), writes=[t_th])
            S.op("pool", lambda e: e.iota(pidi[:], pattern=[[0, 1]], base=0, channel_multiplier=1), writes=[t_th], acc=True)
            cp(S, "dve", thr[:], thi[:], [t_th], [t_th])
            cp(S, "dve", pidf[:], pidi[:], [t_th], [t_th], acc=True)
            S.op("dve", lambda e: e.memset(bacc[:], 0.0), writes=[t_th], acc=True)
            for e_ in range(32):
                stt(S, bacc[:], thr[:], cw[:, pe_i, e_:e_ + 1], bacc[:], ALU.is_ge, ALU.add, [t_th, t_cw], [t_th], acc=True)
            ts(S, "dve", bacc[:], bacc[:], 31.0, 128.0, ALU.min, ALU.mult, [t_th], [t_th], acc=True)
            ts(S, "dve", bacc[:], bacc[:], pidf[:, 0:1], 0.0, ALU.add, ALU.add, [t_th], [t_th], acc=True)
            cp(S, "dve", widx[:], bacc[:], [t_th], [t_widx])
            if STOP == 106:
                S.barrier()
                return
            slp = SbPool(C, st, 2, f"r{l}_sl", [128, 3, GT, 32], F32)
            sfp = SbPool(C, st, 2, f"r{l}_sf", [128, GT, 2], F32)
            for bt in batches:
                gt = len(bt)
                t0i = bt[0]
                ps, k_ps = pp.get()
                for j, ti in enumerate(bt):
                    for kk in range(ti):
                        mm(S, ps[:, j * 32:(j + 1) * 32], C.onb[:], Mall[:, kk, :], kk == 0, False, [t_AB[kk], C.t_c], k_ps)
                    mm(S, ps[:, j * 32:(j + 1) * 32], C.utri[:], Mall[:, ti, :], ti == 0, True, [t_AB[ti], C.t_c], k_ps)
                sl, k_sl = slp.get()
                psv = ps[:, 0:gt * 32].rearrange("p (g n) -> p g n", n=32)
                tt(S, "dve", sl[:, 0, 0:gt, :], psv, cw[:, 5, :].rearrange("p (o n) -> p o n", o=1).to_broadcast([128, gt, 32]), ALU.add, [k_ps, t_cw], [k_sl])
                tt(S, "dve", sl[:, 1, 0:gt, :], sl[:, 0, 0:gt, :], A01[:, t0i:t0i + gt, :], ALU.mult, [k_sl, t_AB[t0i]], [k_sl], acc=True)
                tt(S, "dve", sl[:, 2, 0:gt, :], sl[:, 0, 0:gt, :], B01[:, t0i:t0i + gt, :], ALU.mult, [k_sl, t_AB[t0i]], [k_sl], acc=True)
                sf, k_sf = sfp.get()
                S.op("dve", lambda e, sf=sf, sl=sl, gt=gt: e.reduce_sum(out=sf[:, 0:gt, 0], in_=sl[:, 1, 0:gt, :], axis=AX.X), reads=[k_sl], writes=[k_sf])
                S.op("dve", lambda e, sf=sf, sl=sl, gt=gt: e.reduce_sum(out=sf[:, 0:gt, 1], in_=sl[:, 2, 0:gt, :], axis=AX.X), reads=[k_sl], writes=[k_sf], acc=True)
                cp(S, "dve", sAB[:, t0i:t0i + gt, :], sf[:, 0:gt, :], [k_sf], [t_sAB], acc=True)
                for ti in bt:
                    for k2_ in range(2):
                        S.dma_fn("pool", lambda e, ti=ti, k2_=k2_: e.indirect_dma_start(
                            out=xbuf[:, :], out_offset=bass.IndirectOffsetOnAxis(ap=sAB[:, ti, k2_:k2_ + 1], axis=0),
                            in_=fx_all[:, ti, :], in_offset=None),
                            reads=[t_sAB, t_fx[ti], t_xz], writes=[C.T("xbuf")], acc=True)
        S.barrier()
        if STOP == 107:
            return
        with contextlib.ExitStack() as st:
            sb = lambda name, shape, dt: st.enter_context(nc.sbuf_tensor(f"e{l}_{name}", list(shape), dt))
            wgb = SbPool(C, st, 4, f"e{l}_wgb", [128, 8, 512], BF16)
            wdb = SbPool(C, st, 4, f"e{l}_wdb", [128, 2, 1024], BF16)
            xbp = SbPool(C, st, 3, f"e{l}_xb", [128, 1024], BF16)
            xTp = SbPool(C, st, 3, f"e{l}_xT", [128, 8, 128], BF16)
            sip = SbPool(C, st, 2, f"e{l}_si", [128, 256], F32)
            acp = SbPool(C, st, 3, f"e{l}_ac", [128, 256], BF16)
            aTp = SbPool(C, st, 2, f"e{l}_aT", [128, 2, 128], BF16)
            ybp = SbPool(C, st, 2, f"e{l}_yb", [128, 1024], F32)
            ptx = PsumPool(C, st, 2, f"e{l}_ptx", (128, 8, 128), BF16)
            ptc = PsumPool(C, st, 1, f"e{l}_ptc", (128, 2, 128), BF16)
            pgu = PsumPool(C, st, 2, f"e{l}_pgu")
            py = PsumPool(C, st, 2, f"e{l}_py")
            wgsrc = d["wgu_b"]
            wdsrc = d["wdn_b"]
            nblk = NB
            stA, stB = {}, {}

            def stage_a(b):
                wg, k_wg = wgb.get()
                S.dma_fn("pool", lambda e, wg=wg, b=b: e.indirect_dma_start(
                    out=wg[:].rearrange("p a b -> p (a b)"), out_offset=None, in_=wgsrc[:, :],
                    in_offset=bass.IndirectOffsetOnAxis(ap=widx[:, b:b + 1], axis=0)),
                    reads=[t_widx, C.T("wexp_b")], writes=[k_wg])
                wd, k_wd = wdb.get()
                S.dma_fn("pool", lambda e, wd=wd, b=b: e.indirect_dma_start(
                    out=wd[:].rearrange("p a b -> p (a b)"), out_offset=None, in_=wdsrc[:, :],
                    in_offset=bass.IndirectOffsetOnAxis(ap=widx[:, b:b + 1], axis=0)),
                    reads=[t_widx, C.T("wexp_b")], writes=[k_wd])
                xb, k_xb = xbp.get()
                S.dma("sp", xb[:], xbuf[b * 128:(b + 1) * 128, :], reads=[C.T("xbuf")], writes=[k_xb])
                tp, k_tp = ptx.get()
                for kc in range(8):
                    S.op("pe", lambda e, tp=tp, xb=xb, kc=kc: e.transpose(tp[:, kc, :], xb[:, kc * 128:(kc + 1) * 128], C.idb[:]),
                         reads=[k_xb, C.t_c], writes=[k_tp], acc=(kc > 0))
                xT, k_xT = xTp.get()
                cp(S, "act", xT[:], tp[:], [k_tp], [k_xT])
                stA[b] = (wg, k_wg, wd, k_wd, xT, k_xT)

            def stage_b(b):
                wg, k_wg, wd, k_wd, xT, k_xT = stA.pop(b)
                pg, k_pg = pgu.get()
                for kc in range(8):
                    mm(S, pg[:, :], xT[:, kc, :], wg[:, kc, :], kc == 0, kc == 7, [k_xT, k_wg], k_pg)
                si_, k_si = sip.get()
                act(S, si_[:], pg[:, 0:256], AF.Silu, [k_pg], [k_si])
                ac, k_ac = acp.get()
                tt(S, "dve", ac[:], pg[:, 256:512], si_[:], ALU.mult, [k_pg, k_si], [k_ac])
                stB[b] = (wd, k_wd, ac, k_ac)

            def stage_c(b):
                wd, k_wd, ac, k_ac = stB.pop(b)
                tp2, k_tp2 = ptc.get()
                for c in range(2):
                    S.op("pe", lambda e, tp2=tp2, ac=ac, c=c: e.transpose(tp2[:, c, :], ac[:, c * 128:(c + 1) * 128], C.idb[:]),
                         reads=[k_ac, C.t_c], writes=[k_tp2], acc=(c > 0))
                aT, k_aT = aTp.get()
                cp(S, "act", aT[:], tp2[:], [k_tp2], [k_aT])
                yb, k_yb = ybp.get()
                for hf in range(2):
                    pyt, k_py = py.get()
                    for c in range(2):
                        mm(S, pyt[:, :], aT[:, c, :], wd[:, c, hf * 512:(hf + 1) * 512], c == 0, c == 1, [k_aT, k_wd], k_py)
                    cp(S, "act", yb[:, hf * 512:(hf + 1) * 512], pyt[:, :], [k_py], [k_yb], acc=(hf > 0))
                S.dma("act", ybuf[b * 128:(b + 1) * 128, :], yb[:], reads=[k_yb], writes=[C.T("ybuf")], acc=True)

            for b in range(nblk + 2):
                if b < nblk:
                    stage_a(b)
                if 0 <= b - 1 < nblk:
                    stage_b(b - 1)
                if 0 <= b - 2 < nblk:
                    stage_c(b - 2)
            G2 = sb("G2", [128, 2, 1024], F32)
            fg = sb("fg", [128, 1024], F32)
            t_g2 = Tok()
            for r in range(2):
                S.dma("sp", G2[:, r, :], bcast_row(d["modv"][l][r:r + 1, 5120:6144], 1024), reads=[C.T(f"modv{l}")], writes=[t_g2], acc=True)
            if last:
                S.dma("sp", fg[:], bcast_row(d["final_g"][0:1, :], 1024), writes=[t_g2], acc=True)
            yAp = SbPool(C, st, 3, f"e{l}_yA", [128, 1024], F32)
            yBp = SbPool(C, st, 3, f"e{l}_yB", [128, 1024], F32)
            hp = SbPool(C, st, 4, f"e{l}_h", [128, 1024], F32)
            hnp = SbPool(C, st, 2, f"e{l}_hn", [128, 1024], F32)
            hop = SbPool(C, st, 2, f"e{l}_ho", [128, 1024], F32)
            ssp = SbPool(C, st, 2, f"e{l}_ss", [128, 4], F32)
            sqj = sb("sqj", [128, 1024], BF16)
            t_sqj = Tok()
            pre = {}

            def prefetch(ti):
                tile = tiles[ti]
                ys = []
                for k2, pool_ in enumerate((yAp, yBp)):
                    yt, k_yt = pool_.get()
                    S.dma_fn("pool", lambda e, yt=yt, ti=ti, k2=k2: e.indirect_dma_start(
                        out=yt[:], out_offset=None, in_=ybuf[:, :], in_offset=bass.IndirectOffsetOnAxis(ap=sAB[:, ti, k2:k2 + 1], axis=0)),
                        reads=[t_sAB, C.T("ybuf")], writes=[k_yt])
                    ys.append((yt, k_yt))
                ht, k_h = hp.get()
                S.dma("sp", ht[:], d["h_s"][tile * 128:(tile + 1) * 128, :], reads=[C.T(f"h:{tile}")], writes=[k_h])
                pre[ti] = (ys, ht, k_h)

            prefetch(0)
            for ti, tile in enumerate(tiles):
                r = 0 if tile >= 2 else 1
                if ti + 1 < len(tiles):
                    prefetch(ti + 1)
                ys, ht, k_h = pre.pop(ti)
                (yA, k_yA), (yB, k_yB) = ys
                ts(S, "dve", yA[:], yA[:], w12[:, ti, 0:1], None, ALU.mult, None, [k_yA, t_w12], [k_yA])
                stt(S, yB[:], yB[:], w12[:, ti, 1:2], yA[:], ALU.mult, ALU.add, [k_yB, k_yA, t_w12], [k_yB])
                tt(S, "dve", yB[:], yB[:], G2[:, r, :], ALU.mult, [k_yB, t_g2], [k_yB])
                hn, k_hn = hnp.get()
                tt(S, "dve", hn[:], yB[:], ht[:], ALU.add, [k_yB, k_h], [k_hn])
                if not last:
                    S.dma("act", d["h_s"][tile * 128:(tile + 1) * 128, :], hn[:], reads=[k_hn], writes=[C.T(f"h:{tile}")])
                else:
                    ss, k_ss = ssp.get()
                    act(S, sqj[:], hn[:], AF.Square, [k_hn], [t_sqj, k_ss], accum_out=ss[:, 0:1])
                    act(S, ss[:, 1:2], ss[:, 0:1], AF.Sqrt, [k_ss], [k_ss], acc=True, scale=1.0 / DM, bias=EPS)
                    S.op("dve", lambda e, ss=ss: e.reciprocal(out=ss[:, 2:3], in_=ss[:, 1:2]), reads=[k_ss], writes=[k_ss], acc=True)
                    ho, k_ho = hop.get()
                    stt(S, ho[:], hn[:], ss[:, 2:3], fg[:], ALU.mult, ALU.mult, [k_hn, k_ss, t_g2], [k_ho])
                    S.dma("act", d["out"][(tile - 2) * 128:(tile - 1) * 128, :], ho[:], reads=[k_ho], writes=[C.T(f"out:{tile}")])
    S.barrier()
```
